# Optimizing a Trainium2 kernel written in Bass

```python
import math
import jax, jax.numpy as jnp
from jax import lax
import numpy as np

D_MODEL = 2048
BATCH = 4
SEQ = 8192
DEPTH = 1

CHUNK = 64
Q_BLOCK = 128
ATT_HEADS = 8
ATT_HEAD_DIM = 64
ATT_V_DIM = 2 * ATT_HEAD_DIM
ATT_WIDTH = ATT_HEADS * ATT_V_DIM
SSM_HEADS = 16
SSM_HEAD_DIM = 64
SSM_WIDTH = SSM_HEADS * SSM_HEAD_DIM
SSM_GROUPS = 2
SSM_STATE = 128
SSM_CONV = 4
MIX_WIDTH = ATT_WIDTH + SSM_WIDTH
N_Q = ATT_HEADS * 2 * ATT_HEAD_DIM
N_K = ATT_HEADS * 2 * ATT_HEAD_DIM
N_V = ATT_HEADS * ATT_V_DIM
N_Z = SSM_WIDTH
N_XBC = SSM_WIDTH + 2 * SSM_GROUPS * SSM_STATE
N_DT = SSM_HEADS
IN_COLS = N_Q + N_K + N_V + N_Z + N_XBC + N_DT
N_EXPERT_GROUPS = 4
EXPERTS_PER_GROUP = 8
N_EXPERTS = N_EXPERT_GROUPS * EXPERTS_PER_GROUP
TOP_K = 2
EXPERT_HIDDEN = D_MODEL // 2
MOE_BLOCK = 256
DN_ALPHA = (2 * DEPTH) ** 0.25
DN_BETA = (8 * DEPTH) ** -0.25
LN_EPS = 1e-5
RMS_EPS = 1e-6

kernel_name = "hybrid_diffattn_ssd_hmoe_deepnorm"


def layer_norm(x, g, b):
    xf = x.astype(jnp.float32)
    mu = jnp.mean(xf, -1, keepdims=True)
    var = jnp.mean(jnp.square(xf - mu), -1, keepdims=True)
    return ((xf - mu) * lax.rsqrt(var + LN_EPS) * g + b).astype(x.dtype)


def rms_norm(x, g):
    xf = x.astype(jnp.float32)
    return (xf * lax.rsqrt(jnp.mean(xf * xf, -1, keepdims=True) + RMS_EPS) * g).astype(x.dtype)


def alibi_slopes(n):
    return jnp.exp2(-8.0 * jnp.arange(1, n + 1, dtype=jnp.float32) / n)


def diff_attention(q, k, v, lam, lambda_init, norm_w):
    Bsz, S = q.shape[0], q.shape[1]
    scale = ATT_HEAD_DIM ** -0.5
    slopes = alibi_slopes(ATT_HEADS)
    outs = []
    for qs in range(0, S, Q_BLOCK):
        ke = qs + Q_BLOCK
        s = jnp.einsum('bqhmd,bkhmd->bhmqk', q[:, qs:ke], k[:, :ke],
                       preferred_element_type=jnp.float32) * scale
        t = jnp.arange(qs, ke)
        sp = jnp.arange(ke)
        dist = jnp.abs(t[:, None] - sp[None, :]).astype(jnp.float32)
        allowed = (sp[None, :] // CHUNK) <= (t[:, None] // CHUNK)
        bias = jnp.where(allowed[None], -slopes[:, None, None] * dist[None], -jnp.inf)
        p = jax.nn.softmax(s + bias[None, :, None], axis=-1)
        w = p[:, :, 0] - lam * p[:, :, 1]
        outs.append(jnp.einsum('bhqk,bkhv->bqhv', w.astype(v.dtype), v[:, :ke]))
    o = jnp.concatenate(outs, axis=1)
    o = rms_norm(o, norm_w) * (1.0 - lambda_init)
    return o.reshape(Bsz, S, ATT_WIDTH)


def ssd_mixer(z, xbc, dt_raw, conv_w, conv_b, dt_bias, a_log, d_skip, norm_w):
    Bsz, S, _ = xbc.shape
    f32 = jnp.float32
    xbc = lax.conv_general_dilated(xbc, conv_w[:, None, :].astype(xbc.dtype), window_strides=(1,),
                                   padding=[(SSM_CONV - 1, 0)],
                                   dimension_numbers=('NWC', 'WIO', 'NWC'),
                                   feature_group_count=N_XBC) + conv_b
    xbc = jax.nn.silu(xbc)
    xs, bm, cm = jnp.split(xbc, [SSM_WIDTH, SSM_WIDTH + SSM_GROUPS * SSM_STATE], axis=-1)
    nc = S // CHUNK
    hpg = SSM_HEADS // SSM_GROUPS
    xs = xs.astype(f32).reshape(Bsz, nc, CHUNK, SSM_GROUPS, hpg, SSM_HEAD_DIM)
    bm = bm.astype(f32).reshape(Bsz, nc, CHUNK, SSM_GROUPS, SSM_STATE)
    cm = cm.astype(f32).reshape(Bsz, nc, CHUNK, SSM_GROUPS, SSM_STATE)
    dt = jax.nn.softplus(dt_raw.astype(f32) + dt_bias.astype(f32)).reshape(Bsz, nc, CHUNK, SSM_GROUPS, hpg)
    a_head = -jnp.exp(a_log.astype(f32)).reshape(SSM_GROUPS, hpg)
    acs = jnp.cumsum(jnp.moveaxis(dt * a_head, 2, -1), axis=-1)
    xdt = xs * dt[..., None]
    tril = jnp.tril(jnp.ones((CHUNK, CHUNK), dtype=bool))
    seg = acs[..., :, None] - acs[..., None, :]
    decay_ls = jnp.exp(jnp.where(tril, seg, -jnp.inf))
    cb = jnp.einsum('bclgn,bcsgn->bcgls', cm, bm)
    m = cb[:, :, :, None] * decay_ls
    y_diag = jnp.einsum('bcgels,bcsgep->bclgep', m, xdt)
    decay_states = jnp.moveaxis(jnp.exp(acs[..., -1:] - acs), -1, 2)
    states = jnp.einsum('bclgn,bclgep->bcgepn', bm, xdt * decay_states[..., None])
    chunk_decay = jnp.exp(acs[..., -1])

    def step(h, inp):
        st, dec = inp
        return h * dec[..., None, None] + st, h

    h0 = jnp.zeros((Bsz, SSM_GROUPS, hpg, SSM_HEAD_DIM, SSM_STATE), f32)
    _, prev = lax.scan(step, h0, (jnp.moveaxis(states, 1, 0), jnp.moveaxis(chunk_decay, 1, 0)))
    prev = jnp.moveaxis(prev, 0, 1)
    out_decay = jnp.moveaxis(jnp.exp(acs), -1, 2)
    y_off = jnp.einsum('bclgn,bcgepn->bclgep', cm, prev) * out_decay[..., None]
    y = y_diag + y_off + xs * d_skip.astype(f32).reshape(SSM_GROUPS, hpg)[..., None]
    y = y.reshape(Bsz, S, SSM_WIDTH) * jax.nn.silu(z.astype(f32))
    gsz = SSM_WIDTH // SSM_GROUPS
    y = y.reshape(Bsz, S, SSM_GROUPS, gsz)
    y = y * lax.rsqrt(jnp.mean(y * y, -1, keepdims=True) + RMS_EPS)
    y = y.reshape(Bsz, S, SSM_WIDTH) * norm_w
    return y.astype(z.dtype)


def hierarchical_moe(h, w_rg, b_rg, w_re, b_re, w_gate, w_up, w_down):
    Bsz, S, D = h.shape
    T = Bsz * S
    f32 = jnp.float32
    hf = h.reshape(T, D)
    g_logits = jnp.dot(hf, w_rg, preferred_element_type=f32) + b_rg
    p_group = jax.nn.softmax(g_logits, axis=-1)
    g_prob, g_idx = lax.top_k(p_group, 1)
    g_prob, g_idx = g_prob[:, 0], g_idx[:, 0]
    e_logits = jnp.einsum('td,gde->tge', hf, w_re, preferred_element_type=f32) + b_re
    e_logits = e_logits[jnp.arange(T), g_idx]
    top_logit, top_local = lax.top_k(e_logits, TOP_K)
    top_w = jax.nn.softmax(top_logit, axis=-1) * g_prob[:, None]
    expert_id = g_idx[:, None] * EXPERTS_PER_GROUP + top_local
    n_assign = T * TOP_K
    a_exp = expert_id.reshape(-1).astype(jnp.int32)
    a_tok = jnp.repeat(jnp.arange(T, dtype=jnp.int32), TOP_K)
    a_w = top_w.reshape(-1)
    order = jnp.argsort(a_exp)
    s_exp, s_tok, s_w = a_exp[order], a_tok[order], a_w[order]
    counts = jnp.bincount(a_exp, length=N_EXPERTS)
    starts = jnp.cumsum(counts) - counts
    padded = (counts + MOE_BLOCK - 1) // MOE_BLOCK * MOE_BLOCK
    pad_ends = jnp.cumsum(padded)
    pad_starts = pad_ends - padded
    dest = pad_starts[s_exp] + jnp.arange(n_assign, dtype=jnp.int32) - starts[s_exp]
    n_blocks = -(-n_assign // MOE_BLOCK) + N_EXPERTS
    cap = n_blocks * MOE_BLOCK
    buf_tok = jnp.zeros((cap,), jnp.int32).at[dest].set(s_tok)
    buf_w = jnp.zeros((cap,), f32).at[dest].set(s_w)
    block_exp = jnp.minimum(
        jnp.searchsorted(pad_ends, jnp.arange(n_blocks) * MOE_BLOCK, side='right'), N_EXPERTS - 1)

    def expert_block(args):
        tok, e = args
        xb = hf[tok]
        hid = jax.nn.silu(xb @ w_gate[e]) * (xb @ w_up[e])
        return hid @ w_down[e]

    ys = lax.map(expert_block, (buf_tok.reshape(n_blocks, MOE_BLOCK), block_exp))
    ys = ys.reshape(cap, D) * buf_w[:, None].astype(ys.dtype)
    out = jnp.zeros((T, D), ys.dtype).at[buf_tok].add(ys)
    return out.reshape(Bsz, S, D).astype(h.dtype)


def setup_inputs(seed: int = 0) -> dict:
    key = jax.random.key(seed)
    ks = jax.random.split(key, 26)
    L, D, f32 = DEPTH, D_MODEL, jnp.float32

    def nrm(k, shape, scale):
        return jax.random.normal(k, shape, f32) * scale

    x = nrm(ks[0], (BATCH, SEQ, D), 1.0)
    col_scale = jnp.concatenate([jnp.ones((N_Q + N_K,), f32), jnp.full((N_V,), DN_BETA, f32),
                                 jnp.ones((N_Z + N_XBC + N_DT,), f32)])
    w_in = nrm(ks[1], (L, D, IN_COLS), D ** -0.5) * col_scale
    lambda_q1 = nrm(ks[2], (L, ATT_HEAD_DIM), 0.1)
    lambda_k1 = nrm(ks[3], (L, ATT_HEAD_DIM), 0.1)
    lambda_q2 = nrm(ks[4], (L, ATT_HEAD_DIM), 0.1)
    lambda_k2 = nrm(ks[5], (L, ATT_HEAD_DIM), 0.1)
    attn_norm_w = 1.0 + nrm(ks[6], (L, ATT_V_DIM), 0.02)
    conv_w = nrm(ks[7], (L, SSM_CONV, N_XBC), SSM_CONV ** -0.5)
    conv_b = nrm(ks[8], (L, N_XBC), 0.02)
    dt0 = jnp.exp(jax.random.uniform(ks[9], (L, SSM_HEADS), f32, math.log(1e-3), math.log(1e-1)))
    dt_bias = dt0 + jnp.log(-jnp.expm1(-dt0))
    a_log = jnp.log(jax.random.uniform(ks[10], (L, SSM_HEADS), f32, 1.0, 16.0))
    d_skip = 1.0 + nrm(ks[11], (L, SSM_HEADS), 0.02)
    ssm_norm_w = 1.0 + nrm(ks[12], (L, SSM_WIDTH), 0.02)
    w_out = nrm(ks[13], (L, MIX_WIDTH, D), MIX_WIDTH ** -0.5 * DN_BETA)
    ln1_g = 1.0 + nrm(ks[14], (L, D), 0.02)
    ln1_b = nrm(ks[15], (L, D), 0.02)
    w_router_group = nrm(ks[16], (L, D, N_EXPERT_GROUPS), D ** -0.5)
    b_router_group = nrm(ks[17], (L, N_EXPERT_GROUPS), 0.01)
    w_router_expert = nrm(ks[18], (L, N_EXPERT_GROUPS, D, EXPERTS_PER_GROUP), D ** -0.5)
    b_router_expert = nrm(ks[19], (L, N_EXPERT_GROUPS, EXPERTS_PER_GROUP), 0.01)
    w_gate = nrm(ks[20], (L, N_EXPERTS, D, EXPERT_HIDDEN), D ** -0.5)
    w_up = nrm(ks[21], (L, N_EXPERTS, D, EXPERT_HIDDEN), D ** -0.5 * DN_BETA)
    w_down = nrm(ks[22], (L, N_EXPERTS, EXPERT_HIDDEN, D), EXPERT_HIDDEN ** -0.5 * DN_BETA)
    ln2_g = 1.0 + nrm(ks[23], (L, D), 0.02)
    ln2_b = nrm(ks[24], (L, D), 0.02)
    return {"x": x, "w_in": w_in, "lambda_q1": lambda_q1, "lambda_k1": lambda_k1,
            "lambda_q2": lambda_q2, "lambda_k2": lambda_k2, "attn_norm_w": attn_norm_w,
            "conv_w": conv_w, "conv_b": conv_b, "dt_bias": dt_bias, "a_log": a_log,
            "d_skip": d_skip, "ssm_norm_w": ssm_norm_w, "w_out": w_out,
            "ln1_g": ln1_g, "ln1_b": ln1_b, "w_router_group": w_router_group,
            "b_router_group": b_router_group, "w_router_expert": w_router_expert,
            "b_router_expert": b_router_expert, "w_gate": w_gate, "w_up": w_up,
            "w_down": w_down, "ln2_g": ln2_g, "ln2_b": ln2_b}


def reference(x, w_in, lambda_q1, lambda_k1, lambda_q2, lambda_k2, attn_norm_w, conv_w, conv_b,
              dt_bias, a_log, d_skip, ssm_norm_w, w_out, ln1_g, ln1_b, w_router_group,
              b_router_group, w_router_expert, b_router_expert, w_gate, w_up, w_down,
              ln2_g, ln2_b):
    Bsz, S, _ = x.shape
    f32 = jnp.float32
    offs = [N_Q, N_Q + N_K, N_Q + N_K + N_V, N_Q + N_K + N_V + N_Z,
            N_Q + N_K + N_V + N_Z + N_XBC]
    h = x
    for l in range(DEPTH):
        lambda_init = 0.8 - 0.6 * math.exp(-0.3 * l)
        proj = jnp.einsum('bsd,dc->bsc', h, w_in[l])
        q, k, v, z, xbc, dt_raw = jnp.split(proj, offs, axis=-1)
        q = q.reshape(Bsz, S, ATT_HEADS, 2, ATT_HEAD_DIM)
        k = k.reshape(Bsz, S, ATT_HEADS, 2, ATT_HEAD_DIM)
        v = v.reshape(Bsz, S, ATT_HEADS, ATT_V_DIM)
        lam = (jnp.exp(jnp.sum(lambda_q1[l].astype(f32) * lambda_k1[l].astype(f32)))
               - jnp.exp(jnp.sum(lambda_q2[l].astype(f32) * lambda_k2[l].astype(f32)))
               + lambda_init)
        att = diff_attention(q, k, v, lam, lambda_init, attn_norm_w[l])
        ssm = ssd_mixer(z, xbc, dt_raw, conv_w[l], conv_b[l], dt_bias[l], a_log[l],
                        d_skip[l], ssm_norm_w[l])
        mix = jnp.einsum('bsc,cd->bsd', jnp.concatenate([att, ssm], axis=-1), w_out[l])
        h = layer_norm(DN_ALPHA * h + mix, ln1_g[l], ln1_b[l])
        ffn = hierarchical_moe(h, w_router_group[l], b_router_group[l], w_router_expert[l],
                               b_router_expert[l], w_gate[l], w_up[l], w_down[l])
        h = layer_norm(DN_ALPHA * h + ffn, ln2_g[l], ln2_b[l])
    return h
```

```python
from contextlib import ExitStack
import math
import numpy as np
import concourse.bass as bass
import concourse.mybir as mybir
from concourse.bass_utils import run_bass_kernel_spmd

F32 = mybir.dt.float32
BF16 = mybir.dt.bfloat16
AF = mybir.ActivationFunctionType
ALU = mybir.AluOpType
AX = mybir.AxisListType

D = 2048
IN_COLS = 5648
NEXP = 32
EH = 1024
DN_ALPHA = 2.0 ** 0.25
LN_EPS = 1e-5
RMS_EPS = 1e-6
LAMBDA_INIT = 0.8 - 0.6 * math.exp(0.0)
NEG = -30000.0


class Prog:
    ENGS = ("pe", "act", "dve", "pool", "sp")

    def __init__(self, nc):
        self.nc = nc
        self.ops = []
        self.stack = ExitStack()
        self._n = 0
        self.barrier_at = []
        self.maxops = None

    def sb(self, shape, dtype, name=None):
        self._n += 1
        return self.stack.enter_context(
            self.nc.sbuf_tensor(name or f"sb{self._n}", list(shape), dtype))

    def op(self, eng, fn, reads=(), writes=(), kind="c", accum=False):
        if self.maxops is not None and len(self.ops) >= self.maxops:
            return
        import sys as _sys
        fr = _sys._getframe(1)
        if fr.f_code.co_name in ("dma", "evac", "convert", "layernorm"):
            fr = fr.f_back
        self.ops.append(dict(eng=eng, kind=kind, fn=fn, reads=tuple(reads),
                             writes=tuple(writes), accum=accum, line=fr.f_lineno))

    def dma(self, eng, out, in_, reads=(), writes=(), accum=False, **kw):
        self.op(eng, lambda e: e.dma_start(out=out, in_=in_, **kw), reads, writes,
                kind="d", accum=accum)

    def barrier(self):
        self.barrier_at.append(len(self.ops))

    def emit(self, final_wait_keys=()):
        nc = self.nc
        ops = self.ops
        n = len(ops)
        writers = {}
        gen_first = {}
        readers = {}
        deps = [None] * n
        needed = [False] * n
        bset = set()
        last_of = {}
        barriers = set(self.barrier_at)
        def bank_of(k):
            if isinstance(k, tuple) and k[0] == "ps":
                return ("bankx", k[1])
            if isinstance(k, tuple) and k[0] == "O":
                return ("bankx", 4 + k[1] * 2 + k[2] // 2)
            return None

        for o in ops:
            if o["kind"] != "c":
                continue
            bx = {bank_of(k) for k in o["reads"] + o["writes"]} - {None}
            if bx and not o.get("bx_done"):
                o["writes"] = o["writes"] + tuple(bx)
                o["bx_done"] = True
        for i, o in enumerate(ops):
            if i in barriers:
                bset = set(last_of.values())
            d = set(bset)
            sk = (o["eng"], o["kind"])
            for k in o["reads"]:
                d.update(writers.get(k, {}).values())
            if not o["accum"]:
                for k in o["writes"]:
                    d.update(writers.get(k, {}).values())
                    d.update(readers.get(k, ()))
            else:
                for k in o["writes"]:
                    if k in gen_first:
                        d.add(gen_first[k])
                    if isinstance(k, tuple) and k[0] == "bankx":
                        d.update(writers.get(k, {}).values())
                        d.update(readers.get(k, ()))
            d.discard(i)
            if o["eng"] == "pe" and o["kind"] == "c":
                d = {j for j in d if not (ops[j]["eng"] == "pe" and ops[j]["kind"] == "c")}
            deps[i] = d
            for j in d:
                needed[j] = True
            for k in o["writes"]:
                if o["accum"]:
                    writers.setdefault(k, {})[sk] = i
                else:
                    writers[k] = {sk: i}
                    readers[k] = []
                    gen_first[k] = i
            for k in o["reads"]:
                readers.setdefault(k, []).append(i)
            last_of[sk] = i
            if o["kind"] == "d":
                needed[i] = True
        last_writer = {k: max(v.values()) for k, v in writers.items()}
        final = set()
        for k in final_wait_keys:
            final.update(writers.get(k, {}).values())
        for j in final:
            needed[j] = True
        semkeys = sorted({(o["eng"], o["kind"]) for o in ops})
        sems = {sk: self.stack.enter_context(nc.semaphore(f"s_{sk[0]}_{sk[1]}"))
                for sk in semkeys}
        cnt = {sk: 0 for sk in semkeys}
        val = [0] * n
        for i, o in enumerate(ops):
            if needed[i]:
                sk = (o["eng"], o["kind"])
                cnt[sk] += 16 if o["kind"] == "d" else 1
                val[i] = cnt[sk]
        self.sem_counts = cnt
        engmap = {"pe": "tensor", "act": "scalar", "dve": "vector", "pool": "gpsimd",
                  "sp": "sync"}
        block = self.stack.enter_context(nc.Block())
        for eng in self.ENGS:
            idxs = [i for i, o in enumerate(ops) if o["eng"] == eng]
            if not idxs and eng != "sp":
                continue

            def body(e, idxs=idxs, eng=eng):
                waited = {}
                for i in idxs:
                    o = ops[i]
                    need = {}
                    for j in deps[i]:
                        sk = (ops[j]["eng"], ops[j]["kind"])
                        if val[j] > need.get(sk, 0):
                            need[sk] = val[j]
                    for sk, v in need.items():
                        if waited.get(sk, 0) < v:
                            e.wait_ge(sems[sk], v)
                            waited[sk] = v
                    ins = o["fn"](e)
                    if needed[i]:
                        ins.then_inc(sems[(o["eng"], o["kind"])],
                                     16 if o["kind"] == "d" else 1)
                if eng == "sp":
                    need = {}
                    for j in final:
                        sk = (ops[j]["eng"], ops[j]["kind"])
                        need[sk] = max(need.get(sk, 0), val[j])
                    for sk, v in need.items():
                        if waited.get(sk, 0) < v:
                            e.wait_ge(sems[sk], v)

            getattr(block, engmap[eng])(body)

    def close(self):
        self.stack.close()


def _dsz(dt):
    return 4 if dt == F32 else 2


class Arena:
    def __init__(self, P, nbytes):
        self.t = P.sb([128, nbytes // 4], F32, name="arena")
        self.off = 0
        self.cap = nbytes

    def alloc(self, free, dtype, parts=128):
        free = tuple(free)
        nel = int(np.prod(free))
        sz = (nel * _dsz(dtype) + 63) // 64 * 64
        assert self.off + sz <= self.cap, (self.off, sz, self.cap)
        ap = self.t[0:parts, self.off // 4:(self.off + sz) // 4]
        self.off += sz
        if dtype != F32:
            ap = ap.bitcast(dtype)
        ap = ap[:, 0:nel]
        if len(free) == 2:
            ap = ap.rearrange("p (a b) -> p a b", a=free[0])
        elif len(free) == 3:
            ap = ap.rearrange("p (a b c) -> p a b c", a=free[0], b=free[1])
        return ap

    def reset(self):
        self.off = 0


def win_groups():
    g = []
    for h in range(8):
        g.append(("q", h * 128, 128))
    for h in range(8):
        g.append(("k", 1024 + h * 128, 128))
    for i in range(2):
        g.append(("v", 2048 + i * 512, 512))
    for i in range(2):
        g.append(("z", 3072 + i * 512, 512))
    for i in range(12):
        g.append(("x", 4096 + i * 128, 128))
    g.append(("d", 5632, 16))
    return g


def build_program(S, stop=None, start=0):
    SO = S // 2
    SA = S
    SC = S - SO
    NB = SA // 128
    nc = bass.Bass("TRN2", target_bir_lowering=False)

    def din(name, shape, dt=F32):
        kind = "ExternalInput"
        if start > 0 and name in ("w_in", "w_out", "w_gate", "w_up", "w_down", "xT"):
            kind = "Internal"
        return nc.dram_tensor(name, list(shape), dt, kind=kind).ap()

    PROD = dict(win_b=0, wout_b=0, wg_b=0, wu_b=0, wd_b=0, qs=1, ks=1, vs=1, zs=1, xbcs=1, dts=1,
                mixT=3, h1s=4, h1T=4)

    def dscr(name, shape, dt):
        dbg = stop is not None and (stop == 0 or not name.startswith("w"))
        kind = "ExternalOutput" if dbg else "Internal"
        if start > 0 and PROD[name] < start and not name.startswith("w"):
            kind = "ExternalInput"
        if start > 0 and name.startswith("w"):
            kind = "ExternalInput" if (start == 4 and name == "wout_b") else "Internal"
        return nc.dram_tensor(name, list(shape), dt, kind=kind).ap()

    xT = din("xT", [D, SA])
    xo = din("xo", [SO, D])
    w_in = din("w_in", [D, IN_COLS])
    w_out = din("w_out", [D, D])
    w_gate = din("w_gate", [NEXP, D, EH])
    w_up = din("w_up", [NEXP, D, EH])
    w_down = din("w_down", [NEXP, EH, D])
    wr = din("wr", [D, 36])
    br = din("br", [1, 36])
    lamv = din("lamv", [1, 256])
    anw = din("anw", [1, 128])
    cw = din("cw", [128, 48])
    cb = din("cb", [128, 12])
    dtb = din("dtb", [1, 16])
    alog = din("alog", [1, 16])
    dsk = din("dsk", [1, 16])
    snw = din("snw", [1, 1024])
    ln1g = din("ln1g", [1, D])
    ln1b = din("ln1b", [1, D])
    ln2g = din("ln2g", [1, D])
    ln2b = din("ln2b", [1, D])
    ident_in = din("ident", [128, 128])
    qtab = din("qtab", [8, 4, SO])
    ktab = din("ktab", [8, 4, SA])
    ttab = din("ttab", [128, 8 * 896])
    tri_in = din("tri", [64, 64])
    umat_in = din("umat", [64, 64])
    flag_in = din("flag", [128, 1])
    out = nc.dram_tensor("out", [SO, D], F32, kind="ExternalOutput").ap()

    groups = win_groups()
    goff = []
    o = 0
    for (_, _, gc) in groups:
        goff.append(o)
        o += 128 * 16 * gc
    win_b = dscr("win_b", [o], BF16)
    wout_b = dscr("wout_b", [128, 16 * D], BF16)
    wg_b = dscr("wg_b", [NEXP * 4, 128, 16 * 256], BF16)
    wu_b = dscr("wu_b", [NEXP * 4, 128, 16 * 256], BF16)
    wd_b = dscr("wd_b", [NEXP * 4, 128, 2 * D], BF16)
    qs = dscr("qs", [16, 64, SO], BF16)
    ks = dscr("ks", [16, 64, SA], BF16)
    vs = dscr("vs", [SA, 1024], BF16)
    zs = dscr("zs", [SO, 1024], F32)
    xbcs = dscr("xbcs", [1536, SA], F32)
    dts = dscr("dts", [SA, 16], F32)
    mixT = dscr("mixT", [D, SO], BF16)
    h1s = dscr("h1s", [SO, D], F32)
    h1T = dscr("h1T", [D, SO], BF16)

    P = Prog(nc)
    A = Arena(P, 176 * 1024)
    pp = P.stack.enter_context(nc.psum_tensor("pp", [128, 4096], F32))

    def bank(b, parts=128, n=512, off=0):
        return pp[0:parts, b * 512 + off:b * 512 + off + n]

    def bankbf(b, parts=128, n=1024, off=0):
        return pp[0:parts, b * 512:(b + 1) * 512].bitcast(BF16)[:, off:off + n]

    ident_f = P.sb([128, 128], F32, "ident_f")
    ident_b = P.sb([128, 128], BF16, "ident_b")
    Wall = P.sb([128, (SO // 128) * 32], F32, "Wall")
    epsln = P.sb([128, 1], F32, "epsln")
    epsrms = P.sb([128, 1], F32, "epsrms")
    P.dma("sp", ident_f[:], ident_in, writes=["ident_f"])
    P.dma("pool", ident_b[:], ident_in, writes=["ident_b"])
    P.op("dve", lambda e: e.memset(epsln[:], LN_EPS), writes=["eps"])
    P.op("dve", lambda e: e.memset(epsrms[:], RMS_EPS), writes=["eps2"])
    onec = P.sb([128, 1], F32, "onec")
    P.op("dve", lambda e: e.memset(onec[:], 1.0), writes=["onec"])

    n_setup = len(P.ops)

    def phase_begin(k):
        if start == k and k > 0:
            del P.ops[n_setup:]
            P.barrier_at.clear()
            import os
            if os.environ.get("KMAXOPS"):
                P.maxops = n_setup + int(os.environ["KMAXOPS"])

    evac_rr = [0]

    def evac(out_ap, in_ap, reads, writes, scale=None, accum=False):
        evac_rr[0] ^= 1
        if evac_rr[0]:
            if scale is None:
                P.op("act", lambda e: e.activation(out=out_ap, in_=in_ap, func=AF.Copy),
                     reads, writes, accum=accum)
            else:
                P.op("act", lambda e: e.activation(out=out_ap, in_=in_ap, func=AF.Copy,
                                                   scale=scale), reads, writes, accum=accum)
        else:
            if scale is None:
                P.op("dve", lambda e: e.tensor_copy(out=out_ap, in_=in_ap), reads, writes, accum=accum)
            else:
                P.op("dve", lambda e: e.tensor_scalar(out=out_ap, in0=in_ap, scalar1=scale,
                                                      scalar2=None, op0=ALU.mult),
                     reads, writes, accum=accum)

    cast_rr = [0]

    def convert(src_ap, dst_ap, free_shape, tag):
        b = cast_rr[0] % 3
        cast_rr[0] += 1
        st = stg[b]
        cv = cvt[b]
        nel = int(np.prod(free_shape))
        sv = st[:, 0:nel]
        if len(free_shape) == 2:
            sv = sv.rearrange("p (a b) -> p a b", a=free_shape[0])
        P.dma("sp", sv, src_ap, reads=[], writes=[("stg", b)])
        eng = ("dve", "act", "pool")[b]
        if eng == "act":
            P.op("act", lambda e: e.activation(out=cv[:, 0:nel], in_=st[:, 0:nel], func=AF.Copy),
                 reads=[("stg", b)], writes=[("cvt", b)])
        else:
            P.op(eng, lambda e: e.tensor_copy(out=cv[:, 0:nel], in_=st[:, 0:nel]),
                 reads=[("stg", b)], writes=[("cvt", b)])
        P.dma("sp", dst_ap, cv[:, 0:nel], reads=[("cvt", b)], writes=[tag], accum=True)

    stg = [A.alloc([4096], F32) for _ in range(3)]
    cvt = [A.alloc([4096], BF16) for _ in range(3)]
    for gi, (kind, c0, gc) in enumerate(groups):
        nel = 16 * gc
        dst = win_b[goff[gi]:goff[gi] + 128 * nel].rearrange("(p f) -> p f", p=128)
        if nel <= 4096:
            convert(w_in[:, c0:c0 + gc].rearrange("(kc p) c -> p kc c", p=128), dst,
                    [16, gc], "win_b")
        else:
            for half in range(2):
                convert(w_in[half * 1024:(half + 1) * 1024, c0:c0 + gc]
                        .rearrange("(kc p) c -> p kc c", p=128),
                        dst[:, half * 8 * gc:(half + 1) * 8 * gc], [8, gc], "win_b")
    for kc2 in range(8):
        convert(w_out[kc2 * 256:(kc2 + 1) * 256, :].rearrange("(kc p) c -> p kc c", p=128),
                wout_b[:, kc2 * 2 * D:(kc2 + 1) * 2 * D], [2, D], "wout_b")
    for e_ in range(NEXP):
        for q in range(4):
            convert(w_gate[e_, :, q * 256:(q + 1) * 256].rearrange("(kc p) c -> p kc c", p=128),
                    wg_b[e_ * 4 + q], [16, 256], "wg_b")
            convert(w_up[e_, :, q * 256:(q + 1) * 256].rearrange("(kc p) c -> p kc c", p=128),
                    wu_b[e_ * 4 + q], [16, 256], "wu_b")
            convert(w_down[e_, q * 256:(q + 1) * 256, :].rearrange("(kc p) c -> p kc c", p=128),
                    wd_b[e_ * 4 + q], [2, D], "wd_b")
    if stop == 0:
        P.emit(final_wait_keys=["win_b","wout_b","wg_b","wu_b","wd_b"])
        P.close()
        return nc
    P.barrier()
    A.reset()

    phase_begin(1)
    xTb = [A.alloc([16, 512], BF16) for _ in range(2)]
    wbuf = [A.alloc([16 * 512], BF16) for _ in range(3)]
    ebuf = [A.alloc([512], F32) for _ in range(4)]
    NT = SA // 512
    wrr = 0
    err = 0
    prr = 0
    for tt in range(NT):
        own = tt * 512 >= SC
        t0 = tt * 512
        to = t0 - SC
        xb = xTb[tt % 2]
        xk = ("xTb", tt % 2)
        P.dma("pool", xb, xT[:, t0:t0 + 512].rearrange("(kc p) t -> p kc t", p=128),
              writes=[xk])
        for gi, (kind, c0, gc) in enumerate(groups):
            if kind in ("q", "z") and not own:
                continue
            wb = wbuf[wrr % 3]
            wk = ("wbuf", wrr % 3)
            wrr += 1
            wv = wb[:, 0:16 * gc].rearrange("p (kc c) -> p kc c", kc=16)
            P.dma("sp", wb[:, 0:16 * gc],
                  win_b[goff[gi]:goff[gi] + 128 * 16 * gc].rearrange("(p f) -> p f", p=128),
                  reads=["win_b"], writes=[wk])
            if kind in ("q", "k"):
                h = (c0 % 1024) // 128
                for m in range(2):
                    b = prr % 8
                    prr += 1
                    for kc in range(16):
                        P.op("pe", lambda e, b=b, kc=kc, m=m, wv=wv, xb=xb: e.matmul(
                            bank(b, 64), lhsT=wv[:, kc, m * 64:(m + 1) * 64], rhs=xb[:, kc, :],
                            start=(kc == 0), stop=(kc == 15)),
                            reads=[wk, xk], writes=[("ps", b)])
                    eb = ebuf[err % 4]
                    ek = ("ebuf", err % 4)
                    err += 1
                    ebv = eb.bitcast(BF16)[0:64, 0:512]
                    evac(ebv, bank(b, 64), [("ps", b)], [ek],
                         scale=(0.125 if kind == "q" else None))
                    if kind == "q":
                        P.dma("sp", qs[h * 2 + m, :, to:to + 512], ebv, reads=[ek], writes=["qs"], accum=True)
                    else:
                        P.dma("sp", ks[h * 2 + m, :, t0:t0 + 512], ebv, reads=[ek], writes=["ks"], accum=True)
            elif kind == "x":
                g = (c0 - 4096) // 128
                b = prr % 8
                prr += 1
                for kc in range(16):
                    P.op("pe", lambda e, b=b, kc=kc, wv=wv, xb=xb: e.matmul(
                        bank(b), lhsT=wv[:, kc, :], rhs=xb[:, kc, :],
                        start=(kc == 0), stop=(kc == 15)),
                        reads=[wk, xk], writes=[("ps", b)])
                eb = ebuf[err % 4]
                ek = ("ebuf", err % 4)
                err += 1
                evac(eb, bank(b), [("ps", b)], [ek])
                P.dma("sp", xbcs[g * 128:(g + 1) * 128, t0:t0 + 512], eb, reads=[ek],
                      writes=["xbcs"], accum=True)
            else:
                for ts in range(4):
                    b = prr % 8
                    prr += 1
                    for kc in range(16):
                        P.op("pe", lambda e, b=b, kc=kc, wv=wv, xb=xb, ts=ts, gc=gc: e.matmul(
                            bank(b, 128, gc), lhsT=xb[:, kc, ts * 128:(ts + 1) * 128],
                            rhs=wv[:, kc, :], start=(kc == 0), stop=(kc == 15)),
                            reads=[wk, xk], writes=[("ps", b)])
                    eb = ebuf[err % 4]
                    ek = ("ebuf", err % 4)
                    err += 1
                    if kind == "v":
                        ebv = eb.bitcast(BF16)[:, 0:512]
                        evac(ebv, bank(b), [("ps", b)], [ek])
                        P.dma("sp", vs[t0 + ts * 128:t0 + (ts + 1) * 128, c0 - 2048:c0 - 2048 + 512],
                              ebv, reads=[ek], writes=["vs"], accum=True)
                    elif kind == "z":
                        evac(eb, bank(b), [("ps", b)], [ek])
                        P.dma("sp", zs[to + ts * 128:to + (ts + 1) * 128, c0 - 3072:c0 - 3072 + 512],
                              eb, reads=[ek], writes=["zs"], accum=True)
                    else:
                        evac(eb[:, 0:16], bank(b, 128, 16), [("ps", b)], [ek])
                        P.dma("sp", dts[t0 + ts * 128:t0 + (ts + 1) * 128, :], eb[:, 0:16],
                              reads=[ek], writes=["dts"], accum=True)
    if stop == 1:
        P.emit(final_wait_keys=["qs","ks","vs","zs","xbcs","dts"])
        P.close()
        return nc
    P.barrier()
    A.reset()

    phase_begin(2)
    KA = [[A.alloc([SA], BF16, parts=68) for m in range(2)] for _ in range(2)]
    QA = [[A.alloc([SO], BF16, parts=68) for m in range(2)] for _ in range(2)]
    VA = [A.alloc([NB, 130], BF16) for _ in range(2)]
    PT = [A.alloc([512], BF16) for _ in range(3)]
    TT = A.alloc([8, 896], BF16)
    lamt = A.alloc([256], F32)
    lsc = A.alloc([64], F32)
    lam4 = A.alloc([8], F32)
    nwb = A.alloc([128], F32)
    ep_a = [A.alloc([128], F32) for _ in range(2)]
    ep_o = [A.alloc([128], F32) for _ in range(2)]
    ep_j = A.alloc([128], F32)
    ep_s = [A.alloc([8], F32) for _ in range(2)]
    ob = [A.alloc([128], BF16) for _ in range(2)]
    mo = [A.alloc([512], BF16) for _ in range(2)]

    P.dma("pool", TT, ttab.rearrange("p (h c) -> p h c", h=8), writes=["TT"])
    P.dma("sp", lamt, lamv.partition_broadcast(128), writes=["lamt"])
    P.dma("sp", nwb, anw.partition_broadcast(128), writes=["nwb"])
    P.op("dve", lambda e: e.tensor_scalar(out=nwb, in0=nwb, scalar1=(1.0 - LAMBDA_INIT),
                                          scalar2=None, op0=ALU.mult), reads=["nwb"], writes=["nwb"])
    for i in range(2):
        P.op("dve", lambda e, i=i: e.tensor_tensor(out=lsc, in0=lamt[:, i * 128:i * 128 + 64],
                                                  in1=lamt[:, i * 128 + 64:i * 128 + 128],
                                                  op=ALU.mult), reads=["lamt"], writes=["lsc"])
        P.op("dve", lambda e, i=i: e.tensor_reduce(out=lam4[:, i:i + 1], in_=lsc, axis=AX.X,
                                                  op=ALU.add), reads=["lsc"], writes=["lam4"])
    P.op("act", lambda e: e.activation(out=lam4[:, 4:6], in_=lam4[:, 0:2], func=AF.Exp),
         reads=["lam4"], writes=["lam4"])
    P.op("dve", lambda e: e.scalar_tensor_tensor(out=lam4[:, 2:3], in0=lam4[:, 5:6],
                                                 scalar=-LAMBDA_INIT, in1=lam4[:, 4:5],
                                                 op0=ALU.add, op1=ALU.subtract),
         reads=["lam4"], writes=["neglam"])
    neglam = lam4[:, 2:3]
    for vb in range(2):
        P.op("dve", lambda e, vb=vb: e.memset(VA[vb][:, :, 128:130], 1.0), writes=[("VA", vb)])

    NQT = SO // 512
    strr = 0
    ptrr = 0
    eprr = 0
    for h in range(8):
        hb = h % 2
        kkey = ("KA", hb)
        qkey = ("QA", hb)
        vkey = ("VA", hb)
        for m in range(2):
            P.dma("sp", KA[hb][m][0:64, :], ks[h * 2 + m], reads=["ks"], writes=[kkey], accum=(m > 0))
            for c0_ in range(0, SA, 2048):
                c1_ = min(SA, c0_ + 2048)
                P.dma("pool", KA[hb][m][64:68, c0_:c1_], ktab[h, :, c0_:c1_], writes=[kkey], accum=True)
            P.dma("sp", QA[hb][m][0:64, :], qs[h * 2 + m], reads=["qs"], writes=[qkey], accum=(m > 0))
            for c0_ in range(0, SO, 2048):
                c1_ = min(SO, c0_ + 2048)
                P.dma("pool", QA[hb][m][64:68, c0_:c1_], qtab[h, :, c0_:c1_], writes=[qkey], accum=True)
        for j0 in range(0, NB, 16):
            j1 = min(NB, j0 + 16)
            P.dma("sp", VA[hb][:, j0:j1, 0:128],
                  vs[j0 * 128:j1 * 128, h * 128:(h + 1) * 128].rearrange("(j p) d -> p j d", p=128),
                  reads=["vs"], writes=[vkey], accum=(j0 > 0))
        for qt in range(NQT):
            gb = (SC + qt * 512) // 128
            okeys = [[("O", m, i) for i in range(4)] for m in range(2)]
            for j in range(gb + 4):
                r = j - gb
                for m in range(2):
                    sb_ = strr % 3
                    strr += 1
                    P.op("pe", lambda e, sb_=sb_, m=m, j=j, qt=qt, hb=hb, r=r: e.matmul(
                        bank(sb_), lhsT=KA[hb][m][:, j * 128:(j + 1) * 128],
                        rhs=QA[hb][m][:, qt * 512:(qt + 1) * 512], start=True, stop=(r < 0)),
                        reads=[kkey, qkey], writes=[("ps", sb_)])
                    if r >= 0:
                        P.op("pe", lambda e, sb_=sb_, h=h, r=r: e.matmul(
                            bank(sb_), lhsT=ident_b[:], rhs=TT[:, h, (3 - r) * 128:(3 - r) * 128 + 512],
                            start=False, stop=True),
                            reads=["ident_b", "TT"], writes=[("ps", sb_)])
                    pb = ptrr % 3
                    ptrr += 1
                    P.op("act", lambda e, sb_=sb_, pb=pb: e.activation(
                        out=PT[pb], in_=bank(sb_), func=AF.Exp),
                        reads=[("ps", sb_)], writes=[("PT", pb)])
                    for i in range(4):
                        if r >= 0 and i < r:
                            continue
                        ob_ = 4 + m * 2 + i // 2
                        P.op("pe", lambda e, pb=pb, i=i, j=j, hb=hb, ob_=ob_, gb=gb: e.matmul(
                            bank(ob_, 128, 129, (i % 2) * 256), lhsT=PT[pb][:, i * 128:(i + 1) * 128],
                            rhs=VA[hb][:, j, 0:129], start=(j == 0 and i % 2 == 0), stop=(j == gb + i)),
                            reads=[("PT", pb), vkey], writes=[okeys[m][i]])
            mb = (h * NQT + qt) % 2
            for i in range(4):
                eb_ = eprr % 2
                O0 = bank(4 + i // 2, 128, 129, (i % 2) * 256)
                O1 = bank(6 + i // 2, 128, 129, (i % 2) * 256)
                es = ep_s[eb_]
                ek = ("ep", eb_)
                P.op("dve", lambda e, es=es, O0=O0: e.reciprocal(out=es[:, 0:1], in_=O0[:, 128:129]),
                     reads=[okeys[0][i]], writes=[ek])
                P.op("dve", lambda e, es=es, O1=O1: e.reciprocal(out=es[:, 1:2], in_=O1[:, 128:129]),
                     reads=[okeys[1][i]], writes=[ek])
                P.op("dve", lambda e, es=es: e.tensor_tensor(out=es[:, 2:3], in0=es[:, 1:2], in1=neglam,
                                                             op=ALU.mult),
                     reads=[ek, "neglam"], writes=[ek])
                P.op("dve", lambda e, es=es, O0=O0, eb_=eb_: e.tensor_scalar(
                    out=ep_a[eb_], in0=O0[:, 0:128], scalar1=es[:, 0:1], scalar2=None, op0=ALU.mult),
                    reads=[ek, okeys[0][i]], writes=[("epa", eb_)])
                P.op("dve", lambda e, es=es, O1=O1, eb_=eb_: e.scalar_tensor_tensor(
                    out=ep_o[eb_], in0=O1[:, 0:128], scalar=es[:, 2:3], in1=ep_a[eb_],
                    op0=ALU.mult, op1=ALU.add),
                    reads=[ek, okeys[1][i], ("epa", eb_)], writes=[("epo", eb_)])
                P.op("act", lambda e, es=es, eb_=eb_: e.activation(
                    out=ep_j, in_=ep_o[eb_], func=AF.Square, accum_out=es[:, 3:4]),
                    reads=[("epo", eb_)], writes=[("ep2", eb_), "epj"])
                P.op("act", lambda e, es=es: e.activation(
                    out=es[:, 4:5], in_=es[:, 3:4], func=AF.Sqrt, bias=epsrms[:], scale=1.0 / 128),
                    reads=[("ep2", eb_), "eps2"], writes=[("ep3", eb_)])
                P.op("dve", lambda e, es=es: e.reciprocal(out=es[:, 5:6], in_=es[:, 4:5]),
                     reads=[("ep3", eb_)], writes=[("ep4", eb_)])
                P.op("dve", lambda e, es=es, eb_=eb_: e.scalar_tensor_tensor(
                    out=ob[eb_], in0=ep_o[eb_], scalar=es[:, 5:6], in1=nwb, op0=ALU.mult, op1=ALU.mult),
                    reads=[("ep4", eb_), ("epo", eb_), "nwb"], writes=[("ob", eb_)])
                P.op("pe", lambda e, eb_=eb_, i=i: e.transpose(
                    bankbf(3, 128, 128, i * 128), ob[eb_], ident_b[:]),
                    reads=[("ob", eb_), "ident_b"], writes=[("ps", 3, i)])
                evac(mo[mb][:, i * 128:(i + 1) * 128], bankbf(3, 128, 128, i * 128),
                     [("ps", 3, i)], [("mo", mb)], accum=(i > 0))
                eprr += 1
            P.dma("sp", mixT[h * 128:(h + 1) * 128, qt * 512:(qt + 1) * 512], mo[mb],
                  reads=[("mo", mb)], writes=["mixT"], accum=True)
    if stop == 2:
        P.emit(final_wait_keys=["mixT"])
        P.close()
        return nc
    P.barrier()
    A.reset()

    phase_begin(3)
    NCH = SA // 64
    cwt = A.alloc([12, 4], F32)
    cbt = A.alloc([12], F32)
    tri = A.alloc([64], F32, parts=64)
    umat = A.alloc([64], F32, parts=64)
    ones64 = A.alloc([128], F32, parts=64)
    flagt = A.alloc([1], F32)
    dtbt = A.alloc([16], F32, parts=64)
    Aneg = A.alloc([16], F32, parts=64)
    Dt = A.alloc([16], F32, parts=64)
    snwt = A.alloc([1024], F32, parts=64)
    dtall = A.alloc([NCH, 16], F32, parts=64)
    aall = A.alloc([NCH, 16], F32, parts=64)
    xc = [A.alloc([12, 515], F32) for _ in range(1)]
    acc = A.alloc([512], F32)
    xcv = A.alloc([8, 512], F32)
    bct = A.alloc([4, 512], BF16)
    Rt = A.alloc([16, 64], F32, parts=64)
    Et = A.alloc([16, 64], F32, parts=64)
    Ebt = A.alloc([16, 64], F32)
    mcb = A.alloc([2, 64], F32, parts=64)
    Mt = A.alloc([16, 64], BF16, parts=64)
    Mtmp = A.alloc([16, 64], F32, parts=64)
    x32 = A.alloc([1024], F32, parts=64)
    xbf = A.alloc([1024], BF16, parts=64)
    wsc = A.alloc([16], F32, parts=64)
    xw = A.alloc([1024], BF16, parts=64)
    Btok = A.alloc([2, 128], BF16, parts=64)
    Cs = A.alloc([16, 64], BF16)
    Hs = A.alloc([1024], F32)
    Hbf = A.alloc([1024], BF16)
    yt = A.alloc([1024], F32, parts=64)
    zt = A.alloc([1024], F32, parts=64)
    ysq = A.alloc([1024], F32, parts=64)
    ss = A.alloc([8], F32, parts=64)
    ybf = A.alloc([1024], BF16, parts=64)
    ymo = A.alloc([8, 64], BF16)

    P.dma("sp", cwt, cw.rearrange("p (g k) -> p g k", g=12), writes=["cwt"])
    P.dma("sp", cbt, cb, writes=["cbt"])
    P.dma("sp", tri, tri_in, writes=["tri"])
    P.dma("sp", umat, umat_in, writes=["umat"])
    P.dma("sp", flagt, flag_in, writes=["flagt"])
    P.dma("sp", dtbt, dtb.partition_broadcast(64), writes=["dtbt"])
    P.dma("sp", Aneg, alog.partition_broadcast(64), writes=["Aneg"])
    P.dma("sp", Dt, dsk.partition_broadcast(64), writes=["Dt"])
    P.dma("sp", snwt, snw.partition_broadcast(64), writes=["snwt"])
    for c0_ in range(0, NCH, 32):
        c1_ = min(NCH, c0_ + 32)
        P.dma("sp", dtall[:, c0_:c1_, :], dts[c0_ * 64:c1_ * 64, :].rearrange("(c l) h -> l c h", l=64),
              reads=["dts"], writes=["dtall"], accum=(c0_ > 0))
    P.op("dve", lambda e: e.memset(ones64, 1.0), writes=["ones64"])
    P.op("dve", lambda e: e.memset(Hs, 0.0), writes=["Hs"])
    P.op("dve", lambda e: e.memset(Hbf, 0.0), writes=["Hbf"])
    P.op("act", lambda e: e.activation(out=Aneg, in_=Aneg, func=AF.Exp), reads=["Aneg"], writes=["Aneg"])
    P.op("dve", lambda e: e.tensor_scalar(out=Aneg, in0=Aneg, scalar1=-1.0, scalar2=None, op0=ALU.mult),
         reads=["Aneg"], writes=["Aneg"])
    P.op("dve", lambda e: e.tensor_tensor(out=dtall, in0=dtall,
                                          in1=dtbt.unsqueeze(1).to_broadcast([64, NCH, 16]), op=ALU.add),
         reads=["dtall", "dtbt"], writes=["dtall"])
    P.op("act", lambda e: e.activation(out=dtall, in_=dtall, func=AF.Exp), reads=["dtall"], writes=["dtall"])
    P.op("act", lambda e: e.activation(out=dtall, in_=dtall, func=AF.Ln, bias=onec[0:64, :], scale=1.0),
         reads=["dtall", "onec"], writes=["dtall"])
    P.op("dve", lambda e: e.tensor_tensor(out=aall, in0=dtall,
                                          in1=Aneg.unsqueeze(1).to_broadcast([64, NCH, 16]), op=ALU.mult),
         reads=["dtall", "Aneg"], writes=["aall"])

    for blk in range(SA // 512):
        t0 = blk * 512
        xcb = xc[0]
        if t0 == 0:
            P.op("dve", lambda e: e.memset(xcb[:, :, 0:3], 0.0), writes=["xc"])
            P.dma("sp", xcb[:, :, 3:515], xbcs[:, 0:512].rearrange("(g p) t -> p g t", p=128),
                  reads=["xbcs"], writes=["xc"])
        else:
            P.dma("sp", xcb, xbcs[:, t0 - 3:t0 + 512].rearrange("(g p) t -> p g t", p=128),
                  reads=["xbcs"], writes=["xc"])
        for g in range(12):
            P.op("dve", lambda e, g=g: e.tensor_scalar(out=acc, in0=xcb[:, g, 3:515],
                                                      scalar1=cwt[:, g, 3:4], scalar2=None, op0=ALU.mult),
                 reads=["xc", "cwt"], writes=["acc"])
            for k in range(3):
                P.op("dve", lambda e, g=g, k=k: e.scalar_tensor_tensor(
                    out=acc, in0=xcb[:, g, k:k + 512], scalar=cwt[:, g, k:k + 1], in1=acc,
                    op0=ALU.mult, op1=ALU.add), reads=["xc", "cwt", "acc"], writes=["acc"])
            if g < 8:
                P.op("act", lambda e, g=g: e.activation(out=xcv[:, g, :], in_=acc, func=AF.Silu,
                                                       bias=cbt[:, g:g + 1], scale=1.0),
                     reads=["acc", "cbt"], writes=[("xcv", g)])
            else:
                P.op("act", lambda e, g=g: e.activation(out=bct[:, g - 8, :], in_=acc, func=AF.Silu,
                                                       bias=cbt[:, g:g + 1], scale=1.0),
                     reads=["acc", "cbt"], writes=[("bct", g - 8)])
        for cl in range(8):
            c = blk * 8 + cl
            own = c * 64 >= SC
            csl = slice(cl * 64, (cl + 1) * 64)
            a_c = aall[:, c, :]
            dt_c = dtall[:, c, :]
            for g in range(8):
                P.op("pe", lambda e, g=g, csl=csl: e.transpose(
                    bank(g // 4, 64, 128, (g % 4) * 128), xcv[:, g, csl], ident_f[:]),
                    reads=[("xcv", g), "ident_f"], writes=[("ps", g // 4)])
            for hb_ in range(2):
                P.op("act", lambda e, hb_=hb_: e.activation(
                    out=x32[:, hb_ * 512:(hb_ + 1) * 512], in_=bank(hb_, 64), func=AF.Copy),
                    reads=[("ps", hb_)], writes=[("x32", hb_)])
                P.op("act", lambda e, hb_=hb_: e.activation(
                    out=xbf[:, hb_ * 512:(hb_ + 1) * 512], in_=bank(hb_, 64), func=AF.Copy),
                    reads=[("ps", hb_)], writes=[("xbf", hb_)])
            for g2 in range(2):
                P.op("pe", lambda e, g2=g2, csl=csl: e.transpose(
                    bankbf(7, 64, 128, 512 + g2 * 128), bct[:, g2, csl], ident_b[:]),
                    reads=[("bct", g2), "ident_b"], writes=[("ps", 7, "b")])
            P.op("dve", lambda e: e.tensor_copy(out=Btok.rearrange("p a b -> p (a b)"),
                                                in_=bankbf(7, 64, 256, 512)),
                 reads=[("ps", 7, "b")], writes=["Btok"])
            P.op("dve", lambda e, a_c=a_c: e.tensor_tensor(
                out=Rt, in0=tri.unsqueeze(1).to_broadcast([64, 16, 64]),
                in1=a_c.unsqueeze(2).to_broadcast([64, 16, 64]), op=ALU.mult),
                reads=["tri", "aall"], writes=["Rt"])
            Rf = Rt.rearrange("p a b -> p (a b)")
            for hb_ in range(2):
                P.op("pe", lambda e, hb_=hb_: e.matmul(
                    bank(2 + hb_, 64), lhsT=umat, rhs=Rf[:, hb_ * 512:(hb_ + 1) * 512],
                    start=True, stop=True), reads=["umat", "Rt"], writes=[("ps", 2 + hb_)])
                P.op("pe", lambda e, hb_=hb_: e.matmul(
                    bank(4 + hb_), lhsT=ones64, rhs=Rf[:, hb_ * 512:(hb_ + 1) * 512],
                    start=True, stop=True), reads=["ones64", "Rt"], writes=[("ps", 4 + hb_)])
            Ef = Et.rearrange("p a b -> p (a b)")
            Ebf = Ebt.rearrange("p a b -> p (a b)")
            for hb_ in range(2):
                P.op("act", lambda e, hb_=hb_: e.activation(
                    out=Ef[:, hb_ * 512:(hb_ + 1) * 512], in_=bank(2 + hb_, 64), func=AF.Exp),
                    reads=[("ps", 2 + hb_)], writes=[("Et", hb_)])
                P.op("act", lambda e, hb_=hb_: e.activation(
                    out=Ebf[:, hb_ * 512:(hb_ + 1) * 512], in_=bank(4 + hb_), func=AF.Exp),
                    reads=[("ps", 4 + hb_)], writes=[("Ebt", hb_)])
            ekeys = [("Et", 0), ("Et", 1)]
            ebkeys = [("Ebt", 0), ("Ebt", 1)]
            P.op("dve", lambda e, dt_c=dt_c: e.tensor_tensor(out=wsc, in0=dt_c, in1=Et[:, :, 63],
                                                            op=ALU.mult),
                 reads=ekeys + ["dtall"], writes=["wsc"])
            P.op("dve", lambda e: e.tensor_tensor(
                out=xw.rearrange("p (h d) -> p h d", h=16),
                in0=x32.rearrange("p (h d) -> p h d", h=16),
                in1=wsc.unsqueeze(2).to_broadcast([64, 16, 64]), op=ALU.mult),
                reads=["wsc", ("x32", 0), ("x32", 1)], writes=["xw"])
            if own:
                if c * 64 == SC:
                    P.op("dve", lambda e: e.tensor_scalar(out=Hs, in0=Hs, scalar1=flagt[:, 0:1],
                                                          scalar2=None, op0=ALU.mult),
                         reads=["Hs", "flagt"], writes=["Hs"])
                    P.op("dve", lambda e: e.tensor_copy(out=Hbf, in_=Hs), reads=["Hs"], writes=["Hbf"])
                for g2 in range(2):
                    P.op("pe", lambda e, g2=g2, csl=csl: e.matmul(
                        bank(6, 64, 64, g2 * 64), lhsT=bct[:, g2, csl], rhs=bct[:, 2 + g2, csl],
                        start=True, stop=True), reads=[("bct", g2), ("bct", 2 + g2)],
                        writes=[("ps", 6, "cb")])
                P.op("dve", lambda e: e.tensor_tensor(
                    out=mcb, in0=bank(6, 64, 128).rearrange("p (g l) -> p g l", g=2),
                    in1=tri.unsqueeze(1).to_broadcast([64, 2, 64]), op=ALU.mult),
                    reads=[("ps", 6, "cb"), "tri"], writes=["mcb"])
                for g2 in range(2):
                    P.op("dve", lambda e, g2=g2: e.tensor_tensor(
                        out=Mtmp[:, g2 * 8:(g2 + 1) * 8, :], in0=Et[:, g2 * 8:(g2 + 1) * 8, :],
                        in1=mcb[:, g2:g2 + 1, :].to_broadcast([64, 8, 64]), op=ALU.mult),
                        reads=ekeys + ["mcb"], writes=[("Mtmp", g2)])
                P.op("dve", lambda e, dt_c=dt_c: e.tensor_tensor(
                    out=Mt, in0=Mtmp, in1=dt_c.unsqueeze(2).to_broadcast([64, 16, 64]), op=ALU.mult),
                    reads=[("Mtmp", 0), ("Mtmp", 1), "dtall"], writes=["Mt"])
                for g2 in range(2):
                    P.op("dve", lambda e, g2=g2, csl=csl: e.tensor_tensor(
                        out=Cs[:, g2 * 8:(g2 + 1) * 8, :], in0=Ebt[:, g2 * 8:(g2 + 1) * 8, :],
                        in1=bct[:, 2 + g2, csl].unsqueeze(1).to_broadcast([128, 8, 64]), op=ALU.mult),
                        reads=ebkeys + [("bct", 2 + g2)], writes=[("Cs", g2)])
                for hh in range(16):
                    yb = bank(hh // 8, 64, 64, (hh % 8) * 64)
                    P.op("pe", lambda e, hh=hh, yb=yb: e.matmul(
                        yb, lhsT=Mt[:, hh, :], rhs=xbf[:, hh * 64:(hh + 1) * 64], start=True, stop=False),
                        reads=["Mt", ("xbf", hh // 8), ("x32", hh // 8)], writes=[("ps", hh // 8)])
                    P.op("pe", lambda e, hh=hh, yb=yb: e.matmul(
                        yb, lhsT=Cs[:, hh, :], rhs=Hbf[:, hh * 64:(hh + 1) * 64], start=False, stop=True),
                        reads=[("Cs", hh // 8), "Hbf"], writes=[("ps", hh // 8)])
            for g2 in range(2):
                P.op("pe", lambda e, g2=g2: e.matmul(
                    bank(2 + g2), lhsT=Btok[:, g2, :], rhs=xw[:, g2 * 512:(g2 + 1) * 512],
                    start=True, stop=True), reads=["Btok", "xw"] + ekeys, writes=[("ps", 2 + g2)])
            P.op("dve", lambda e: e.tensor_tensor(
                out=Hs.rearrange("p (h d) -> p h d", h=16), in0=Hs.rearrange("p (h d) -> p h d", h=16),
                in1=Ebt[:, :, 63:64].to_broadcast([128, 16, 64]), op=ALU.mult),
                reads=["Hs", "Hbf"] + ebkeys, writes=["Hs"])
            for g2 in range(2):
                P.op("dve", lambda e, g2=g2: e.tensor_tensor(
                    out=Hs[:, g2 * 512:(g2 + 1) * 512], in0=Hs[:, g2 * 512:(g2 + 1) * 512],
                    in1=bank(2 + g2), op=ALU.add), reads=["Hs", ("ps", 2 + g2)], writes=["Hs"])
            if own:
                to = c * 64 - SC
                P.dma("sp", zt, zs[to:to + 64, :], reads=["zs"], writes=["zt"])
                P.op("dve", lambda e: e.tensor_tensor(
                    out=yt.rearrange("p (h d) -> p h d", h=16), in0=x32.rearrange("p (h d) -> p h d", h=16),
                    in1=Dt.unsqueeze(2).to_broadcast([64, 16, 64]), op=ALU.mult),
                    reads=[("x32", 0), ("x32", 1), "Dt"], writes=["yt"])
                for hb_ in range(2):
                    P.op("dve", lambda e, hb_=hb_: e.tensor_tensor(
                        out=yt[:, hb_ * 512:(hb_ + 1) * 512], in0=yt[:, hb_ * 512:(hb_ + 1) * 512],
                        in1=bank(hb_, 64), op=ALU.add), reads=["yt", ("ps", hb_)], writes=["yt"])
                P.op("act", lambda e: e.activation(out=zt, in_=zt, func=AF.Silu), reads=["zt"], writes=["zt"])
                P.op("dve", lambda e: e.tensor_tensor(out=yt, in0=yt, in1=zt, op=ALU.mult),
                     reads=["yt", "zt"], writes=["yt"])
                for g2 in range(2):
                    P.op("act", lambda e, g2=g2: e.activation(
                        out=ysq[:, g2 * 512:(g2 + 1) * 512], in_=yt[:, g2 * 512:(g2 + 1) * 512],
                        func=AF.Square, accum_out=ss[:, g2:g2 + 1]), reads=["yt"], writes=["ysq", ("ss", g2)])
                P.op("act", lambda e: e.activation(out=ss[:, 2:4], in_=ss[:, 0:2], func=AF.Sqrt,
                                                   bias=epsrms[0:64, :], scale=1.0 / 512),
                     reads=[("ss", 0), ("ss", 1), "eps2"], writes=["ss2"])
                P.op("dve", lambda e: e.reciprocal(out=ss[:, 4:6], in_=ss[:, 2:4]), reads=["ss2"],
                     writes=["ss3"])
                for g2 in range(2):
                    P.op("dve", lambda e, g2=g2: e.scalar_tensor_tensor(
                        out=ybf[:, g2 * 512:(g2 + 1) * 512], in0=yt[:, g2 * 512:(g2 + 1) * 512],
                        scalar=ss[:, 4 + g2:5 + g2], in1=snwt[:, g2 * 512:(g2 + 1) * 512],
                        op0=ALU.mult, op1=ALU.mult), reads=["yt", "ss3", "snwt"], writes=["ybf"])
                for g in range(8):
                    P.op("pe", lambda e, g=g: e.transpose(
                        bankbf(7, 128, 64, g * 64), ybf[:, g * 128:(g + 1) * 128], ident_b[0:64, 0:64]),
                        reads=["ybf", "ident_b"], writes=[("ps", 7)])
                P.op("act", lambda e: e.activation(out=ymo.rearrange("p a b -> p (a b)"),
                                                   in_=bankbf(7, 128, 512, 0), func=AF.Copy),
                     reads=[("ps", 7)], writes=["ymo"])
                P.dma("sp", mixT[1024:2048, to:to + 64].rearrange("(g p) t -> p g t", p=128), ymo,
                      reads=["ymo"], writes=["mixT"], accum=True)
            P.op("act", lambda e: e.activation(out=Hbf, in_=Hs, func=AF.Copy), reads=["Hs"], writes=["Hbf"])
    if stop == 3:
        P.emit(final_wait_keys=["mixT"])
        P.close()
        return nc
    P.barrier()
    A.reset()

    phase_begin(4)
    wo = A.alloc([16, D], BF16)
    mxt = [A.alloc([16, 512], BF16) for _ in range(2)]
    xot = [A.alloc([D], F32) for _ in range(2)]
    rt = [A.alloc([D], F32) for _ in range(2)]
    g1 = A.alloc([D], F32)
    b1 = A.alloc([D], F32)
    st6 = A.alloc([4, 6], F32)
    mv = A.alloc([8], F32)
    hT32 = A.alloc([16, 128], F32)
    hTb = A.alloc([16, 128], BF16)
    wr32 = A.alloc([16, 36], F32)
    brt = A.alloc([36], F32)
    lg = A.alloc([36], F32)
    rs = A.alloc([16], F32)
    Gt = A.alloc([4], F32)
    elm = A.alloc([4, 8], F32)
    top8 = A.alloc([8], F32)
    msk = A.alloc([32], F32)
    ext = A.alloc([32], F32)
    P.dma("sp", wo.rearrange("p a b -> p (a b)"), wout_b, reads=["wout_b"], writes=["wo"])
    P.dma("sp", g1, ln1g.partition_broadcast(128), writes=["g1"])
    P.dma("sp", b1, ln1b.partition_broadcast(128), writes=["b1"])
    P.dma("sp", wr32, wr.rearrange("(kc p) c -> p kc c", p=128), writes=["wr32"])
    P.dma("sp", brt, br.partition_broadcast(128), writes=["brt"])

    def layernorm(src, dst, gk, bk, gt, bt, rkey, okey, tag, st6, mv):
        for cc in range(4):
            P.op("dve", lambda e, cc=cc: e.bn_stats(out=st6[:, cc, :], in_=src[:, cc * 512:(cc + 1) * 512]),
                 reads=[rkey], writes=[("st6", tag)])
        P.op("dve", lambda e: e.bn_aggr(out=mv[:, 0:2], in_=st6.rearrange("p a b -> p (a b)")),
             reads=[("st6", tag)], writes=[("mv", tag)])
        P.op("act", lambda e: e.activation(out=mv[:, 2:3], in_=mv[:, 1:2], func=AF.Sqrt,
                                           bias=epsln[:], scale=1.0),
             reads=[("mv", tag), "eps"], writes=[("mv2", tag)])
        P.op("dve", lambda e: e.reciprocal(out=mv[:, 3:4], in_=mv[:, 2:3]), reads=[("mv2", tag)],
             writes=[("mv3", tag)])
        P.op("dve", lambda e: e.tensor_scalar(out=src, in0=src, scalar1=mv[:, 0:1], scalar2=mv[:, 3:4],
                                              op0=ALU.subtract, op1=ALU.mult),
             reads=[rkey, ("mv3", tag), ("mv", tag)], writes=[rkey])
        P.op("dve", lambda e: e.tensor_tensor(out=src, in0=src, in1=gt, op=ALU.mult),
             reads=[rkey, gk], writes=[rkey])
        P.op("dve", lambda e: e.tensor_tensor(out=dst, in0=src, in1=bt, op=ALU.add),
             reads=[rkey, bk], writes=[okey])

    for t4 in range(SO // 512):
        mb = t4 % 2
        P.dma("sp", mxt[mb], mixT[:, t4 * 512:(t4 + 1) * 512].rearrange("(kc p) t -> p kc t", p=128),
              reads=["mixT"], writes=[("mxt", mb)])
        for ts in range(4):
            ti = t4 * 4 + ts
            tb = ti % 2
            tok0 = ti * 128
            P.dma("sp", xot[tb], xo[tok0:tok0 + 128, :], writes=[("xot", tb)])
            for cbk in range(4):
                for kc in range(16):
                    P.op("pe", lambda e, cbk=cbk, kc=kc, ts=ts, mb=mb: e.matmul(
                        bank(cbk), lhsT=mxt[mb][:, kc, ts * 128:(ts + 1) * 128],
                        rhs=wo[:, kc, cbk * 512:(cbk + 1) * 512], start=(kc == 0), stop=(kc == 15)),
                        reads=[("mxt", mb), "wo"], writes=[("ps", cbk)])
                P.op("dve", lambda e, cbk=cbk, tb=tb: e.scalar_tensor_tensor(
                    out=rt[tb][:, cbk * 512:(cbk + 1) * 512], in0=xot[tb][:, cbk * 512:(cbk + 1) * 512],
                    scalar=DN_ALPHA, in1=bank(cbk), op0=ALU.mult, op1=ALU.add),
                    reads=[("xot", tb), ("ps", cbk)], writes=[("rt", tb)])
            layernorm(rt[tb], rt[tb], "g1", "b1", g1, b1, ("rt", tb), ("rt", tb), "a", st6, mv)
            P.dma("sp", h1s[tok0:tok0 + 128, :], rt[tb], reads=[("rt", tb)], writes=["h1s"], accum=True)
            for half in range(2):
                for k8 in range(8):
                    kc = half * 8 + k8
                    P.op("pe", lambda e, kc=kc, k8=k8, half=half, tb=tb: e.transpose(
                        bank(4 + half * 2 + k8 // 4, 128, 128, (k8 % 4) * 128),
                        rt[tb][:, kc * 128:(kc + 1) * 128], ident_f[:]),
                        reads=[("rt", tb), "ident_f"], writes=[("ps", 4 + half * 2 + k8 // 4)])
                for q2 in range(2):
                    bq = 4 + half * 2 + q2
                    kc0 = half * 8 + q2 * 4
                    P.op("act", lambda e, bq=bq, kc0=kc0: e.activation(
                        out=hT32[:, kc0:kc0 + 4, :].rearrange("p a b -> p (a b)"), in_=bank(bq), func=AF.Copy),
                        reads=[("ps", bq)], writes=[("hT32", kc0)])
                    P.op("dve", lambda e, bq=bq, kc0=kc0: e.tensor_copy(
                        out=hTb[:, kc0:kc0 + 4, :].rearrange("p a b -> p (a b)"), in_=bank(bq)),
                        reads=[("ps", bq)], writes=[("hTb", kc0)])
            P.dma("sp", h1T[:, tok0:tok0 + 128].rearrange("(kc p) t -> p kc t", p=128), hTb,
                  reads=[("hTb", k) for k in (0, 4, 8, 12)], writes=["h1T"], accum=True)
            for kc in range(16):
                P.op("pe", lambda e, kc=kc: e.matmul(
                    bank(0, 128, 36), lhsT=hT32[:, kc, :], rhs=wr32[:, kc, :], start=(kc == 0), stop=(kc == 15)),
                    reads=[("hT32", (kc // 4) * 4), "wr32"], writes=[("ps", 0)])
            P.op("dve", lambda e: e.tensor_tensor(out=lg, in0=bank(0, 128, 36), in1=brt, op=ALU.add),
                 reads=[("ps", 0), "brt"], writes=["lg"])
            gl = lg[:, 0:4]
            el = lg[:, 4:36].rearrange("p (g e) -> p g e", g=4)
            P.op("dve", lambda e: e.tensor_reduce(out=rs[:, 0:1], in_=gl, axis=AX.X, op=ALU.max),
                 reads=["lg"], writes=["rs0"])
            P.op("dve", lambda e: e.tensor_scalar(out=rs[:, 1:2], in0=rs[:, 0:1], scalar1=-1.0, scalar2=None,
                                                  op0=ALU.mult), reads=["rs0"], writes=["rs1"])
            P.op("act", lambda e: e.activation(out=Gt, in_=gl, func=AF.Exp, bias=rs[:, 1:2], scale=1.0,
                                               accum_out=rs[:, 2:3]),
                 reads=["lg", "rs1"], writes=["Gt", "rs2"])
            P.op("dve", lambda e: e.reciprocal(out=rs[:, 3:4], in_=rs[:, 2:3]), reads=["rs2"], writes=["rs3"])
            P.op("dve", lambda e: e.tensor_scalar(out=Gt, in0=gl, scalar1=rs[:, 0:1], scalar2=None,
                                                  op0=ALU.is_ge), reads=["lg", "rs0", "Gt"], writes=["Gt"])
            P.op("dve", lambda e: e.tensor_scalar(out=Gt, in0=Gt, scalar1=-NEG, scalar2=NEG,
                                                  op0=ALU.mult, op1=ALU.add), reads=["Gt"], writes=["Gt"])
            P.op("dve", lambda e: e.tensor_tensor(out=elm, in0=el, in1=Gt.unsqueeze(2).to_broadcast([128, 4, 8]),
                                                  op=ALU.add), reads=["lg", "Gt"], writes=["elm"])
            elf = elm.rearrange("p a b -> p (a b)")
            P.op("dve", lambda e: e.max(out=top8, in_=elf), reads=["elm"], writes=["top8"])
            P.op("dve", lambda e: e.tensor_scalar(out=msk, in0=elf, scalar1=top8[:, 1:2], scalar2=None,
                                                  op0=ALU.is_ge), reads=["elm", "top8"], writes=["msk"])
            P.op("dve", lambda e: e.tensor_scalar(out=rs[:, 4:5], in0=top8[:, 0:1], scalar1=-1.0, scalar2=None,
                                                  op0=ALU.mult), reads=["top8"], writes=["rs4"])
            P.op("act", lambda e: e.activation(out=ext, in_=elf, func=AF.Exp, bias=rs[:, 4:5], scale=1.0),
                 reads=["elm", "rs4"], writes=["ext"])
            P.op("act", lambda e: e.activation(out=rs[:, 5:6], in_=top8[:, 1:2], func=AF.Exp, bias=rs[:, 4:5],
                                               scale=1.0), reads=["top8", "rs4"], writes=["rs5"])
            P.op("dve", lambda e: e.tensor_scalar(out=rs[:, 6:7], in0=rs[:, 5:6], scalar1=1.0, scalar2=None,
                                                  op0=ALU.add), reads=["rs5"], writes=["rs6"])
            P.op("dve", lambda e: e.reciprocal(out=rs[:, 7:8], in_=rs[:, 6:7]), reads=["rs6"], writes=["rs7"])
            P.op("dve", lambda e: e.tensor_tensor(out=rs[:, 8:9], in0=rs[:, 7:8], in1=rs[:, 3:4], op=ALU.mult),
                 reads=["rs7", "rs3"], writes=["rs8"])
            P.op("dve", lambda e, ti=ti: e.scalar_tensor_tensor(
                out=Wall[:, ti * 32:(ti + 1) * 32], in0=ext, scalar=rs[:, 8:9], in1=msk,
                op0=ALU.mult, op1=ALU.mult), reads=["ext", "rs8", "msk"], writes=["Wall"])
    if stop == 4:
        wall_out = nc.dram_tensor("Wall_out", [128, (SO // 128) * 32], F32, kind="ExternalOutput").ap()
        P.dma("sp", wall_out, Wall[:], reads=["Wall"], writes=["wall_out"])
        P.emit(final_wait_keys=["h1s","h1T","wall_out"])
        P.close()
        return nc
    P.barrier()
    A.reset()

    phase_begin(5)
    if start == 5:
        wall_in = nc.dram_tensor("Wall_in", [128, (SO // 128) * 32], F32, kind="ExternalInput").ap()
        P.dma("sp", Wall[:], wall_in, writes=["Wall"])
    TTK = min(512, SO)
    NTS = TTK // 128
    NTH = TTK // 512
    hTt = A.alloc([16, TTK], BF16)
    oacc = A.alloc([NTS, D], F32)
    gq = [A.alloc([16, 256], BF16) for _ in range(2)]
    uq = [A.alloc([16, 256], BF16) for _ in range(2)]
    dq = [A.alloc([2, D], BF16) for _ in range(2)]
    sg = [A.alloc([512], F32) for _ in range(2)]
    hid = [A.alloc([2, TTK], BF16) for _ in range(2)]
    g2t = A.alloc([D], F32)
    b2t = A.alloc([D], F32)
    h1t = [A.alloc([D], F32) for _ in range(2)]
    st6b = A.alloc([4, 6], F32)
    mvb = A.alloc([8], F32)
    P.dma("sp", g2t, ln2g.partition_broadcast(128), writes=["g2"])
    P.dma("sp", b2t, ln2b.partition_broadcast(128), writes=["b2"])
    wq = 0
    prr = 0
    for tt in range(SO // TTK):
        tk0 = tt * TTK
        P.dma("sp", hTt, h1T[:, tk0:tk0 + TTK].rearrange("(kc p) t -> p kc t", p=128),
              reads=["h1T"], writes=["hTt"])
        P.op("pool", lambda e: e.memset(oacc.rearrange("p a b -> p (a b)"), 0.0),
             writes=[("oacc", ts_) for ts_ in range(NTS)])
        for e_ in range(NEXP):
            for q in range(4):
                wbi = wq % 2
                wq += 1
                P.dma("sp", gq[wbi].rearrange("p a b -> p (a b)"), wg_b[e_ * 4 + q], reads=["wg_b"],
                      writes=[("gq", wbi)])
                P.dma("sp", uq[wbi].rearrange("p a b -> p (a b)"), wu_b[e_ * 4 + q], reads=["wu_b"],
                      writes=[("uq", wbi)])
                P.dma("sp", dq[wbi].rearrange("p a b -> p (a b)"), wd_b[e_ * 4 + q], reads=["wd_b"],
                      writes=[("dq", wbi)])
                hk = ("hid", wbi)
                for hc in range(2):
                    for th in range(NTH):
                        bg = (prr % 2) * 2
                        prr += 1
                        for kc in range(16):
                            P.op("pe", lambda e, bg=bg, kc=kc, hc=hc, th=th, wbi=wbi: e.matmul(
                                bank(bg), lhsT=gq[wbi][:, kc, hc * 128:(hc + 1) * 128],
                                rhs=hTt[:, kc, th * 512:(th + 1) * 512], start=(kc == 0), stop=(kc == 15)),
                                reads=[("gq", wbi), "hTt"], writes=[("ps", bg)])
                        for kc in range(16):
                            P.op("pe", lambda e, bg=bg, kc=kc, hc=hc, th=th, wbi=wbi: e.matmul(
                                bank(bg + 1), lhsT=uq[wbi][:, kc, hc * 128:(hc + 1) * 128],
                                rhs=hTt[:, kc, th * 512:(th + 1) * 512], start=(kc == 0), stop=(kc == 15)),
                                reads=[("uq", wbi), "hTt"], writes=[("ps", bg + 1)])
                        sgi = (bg // 2)
                        P.op("act", lambda e, bg=bg, sgi=sgi: e.activation(out=sg[sgi], in_=bank(bg), func=AF.Silu),
                             reads=[("ps", bg)], writes=[("sg", sgi)])
                        P.op("dve", lambda e, bg=bg, sgi=sgi, hc=hc, th=th, wbi=wbi: e.tensor_tensor(
                            out=hid[wbi][:, hc, th * 512:(th + 1) * 512], in0=sg[sgi], in1=bank(bg + 1),
                            op=ALU.mult), reads=[("sg", sgi), ("ps", bg + 1)], writes=[hk],
                            accum=not (hc == 0 and th == 0))
                for ts in range(NTS):
                    ti = tt * NTS + ts
                    for cbk in range(4):
                        bd = 4 + (ts * 4 + cbk) % 4
                        for hc in range(2):
                            P.op("pe", lambda e, bd=bd, hc=hc, ts=ts, cbk=cbk, wbi=wbi: e.matmul(
                                bank(bd), lhsT=hid[wbi][:, hc, ts * 128:(ts + 1) * 128],
                                rhs=dq[wbi][:, hc, cbk * 512:(cbk + 1) * 512], start=(hc == 0), stop=(hc == 1)),
                                reads=[hk, ("dq", wbi)], writes=[("ps", bd)])
                        P.op("dve", lambda e, bd=bd, ts=ts, cbk=cbk, ti=ti, e_=e_: e.scalar_tensor_tensor(
                            out=oacc[:, ts, cbk * 512:(cbk + 1) * 512], in0=bank(bd),
                            scalar=Wall[:, ti * 32 + e_:ti * 32 + e_ + 1],
                            in1=oacc[:, ts, cbk * 512:(cbk + 1) * 512], op0=ALU.mult, op1=ALU.add),
                            reads=[("ps", bd), "Wall", ("oacc", ts)], writes=[("oacc", ts)])
        for ts in range(NTS):
            tok0 = tk0 + ts * 128
            hb_ = ts % 2
            P.dma("sp", h1t[hb_], h1s[tok0:tok0 + 128, :], reads=["h1s"], writes=[("h1t", hb_)])
            P.op("dve", lambda e, hb_=hb_, ts=ts: e.scalar_tensor_tensor(
                out=h1t[hb_], in0=h1t[hb_], scalar=DN_ALPHA, in1=oacc[:, ts, :], op0=ALU.mult, op1=ALU.add),
                reads=[("h1t", hb_), ("oacc", ts)], writes=[("h1t", hb_)])
            layernorm(h1t[hb_], h1t[hb_], "g2", "b2", g2t, b2t, ("h1t", hb_), ("h1t", hb_), "b", st6b, mvb)
            P.dma("sp", out[tok0:tok0 + 128, :], h1t[hb_], reads=[("h1t", hb_)], writes=["out"], accum=True)
    P.emit(final_wait_keys=["out"])
    P.close()
    return nc


def _consts(S, hf):
    SO = S // 2
    SC = S - SO
    slopes = np.exp2(-8.0 * np.arange(1, 9, dtype=np.float64) / 8)
    tok = np.arange(S)
    blk = tok // 128
    rel = tok % 128
    ktab = np.zeros((8, 4, S), np.float32)
    qtab = np.zeros((8, 4, SO), np.float32)
    for h in range(8):
        sl = slopes[h]
        ktab[h, 0] = 1.0
        ktab[h, 1] = 1.0
        ktab[h, 2] = sl * 128 * blk
        ktab[h, 3] = sl * rel
        if hf == 0:
            ktab[h, 2, :SC] += NEG
        qtab[h, 0] = -sl * 128 * blk[SC:]
        qtab[h, 1] = -sl * rel[SC:]
        qtab[h, 2] = 1.0
        qtab[h, 3] = 1.0
    ttab = np.zeros((128, 8, 896), np.float32)
    s = np.arange(128)[:, None]
    t = np.arange(128)[None, :]
    for h in range(8):
        d = np.zeros((128, 128), np.float64)
        fut = (s > t) & ((s // 64) == (t // 64))
        d[fut] = (-2.0 * slopes[h] * (s - t))[fut]
        d[(s // 64) > (t // 64)] = NEG
        ttab[:, h, 0:384] = NEG
        ttab[:, h, 384:512] = d
    j = np.arange(64)[:, None]
    l = np.arange(64)[None, :]
    tri = (j <= l).astype(np.float32)
    umat = (j > l).astype(np.float32)
    flag = np.full((128, 1), 1.0 if hf == 1 else 0.0, np.float32)
    return dict(ktab=ktab, qtab=qtab, ttab=ttab.reshape(128, 8 * 896), tri=tri, umat=umat, flag=flag,
                ident=np.eye(128, dtype=np.float32))


_CACHE = {}


def kernel(x, w_in, lambda_q1, lambda_k1, lambda_q2, lambda_k2, attn_norm_w, conv_w, conv_b,
           dt_bias, a_log, d_skip, ssm_norm_w, w_out, ln1_g, ln1_b, w_router_group,
           b_router_group, w_router_expert, b_router_expert, w_gate, w_up, w_down,
           ln2_g, ln2_b):
    x = np.asarray(x, np.float32)
    B, S, _ = x.shape
    SO = S // 2
    f = lambda a: np.ascontiguousarray(np.asarray(a, np.float32))
    if S not in _CACHE:
        _CACHE[S] = build_program(S)
    nc = _CACHE[S]
    wr = np.concatenate([f(w_router_group)[0], np.transpose(f(w_router_expert)[0], (1, 0, 2)).reshape(D, 32)], 1)
    br = np.concatenate([f(b_router_group)[0], f(b_router_expert)[0].reshape(32)])[None]
    lamv = np.concatenate([f(lambda_q1)[0], f(lambda_k1)[0], f(lambda_q2)[0], f(lambda_k2)[0]])[None]
    cw = np.ascontiguousarray(f(conv_w)[0].T.reshape(12, 128, 4).transpose(1, 0, 2).reshape(128, 48))
    cb = np.ascontiguousarray(f(conv_b)[0].reshape(12, 128).T)
    shared = dict(
        w_in=f(w_in)[0], w_out=f(w_out)[0], w_gate=f(w_gate)[0], w_up=f(w_up)[0], w_down=f(w_down)[0],
        wr=f(wr), br=f(br), lamv=f(lamv), anw=f(attn_norm_w), cw=cw, cb=cb, dtb=f(dt_bias), alog=f(a_log),
        dsk=f(d_skip), snw=f(ssm_norm_w), ln1g=f(ln1_g), ln1b=f(ln1_b), ln2g=f(ln2_g), ln2b=f(ln2_b))
    in_maps = []
    for b in range(B):
        for hf in range(2):
            m = dict(shared)
            m.update(_consts(S, hf))
            own = x[b, hf * SO:(hf + 1) * SO]
            ctx = x[b, 0:SO] if hf == 1 else np.zeros_like(own)
            m["xT"] = np.ascontiguousarray(np.concatenate([ctx, own], 0).T)
            m["xo"] = np.ascontiguousarray(own)
            in_maps.append(m)
    res = run_bass_kernel_spmd(nc, in_maps, core_ids=list(range(B * 2)))
    outp = np.zeros((B, S, D), np.float32)
    for b in range(B):
        for hf in range(2):
            outp[b, hf * SO:(hf + 1) * SO] = res.results[b * 2 + hf]["out"]
    return outp
```

```python
from contextlib import ExitStack
import math
import numpy as np
import concourse.bass as bass
import concourse.mybir as mybir
from concourse.bass_utils import run_bass_kernel_spmd

F32 = mybir.dt.float32
BF16 = mybir.dt.bfloat16
AF = mybir.ActivationFunctionType
ALU = mybir.AluOpType
AX = mybir.AxisListType

D = 2048
IN_COLS = 5648
NEXP = 32
EH = 1024
DN_ALPHA = 2.0 ** 0.25
LN_EPS = 1e-5
RMS_EPS = 1e-6
LAMBDA_INIT = 0.8 - 0.6 * math.exp(0.0)
NEG = -30000.0


class Prog:
    ENGS = ("pe", "act", "dve", "pool", "sp")

    def __init__(self, nc):
        self.nc = nc
        self.ops = []
        self.stack = ExitStack()
        self._n = 0
        self.barrier_at = []
        self.maxops = None

    def sb(self, shape, dtype, name=None):
        self._n += 1
        return self.stack.enter_context(
            self.nc.sbuf_tensor(name or f"sb{self._n}", list(shape), dtype))

    def op(self, eng, fn, reads=(), writes=(), kind="c", accum=False):
        if self.maxops is not None and len(self.ops) >= self.maxops:
            return
        import sys as _sys
        fr = _sys._getframe(1)
        if fr.f_code.co_name in ("dma", "evac", "convert", "layernorm"):
            fr = fr.f_back
        self.ops.append(dict(eng=eng, kind=kind, fn=fn, reads=tuple(reads),
                             writes=tuple(writes), accum=accum, line=fr.f_lineno))

    def dma(self, eng, out, in_, reads=(), writes=(), accum=False, **kw):
        self.op(eng, lambda e: e.dma_start(out=out, in_=in_, **kw), reads, writes,
                kind="d", accum=accum)

    def barrier(self):
        self.barrier_at.append(len(self.ops))

    def emit(self, final_wait_keys=()):
        nc = self.nc
        ops = self.ops
        n = len(ops)
        writers = {}
        gen_first = {}
        readers = {}
        deps = [None] * n
        needed = [False] * n
        bset = set()
        last_of = {}
        barriers = set(self.barrier_at)
        def bank_of(k):
            if isinstance(k, tuple) and k[0] == "ps":
                return ("bankx", k[1])
            if isinstance(k, tuple) and k[0] == "O":
                return ("bankx", 4 + k[1] * 2 + k[2] // 2)
            return None

        for o in ops:
            if o["kind"] != "c":
                continue
            bx = {bank_of(k) for k in o["reads"] + o["writes"]} - {None}
            if bx and not o.get("bx_done"):
                o["writes"] = o["writes"] + tuple(bx)
                o["bx_done"] = True
        for i, o in enumerate(ops):
            if i in barriers:
                bset = set(last_of.values())
            d = set(bset)
            sk = (o["eng"], o["kind"])
            for k in o["reads"]:
                d.update(writers.get(k, {}).values())
            if not o["accum"]:
                for k in o["writes"]:
                    d.update(writers.get(k, {}).values())
                    d.update(readers.get(k, ()))
            else:
                for k in o["writes"]:
                    if k in gen_first:
                        d.add(gen_first[k])
                    if isinstance(k, tuple) and k[0] == "bankx":
                        d.update(writers.get(k, {}).values())
                        d.update(readers.get(k, ()))
            d.discard(i)
            if o["eng"] == "pe" and o["kind"] == "c":
                d = {j for j in d if not (ops[j]["eng"] == "pe" and ops[j]["kind"] == "c")}
            deps[i] = d
            for j in d:
                needed[j] = True
            for k in o["writes"]:
                if o["accum"]:
                    writers.setdefault(k, {})[sk] = i
                else:
                    writers[k] = {sk: i}
                    readers[k] = []
                    gen_first[k] = i
            for k in o["reads"]:
                readers.setdefault(k, []).append(i)
            last_of[sk] = i
            if o["kind"] == "d":
                needed[i] = True
        last_writer = {k: max(v.values()) for k, v in writers.items()}
        final = set()
        for k in final_wait_keys:
            final.update(writers.get(k, {}).values())
        for j in final:
            needed[j] = True
        semkeys = sorted({(o["eng"], o["kind"]) for o in ops})
        sems = {sk: self.stack.enter_context(nc.semaphore(f"s_{sk[0]}_{sk[1]}"))
                for sk in semkeys}
        cnt = {sk: 0 for sk in semkeys}
        val = [0] * n
        for i, o in enumerate(ops):
            if needed[i]:
                sk = (o["eng"], o["kind"])
                cnt[sk] += 16 if o["kind"] == "d" else 1
                val[i] = cnt[sk]
        self.sem_counts = cnt
        engmap = {"pe": "tensor", "act": "scalar", "dve": "vector", "pool": "gpsimd",
                  "sp": "sync"}
        block = self.stack.enter_context(nc.Block())
        for eng in self.ENGS:
            idxs = [i for i, o in enumerate(ops) if o["eng"] == eng]
            if not idxs and eng != "sp":
                continue

            def body(e, idxs=idxs, eng=eng):
                waited = {}
                for i in idxs:
                    o = ops[i]
                    need = {}
                    for j in deps[i]:
                        sk = (ops[j]["eng"], ops[j]["kind"])
                        if val[j] > need.get(sk, 0):
                            need[sk] = val[j]
                    for sk, v in need.items():
                        if waited.get(sk, 0) < v:
                            e.wait_ge(sems[sk], v)
                            waited[sk] = v
                    ins = o["fn"](e)
                    if needed[i]:
                        ins.then_inc(sems[(o["eng"], o["kind"])],
                                     16 if o["kind"] == "d" else 1)
                if eng == "sp":
                    need = {}
                    for j in final:
                        sk = (ops[j]["eng"], ops[j]["kind"])
                        need[sk] = max(need.get(sk, 0), val[j])
                    for sk, v in need.items():
                        if waited.get(sk, 0) < v:
                            e.wait_ge(sems[sk], v)

            getattr(block, engmap[eng])(body)

    def close(self):
        self.stack.close()


def _dsz(dt):
    return 4 if dt == F32 else 2


class Arena:
    def __init__(self, P, nbytes):
        self.t = P.sb([128, nbytes // 4], F32, name="arena")
        self.off = 0
        self.cap = nbytes

    def alloc(self, free, dtype, parts=128):
        free = tuple(free)
        nel = int(np.prod(free))
        sz = (nel * _dsz(dtype) + 63) // 64 * 64
        assert self.off + sz <= self.cap, (self.off, sz, self.cap)
        ap = self.t[0:parts, self.off // 4:(self.off + sz) // 4]
        self.off += sz
        if dtype != F32:
            ap = ap.bitcast(dtype)
        ap = ap[:, 0:nel]
        if len(free) == 2:
            ap = ap.rearrange("p (a b) -> p a b", a=free[0])
        elif len(free) == 3:
            ap = ap.rearrange("p (a b c) -> p a b c", a=free[0], b=free[1])
        return ap

    def reset(self):
        self.off = 0


def win_groups():
    g = []
    for h in range(8):
        g.append(("q", h * 128, 128))
    for h in range(8):
        g.append(("k", 1024 + h * 128, 128))
    for i in range(2):
        g.append(("v", 2048 + i * 512, 512))
    for i in range(2):
        g.append(("z", 3072 + i * 512, 512))
    for i in range(12):
        g.append(("x", 4096 + i * 128, 128))
    g.append(("d", 5632, 16))
    return g


def build_program(S, stop=None, start=0):
    SO = S // 2
    SA = S
    SC = S - SO
    NB = SA // 128
    nc = bass.Bass("TRN2", target_bir_lowering=False)

    def din(name, shape, dt=F32):
        kind = "ExternalInput"
        if start > 0 and name in ("w_in", "w_out", "w_gate", "w_up", "w_down", "xT"):
            kind = "Internal"
        return nc.dram_tensor(name, list(shape), dt, kind=kind).ap()

    PROD = dict(win_b=0, wout_b=0, wg_b=0, wu_b=0, wd_b=0, qs=1, ks=1, vs=1, zs=1, xbcs=1, dts=1,
                mixT=3, h1s=4, h1T=4, xsort=5, ysort=5, h1b=4)

    def dscr(name, shape, dt):
        dbg = stop is not None and (stop == 0 or not name.startswith("w"))
        kind = "ExternalOutput" if dbg else "Internal"
        if start > 0 and PROD[name] < start and not name.startswith("w"):
            kind = "ExternalInput"
        if start > 0 and name.startswith("w"):
            kind = "ExternalInput" if (start == 4 and name == "wout_b") else "Internal"
        return nc.dram_tensor(name, list(shape), dt, kind=kind).ap()

    xT = din("xT", [D, SA])
    xo = din("xo", [SO, D])
    w_in = din("w_in", [D, IN_COLS])
    w_out = din("w_out", [D, D])
    w_gate = din("w_gate", [NEXP, D, EH])
    w_up = din("w_up", [NEXP, D, EH])
    w_down = din("w_down", [NEXP, EH, D])
    wr = din("wr", [D, 36])
    br = din("br", [1, 36])
    lamv = din("lamv", [1, 256])
    anw = din("anw", [1, 128])
    cw = din("cw", [128, 48])
    cb = din("cb", [128, 12])
    dtb = din("dtb", [1, 16])
    alog = din("alog", [1, 16])
    dsk = din("dsk", [1, 16])
    snw = din("snw", [1, 1024])
    ln1g = din("ln1g", [1, D])
    ln1b = din("ln1b", [1, D])
    ln2g = din("ln2g", [1, D])
    ln2b = din("ln2b", [1, D])
    ident_in = din("ident", [128, 128])
    qtab = din("qtab", [8, 4, SO])
    ktab = din("ktab", [8, 4, SA])
    ttab = din("ttab", [128, 8 * 896])
    tri_in = din("tri", [64, 64])
    umat_in = din("umat", [64, 64])
    flag_in = din("flag", [128, 1])
    out = nc.dram_tensor("out", [SO, D], F32, kind="ExternalOutput").ap()

    groups = win_groups()
    goff = []
    o = 0
    for (_, _, gc) in groups:
        goff.append(o)
        o += 128 * 16 * gc
    win_b = dscr("win_b", [o], BF16)
    wout_b = dscr("wout_b", [128, 16 * D], BF16)
    wg_b = dscr("wg_b", [NEXP * 128, 16 * EH], BF16)
    wu_b = dscr("wu_b", [NEXP * 128, 16 * EH], BF16)
    wd_b = dscr("wd_b", [NEXP * 128, 8 * D], BF16)
    BLK = 256
    NTT = SO // 128
    NBLK = (2 * SO) // BLK + NEXP
    xsort = dscr("xsort", [NBLK * BLK, D], BF16)
    ysort = dscr("ysort", [NBLK * BLK, D], F32)
    h1b = dscr("h1b", [SO, D], BF16)
    lmat_in = din("lmat", [128, 128])
    thr_in = din("thr256", [128, 32])
    bthr_in = din("bthr", [128, NBLK])
    pidx_in = din("pidx", [128, 1])
    qs = dscr("qs", [16, 64, SO], BF16)
    ks = dscr("ks", [16, 64, SA], BF16)
    vs = dscr("vs", [SA, 1024], BF16)
    zs = dscr("zs", [SO, 1024], F32)
    xbcs = dscr("xbcs", [1536, SA], F32)
    dts = dscr("dts", [SA, 16], F32)
    mixT = dscr("mixT", [D, SO], BF16)
    h1s = dscr("h1s", [SO, D], F32)
    h1T = dscr("h1T", [D, SO], BF16)

    P = Prog(nc)
    A = Arena(P, 176 * 1024)
    pp = P.stack.enter_context(nc.psum_tensor("pp", [128, 4096], F32))

    def bank(b, parts=128, n=512, off=0):
        return pp[0:parts, b * 512 + off:b * 512 + off + n]

    def bankbf(b, parts=128, n=1024, off=0):
        return pp[0:parts, b * 512:(b + 1) * 512].bitcast(BF16)[:, off:off + n]

    ident_f = P.sb([128, 128], F32, "ident_f")
    ident_b = P.sb([128, 128], BF16, "ident_b")
    Wall = P.sb([128, (SO // 128) * 32], F32, "Wall")
    epsln = P.sb([128, 1], F32, "epsln")
    epsrms = P.sb([128, 1], F32, "epsrms")
    P.dma("sp", ident_f[:], ident_in, writes=["ident_f"])
    P.dma("pool", ident_b[:], ident_in, writes=["ident_b"])
    P.op("dve", lambda e: e.memset(epsln[:], LN_EPS), writes=["eps"])
    P.op("dve", lambda e: e.memset(epsrms[:], RMS_EPS), writes=["eps2"])
    onec = P.sb([128, 1], F32, "onec")
    P.op("dve", lambda e: e.memset(onec[:], 1.0), writes=["onec"])

    n_setup = len(P.ops)

    def phase_begin(k):
        if start == k and k > 0:
            del P.ops[n_setup:]
            P.barrier_at.clear()
            import os
            if os.environ.get("KMAXOPS"):
                P.maxops = n_setup + int(os.environ["KMAXOPS"])

    evac_rr = [0]

    def evac(out_ap, in_ap, reads, writes, scale=None, accum=False):
        evac_rr[0] ^= 1
        if evac_rr[0]:
            if scale is None:
                P.op("act", lambda e: e.activation(out=out_ap, in_=in_ap, func=AF.Copy),
                     reads, writes, accum=accum)
            else:
                P.op("act", lambda e: e.activation(out=out_ap, in_=in_ap, func=AF.Copy,
                                                   scale=scale), reads, writes, accum=accum)
        else:
            if scale is None:
                P.op("dve", lambda e: e.tensor_copy(out=out_ap, in_=in_ap), reads, writes, accum=accum)
            else:
                P.op("dve", lambda e: e.tensor_scalar(out=out_ap, in0=in_ap, scalar1=scale,
                                                      scalar2=None, op0=ALU.mult),
                     reads, writes, accum=accum)

    cast_rr = [0]

    def convert(src_ap, dst_ap, free_shape, tag, dst3d=False):
        b = cast_rr[0] % 3
        cast_rr[0] += 1
        st = stg[b]
        cv = cvt[b]
        nel = int(np.prod(free_shape))
        sv = st[:, 0:nel]
        if len(free_shape) == 2:
            sv = sv.rearrange("p (a b) -> p a b", a=free_shape[0])
        P.dma("sp", sv, src_ap, reads=[], writes=[("stg", b)])
        eng = ("dve", "act", "pool")[b]
        if eng == "act":
            P.op("act", lambda e: e.activation(out=cv[:, 0:nel], in_=st[:, 0:nel], func=AF.Copy),
                 reads=[("stg", b)], writes=[("cvt", b)])
        else:
            P.op(eng, lambda e: e.tensor_copy(out=cv[:, 0:nel], in_=st[:, 0:nel]),
                 reads=[("stg", b)], writes=[("cvt", b)])
        cvv = cv[:, 0:nel]
        if dst3d:
            cvv = cvv.rearrange("p (a b) -> p a b", a=free_shape[0])
        P.dma("sp", dst_ap, cvv, reads=[("cvt", b)], writes=[tag], accum=True)

    stg = [A.alloc([4096], F32) for _ in range(3)]
    cvt = [A.alloc([4096], BF16) for _ in range(3)]
    for gi, (kind, c0, gc) in enumerate(groups):
        nel = 16 * gc
        dst = win_b[goff[gi]:goff[gi] + 128 * nel].rearrange("(p f) -> p f", p=128)
        if nel <= 4096:
            convert(w_in[:, c0:c0 + gc].rearrange("(kc p) c -> p kc c", p=128), dst,
                    [16, gc], "win_b")
        else:
            for half in range(2):
                convert(w_in[half * 1024:(half + 1) * 1024, c0:c0 + gc]
                        .rearrange("(kc p) c -> p kc c", p=128),
                        dst[:, half * 8 * gc:(half + 1) * 8 * gc], [8, gc], "win_b")
    for kc2 in range(8):
        convert(w_out[kc2 * 256:(kc2 + 1) * 256, :].rearrange("(kc p) c -> p kc c", p=128),
                wout_b[:, kc2 * 2 * D:(kc2 + 1) * 2 * D], [2, D], "wout_b")
    for e_ in range(NEXP):
        for q in range(4):
            convert(w_gate[e_, :, q * 256:(q + 1) * 256].rearrange("(kc p) c -> p kc c", p=128),
                    wg_b[e_ * 128:(e_ + 1) * 128, :].rearrange("p (kc c) -> p kc c", kc=16)[:, :, q * 256:(q + 1) * 256],
                    [16, 256], "wg_b", dst3d=True)
            convert(w_up[e_, :, q * 256:(q + 1) * 256].rearrange("(kc p) c -> p kc c", p=128),
                    wu_b[e_ * 128:(e_ + 1) * 128, :].rearrange("p (kc c) -> p kc c", kc=16)[:, :, q * 256:(q + 1) * 256],
                    [16, 256], "wu_b", dst3d=True)
            convert(w_down[e_, q * 256:(q + 1) * 256, :].rearrange("(kc p) c -> p kc c", p=128),
                    wd_b[e_ * 128:(e_ + 1) * 128, q * 2 * D:(q + 1) * 2 * D], [2, D], "wd_b")
    if stop == 0:
        P.emit(final_wait_keys=["win_b","wout_b","wg_b","wu_b","wd_b"])
        P.close()
        return nc
    P.barrier()
    A.reset()

    phase_begin(1)
    xTb = [A.alloc([16, 512], BF16) for _ in range(2)]
    wbuf = [A.alloc([16 * 512], BF16) for _ in range(3)]
    ebuf = [A.alloc([512], F32) for _ in range(4)]
    NT = SA // 512
    wrr = 0
    err = 0
    prr = 0
    for tt in range(NT):
        own = tt * 512 >= SC
        t0 = tt * 512
        to = t0 - SC
        xb = xTb[tt % 2]
        xk = ("xTb", tt % 2)
        P.dma("pool", xb, xT[:, t0:t0 + 512].rearrange("(kc p) t -> p kc t", p=128),
              writes=[xk])
        for gi, (kind, c0, gc) in enumerate(groups):
            if kind in ("q", "z") and not own:
                continue
            wb = wbuf[wrr % 3]
            wk = ("wbuf", wrr % 3)
            wrr += 1
            wv = wb[:, 0:16 * gc].rearrange("p (kc c) -> p kc c", kc=16)
            P.dma("sp", wb[:, 0:16 * gc],
                  win_b[goff[gi]:goff[gi] + 128 * 16 * gc].rearrange("(p f) -> p f", p=128),
                  reads=["win_b"], writes=[wk])
            if kind in ("q", "k"):
                h = (c0 % 1024) // 128
                for m in range(2):
                    b = prr % 8
                    prr += 1
                    for kc in range(16):
                        P.op("pe", lambda e, b=b, kc=kc, m=m, wv=wv, xb=xb: e.matmul(
                            bank(b, 64), lhsT=wv[:, kc, m * 64:(m + 1) * 64], rhs=xb[:, kc, :],
                            start=(kc == 0), stop=(kc == 15)),
                            reads=[wk, xk], writes=[("ps", b)])
                    eb = ebuf[err % 4]
                    ek = ("ebuf", err % 4)
                    err += 1
                    ebv = eb.bitcast(BF16)[0:64, 0:512]
                    evac(ebv, bank(b, 64), [("ps", b)], [ek],
                         scale=(0.125 if kind == "q" else None))
                    if kind == "q":
                        P.dma("sp", qs[h * 2 + m, :, to:to + 512], ebv, reads=[ek], writes=["qs"], accum=True)
                    else:
                        P.dma("sp", ks[h * 2 + m, :, t0:t0 + 512], ebv, reads=[ek], writes=["ks"], accum=True)
            elif kind == "x":
                g = (c0 - 4096) // 128
                b = prr % 8
                prr += 1
                for kc in range(16):
                    P.op("pe", lambda e, b=b, kc=kc, wv=wv, xb=xb: e.matmul(
                        bank(b), lhsT=wv[:, kc, :], rhs=xb[:, kc, :],
                        start=(kc == 0), stop=(kc == 15)),
                        reads=[wk, xk], writes=[("ps", b)])
                eb = ebuf[err % 4]
                ek = ("ebuf", err % 4)
                err += 1
                evac(eb, bank(b), [("ps", b)], [ek])
                P.dma("sp", xbcs[g * 128:(g + 1) * 128, t0:t0 + 512], eb, reads=[ek],
                      writes=["xbcs"], accum=True)
            else:
                for ts in range(4):
                    b = prr % 8
                    prr += 1
                    for kc in range(16):
                        P.op("pe", lambda e, b=b, kc=kc, wv=wv, xb=xb, ts=ts, gc=gc: e.matmul(
                            bank(b, 128, gc), lhsT=xb[:, kc, ts * 128:(ts + 1) * 128],
                            rhs=wv[:, kc, :], start=(kc == 0), stop=(kc == 15)),
                            reads=[wk, xk], writes=[("ps", b)])
                    eb = ebuf[err % 4]
                    ek = ("ebuf", err % 4)
                    err += 1
                    if kind == "v":
                        ebv = eb.bitcast(BF16)[:, 0:512]
                        evac(ebv, bank(b), [("ps", b)], [ek])
                        P.dma("sp", vs[t0 + ts * 128:t0 + (ts + 1) * 128, c0 - 2048:c0 - 2048 + 512],
                              ebv, reads=[ek], writes=["vs"], accum=True)
                    elif kind == "z":
                        evac(eb, bank(b), [("ps", b)], [ek])
                        P.dma("sp", zs[to + ts * 128:to + (ts + 1) * 128, c0 - 3072:c0 - 3072 + 512],
                              eb, reads=[ek], writes=["zs"], accum=True)
                    else:
                        evac(eb[:, 0:16], bank(b, 128, 16), [("ps", b)], [ek])
                        P.dma("sp", dts[t0 + ts * 128:t0 + (ts + 1) * 128, :], eb[:, 0:16],
                              reads=[ek], writes=["dts"], accum=True)
    if stop == 1:
        P.emit(final_wait_keys=["qs","ks","vs","zs","xbcs","dts"])
        P.close()
        return nc
    P.barrier()
    A.reset()

    phase_begin(2)
    KA = [[A.alloc([SA], BF16, parts=68) for m in range(2)] for _ in range(2)]
    QA = [[A.alloc([SO], BF16, parts=68) for m in range(2)] for _ in range(2)]
    VA = [A.alloc([NB, 130], BF16) for _ in range(2)]
    PT = [A.alloc([512], BF16) for _ in range(3)]
    TT = A.alloc([8, 896], BF16)
    lamt = A.alloc([256], F32)
    lsc = A.alloc([64], F32)
    lam4 = A.alloc([8], F32)
    nwb = A.alloc([128], F32)
    ep_a = [A.alloc([128], F32) for _ in range(2)]
    ep_o = [A.alloc([128], F32) for _ in range(2)]
    ep_j = A.alloc([128], F32)
    ep_s = [A.alloc([8], F32) for _ in range(2)]
    ob = [A.alloc([128], BF16) for _ in range(2)]
    mo = [A.alloc([512], BF16) for _ in range(2)]

    P.dma("pool", TT, ttab.rearrange("p (h c) -> p h c", h=8), writes=["TT"])
    P.dma("sp", lamt, lamv.partition_broadcast(128), writes=["lamt"])
    P.dma("sp", nwb, anw.partition_broadcast(128), writes=["nwb"])
    P.op("dve", lambda e: e.tensor_scalar(out=nwb, in0=nwb, scalar1=(1.0 - LAMBDA_INIT),
                                          scalar2=None, op0=ALU.mult), reads=["nwb"], writes=["nwb"])
    for i in range(2):
        P.op("dve", lambda e, i=i: e.tensor_tensor(out=lsc, in0=lamt[:, i * 128:i * 128 + 64],
                                                  in1=lamt[:, i * 128 + 64:i * 128 + 128],
                                                  op=ALU.mult), reads=["lamt"], writes=["lsc"])
        P.op("dve", lambda e, i=i: e.tensor_reduce(out=lam4[:, i:i + 1], in_=lsc, axis=AX.X,
                                                  op=ALU.add), reads=["lsc"], writes=["lam4"])
    P.op("act", lambda e: e.activation(out=lam4[:, 4:6], in_=lam4[:, 0:2], func=AF.Exp),
         reads=["lam4"], writes=["lam4"])
    P.op("dve", lambda e: e.scalar_tensor_tensor(out=lam4[:, 2:3], in0=lam4[:, 5:6],
                                                 scalar=-LAMBDA_INIT, in1=lam4[:, 4:5],
                                                 op0=ALU.add, op1=ALU.subtract),
         reads=["lam4"], writes=["neglam"])
    neglam = lam4[:, 2:3]
    for vb in range(2):
        P.op("dve", lambda e, vb=vb: e.memset(VA[vb][:, :, 128:130], 1.0), writes=[("VA", vb)])

    NQT = SO // 512
    strr = 0
    ptrr = 0
    eprr = 0
    for h in range(8):
        hb = h % 2
        kkey = ("KA", hb)
        qkey = ("QA", hb)
        vkey = ("VA", hb)
        for m in range(2):
            P.dma("sp", KA[hb][m][0:64, :], ks[h * 2 + m], reads=["ks"], writes=[kkey], accum=(m > 0))
            for c0_ in range(0, SA, 2048):
                c1_ = min(SA, c0_ + 2048)
                P.dma("pool", KA[hb][m][64:68, c0_:c1_], ktab[h, :, c0_:c1_], writes=[kkey], accum=True)
            P.dma("sp", QA[hb][m][0:64, :], qs[h * 2 + m], reads=["qs"], writes=[qkey], accum=(m > 0))
            for c0_ in range(0, SO, 2048):
                c1_ = min(SO, c0_ + 2048)
                P.dma("pool", QA[hb][m][64:68, c0_:c1_], qtab[h, :, c0_:c1_], writes=[qkey], accum=True)
        for j0 in range(0, NB, 16):
            j1 = min(NB, j0 + 16)
            P.dma("sp", VA[hb][:, j0:j1, 0:128],
                  vs[j0 * 128:j1 * 128, h * 128:(h + 1) * 128].rearrange("(j p) d -> p j d", p=128),
                  reads=["vs"], writes=[vkey], accum=(j0 > 0))
        for qt in range(NQT):
            gb = (SC + qt * 512) // 128
            okeys = [[("O", m, i) for i in range(4)] for m in range(2)]
            for j in range(gb + 4):
                r = j - gb
                for m in range(2):
                    sb_ = strr % 3
                    strr += 1
                    P.op("pe", lambda e, sb_=sb_, m=m, j=j, qt=qt, hb=hb, r=r: e.matmul(
                        bank(sb_), lhsT=KA[hb][m][:, j * 128:(j + 1) * 128],
                        rhs=QA[hb][m][:, qt * 512:(qt + 1) * 512], start=True, stop=(r < 0)),
                        reads=[kkey, qkey], writes=[("ps", sb_)])
                    if r >= 0:
                        P.op("pe", lambda e, sb_=sb_, h=h, r=r: e.matmul(
                            bank(sb_), lhsT=ident_b[:], rhs=TT[:, h, (3 - r) * 128:(3 - r) * 128 + 512],
                            start=False, stop=True),
                            reads=["ident_b", "TT"], writes=[("ps", sb_)])
                    pb = ptrr % 3
                    ptrr += 1
                    P.op("act", lambda e, sb_=sb_, pb=pb: e.activation(
                        out=PT[pb], in_=bank(sb_), func=AF.Exp),
                        reads=[("ps", sb_)], writes=[("PT", pb)])
                    for i in range(4):
                        if r >= 0 and i < r:
                            continue
                        ob_ = 4 + m * 2 + i // 2
                        P.op("pe", lambda e, pb=pb, i=i, j=j, hb=hb, ob_=ob_, gb=gb: e.matmul(
                            bank(ob_, 128, 129, (i % 2) * 256), lhsT=PT[pb][:, i * 128:(i + 1) * 128],
                            rhs=VA[hb][:, j, 0:129], start=(j == 0 and i % 2 == 0), stop=(j == gb + i)),
                            reads=[("PT", pb), vkey], writes=[okeys[m][i]])
            mb = (h * NQT + qt) % 2
            for i in range(4):
                eb_ = eprr % 2
                O0 = bank(4 + i // 2, 128, 129, (i % 2) * 256)
                O1 = bank(6 + i // 2, 128, 129, (i % 2) * 256)
                es = ep_s[eb_]
                ek = ("ep", eb_)
                P.op("dve", lambda e, es=es, O0=O0: e.reciprocal(out=es[:, 0:1], in_=O0[:, 128:129]),
                     reads=[okeys[0][i]], writes=[ek])
                P.op("dve", lambda e, es=es, O1=O1: e.reciprocal(out=es[:, 1:2], in_=O1[:, 128:129]),
                     reads=[okeys[1][i]], writes=[ek])
                P.op("dve", lambda e, es=es: e.tensor_tensor(out=es[:, 2:3], in0=es[:, 1:2], in1=neglam,
                                                             op=ALU.mult),
                     reads=[ek, "neglam"], writes=[ek])
                P.op("dve", lambda e, es=es, O0=O0, eb_=eb_: e.tensor_scalar(
                    out=ep_a[eb_], in0=O0[:, 0:128], scalar1=es[:, 0:1], scalar2=None, op0=ALU.mult),
                    reads=[ek, okeys[0][i]], writes=[("epa", eb_)])
                P.op("dve", lambda e, es=es, O1=O1, eb_=eb_: e.scalar_tensor_tensor(
                    out=ep_o[eb_], in0=O1[:, 0:128], scalar=es[:, 2:3], in1=ep_a[eb_],
                    op0=ALU.mult, op1=ALU.add),
                    reads=[ek, okeys[1][i], ("epa", eb_)], writes=[("epo", eb_)])
                P.op("act", lambda e, es=es, eb_=eb_: e.activation(
                    out=ep_j, in_=ep_o[eb_], func=AF.Square, accum_out=es[:, 3:4]),
                    reads=[("epo", eb_)], writes=[("ep2", eb_), "epj"])
                P.op("act", lambda e, es=es: e.activation(
                    out=es[:, 4:5], in_=es[:, 3:4], func=AF.Sqrt, bias=epsrms[:], scale=1.0 / 128),
                    reads=[("ep2", eb_), "eps2"], writes=[("ep3", eb_)])
                P.op("dve", lambda e, es=es: e.reciprocal(out=es[:, 5:6], in_=es[:, 4:5]),
                     reads=[("ep3", eb_)], writes=[("ep4", eb_)])
                P.op("dve", lambda e, es=es, eb_=eb_: e.scalar_tensor_tensor(
                    out=ob[eb_], in0=ep_o[eb_], scalar=es[:, 5:6], in1=nwb, op0=ALU.mult, op1=ALU.mult),
                    reads=[("ep4", eb_), ("epo", eb_), "nwb"], writes=[("ob", eb_)])
                P.op("pe", lambda e, eb_=eb_, i=i: e.transpose(
                    bankbf(3, 128, 128, i * 128), ob[eb_], ident_b[:]),
                    reads=[("ob", eb_), "ident_b"], writes=[("ps", 3, i)])
                evac(mo[mb][:, i * 128:(i + 1) * 128], bankbf(3, 128, 128, i * 128),
                     [("ps", 3, i)], [("mo", mb)], accum=(i > 0))
                eprr += 1
            P.dma("sp", mixT[h * 128:(h + 1) * 128, qt * 512:(qt + 1) * 512], mo[mb],
                  reads=[("mo", mb)], writes=["mixT"], accum=True)
    if stop == 2:
        P.emit(final_wait_keys=["mixT"])
        P.close()
        return nc
    P.barrier()
    A.reset()

    phase_begin(3)
    NCH = SA // 64
    cwt = A.alloc([12, 4], F32)
    cbt = A.alloc([12], F32)
    tri = A.alloc([64], F32, parts=64)
    umat = A.alloc([64], F32, parts=64)
    ones64 = A.alloc([128], F32, parts=64)
    flagt = A.alloc([1], F32)
    dtbt = A.alloc([16], F32, parts=64)
    Aneg = A.alloc([16], F32, parts=64)
    Dt = A.alloc([16], F32, parts=64)
    snwt = A.alloc([1024], F32, parts=64)
    dtall = A.alloc([NCH, 16], F32, parts=64)
    aall = A.alloc([NCH, 16], F32, parts=64)
    xc = [A.alloc([12, 515], F32) for _ in range(1)]
    acc = A.alloc([512], F32)
    xcv = A.alloc([8, 512], F32)
    bct = A.alloc([4, 512], BF16)
    Rt = A.alloc([16, 64], F32, parts=64)
    Et = A.alloc([16, 64], F32, parts=64)
    Ebt = A.alloc([16, 64], F32)
    mcb = A.alloc([2, 64], F32, parts=64)
    Mt = A.alloc([16, 64], BF16, parts=64)
    Mtmp = A.alloc([16, 64], F32, parts=64)
    x32 = A.alloc([1024], F32, parts=64)
    xbf = A.alloc([1024], BF16, parts=64)
    wsc = A.alloc([16], F32, parts=64)
    xw = A.alloc([1024], BF16, parts=64)
    Btok = A.alloc([2, 128], BF16, parts=64)
    Cs = A.alloc([16, 64], BF16)
    Hs = A.alloc([1024], F32)
    Hbf = A.alloc([1024], BF16)
    yt = A.alloc([1024], F32, parts=64)
    zt = A.alloc([1024], F32, parts=64)
    ysq = A.alloc([1024], F32, parts=64)
    ss = A.alloc([8], F32, parts=64)
    ybf = A.alloc([1024], BF16, parts=64)
    ymo = A.alloc([8, 64], BF16)

    P.dma("sp", cwt, cw.rearrange("p (g k) -> p g k", g=12), writes=["cwt"])
    P.dma("sp", cbt, cb, writes=["cbt"])
    P.dma("sp", tri, tri_in, writes=["tri"])
    P.dma("sp", umat, umat_in, writes=["umat"])
    P.dma("sp", flagt, flag_in, writes=["flagt"])
    P.dma("sp", dtbt, dtb.partition_broadcast(64), writes=["dtbt"])
    P.dma("sp", Aneg, alog.partition_broadcast(64), writes=["Aneg"])
    P.dma("sp", Dt, dsk.partition_broadcast(64), writes=["Dt"])
    P.dma("sp", snwt, snw.partition_broadcast(64), writes=["snwt"])
    for c0_ in range(0, NCH, 32):
        c1_ = min(NCH, c0_ + 32)
        P.dma("sp", dtall[:, c0_:c1_, :], dts[c0_ * 64:c1_ * 64, :].rearrange("(c l) h -> l c h", l=64),
              reads=["dts"], writes=["dtall"], accum=(c0_ > 0))
    P.op("dve", lambda e: e.memset(ones64, 1.0), writes=["ones64"])
    P.op("dve", lambda e: e.memset(Hs, 0.0), writes=["Hs"])
    P.op("dve", lambda e: e.memset(Hbf, 0.0), writes=["Hbf"])
    P.op("act", lambda e: e.activation(out=Aneg, in_=Aneg, func=AF.Exp), reads=["Aneg"], writes=["Aneg"])
    P.op("dve", lambda e: e.tensor_scalar(out=Aneg, in0=Aneg, scalar1=-1.0, scalar2=None, op0=ALU.mult),
         reads=["Aneg"], writes=["Aneg"])
    P.op("dve", lambda e: e.tensor_tensor(out=dtall, in0=dtall,
                                          in1=dtbt.unsqueeze(1).to_broadcast([64, NCH, 16]), op=ALU.add),
         reads=["dtall", "dtbt"], writes=["dtall"])
    P.op("act", lambda e: e.activation(out=dtall, in_=dtall, func=AF.Exp), reads=["dtall"], writes=["dtall"])
    P.op("act", lambda e: e.activation(out=dtall, in_=dtall, func=AF.Ln, bias=onec[0:64, :], scale=1.0),
         reads=["dtall", "onec"], writes=["dtall"])
    P.op("dve", lambda e: e.tensor_tensor(out=aall, in0=dtall,
                                          in1=Aneg.unsqueeze(1).to_broadcast([64, NCH, 16]), op=ALU.mult),
         reads=["dtall", "Aneg"], writes=["aall"])

    for blk in range(SA // 512):
        t0 = blk * 512
        xcb = xc[0]
        if t0 == 0:
            P.op("dve", lambda e: e.memset(xcb[:, :, 0:3], 0.0), writes=["xc"])
            P.dma("sp", xcb[:, :, 3:515], xbcs[:, 0:512].rearrange("(g p) t -> p g t", p=128),
                  reads=["xbcs"], writes=["xc"])
        else:
            P.dma("sp", xcb, xbcs[:, t0 - 3:t0 + 512].rearrange("(g p) t -> p g t", p=128),
                  reads=["xbcs"], writes=["xc"])
        for g in range(12):
            P.op("dve", lambda e, g=g: e.tensor_scalar(out=acc, in0=xcb[:, g, 3:515],
                                                      scalar1=cwt[:, g, 3:4], scalar2=None, op0=ALU.mult),
                 reads=["xc", "cwt"], writes=["acc"])
            for k in range(3):
                P.op("dve", lambda e, g=g, k=k: e.scalar_tensor_tensor(
                    out=acc, in0=xcb[:, g, k:k + 512], scalar=cwt[:, g, k:k + 1], in1=acc,
                    op0=ALU.mult, op1=ALU.add), reads=["xc", "cwt", "acc"], writes=["acc"])
            if g < 8:
                P.op("act", lambda e, g=g: e.activation(out=xcv[:, g, :], in_=acc, func=AF.Silu,
                                                       bias=cbt[:, g:g + 1], scale=1.0),
                     reads=["acc", "cbt"], writes=[("xcv", g)])
            else:
                P.op("act", lambda e, g=g: e.activation(out=bct[:, g - 8, :], in_=acc, func=AF.Silu,
                                                       bias=cbt[:, g:g + 1], scale=1.0),
                     reads=["acc", "cbt"], writes=[("bct", g - 8)])
        for cl in range(8):
            c = blk * 8 + cl
            own = c * 64 >= SC
            csl = slice(cl * 64, (cl + 1) * 64)
            a_c = aall[:, c, :]
            dt_c = dtall[:, c, :]
            for g in range(8):
                P.op("pe", lambda e, g=g, csl=csl: e.transpose(
                    bank(g // 4, 64, 128, (g % 4) * 128), xcv[:, g, csl], ident_f[:]),
                    reads=[("xcv", g), "ident_f"], writes=[("ps", g // 4)])
            for hb_ in range(2):
                P.op("act", lambda e, hb_=hb_: e.activation(
                    out=x32[:, hb_ * 512:(hb_ + 1) * 512], in_=bank(hb_, 64), func=AF.Copy),
                    reads=[("ps", hb_)], writes=[("x32", hb_)])
                P.op("act", lambda e, hb_=hb_: e.activation(
                    out=xbf[:, hb_ * 512:(hb_ + 1) * 512], in_=bank(hb_, 64), func=AF.Copy),
                    reads=[("ps", hb_)], writes=[("xbf", hb_)])
            for g2 in range(2):
                P.op("pe", lambda e, g2=g2, csl=csl: e.transpose(
                    bankbf(7, 64, 128, 512 + g2 * 128), bct[:, g2, csl], ident_b[:]),
                    reads=[("bct", g2), "ident_b"], writes=[("ps", 7, "b")])
            P.op("dve", lambda e: e.tensor_copy(out=Btok.rearrange("p a b -> p (a b)"),
                                                in_=bankbf(7, 64, 256, 512)),
                 reads=[("ps", 7, "b")], writes=["Btok"])
            P.op("dve", lambda e, a_c=a_c: e.tensor_tensor(
                out=Rt, in0=tri.unsqueeze(1).to_broadcast([64, 16, 64]),
                in1=a_c.unsqueeze(2).to_broadcast([64, 16, 64]), op=ALU.mult),
                reads=["tri", "aall"], writes=["Rt"])
            Rf = Rt.rearrange("p a b -> p (a b)")
            for hb_ in range(2):
                P.op("pe", lambda e, hb_=hb_: e.matmul(
                    bank(2 + hb_, 64), lhsT=umat, rhs=Rf[:, hb_ * 512:(hb_ + 1) * 512],
                    start=True, stop=True), reads=["umat", "Rt"], writes=[("ps", 2 + hb_)])
                P.op("pe", lambda e, hb_=hb_: e.matmul(
                    bank(4 + hb_), lhsT=ones64, rhs=Rf[:, hb_ * 512:(hb_ + 1) * 512],
                    start=True, stop=True), reads=["ones64", "Rt"], writes=[("ps", 4 + hb_)])
            Ef = Et.rearrange("p a b -> p (a b)")
            Ebf = Ebt.rearrange("p a b -> p (a b)")
            for hb_ in range(2):
                P.op("act", lambda e, hb_=hb_: e.activation(
                    out=Ef[:, hb_ * 512:(hb_ + 1) * 512], in_=bank(2 + hb_, 64), func=AF.Exp),
                    reads=[("ps", 2 + hb_)], writes=[("Et", hb_)])
                P.op("act", lambda e, hb_=hb_: e.activation(
                    out=Ebf[:, hb_ * 512:(hb_ + 1) * 512], in_=bank(4 + hb_), func=AF.Exp),
                    reads=[("ps", 4 + hb_)], writes=[("Ebt", hb_)])
            ekeys = [("Et", 0), ("Et", 1)]
            ebkeys = [("Ebt", 0), ("Ebt", 1)]
            P.op("dve", lambda e, dt_c=dt_c: e.tensor_tensor(out=wsc, in0=dt_c, in1=Et[:, :, 63],
                                                            op=ALU.mult),
                 reads=ekeys + ["dtall"], writes=["wsc"])
            P.op("dve", lambda e: e.tensor_tensor(
                out=xw.rearrange("p (h d) -> p h d", h=16),
                in0=x32.rearrange("p (h d) -> p h d", h=16),
                in1=wsc.unsqueeze(2).to_broadcast([64, 16, 64]), op=ALU.mult),
                reads=["wsc", ("x32", 0), ("x32", 1)], writes=["xw"])
            if own:
                if c * 64 == SC:
                    P.op("dve", lambda e: e.tensor_scalar(out=Hs, in0=Hs, scalar1=flagt[:, 0:1],
                                                          scalar2=None, op0=ALU.mult),
                         reads=["Hs", "flagt"], writes=["Hs"])
                    P.op("dve", lambda e: e.tensor_copy(out=Hbf, in_=Hs), reads=["Hs"], writes=["Hbf"])
                for g2 in range(2):
                    P.op("pe", lambda e, g2=g2, csl=csl: e.matmul(
                        bank(6, 64, 64, g2 * 64), lhsT=bct[:, g2, csl], rhs=bct[:, 2 + g2, csl],
                        start=True, stop=True), reads=[("bct", g2), ("bct", 2 + g2)],
                        writes=[("ps", 6, "cb")])
                P.op("dve", lambda e: e.tensor_tensor(
                    out=mcb, in0=bank(6, 64, 128).rearrange("p (g l) -> p g l", g=2),
                    in1=tri.unsqueeze(1).to_broadcast([64, 2, 64]), op=ALU.mult),
                    reads=[("ps", 6, "cb"), "tri"], writes=["mcb"])
                for g2 in range(2):
                    P.op("dve", lambda e, g2=g2: e.tensor_tensor(
                        out=Mtmp[:, g2 * 8:(g2 + 1) * 8, :], in0=Et[:, g2 * 8:(g2 + 1) * 8, :],
                        in1=mcb[:, g2:g2 + 1, :].to_broadcast([64, 8, 64]), op=ALU.mult),
                        reads=ekeys + ["mcb"], writes=[("Mtmp", g2)])
                P.op("dve", lambda e, dt_c=dt_c: e.tensor_tensor(
                    out=Mt, in0=Mtmp, in1=dt_c.unsqueeze(2).to_broadcast([64, 16, 64]), op=ALU.mult),
                    reads=[("Mtmp", 0), ("Mtmp", 1), "dtall"], writes=["Mt"])
                for g2 in range(2):
                    P.op("dve", lambda e, g2=g2, csl=csl: e.tensor_tensor(
                        out=Cs[:, g2 * 8:(g2 + 1) * 8, :], in0=Ebt[:, g2 * 8:(g2 + 1) * 8, :],
                        in1=bct[:, 2 + g2, csl].unsqueeze(1).to_broadcast([128, 8, 64]), op=ALU.mult),
                        reads=ebkeys + [("bct", 2 + g2)], writes=[("Cs", g2)])
                for hh in range(16):
                    yb = bank(hh // 8, 64, 64, (hh % 8) * 64)
                    P.op("pe", lambda e, hh=hh, yb=yb: e.matmul(
                        yb, lhsT=Mt[:, hh, :], rhs=xbf[:, hh * 64:(hh + 1) * 64], start=True, stop=False),
                        reads=["Mt", ("xbf", hh // 8), ("x32", hh // 8)], writes=[("ps", hh // 8)])
                    P.op("pe", lambda e, hh=hh, yb=yb: e.matmul(
                        yb, lhsT=Cs[:, hh, :], rhs=Hbf[:, hh * 64:(hh + 1) * 64], start=False, stop=True),
                        reads=[("Cs", hh // 8), "Hbf"], writes=[("ps", hh // 8)])
            for g2 in range(2):
                P.op("pe", lambda e, g2=g2: e.matmul(
                    bank(2 + g2), lhsT=Btok[:, g2, :], rhs=xw[:, g2 * 512:(g2 + 1) * 512],
                    start=True, stop=True), reads=["Btok", "xw"] + ekeys, writes=[("ps", 2 + g2)])
            P.op("dve", lambda e: e.tensor_tensor(
                out=Hs.rearrange("p (h d) -> p h d", h=16), in0=Hs.rearrange("p (h d) -> p h d", h=16),
                in1=Ebt[:, :, 63:64].to_broadcast([128, 16, 64]), op=ALU.mult),
                reads=["Hs", "Hbf"] + ebkeys, writes=["Hs"])
            for g2 in range(2):
                P.op("dve", lambda e, g2=g2: e.tensor_tensor(
                    out=Hs[:, g2 * 512:(g2 + 1) * 512], in0=Hs[:, g2 * 512:(g2 + 1) * 512],
                    in1=bank(2 + g2), op=ALU.add), reads=["Hs", ("ps", 2 + g2)], writes=["Hs"])
            if own:
                to = c * 64 - SC
                P.dma("sp", zt, zs[to:to + 64, :], reads=["zs"], writes=["zt"])
                P.op("dve", lambda e: e.tensor_tensor(
                    out=yt.rearrange("p (h d) -> p h d", h=16), in0=x32.rearrange("p (h d) -> p h d", h=16),
                    in1=Dt.unsqueeze(2).to_broadcast([64, 16, 64]), op=ALU.mult),
                    reads=[("x32", 0), ("x32", 1), "Dt"], writes=["yt"])
                for hb_ in range(2):
                    P.op("dve", lambda e, hb_=hb_: e.tensor_tensor(
                        out=yt[:, hb_ * 512:(hb_ + 1) * 512], in0=yt[:, hb_ * 512:(hb_ + 1) * 512],
                        in1=bank(hb_, 64), op=ALU.add), reads=["yt", ("ps", hb_)], writes=["yt"])
                P.op("act", lambda e: e.activation(out=zt, in_=zt, func=AF.Silu), reads=["zt"], writes=["zt"])
                P.op("dve", lambda e: e.tensor_tensor(out=yt, in0=yt, in1=zt, op=ALU.mult),
                     reads=["yt", "zt"], writes=["yt"])
                for g2 in range(2):
                    P.op("act", lambda e, g2=g2: e.activation(
                        out=ysq[:, g2 * 512:(g2 + 1) * 512], in_=yt[:, g2 * 512:(g2 + 1) * 512],
                        func=AF.Square, accum_out=ss[:, g2:g2 + 1]), reads=["yt"], writes=["ysq", ("ss", g2)])
                P.op("act", lambda e: e.activation(out=ss[:, 2:4], in_=ss[:, 0:2], func=AF.Sqrt,
                                                   bias=epsrms[0:64, :], scale=1.0 / 512),
                     reads=[("ss", 0), ("ss", 1), "eps2"], writes=["ss2"])
                P.op("dve", lambda e: e.reciprocal(out=ss[:, 4:6], in_=ss[:, 2:4]), reads=["ss2"],
                     writes=["ss3"])
                for g2 in range(2):
                    P.op("dve", lambda e, g2=g2: e.scalar_tensor_tensor(
                        out=ybf[:, g2 * 512:(g2 + 1) * 512], in0=yt[:, g2 * 512:(g2 + 1) * 512],
                        scalar=ss[:, 4 + g2:5 + g2], in1=snwt[:, g2 * 512:(g2 + 1) * 512],
                        op0=ALU.mult, op1=ALU.mult), reads=["yt", "ss3", "snwt"], writes=["ybf"])
                for g in range(8):
                    P.op("pe", lambda e, g=g: e.transpose(
                        bankbf(7, 128, 64, g * 64), ybf[:, g * 128:(g + 1) * 128], ident_b[0:64, 0:64]),
                        reads=["ybf", "ident_b"], writes=[("ps", 7)])
                P.op("act", lambda e: e.activation(out=ymo.rearrange("p a b -> p (a b)"),
                                                   in_=bankbf(7, 128, 512, 0), func=AF.Copy),
                     reads=[("ps", 7)], writes=["ymo"])
                P.dma("sp", mixT[1024:2048, to:to + 64].rearrange("(g p) t -> p g t", p=128), ymo,
                      reads=["ymo"], writes=["mixT"], accum=True)
            P.op("act", lambda e: e.activation(out=Hbf, in_=Hs, func=AF.Copy), reads=["Hs"], writes=["Hbf"])
    if stop == 3:
        P.emit(final_wait_keys=["mixT"])
        P.close()
        return nc
    P.barrier()
    A.reset()

    phase_begin(4)
    wo = A.alloc([16, D], BF16)
    mxt = [A.alloc([16, 512], BF16) for _ in range(2)]
    xot = [A.alloc([D], F32) for _ in range(2)]
    rt = [A.alloc([D], F32) for _ in range(2)]
    g1 = A.alloc([D], F32)
    b1 = A.alloc([D], F32)
    st6 = A.alloc([4, 6], F32)
    mv = A.alloc([8], F32)
    hT32 = A.alloc([16, 128], F32)
    hTb = A.alloc([16, 128], BF16)
    hbt = [A.alloc([D], BF16) for _ in range(2)]
    wr32 = A.alloc([16, 36], F32)
    brt = A.alloc([36], F32)
    lg = A.alloc([36], F32)
    rs = A.alloc([16], F32)
    Gt = A.alloc([4], F32)
    elm = A.alloc([4, 8], F32)
    top8 = A.alloc([8], F32)
    msk = A.alloc([32], F32)
    ext = A.alloc([32], F32)
    P.dma("sp", wo.rearrange("p a b -> p (a b)"), wout_b, reads=["wout_b"], writes=["wo"])
    P.dma("sp", g1, ln1g.partition_broadcast(128), writes=["g1"])
    P.dma("sp", b1, ln1b.partition_broadcast(128), writes=["b1"])
    P.dma("sp", wr32, wr.rearrange("(kc p) c -> p kc c", p=128), writes=["wr32"])
    P.dma("sp", brt, br.partition_broadcast(128), writes=["brt"])

    def layernorm(src, dst, gk, bk, gt, bt, rkey, okey, tag, st6, mv):
        for cc in range(4):
            P.op("dve", lambda e, cc=cc: e.bn_stats(out=st6[:, cc, :], in_=src[:, cc * 512:(cc + 1) * 512]),
                 reads=[rkey], writes=[("st6", tag)])
        P.op("dve", lambda e: e.bn_aggr(out=mv[:, 0:2], in_=st6.rearrange("p a b -> p (a b)")),
             reads=[("st6", tag)], writes=[("mv", tag)])
        P.op("act", lambda e: e.activation(out=mv[:, 2:3], in_=mv[:, 1:2], func=AF.Sqrt,
                                           bias=epsln[:], scale=1.0),
             reads=[("mv", tag), "eps"], writes=[("mv2", tag)])
        P.op("dve", lambda e: e.reciprocal(out=mv[:, 3:4], in_=mv[:, 2:3]), reads=[("mv2", tag)],
             writes=[("mv3", tag)])
        P.op("dve", lambda e: e.tensor_scalar(out=src, in0=src, scalar1=mv[:, 0:1], scalar2=mv[:, 3:4],
                                              op0=ALU.subtract, op1=ALU.mult),
             reads=[rkey, ("mv3", tag), ("mv", tag)], writes=[rkey])
        P.op("dve", lambda e: e.tensor_tensor(out=src, in0=src, in1=gt, op=ALU.mult),
             reads=[rkey, gk], writes=[rkey])
        P.op("dve", lambda e: e.tensor_tensor(out=dst, in0=src, in1=bt, op=ALU.add),
             reads=[rkey, bk], writes=[okey])

    for t4 in range(SO // 512):
        mb = t4 % 2
        P.dma("sp", mxt[mb], mixT[:, t4 * 512:(t4 + 1) * 512].rearrange("(kc p) t -> p kc t", p=128),
              reads=["mixT"], writes=[("mxt", mb)])
        for ts in range(4):
            ti = t4 * 4 + ts
            tb = ti % 2
            tok0 = ti * 128
            P.dma("sp", xot[tb], xo[tok0:tok0 + 128, :], writes=[("xot", tb)])
            for cbk in range(4):
                for kc in range(16):
                    P.op("pe", lambda e, cbk=cbk, kc=kc, ts=ts, mb=mb: e.matmul(
                        bank(cbk), lhsT=mxt[mb][:, kc, ts * 128:(ts + 1) * 128],
                        rhs=wo[:, kc, cbk * 512:(cbk + 1) * 512], start=(kc == 0), stop=(kc == 15)),
                        reads=[("mxt", mb), "wo"], writes=[("ps", cbk)])
                P.op("dve", lambda e, cbk=cbk, tb=tb: e.scalar_tensor_tensor(
                    out=rt[tb][:, cbk * 512:(cbk + 1) * 512], in0=xot[tb][:, cbk * 512:(cbk + 1) * 512],
                    scalar=DN_ALPHA, in1=bank(cbk), op0=ALU.mult, op1=ALU.add),
                    reads=[("xot", tb), ("ps", cbk)], writes=[("rt", tb)])
            layernorm(rt[tb], rt[tb], "g1", "b1", g1, b1, ("rt", tb), ("rt", tb), "a", st6, mv)
            P.dma("sp", h1s[tok0:tok0 + 128, :], rt[tb], reads=[("rt", tb)], writes=["h1s"], accum=True)
            P.op("act", lambda e, tb=tb: e.activation(out=hbt[tb], in_=rt[tb], func=AF.Copy),
                 reads=[("rt", tb)], writes=[("hbt", tb)])
            P.dma("sp", h1b[tok0:tok0 + 128, :], hbt[tb], reads=[("hbt", tb)], writes=["h1b"], accum=True)
            for half in range(2):
                for k8 in range(8):
                    kc = half * 8 + k8
                    P.op("pe", lambda e, kc=kc, k8=k8, half=half, tb=tb: e.transpose(
                        bank(4 + half * 2 + k8 // 4, 128, 128, (k8 % 4) * 128),
                        rt[tb][:, kc * 128:(kc + 1) * 128], ident_f[:]),
                        reads=[("rt", tb), "ident_f"], writes=[("ps", 4 + half * 2 + k8 // 4)])
                for q2 in range(2):
                    bq = 4 + half * 2 + q2
                    kc0 = half * 8 + q2 * 4
                    P.op("act", lambda e, bq=bq, kc0=kc0: e.activation(
                        out=hT32[:, kc0:kc0 + 4, :].rearrange("p a b -> p (a b)"), in_=bank(bq), func=AF.Copy),
                        reads=[("ps", bq)], writes=[("hT32", kc0)])
            for kc in range(16):
                P.op("pe", lambda e, kc=kc: e.matmul(
                    bank(0, 128, 36), lhsT=hT32[:, kc, :], rhs=wr32[:, kc, :], start=(kc == 0), stop=(kc == 15)),
                    reads=[("hT32", (kc // 4) * 4), "wr32"], writes=[("ps", 0)])
            P.op("dve", lambda e: e.tensor_tensor(out=lg, in0=bank(0, 128, 36), in1=brt, op=ALU.add),
                 reads=[("ps", 0), "brt"], writes=["lg"])
            gl = lg[:, 0:4]
            el = lg[:, 4:36].rearrange("p (g e) -> p g e", g=4)
            P.op("dve", lambda e: e.tensor_reduce(out=rs[:, 0:1], in_=gl, axis=AX.X, op=ALU.max),
                 reads=["lg"], writes=["rs0"])
            P.op("dve", lambda e: e.tensor_scalar(out=rs[:, 1:2], in0=rs[:, 0:1], scalar1=-1.0, scalar2=None,
                                                  op0=ALU.mult), reads=["rs0"], writes=["rs1"])
            P.op("act", lambda e: e.activation(out=Gt, in_=gl, func=AF.Exp, bias=rs[:, 1:2], scale=1.0,
                                               accum_out=rs[:, 2:3]),
                 reads=["lg", "rs1"], writes=["Gt", "rs2"])
            P.op("dve", lambda e: e.reciprocal(out=rs[:, 3:4], in_=rs[:, 2:3]), reads=["rs2"], writes=["rs3"])
            P.op("dve", lambda e: e.tensor_scalar(out=Gt, in0=gl, scalar1=rs[:, 0:1], scalar2=None,
                                                  op0=ALU.is_ge), reads=["lg", "rs0", "Gt"], writes=["Gt"])
            P.op("dve", lambda e: e.tensor_scalar(out=Gt, in0=Gt, scalar1=-NEG, scalar2=NEG,
                                                  op0=ALU.mult, op1=ALU.add), reads=["Gt"], writes=["Gt"])
            P.op("dve", lambda e: e.tensor_tensor(out=elm, in0=el, in1=Gt.unsqueeze(2).to_broadcast([128, 4, 8]),
                                                  op=ALU.add), reads=["lg", "Gt"], writes=["elm"])
            elf = elm.rearrange("p a b -> p (a b)")
            P.op("dve", lambda e: e.max(out=top8, in_=elf), reads=["elm"], writes=["top8"])
            P.op("dve", lambda e: e.tensor_scalar(out=msk, in0=elf, scalar1=top8[:, 1:2], scalar2=None,
                                                  op0=ALU.is_ge), reads=["elm", "top8"], writes=["msk"])
            P.op("dve", lambda e: e.tensor_scalar(out=rs[:, 4:5], in0=top8[:, 0:1], scalar1=-1.0, scalar2=None,
                                                  op0=ALU.mult), reads=["top8"], writes=["rs4"])
            P.op("act", lambda e: e.activation(out=ext, in_=elf, func=AF.Exp, bias=rs[:, 4:5], scale=1.0),
                 reads=["elm", "rs4"], writes=["ext"])
            P.op("act", lambda e: e.activation(out=rs[:, 5:6], in_=top8[:, 1:2], func=AF.Exp, bias=rs[:, 4:5],
                                               scale=1.0), reads=["top8", "rs4"], writes=["rs5"])
            P.op("dve", lambda e: e.tensor_scalar(out=rs[:, 6:7], in0=rs[:, 5:6], scalar1=1.0, scalar2=None,
                                                  op0=ALU.add), reads=["rs5"], writes=["rs6"])
            P.op("dve", lambda e: e.reciprocal(out=rs[:, 7:8], in_=rs[:, 6:7]), reads=["rs6"], writes=["rs7"])
            P.op("dve", lambda e: e.tensor_tensor(out=rs[:, 8:9], in0=rs[:, 7:8], in1=rs[:, 3:4], op=ALU.mult),
                 reads=["rs7", "rs3"], writes=["rs8"])
            P.op("dve", lambda e, ti=ti: e.scalar_tensor_tensor(
                out=Wall[:, ti * 32:(ti + 1) * 32], in0=ext, scalar=rs[:, 8:9], in1=msk,
                op0=ALU.mult, op1=ALU.mult), reads=["ext", "rs8", "msk"], writes=["Wall"])
    if stop == 4:
        wall_out = nc.dram_tensor("Wall_out", [128, (SO // 128) * 32], F32, kind="ExternalOutput").ap()
        P.dma("sp", wall_out, Wall[:], reads=["Wall"], writes=["wall_out"])
        P.emit(final_wait_keys=["h1s","h1b","wall_out"])
        P.close()
        return nc
    P.barrier()
    A.reset()

    phase_begin(5)
    if start == 5:
        wall_in = nc.dram_tensor("Wall_in", [128, (SO // 128) * 32], F32, kind="ExternalInput").ap()
        P.dma("sp", Wall[:], wall_in, writes=["Wall"])
    I32 = mybir.dt.int32
    U32 = mybir.dt.uint32
    didx = P.sb([128, NTT * 2], I32, "didx")
    wk = P.sb([128, NTT * 2], F32, "wk")
    gidx = P.sb([128, NBLK], I32, "gidx")
    lmat_b = A.alloc([128], BF16)
    ones_b = A.alloc([128], BF16)
    sel = A.alloc([NTT, 32], BF16)
    carry = A.alloc([32], F32)
    rank = A.alloc([NTT, 32], F32)
    dest = A.alloc([NTT, 32], F32)
    dsel = A.alloc([NTT, 32], F32)
    t8 = A.alloc([NTT, 8], F32)
    thr = A.alloc([32], F32)
    cmpt = A.alloc([32, 32], F32)
    nbe = A.alloc([32], F32)
    padded = A.alloc([32], F32)
    pend = A.alloc([32], F32)
    pstart = A.alloc([32], F32)
    onesf = A.alloc([32], F32)
    bthr = A.alloc([NBLK], F32)
    cmpb = A.alloc([NBLK, 32], F32)
    ebt = A.alloc([NBLK], F32)
    pidx = A.alloc([1], F32)
    didf = A.alloc([NTT, 2], F32)
    junk = A.alloc([32], F32)
    Wv = Wall[:].rearrange("p (t e) -> p t e", e=32)
    P.dma("pool", lmat_b, lmat_in, writes=["lmat_b"])
    P.dma("sp", thr, thr_in, writes=["thr"])
    P.dma("sp", bthr, bthr_in, writes=["bthr"])
    P.dma("sp", pidx, pidx_in, writes=["pidx"])
    P.op("dve", lambda e: e.memset(ones_b, 1.0), writes=["ones_b"])
    P.op("dve", lambda e: e.memset(onesf, 1.0), writes=["onesf"])
    P.op("dve", lambda e: e.memset(carry, 0.0), writes=["carry"])
    P.op("dve", lambda e: e.tensor_single_scalar(out=sel, in_=Wv, scalar=0.0, op=ALU.is_gt),
         reads=["Wall"], writes=["sel"])
    for ti in range(NTT):
        bk = ti % 2
        P.op("pe", lambda e, ti=ti, bk=bk: e.matmul(bank(bk, 128, 32, 0), lhsT=lmat_b, rhs=sel[:, ti, :],
                                                   start=True, stop=True),
             reads=["lmat_b", "sel"], writes=[("ps", bk)])
        P.op("pe", lambda e, ti=ti, bk=bk: e.matmul(bank(bk, 128, 32, 32), lhsT=ones_b, rhs=sel[:, ti, :],
                                                   start=True, stop=True),
             reads=["ones_b", "sel"], writes=[("ps", bk)])
        P.op("dve", lambda e, ti=ti, bk=bk: e.tensor_tensor(out=rank[:, ti, :], in0=bank(bk, 128, 32, 0),
                                                           in1=carry, op=ALU.add),
             reads=[("ps", bk), "carry"], writes=[("rank", ti)])
        P.op("dve", lambda e, bk=bk: e.tensor_tensor(out=carry, in0=bank(bk, 128, 32, 32), in1=carry, op=ALU.add),
             reads=[("ps", bk), "carry"], writes=["carry"])
    rkeys = [("rank", ti) for ti in range(NTT)]
    P.op("dve", lambda e: e.tensor_tensor(out=cmpt, in0=carry.unsqueeze(2).to_broadcast([128, 32, 32]),
                                          in1=thr.unsqueeze(1).to_broadcast([128, 32, 32]), op=ALU.is_gt),
         reads=["carry", "thr"], writes=["cmpt"])
    P.op("dve", lambda e: e.tensor_reduce(out=nbe, in_=cmpt, axis=AX.X, op=ALU.add), reads=["cmpt"], writes=["nbe"])
    P.op("dve", lambda e: e.tensor_scalar(out=padded, in0=nbe, scalar1=float(BLK), scalar2=None, op0=ALU.mult),
         reads=["nbe"], writes=["padded"])
    P.op("dve", lambda e: e.tensor_tensor_scan(out=pend, data0=onesf, data1=padded, initial=0.0,
                                               op0=ALU.mult, op1=ALU.add),
         reads=["onesf", "padded"], writes=["pend"])
    P.op("dve", lambda e: e.tensor_tensor(out=pstart, in0=pend, in1=padded, op=ALU.subtract),
         reads=["pend", "padded"], writes=["pstart"])
    P.op("dve", lambda e: e.tensor_tensor(out=dest, in0=rank, in1=pstart.unsqueeze(1).to_broadcast([128, NTT, 32]),
                                          op=ALU.add), reads=rkeys + ["pstart"], writes=["dest"])
    P.op("dve", lambda e: e.scalar_tensor_tensor(out=dsel.rearrange("p a b -> p (a b)"),
                                                 in0=dest.rearrange("p a b -> p (a b)"), scalar=1.0,
                                                 in1=sel.rearrange("p a b -> p (a b)"), op0=ALU.add, op1=ALU.mult),
         reads=["dest", "sel"], writes=["dsel"])
    for ti in range(NTT):
        P.op("dve", lambda e, ti=ti: e.max(out=t8[:, ti, :], in_=dsel[:, ti, :]), reads=["dsel"],
             writes=[("t8", ti)])
        for k in range(2):
            P.op("dve", lambda e, ti=ti, k=k: e.scalar_tensor_tensor(
                out=junk, in0=dsel[:, ti, :], scalar=t8[:, ti, k:k + 1], in1=Wv[:, ti, :],
                op0=ALU.is_equal, op1=ALU.mult, accum_out=wk[:, ti * 2 + k:ti * 2 + k + 1]),
                reads=["dsel", ("t8", ti), "Wall"], writes=["junk", ("wk", ti, k)])
    tkeys = [("t8", ti) for ti in range(NTT)]
    P.op("dve", lambda e: e.tensor_scalar(out=didf, in0=t8[:, :, 0:2], scalar1=-1.0, scalar2=None, op0=ALU.add),
         reads=tkeys, writes=["didf"])
    P.op("dve", lambda e: e.tensor_copy(out=didx[:], in_=didf.rearrange("p a b -> p (a b)")),
         reads=["didf"], writes=["didx"])
    P.op("dve", lambda e: e.tensor_tensor(out=cmpb, in0=pend.unsqueeze(1).to_broadcast([128, NBLK, 32]),
                                          in1=bthr.unsqueeze(2).to_broadcast([128, NBLK, 32]), op=ALU.is_le),
         reads=["pend", "bthr"], writes=["cmpb"])
    P.op("dve", lambda e: e.tensor_reduce(out=ebt, in_=cmpb, axis=AX.X, op=ALU.add), reads=["cmpb"], writes=["ebt"])
    P.op("dve", lambda e: e.tensor_scalar(out=ebt, in0=ebt, scalar1=float(NEXP - 1), scalar2=128.0,
                                          op0=ALU.min, op1=ALU.mult), reads=["ebt"], writes=["ebt"])
    P.op("dve", lambda e: e.tensor_scalar(out=ebt, in0=ebt, scalar1=pidx[:, 0:1], scalar2=None, op0=ALU.add),
         reads=["ebt", "pidx"], writes=["ebt"])
    P.op("dve", lambda e: e.tensor_copy(out=gidx[:], in_=ebt), reads=["ebt"], writes=["gidx"])
    if stop == 51:
        o1 = nc.dram_tensor("didx_o", [128, NTT * 2], I32, kind="ExternalOutput").ap()
        o2 = nc.dram_tensor("wk_o", [128, NTT * 2], F32, kind="ExternalOutput").ap()
        o3 = nc.dram_tensor("gidx_o", [128, NBLK], I32, kind="ExternalOutput").ap()
        o4 = nc.dram_tensor("pend_o", [128, 32], F32, kind="ExternalOutput").ap()
        P.dma("sp", o1, didx[:], reads=["didx"], writes=["o1"])
        P.dma("sp", o2, wk[:], reads=[("wk", ti, k) for ti in range(NTT) for k in range(2)], writes=["o2"])
        P.dma("sp", o3, gidx[:], reads=["gidx"], writes=["o3"])
        P.dma("sp", o4, pend, reads=["pend"], writes=["o4"])
        P.emit(final_wait_keys=["o1", "o2", "o3", "o4"])
        P.close()
        return nc
    P.barrier()
    A.reset()
    xrow = [A.alloc([D], BF16) for _ in range(2)]
    Gw = A.alloc([16, EH], BF16)
    Uw = A.alloc([16, EH], BF16)
    Dw = A.alloc([8, D], BF16)
    xblk = [A.alloc([D], BF16) for _ in range(2)]
    xTb5 = A.alloc([16, BLK], BF16)
    sg5 = [A.alloc([BLK], F32) for _ in range(2)]
    hid5 = A.alloc([8, BLK], BF16)
    yst = [A.alloc([D], F32) for _ in range(2)]
    for ti in range(NTT):
        xb_ = ti % 2
        P.dma("sp", xrow[xb_], h1b[ti * 128:(ti + 1) * 128, :], reads=["h1b"], writes=[("xrow", xb_), "spq"])
        for k in range(2):
            P.op("pool", lambda e, ti=ti, k=k, xb_=xb_: e.indirect_dma_start(
                out=xsort, out_offset=bass.IndirectOffsetOnAxis(
                    ap=didx[:, ti * 2 + k:ti * 2 + k + 1].bitcast(U32), axis=0),
                in_=xrow[xb_], in_offset=None),
                reads=[("xrow", xb_), "didx"], writes=["xsort", "poolq"], kind="d")
    erow = 0
    for b in range(NBLK):
        gio = bass.IndirectOffsetOnAxis
        P.op("pool", lambda e, b=b: e.indirect_dma_start(
            out=Gw.rearrange("p a b -> p (a b)"), out_offset=None, in_=wg_b,
            in_offset=bass.IndirectOffsetOnAxis(ap=gidx[:, b:b + 1].bitcast(U32), axis=0)),
            reads=["gidx", "wg_b"], writes=["Gw", "poolq"], kind="d")
        P.op("pool", lambda e, b=b: e.indirect_dma_start(
            out=Uw.rearrange("p a b -> p (a b)"), out_offset=None, in_=wu_b,
            in_offset=bass.IndirectOffsetOnAxis(ap=gidx[:, b:b + 1].bitcast(U32), axis=0)),
            reads=["gidx", "wu_b"], writes=["Uw", "poolq"], kind="d")
        P.op("pool", lambda e, b=b: e.indirect_dma_start(
            out=Dw.rearrange("p a b -> p (a b)"), out_offset=None, in_=wd_b,
            in_offset=bass.IndirectOffsetOnAxis(ap=gidx[:, b:b + 1].bitcast(U32), axis=0)),
            reads=["gidx", "wd_b"], writes=["Dw", "poolq"], kind="d")
        for st in range(2):
            r0 = b * BLK + st * 128
            P.dma("sp", xblk[st], xsort[r0:r0 + 128, :], reads=["xsort"], writes=[("xblk", st), "spq"])
            for k4 in range(4):
                bk = k4 % 2
                for kk in range(4):
                    kc = k4 * 4 + kk
                    P.op("pe", lambda e, st=st, kc=kc, kk=kk, bk=bk: e.transpose(
                        bankbf(bk, 128, 128, kk * 128), xblk[st][:, kc * 128:(kc + 1) * 128], ident_b[:]),
                        reads=[("xblk", st), "ident_b"], writes=[("ps", bk)])
                evac(xTb5[:, k4 * 4:(k4 + 1) * 4, st * 128:(st + 1) * 128],
                     bankbf(bk, 128, 512, 0).rearrange("p (a b) -> p a b", a=4),
                     [("ps", bk)], [("xT5", st, k4)])
        xkeys = [("xT5", st, k4) for st in range(2) for k4 in range(4)]
        for hc in range(8):
            bg = 2 + (hc % 2) * 2
            for kc in range(16):
                P.op("pe", lambda e, bg=bg, kc=kc, hc=hc: e.matmul(
                    bank(bg, 128, BLK), lhsT=Gw[:, kc, hc * 128:(hc + 1) * 128], rhs=xTb5[:, kc, :],
                    start=(kc == 0), stop=(kc == 15)), reads=["Gw"] + xkeys, writes=[("ps", bg)])
            for kc in range(16):
                P.op("pe", lambda e, bg=bg, kc=kc, hc=hc: e.matmul(
                    bank(bg + 1, 128, BLK), lhsT=Uw[:, kc, hc * 128:(hc + 1) * 128], rhs=xTb5[:, kc, :],
                    start=(kc == 0), stop=(kc == 15)), reads=["Uw"] + xkeys, writes=[("ps", bg + 1)])
            sgi = hc % 2
            P.op("act", lambda e, bg=bg, sgi=sgi: e.activation(out=sg5[sgi], in_=bank(bg, 128, BLK), func=AF.Silu),
                 reads=[("ps", bg)], writes=[("sg5", sgi)])
            P.op("dve", lambda e, bg=bg, sgi=sgi, hc=hc: e.tensor_tensor(
                out=hid5[:, hc, :], in0=sg5[sgi], in1=bank(bg + 1, 128, BLK), op=ALU.mult),
                reads=[("sg5", sgi), ("ps", bg + 1)], writes=[("hid5", hc)])
        hkeys = [("hid5", hc) for hc in range(8)]
        for st in range(2):
            r0 = b * BLK + st * 128
            for cbk in range(4):
                bd = 6 + cbk % 2
                for fc in range(8):
                    P.op("pe", lambda e, bd=bd, fc=fc, st=st, cbk=cbk: e.matmul(
                        bank(bd), lhsT=hid5[:, fc, st * 128:(st + 1) * 128],
                        rhs=Dw[:, fc, cbk * 512:(cbk + 1) * 512], start=(fc == 0), stop=(fc == 7)),
                        reads=hkeys + ["Dw"], writes=[("ps", bd)])
                evac(yst[st][:, cbk * 512:(cbk + 1) * 512], bank(bd), [("ps", bd)], [("yst", st, cbk)])
            P.dma("sp", ysort[r0:r0 + 128, :], yst[st], reads=[("yst", st, c_) for c_ in range(4)],
                  writes=["ysort", "spq"])
    P.barrier()
    A.reset()
    g2t = A.alloc([D], F32)
    b2t = A.alloc([D], F32)
    y0t = [A.alloc([D], F32) for _ in range(2)]
    y1t = [A.alloc([D], F32) for _ in range(2)]
    h1t = [A.alloc([D], F32) for _ in range(2)]
    st6b = A.alloc([4, 6], F32)
    mvb = A.alloc([8], F32)
    P.dma("sp", g2t, ln2g.partition_broadcast(128), writes=["g2"])
    P.dma("sp", b2t, ln2b.partition_broadcast(128), writes=["b2"])
    for ti in range(NTT):
        hb_ = ti % 2
        tok0 = ti * 128
        P.dma("sp", h1t[hb_], h1s[tok0:tok0 + 128, :], reads=["h1s"], writes=[("h1t", hb_), "spq"])
        for k, yt_ in ((0, y0t), (1, y1t)):
            P.op("pool", lambda e, ti=ti, k=k, yt_=yt_, hb_=hb_: e.indirect_dma_start(
                out=yt_[hb_], out_offset=None, in_=ysort,
                in_offset=bass.IndirectOffsetOnAxis(ap=didx[:, ti * 2 + k:ti * 2 + k + 1].bitcast(U32), axis=0)),
                reads=["didx", "ysort"], writes=[("yk", k, hb_), "poolq"], kind="d")
        P.op("dve", lambda e, ti=ti, hb_=hb_: e.tensor_scalar(
            out=y0t[hb_], in0=y0t[hb_], scalar1=wk[:, ti * 2:ti * 2 + 1], scalar2=None, op0=ALU.mult),
            reads=[("yk", 0, hb_)] + [("wk", ti, 0)], writes=[("yk", 0, hb_)])
        P.op("dve", lambda e, ti=ti, hb_=hb_: e.scalar_tensor_tensor(
            out=y0t[hb_], in0=y1t[hb_], scalar=wk[:, ti * 2 + 1:ti * 2 + 2], in1=y0t[hb_],
            op0=ALU.mult, op1=ALU.add),
            reads=[("yk", 0, hb_), ("yk", 1, hb_), ("wk", ti, 1)], writes=[("yk", 0, hb_)])
        P.op("dve", lambda e, hb_=hb_: e.scalar_tensor_tensor(
            out=h1t[hb_], in0=h1t[hb_], scalar=DN_ALPHA, in1=y0t[hb_], op0=ALU.mult, op1=ALU.add),
            reads=[("h1t", hb_), ("yk", 0, hb_)], writes=[("h1t", hb_)])
        layernorm(h1t[hb_], h1t[hb_], "g2", "b2", g2t, b2t, ("h1t", hb_), ("h1t", hb_), "b", st6b, mvb)
        P.dma("sp", out[tok0:tok0 + 128, :], h1t[hb_], reads=[("h1t", hb_)], writes=["out", "spq"])
    P.emit(final_wait_keys=["out"])
    P.close()
    return nc


def _consts(S, hf):
    SO = S // 2
    SC = S - SO
    slopes = np.exp2(-8.0 * np.arange(1, 9, dtype=np.float64) / 8)
    tok = np.arange(S)
    blk = tok // 128
    rel = tok % 128
    ktab = np.zeros((8, 4, S), np.float32)
    qtab = np.zeros((8, 4, SO), np.float32)
    for h in range(8):
        sl = slopes[h]
        ktab[h, 0] = 1.0
        ktab[h, 1] = 1.0
        ktab[h, 2] = sl * 128 * blk
        ktab[h, 3] = sl * rel
        if hf == 0:
            ktab[h, 2, :SC] += NEG
        qtab[h, 0] = -sl * 128 * blk[SC:]
        qtab[h, 1] = -sl * rel[SC:]
        qtab[h, 2] = 1.0
        qtab[h, 3] = 1.0
    ttab = np.zeros((128, 8, 896), np.float32)
    s = np.arange(128)[:, None]
    t = np.arange(128)[None, :]
    for h in range(8):
        d = np.zeros((128, 128), np.float64)
        fut = (s > t) & ((s // 64) == (t // 64))
        d[fut] = (-2.0 * slopes[h] * (s - t))[fut]
        d[(s // 64) > (t // 64)] = NEG
        ttab[:, h, 0:384] = NEG
        ttab[:, h, 384:512] = d
    j = np.arange(64)[:, None]
    l = np.arange(64)[None, :]
    tri = (j <= l).astype(np.float32)
    umat = (j > l).astype(np.float32)
    flag = np.full((128, 1), 1.0 if hf == 1 else 0.0, np.float32)
    nblk = (2 * SO) // 256 + NEXP
    a = np.arange(128)
    lmat = (a[:, None] < a[None, :]).astype(np.float32)
    thr256 = np.tile((256.0 * np.arange(32, dtype=np.float32))[None], (128, 1))
    bthr = np.tile((256.0 * np.arange(nblk, dtype=np.float32))[None], (128, 1))
    pidx = a.astype(np.float32)[:, None]
    return dict(ktab=ktab, qtab=qtab, ttab=ttab.reshape(128, 8 * 896), tri=tri, umat=umat, flag=flag,
                ident=np.eye(128, dtype=np.float32), lmat=lmat, thr256=thr256, bthr=bthr, pidx=pidx)


_CACHE = {}


def kernel(x, w_in, lambda_q1, lambda_k1, lambda_q2, lambda_k2, attn_norm_w, conv_w, conv_b,
           dt_bias, a_log, d_skip, ssm_norm_w, w_out, ln1_g, ln1_b, w_router_group,
           b_router_group, w_router_expert, b_router_expert, w_gate, w_up, w_down,
           ln2_g, ln2_b):
    x = np.asarray(x, np.float32)
    B, S, _ = x.shape
    SO = S // 2
    f = lambda a: np.ascontiguousarray(np.asarray(a, np.float32))
    if S not in _CACHE:
        _CACHE[S] = build_program(S)
    nc = _CACHE[S]
    wr = np.concatenate([f(w_router_group)[0], np.transpose(f(w_router_expert)[0], (1, 0, 2)).reshape(D, 32)], 1)
    br = np.concatenate([f(b_router_group)[0], f(b_router_expert)[0].reshape(32)])[None]
    lamv = np.concatenate([f(lambda_q1)[0], f(lambda_k1)[0], f(lambda_q2)[0], f(lambda_k2)[0]])[None]
    cw = np.ascontiguousarray(f(conv_w)[0].T.reshape(12, 128, 4).transpose(1, 0, 2).reshape(128, 48))
    cb = np.ascontiguousarray(f(conv_b)[0].reshape(12, 128).T)
    shared = dict(
        w_in=f(w_in)[0], w_out=f(w_out)[0], w_gate=f(w_gate)[0], w_up=f(w_up)[0], w_down=f(w_down)[0],
        wr=f(wr), br=f(br), lamv=f(lamv), anw=f(attn_norm_w), cw=cw, cb=cb, dtb=f(dt_bias), alog=f(a_log),
        dsk=f(d_skip), snw=f(ssm_norm_w), ln1g=f(ln1_g), ln1b=f(ln1_b), ln2g=f(ln2_g), ln2b=f(ln2_b))
    in_maps = []
    for b in range(B):
        for hf in range(2):
            m = dict(shared)
            m.update(_consts(S, hf))
            own = x[b, hf * SO:(hf + 1) * SO]
            ctx = x[b, 0:SO] if hf == 1 else np.zeros_like(own)
            m["xT"] = np.ascontiguousarray(np.concatenate([ctx, own], 0).T)
            m["xo"] = np.ascontiguousarray(own)
            in_maps.append(m)
    res = run_bass_kernel_spmd(nc, in_maps, core_ids=list(range(B * 2)))
    outp = np.zeros((B, S, D), np.float32)
    for b in range(B):
        for hf in range(2):
            outp[b, hf * SO:(hf + 1) * SO] = res.results[b * 2 + hf]["out"]
    return outp
```

```python
from contextlib import ExitStack
import math
import numpy as np
import concourse.bass as bass
import concourse.mybir as mybir
from concourse.bass_utils import run_bass_kernel_spmd

F32 = mybir.dt.float32
BF16 = mybir.dt.bfloat16
AF = mybir.ActivationFunctionType
ALU = mybir.AluOpType
AX = mybir.AxisListType

D = 2048
IN_COLS = 5648
NEXP = 32
EH = 1024
DN_ALPHA = 2.0 ** 0.25
LN_EPS = 1e-5
RMS_EPS = 1e-6
LAMBDA_INIT = 0.8 - 0.6 * math.exp(0.0)
NEG = -30000.0


class Prog:
    ENGS = ("pe", "act", "dve", "pool", "sp")

    def __init__(self, nc):
        self.nc = nc
        self.ops = []
        self.stack = ExitStack()
        self._n = 0
        self.barrier_at = []
        self.maxops = None

    def sb(self, shape, dtype, name=None):
        self._n += 1
        return self.stack.enter_context(
            self.nc.sbuf_tensor(name or f"sb{self._n}", list(shape), dtype))

    def op(self, eng, fn, reads=(), writes=(), kind="c", accum=False):
        if self.maxops is not None and len(self.ops) >= self.maxops:
            return
        import sys as _sys
        fr = _sys._getframe(1)
        if fr.f_code.co_name in ("dma", "evac", "convert", "layernorm"):
            fr = fr.f_back
        self.ops.append(dict(eng=eng, kind=kind, fn=fn, reads=tuple(reads),
                             writes=tuple(writes), accum=accum, line=fr.f_lineno))

    def dma(self, eng, out, in_, reads=(), writes=(), accum=False, **kw):
        self.op(eng, lambda e: e.dma_start(out=out, in_=in_, **kw), reads, writes,
                kind="d", accum=accum)

    def barrier(self):
        self.barrier_at.append(len(self.ops))

    def emit(self, final_wait_keys=()):
        nc = self.nc
        ops = self.ops
        n = len(ops)
        writers = {}
        gen_first = {}
        readers = {}
        deps = [None] * n
        needed = [False] * n
        bset = set()
        last_of = {}
        barriers = set(self.barrier_at)
        KSLOT = 16
        dcount = {}
        for o in ops:
            if o["kind"] == "d":
                c_ = dcount.get(o["eng"], 0)
                o["slot"] = c_ % KSLOT
                dcount[o["eng"]] = c_ + 1
            else:
                o["slot"] = 0

        def skof(o):
            return (o["eng"], o["kind"], o["slot"])

        def bank_of(k):
            if isinstance(k, tuple) and k[0] == "ps":
                return ("bankx", k[1])
            if isinstance(k, tuple) and k[0] == "O":
                return ("bankx", 4 + k[1] * 2 + k[2] // 2)
            return None

        for o in ops:
            if o["kind"] != "c":
                continue
            bx = {bank_of(k) for k in o["reads"] + o["writes"]} - {None}
            if bx and not o.get("bx_done"):
                o["writes"] = o["writes"] + tuple(bx)
                o["bx_done"] = True
        for i, o in enumerate(ops):
            if i in barriers:
                bset = set(last_of.values())
            d = set(bset)
            sk = skof(o)
            for k in o["reads"]:
                d.update(writers.get(k, {}).values())
            if not o["accum"]:
                for k in o["writes"]:
                    d.update(writers.get(k, {}).values())
                    d.update(readers.get(k, ()))
            else:
                for k in o["writes"]:
                    if k in gen_first:
                        d.add(gen_first[k])
                    if isinstance(k, tuple) and k[0] == "bankx":
                        d.update(writers.get(k, {}).values())
                        d.update(readers.get(k, ()))
            d.discard(i)
            if o["eng"] == "pe" and o["kind"] == "c":
                d = {j for j in d if not (ops[j]["eng"] == "pe" and ops[j]["kind"] == "c")}
            deps[i] = d
            for j in d:
                needed[j] = True
            for k in o["writes"]:
                if o["accum"]:
                    writers.setdefault(k, {})[sk] = i
                else:
                    writers[k] = {sk: i}
                    readers[k] = []
                    gen_first[k] = i
            for k in o["reads"]:
                readers.setdefault(k, []).append(i)
            last_of[sk] = i
            if o["kind"] == "d":
                needed[i] = True
        last_writer = {k: max(v.values()) for k, v in writers.items()}
        final = set()
        for k in final_wait_keys:
            final.update(writers.get(k, {}).values())
        for j in final:
            needed[j] = True
        semkeys = sorted({skof(o) for o in ops})
        sems = {sk: self.stack.enter_context(nc.semaphore(f"s_{sk[0]}_{sk[1]}_{sk[2]}"))
                for sk in semkeys}
        cnt = {sk: 0 for sk in semkeys}
        val = [0] * n
        for i, o in enumerate(ops):
            if needed[i]:
                sk = skof(o)
                cnt[sk] += 16 if o["kind"] == "d" else 1
                val[i] = cnt[sk]
        self.sem_counts = cnt
        engmap = {"pe": "tensor", "act": "scalar", "dve": "vector", "pool": "gpsimd",
                  "sp": "sync"}
        block = self.stack.enter_context(nc.Block())
        for eng in self.ENGS:
            idxs = [i for i, o in enumerate(ops) if o["eng"] == eng]
            if not idxs and eng != "sp":
                continue

            def body(e, idxs=idxs, eng=eng):
                waited = {}
                for i in idxs:
                    o = ops[i]
                    need = {}
                    for j in deps[i]:
                        sk = skof(ops[j])
                        if val[j] > need.get(sk, 0):
                            need[sk] = val[j]
                    for sk, v in need.items():
                        if waited.get(sk, 0) < v:
                            e.wait_ge(sems[sk], v)
                            waited[sk] = v
                    ins = o["fn"](e)
                    if needed[i]:
                        ins.then_inc(sems[skof(o)],
                                     16 if o["kind"] == "d" else 1)
                if eng == "sp":
                    need = {}
                    for j in final:
                        sk = skof(ops[j])
                        need[sk] = max(need.get(sk, 0), val[j])
                    for sk, v in need.items():
                        if waited.get(sk, 0) < v:
                            e.wait_ge(sems[sk], v)

            getattr(block, engmap[eng])(body)

    def close(self):
        self.stack.close()


def _dsz(dt):
    return 4 if dt == F32 else 2


class Arena:
    def __init__(self, P, nbytes):
        self.t = P.sb([128, nbytes // 4], F32, name="arena")
        self.off = 0
        self.cap = nbytes

    def alloc(self, free, dtype, parts=128):
        free = tuple(free)
        nel = int(np.prod(free))
        sz = (nel * _dsz(dtype) + 63) // 64 * 64
        assert self.off + sz <= self.cap, (self.off, sz, self.cap)
        ap = self.t[0:parts, self.off // 4:(self.off + sz) // 4]
        self.off += sz
        if dtype != F32:
            ap = ap.bitcast(dtype)
        ap = ap[:, 0:nel]
        if len(free) == 2:
            ap = ap.rearrange("p (a b) -> p a b", a=free[0])
        elif len(free) == 3:
            ap = ap.rearrange("p (a b c) -> p a b c", a=free[0], b=free[1])
        return ap

    def reset(self):
        self.off = 0


def win_groups():
    g = []
    for h in range(8):
        g.append(("q", h * 128, 128))
    for h in range(8):
        g.append(("k", 1024 + h * 128, 128))
    for i in range(2):
        g.append(("v", 2048 + i * 512, 512))
    for i in range(2):
        g.append(("z", 3072 + i * 512, 512))
    for i in range(12):
        g.append(("x", 4096 + i * 128, 128))
    g.append(("d", 5632, 16))
    return g


def build_program(S, stop=None, start=0):
    SO = S // 2
    SA = S
    SC = S - SO
    NB = SA // 128
    nc = bass.Bass("TRN2", target_bir_lowering=False)

    def din(name, shape, dt=F32):
        kind = "ExternalInput"
        if start > 0 and name in ("w_in", "w_out", "w_gate", "w_up", "w_down", "xT"):
            kind = "Internal"
        return nc.dram_tensor(name, list(shape), dt, kind=kind).ap()

    PROD = dict(win_b=0, wout_b=0, wg_b=0, wu_b=0, wd_b=0, qs=1, ks=1, vs=1, zs=1, xbcs=1, dts=1,
                mixT=3, h1s=4, h1T=4, xsort=5, ysort=5, h1b=4)

    def dscr(name, shape, dt):
        dbg = stop is not None and (stop == 0 or not name.startswith("w"))
        kind = "ExternalOutput" if dbg else "Internal"
        if start > 0 and PROD[name] < start and not name.startswith("w"):
            kind = "ExternalInput"
        if start > 0 and name.startswith("w"):
            kind = "ExternalInput" if (start == 4 and name == "wout_b") else "Internal"
        return nc.dram_tensor(name, list(shape), dt, kind=kind).ap()

    xT = din("xT", [D, SA])
    xo = din("xo", [SO, D])
    w_in = din("w_in", [D, IN_COLS])
    w_out = din("w_out", [D, D])
    w_gate = din("w_gate", [NEXP, D, EH])
    w_up = din("w_up", [NEXP, D, EH])
    w_down = din("w_down", [NEXP, EH, D])
    wr = din("wr", [D, 36])
    br = din("br", [1, 36])
    lamv = din("lamv", [1, 256])
    anw = din("anw", [1, 128])
    cw = din("cw", [128, 48])
    cb = din("cb", [128, 12])
    dtb = din("dtb", [1, 16])
    alog = din("alog", [1, 16])
    dsk = din("dsk", [1, 16])
    snw = din("snw", [1, 1024])
    ln1g = din("ln1g", [1, D])
    ln1b = din("ln1b", [1, D])
    ln2g = din("ln2g", [1, D])
    ln2b = din("ln2b", [1, D])
    ident_in = din("ident", [128, 128])
    qtab = din("qtab", [8, 4, SO])
    ktab = din("ktab", [8, 4, SA])
    ttab = din("ttab", [128, 8 * 896])
    tri_in = din("tri", [64, 64])
    umat_in = din("umat", [64, 64])
    flag_in = din("flag", [128, 1])
    out = nc.dram_tensor("out", [SO, D], F32, kind="ExternalOutput").ap()

    groups = win_groups()
    goff = []
    o = 0
    for (_, _, gc) in groups:
        goff.append(o)
        o += 128 * 16 * gc
    win_b = dscr("win_b", [o], BF16)
    wout_b = dscr("wout_b", [128, 16 * D], BF16)
    wg_b = dscr("wg_b", [NEXP * 128, 16 * EH], BF16)
    wu_b = dscr("wu_b", [NEXP * 128, 16 * EH], BF16)
    wd_b = dscr("wd_b", [NEXP * 128, 8 * D], BF16)
    BLK = 256
    NTT = SO // 128
    NBLK = (2 * SO) // BLK + NEXP
    xsort = dscr("xsort", [NBLK * BLK, D], BF16)
    ysort = dscr("ysort", [NBLK * BLK, D], F32)
    h1b = dscr("h1b", [SO, D], BF16)
    lmat_in = din("lmat", [128, 128])
    thr_in = din("thr256", [128, 32])
    bthr_in = din("bthr", [128, NBLK])
    pidx_in = din("pidx", [128, 1])
    qs = dscr("qs", [16, 64, SO], BF16)
    ks = dscr("ks", [16, 64, SA], BF16)
    vs = dscr("vs", [SA, 1024], BF16)
    zs = dscr("zs", [SO, 1024], F32)
    xbcs = dscr("xbcs", [1536, SA], F32)
    dts = dscr("dts", [SA, 16], F32)
    mixT = dscr("mixT", [D, SO], BF16)
    h1s = dscr("h1s", [SO, D], F32)
    h1T = dscr("h1T", [D, SO], BF16)

    P = Prog(nc)
    A = Arena(P, 176 * 1024)
    pp = P.stack.enter_context(nc.psum_tensor("pp", [128, 4096], F32))

    def bank(b, parts=128, n=512, off=0):
        return pp[0:parts, b * 512 + off:b * 512 + off + n]

    def bankbf(b, parts=128, n=1024, off=0):
        return pp[0:parts, b * 512:(b + 1) * 512].bitcast(BF16)[:, off:off + n]

    ident_f = P.sb([128, 128], F32, "ident_f")
    ident_b = P.sb([128, 128], BF16, "ident_b")
    Wall = P.sb([128, (SO // 128) * 32], F32, "Wall")
    epsln = P.sb([128, 1], F32, "epsln")
    epsrms = P.sb([128, 1], F32, "epsrms")
    P.dma("sp", ident_f[:], ident_in, writes=["ident_f"])
    P.dma("pool", ident_b[:], ident_in, writes=["ident_b"])
    P.op("dve", lambda e: e.memset(epsln[:], LN_EPS), writes=["eps"])
    P.op("dve", lambda e: e.memset(epsrms[:], RMS_EPS), writes=["eps2"])
    onec = P.sb([128, 1], F32, "onec")
    P.op("dve", lambda e: e.memset(onec[:], 1.0), writes=["onec"])

    n_setup = len(P.ops)

    def phase_begin(k):
        if start == k and k > 0:
            del P.ops[n_setup:]
            P.barrier_at.clear()
            import os
            if os.environ.get("KMAXOPS"):
                P.maxops = n_setup + int(os.environ["KMAXOPS"])

    evac_rr = [0]

    def evac(out_ap, in_ap, reads, writes, scale=None, accum=False):
        evac_rr[0] ^= 1
        if evac_rr[0]:
            if scale is None:
                P.op("act", lambda e: e.activation(out=out_ap, in_=in_ap, func=AF.Copy),
                     reads, writes, accum=accum)
            else:
                P.op("act", lambda e: e.activation(out=out_ap, in_=in_ap, func=AF.Copy,
                                                   scale=scale), reads, writes, accum=accum)
        else:
            if scale is None:
                P.op("dve", lambda e: e.tensor_copy(out=out_ap, in_=in_ap), reads, writes, accum=accum)
            else:
                P.op("dve", lambda e: e.tensor_scalar(out=out_ap, in0=in_ap, scalar1=scale,
                                                      scalar2=None, op0=ALU.mult),
                     reads, writes, accum=accum)

    cast_rr = [0]
    pend_st = []

    def convert(src_ap, dst_ap, free_shape, tag, dst3d=False):
        b = cast_rr[0] % 3
        cast_rr[0] += 1
        st = stg[b]
        cv = cvt[b]
        nel = int(np.prod(free_shape))
        sv = st[:, 0:nel]
        if len(free_shape) == 2:
            sv = sv.rearrange("p (a b) -> p a b", a=free_shape[0])
        P.dma("sp", sv, src_ap, reads=[], writes=[("stg", b)])
        eng = ("dve", "act")[(cast_rr[0] - 1) % 2]
        if eng == "act":
            P.op("act", lambda e: e.activation(out=cv[:, 0:nel], in_=st[:, 0:nel], func=AF.Copy),
                 reads=[("stg", b)], writes=[("cvt", b)])
        else:
            P.op(eng, lambda e: e.tensor_copy(out=cv[:, 0:nel], in_=st[:, 0:nel]),
                 reads=[("stg", b)], writes=[("cvt", b)])
        cvv = cv[:, 0:nel]
        if dst3d:
            cvv = cvv.rearrange("p (a b) -> p a b", a=free_shape[0])
        pend_st.append(lambda: P.dma("sp", dst_ap, cvv, reads=[("cvt", b)], writes=[tag], accum=True))
        if len(pend_st) > 2:
            pend_st.pop(0)()

    stg = [A.alloc([4096], F32) for _ in range(3)]
    cvt = [A.alloc([4096], BF16) for _ in range(3)]
    for gi, (kind, c0, gc) in enumerate(groups):
        nel = 16 * gc
        dst = win_b[goff[gi]:goff[gi] + 128 * nel].rearrange("(p f) -> p f", p=128)
        if nel <= 4096:
            convert(w_in[:, c0:c0 + gc].rearrange("(kc p) c -> p kc c", p=128), dst,
                    [16, gc], "win_b")
        else:
            for half in range(2):
                convert(w_in[half * 1024:(half + 1) * 1024, c0:c0 + gc]
                        .rearrange("(kc p) c -> p kc c", p=128),
                        dst[:, half * 8 * gc:(half + 1) * 8 * gc], [8, gc], "win_b")
    for kc2 in range(8):
        convert(w_out[kc2 * 256:(kc2 + 1) * 256, :].rearrange("(kc p) c -> p kc c", p=128),
                wout_b[:, kc2 * 2 * D:(kc2 + 1) * 2 * D], [2, D], "wout_b")
    for e_ in range(NEXP):
        for q in range(4):
            convert(w_gate[e_, :, q * 256:(q + 1) * 256].rearrange("(kc p) c -> p kc c", p=128),
                    wg_b[e_ * 128:(e_ + 1) * 128, :].rearrange("p (kc c) -> p kc c", kc=16)[:, :, q * 256:(q + 1) * 256],
                    [16, 256], "wg_b", dst3d=True)
            convert(w_up[e_, :, q * 256:(q + 1) * 256].rearrange("(kc p) c -> p kc c", p=128),
                    wu_b[e_ * 128:(e_ + 1) * 128, :].rearrange("p (kc c) -> p kc c", kc=16)[:, :, q * 256:(q + 1) * 256],
                    [16, 256], "wu_b", dst3d=True)
            convert(w_down[e_, q * 256:(q + 1) * 256, :].rearrange("(kc p) c -> p kc c", p=128),
                    wd_b[e_ * 128:(e_ + 1) * 128, q * 2 * D:(q + 1) * 2 * D], [2, D], "wd_b")
    while pend_st:
        pend_st.pop(0)()
    if stop == 0:
        P.emit(final_wait_keys=["win_b","wout_b","wg_b","wu_b","wd_b"])
        P.close()
        return nc
    P.barrier()
    A.reset()

    phase_begin(1)
    xTb = [A.alloc([16, 512], BF16) for _ in range(2)]
    wbuf = [A.alloc([16 * 512], BF16) for _ in range(3)]
    ebuf = [A.alloc([512], F32) for _ in range(4)]
    NT = SA // 512
    wrr = 0
    err = 0
    prr = 0
    for tt in range(NT):
        own = tt * 512 >= SC
        t0 = tt * 512
        to = t0 - SC
        xb = xTb[tt % 2]
        xk = ("xTb", tt % 2)
        P.dma("pool", xb, xT[:, t0:t0 + 512].rearrange("(kc p) t -> p kc t", p=128),
              writes=[xk])
        for gi, (kind, c0, gc) in enumerate(groups):
            if kind in ("q", "z") and not own:
                continue
            wb = wbuf[wrr % 3]
            wk = ("wbuf", wrr % 3)
            wrr += 1
            wv = wb[:, 0:16 * gc].rearrange("p (kc c) -> p kc c", kc=16)
            P.dma("sp", wb[:, 0:16 * gc],
                  win_b[goff[gi]:goff[gi] + 128 * 16 * gc].rearrange("(p f) -> p f", p=128),
                  reads=["win_b"], writes=[wk])
            if kind in ("q", "k"):
                h = (c0 % 1024) // 128
                for m in range(2):
                    b = prr % 8
                    prr += 1
                    for kc in range(16):
                        P.op("pe", lambda e, b=b, kc=kc, m=m, wv=wv, xb=xb: e.matmul(
                            bank(b, 64), lhsT=wv[:, kc, m * 64:(m + 1) * 64], rhs=xb[:, kc, :],
                            start=(kc == 0), stop=(kc == 15)),
                            reads=[wk, xk], writes=[("ps", b)])
                    eb = ebuf[err % 4]
                    ek = ("ebuf", err % 4)
                    err += 1
                    ebv = eb.bitcast(BF16)[0:64, 0:512]
                    evac(ebv, bank(b, 64), [("ps", b)], [ek],
                         scale=(0.125 if kind == "q" else None))
                    if kind == "q":
                        P.dma("sp", qs[h * 2 + m, :, to:to + 512], ebv, reads=[ek], writes=["qs"], accum=True)
                    else:
                        P.dma("sp", ks[h * 2 + m, :, t0:t0 + 512], ebv, reads=[ek], writes=["ks"], accum=True)
            elif kind == "x":
                g = (c0 - 4096) // 128
                b = prr % 8
                prr += 1
                for kc in range(16):
                    P.op("pe", lambda e, b=b, kc=kc, wv=wv, xb=xb: e.matmul(
                        bank(b), lhsT=wv[:, kc, :], rhs=xb[:, kc, :],
                        start=(kc == 0), stop=(kc == 15)),
                        reads=[wk, xk], writes=[("ps", b)])
                eb = ebuf[err % 4]
                ek = ("ebuf", err % 4)
                err += 1
                evac(eb, bank(b), [("ps", b)], [ek])
                P.dma("sp", xbcs[g * 128:(g + 1) * 128, t0:t0 + 512], eb, reads=[ek],
                      writes=["xbcs"], accum=True)
            else:
                for ts in range(4):
                    b = prr % 8
                    prr += 1
                    for kc in range(16):
                        P.op("pe", lambda e, b=b, kc=kc, wv=wv, xb=xb, ts=ts, gc=gc: e.matmul(
                            bank(b, 128, gc), lhsT=xb[:, kc, ts * 128:(ts + 1) * 128],
                            rhs=wv[:, kc, :], start=(kc == 0), stop=(kc == 15)),
                            reads=[wk, xk], writes=[("ps", b)])
                    eb = ebuf[err % 4]
                    ek = ("ebuf", err % 4)
                    err += 1
                    if kind == "v":
                        ebv = eb.bitcast(BF16)[:, 0:512]
                        evac(ebv, bank(b), [("ps", b)], [ek])
                        P.dma("sp", vs[t0 + ts * 128:t0 + (ts + 1) * 128, c0 - 2048:c0 - 2048 + 512],
                              ebv, reads=[ek], writes=["vs"], accum=True)
                    elif kind == "z":
                        evac(eb, bank(b), [("ps", b)], [ek])
                        P.dma("sp", zs[to + ts * 128:to + (ts + 1) * 128, c0 - 3072:c0 - 3072 + 512],
                              eb, reads=[ek], writes=["zs"], accum=True)
                    else:
                        evac(eb[:, 0:16], bank(b, 128, 16), [("ps", b)], [ek])
                        P.dma("sp", dts[t0 + ts * 128:t0 + (ts + 1) * 128, :], eb[:, 0:16],
                              reads=[ek], writes=["dts"], accum=True)
    if stop == 1:
        P.emit(final_wait_keys=["qs","ks","vs","zs","xbcs","dts"])
        P.close()
        return nc
    P.barrier()
    A.reset()

    phase_begin(2)
    KA = [[A.alloc([SA], BF16, parts=68) for m in range(2)] for _ in range(2)]
    QA = [[A.alloc([SO], BF16, parts=68) for m in range(2)] for _ in range(2)]
    VA = [A.alloc([NB, 130], BF16) for _ in range(2)]
    PT = [A.alloc([512], BF16) for _ in range(3)]
    TT = A.alloc([8, 896], BF16)
    lamt = A.alloc([256], F32)
    lsc = A.alloc([64], F32)
    lam4 = A.alloc([8], F32)
    nwb = A.alloc([128], F32)
    ep_a = [A.alloc([128], F32) for _ in range(2)]
    ep_o = [A.alloc([128], F32) for _ in range(2)]
    ep_j = A.alloc([128], F32)
    ep_s = [A.alloc([8], F32) for _ in range(2)]
    ob = [A.alloc([128], BF16) for _ in range(2)]
    mo = [A.alloc([512], BF16) for _ in range(2)]

    P.dma("pool", TT, ttab.rearrange("p (h c) -> p h c", h=8), writes=["TT"])
    P.dma("sp", lamt, lamv.partition_broadcast(128), writes=["lamt"])
    P.dma("sp", nwb, anw.partition_broadcast(128), writes=["nwb"])
    P.op("dve", lambda e: e.tensor_scalar(out=nwb, in0=nwb, scalar1=(1.0 - LAMBDA_INIT),
                                          scalar2=None, op0=ALU.mult), reads=["nwb"], writes=["nwb"])
    for i in range(2):
        P.op("dve", lambda e, i=i: e.tensor_tensor(out=lsc, in0=lamt[:, i * 128:i * 128 + 64],
                                                  in1=lamt[:, i * 128 + 64:i * 128 + 128],
                                                  op=ALU.mult), reads=["lamt"], writes=["lsc"])
        P.op("dve", lambda e, i=i: e.tensor_reduce(out=lam4[:, i:i + 1], in_=lsc, axis=AX.X,
                                                  op=ALU.add), reads=["lsc"], writes=["lam4"])
    P.op("act", lambda e: e.activation(out=lam4[:, 4:6], in_=lam4[:, 0:2], func=AF.Exp),
         reads=["lam4"], writes=["lam4"])
    P.op("dve", lambda e: e.scalar_tensor_tensor(out=lam4[:, 2:3], in0=lam4[:, 5:6],
                                                 scalar=-LAMBDA_INIT, in1=lam4[:, 4:5],
                                                 op0=ALU.add, op1=ALU.subtract),
         reads=["lam4"], writes=["neglam"])
    neglam = lam4[:, 2:3]
    for vb in range(2):
        P.op("dve", lambda e, vb=vb: e.memset(VA[vb][:, :, 128:130], 1.0), writes=[("VA", vb)])

    NQT = SO // 512
    strr = 0
    ptrr = 0
    eprr = 0
    for h in range(8):
        hb = h % 2
        kkey = ("KA", hb)
        qkey = ("QA", hb)
        vkey = ("VA", hb)
        for m in range(2):
            P.dma("sp", KA[hb][m][0:64, :], ks[h * 2 + m], reads=["ks"], writes=[kkey], accum=(m > 0))
            for c0_ in range(0, SA, 2048):
                c1_ = min(SA, c0_ + 2048)
                P.dma("pool", KA[hb][m][64:68, c0_:c1_], ktab[h, :, c0_:c1_], writes=[kkey], accum=True)
            P.dma("sp", QA[hb][m][0:64, :], qs[h * 2 + m], reads=["qs"], writes=[qkey], accum=(m > 0))
            for c0_ in range(0, SO, 2048):
                c1_ = min(SO, c0_ + 2048)
                P.dma("pool", QA[hb][m][64:68, c0_:c1_], qtab[h, :, c0_:c1_], writes=[qkey], accum=True)
        for j0 in range(0, NB, 16):
            j1 = min(NB, j0 + 16)
            P.dma("sp", VA[hb][:, j0:j1, 0:128],
                  vs[j0 * 128:j1 * 128, h * 128:(h + 1) * 128].rearrange("(j p) d -> p j d", p=128),
                  reads=["vs"], writes=[vkey], accum=(j0 > 0))
        for qt in range(NQT):
            gb = (SC + qt * 512) // 128
            okeys = [[("O", m, i) for i in range(4)] for m in range(2)]
            its = [(j, m) for j in range(gb + 4) for m in range(2)]
            slots = []
            for (j, m) in its:
                slots.append((strr % 3, ptrr % 3))
                strr += 1
                ptrr += 1

            def emit_qk(idx, its=its, slots=slots, gb=gb, qt=qt, hb=hb, h=h, kkey=kkey, qkey=qkey):
                j, m = its[idx]
                sb_, pb = slots[idx]
                r = j - gb
                P.op("pe", lambda e, sb_=sb_, m=m, j=j, qt=qt, hb=hb, r=r: e.matmul(
                    bank(sb_), lhsT=KA[hb][m][:, j * 128:(j + 1) * 128],
                    rhs=QA[hb][m][:, qt * 512:(qt + 1) * 512], start=True, stop=(r < 0)),
                    reads=[kkey, qkey], writes=[("ps", sb_)])
                if r >= 0:
                    P.op("pe", lambda e, sb_=sb_, h=h, r=r: e.matmul(
                        bank(sb_), lhsT=ident_b[:], rhs=TT[:, h, (3 - r) * 128:(3 - r) * 128 + 512],
                        start=False, stop=True),
                        reads=["ident_b", "TT"], writes=[("ps", sb_)])

            def emit_rest(idx, its=its, slots=slots, gb=gb, hb=hb, vkey=vkey, okeys=okeys):
                j, m = its[idx]
                sb_, pb = slots[idx]
                r = j - gb
                P.op("act", lambda e, sb_=sb_, pb=pb: e.activation(
                    out=PT[pb], in_=bank(sb_), func=AF.Exp),
                    reads=[("ps", sb_)], writes=[("PT", pb)])
                for i in range(4):
                    if r >= 0 and i < r:
                        continue
                    ob_ = 4 + m * 2 + i // 2
                    P.op("pe", lambda e, pb=pb, i=i, j=j, hb=hb, ob_=ob_, gb=gb: e.matmul(
                        bank(ob_, 128, 129, (i % 2) * 256), lhsT=PT[pb][:, i * 128:(i + 1) * 128],
                        rhs=VA[hb][:, j, 0:129], start=(j == 0 and i % 2 == 0), stop=(j == gb + i)),
                        reads=[("PT", pb), vkey], writes=[okeys[m][i]])

            LA = 2
            for idx in range(len(its) + LA):
                if idx < len(its):
                    emit_qk(idx)
                if idx - LA >= 0:
                    emit_rest(idx - LA)
            mb = (h * NQT + qt) % 2
            for i in range(4):
                eb_ = eprr % 2
                O0 = bank(4 + i // 2, 128, 129, (i % 2) * 256)
                O1 = bank(6 + i // 2, 128, 129, (i % 2) * 256)
                es = ep_s[eb_]
                ek = ("ep", eb_)
                P.op("dve", lambda e, es=es, O0=O0: e.reciprocal(out=es[:, 0:1], in_=O0[:, 128:129]),
                     reads=[okeys[0][i]], writes=[ek])
                P.op("dve", lambda e, es=es, O1=O1: e.reciprocal(out=es[:, 1:2], in_=O1[:, 128:129]),
                     reads=[okeys[1][i]], writes=[ek])
                P.op("dve", lambda e, es=es: e.tensor_tensor(out=es[:, 2:3], in0=es[:, 1:2], in1=neglam,
                                                             op=ALU.mult),
                     reads=[ek, "neglam"], writes=[ek])
                P.op("dve", lambda e, es=es, O0=O0, eb_=eb_: e.tensor_scalar(
                    out=ep_a[eb_], in0=O0[:, 0:128], scalar1=es[:, 0:1], scalar2=None, op0=ALU.mult),
                    reads=[ek, okeys[0][i]], writes=[("epa", eb_)])
                P.op("dve", lambda e, es=es, O1=O1, eb_=eb_: e.scalar_tensor_tensor(
                    out=ep_o[eb_], in0=O1[:, 0:128], scalar=es[:, 2:3], in1=ep_a[eb_],
                    op0=ALU.mult, op1=ALU.add),
                    reads=[ek, okeys[1][i], ("epa", eb_)], writes=[("epo", eb_)])
                P.op("act", lambda e, es=es, eb_=eb_: e.activation(
                    out=ep_j, in_=ep_o[eb_], func=AF.Square, accum_out=es[:, 3:4]),
                    reads=[("epo", eb_)], writes=[("ep2", eb_), "epj"])
                P.op("act", lambda e, es=es: e.activation(
                    out=es[:, 4:5], in_=es[:, 3:4], func=AF.Sqrt, bias=epsrms[:], scale=1.0 / 128),
                    reads=[("ep2", eb_), "eps2"], writes=[("ep3", eb_)])
                P.op("dve", lambda e, es=es: e.reciprocal(out=es[:, 5:6], in_=es[:, 4:5]),
                     reads=[("ep3", eb_)], writes=[("ep4", eb_)])
                P.op("dve", lambda e, es=es, eb_=eb_: e.scalar_tensor_tensor(
                    out=ob[eb_], in0=ep_o[eb_], scalar=es[:, 5:6], in1=nwb, op0=ALU.mult, op1=ALU.mult),
                    reads=[("ep4", eb_), ("epo", eb_), "nwb"], writes=[("ob", eb_)])
                P.op("pe", lambda e, eb_=eb_, i=i: e.transpose(
                    bankbf(3, 128, 128, i * 128), ob[eb_], ident_b[:]),
                    reads=[("ob", eb_), "ident_b"], writes=[("ps", 3, i)])
                evac(mo[mb][:, i * 128:(i + 1) * 128], bankbf(3, 128, 128, i * 128),
                     [("ps", 3, i)], [("mo", mb)], accum=(i > 0))
                eprr += 1
            P.dma("sp", mixT[h * 128:(h + 1) * 128, qt * 512:(qt + 1) * 512], mo[mb],
                  reads=[("mo", mb)], writes=["mixT"], accum=True)
    if stop == 2:
        P.emit(final_wait_keys=["mixT"])
        P.close()
        return nc
    P.barrier()
    A.reset()

    phase_begin(3)
    NCH = SA // 64
    cwt = A.alloc([12, 4], F32)
    cbt = A.alloc([12], F32)
    tri = A.alloc([64], F32, parts=64)
    umat = A.alloc([64], F32, parts=64)
    ones64 = A.alloc([128], F32, parts=64)
    flagt = A.alloc([1], F32)
    dtbt = A.alloc([16], F32, parts=64)
    Aneg = A.alloc([16], F32, parts=64)
    Dt = A.alloc([16], F32, parts=64)
    snwt = A.alloc([1024], F32, parts=64)
    dtall = A.alloc([NCH, 16], F32, parts=64)
    aall = A.alloc([NCH, 16], F32, parts=64)
    xc = [A.alloc([12, 515], F32) for _ in range(1)]
    acc = A.alloc([512], F32)
    xcv = A.alloc([8, 512], F32)
    bct = A.alloc([4, 512], BF16)
    Rt = A.alloc([16, 64], F32, parts=64)
    Et = A.alloc([16, 64], F32, parts=64)
    Ebt = A.alloc([16, 64], F32)
    mcb = A.alloc([2, 64], F32, parts=64)
    Mt = A.alloc([16, 64], BF16, parts=64)
    Mtmp = A.alloc([16, 64], F32, parts=64)
    x32 = A.alloc([1024], F32, parts=64)
    xbf = A.alloc([1024], BF16, parts=64)
    wsc = A.alloc([16], F32, parts=64)
    xw = A.alloc([1024], BF16, parts=64)
    Btok = A.alloc([2, 128], BF16, parts=64)
    Cs = A.alloc([16, 64], BF16)
    Hs = A.alloc([1024], F32)
    Hbf = A.alloc([1024], BF16)
    yt = A.alloc([1024], F32, parts=64)
    zt = A.alloc([1024], F32, parts=64)
    ysq = A.alloc([1024], F32, parts=64)
    ss = A.alloc([8], F32, parts=64)
    ybf = A.alloc([1024], BF16, parts=64)
    ymo = A.alloc([8, 64], BF16)

    P.dma("sp", cwt, cw.rearrange("p (g k) -> p g k", g=12), writes=["cwt"])
    P.dma("sp", cbt, cb, writes=["cbt"])
    P.dma("sp", tri, tri_in, writes=["tri"])
    P.dma("sp", umat, umat_in, writes=["umat"])
    P.dma("sp", flagt, flag_in, writes=["flagt"])
    P.dma("sp", dtbt, dtb.partition_broadcast(64), writes=["dtbt"])
    P.dma("sp", Aneg, alog.partition_broadcast(64), writes=["Aneg"])
    P.dma("sp", Dt, dsk.partition_broadcast(64), writes=["Dt"])
    P.dma("sp", snwt, snw.partition_broadcast(64), writes=["snwt"])
    for c0_ in range(0, NCH, 32):
        c1_ = min(NCH, c0_ + 32)
        P.dma("sp", dtall[:, c0_:c1_, :], dts[c0_ * 64:c1_ * 64, :].rearrange("(c l) h -> l c h", l=64),
              reads=["dts"], writes=["dtall"], accum=(c0_ > 0))
    P.op("dve", lambda e: e.memset(ones64, 1.0), writes=["ones64"])
    P.op("dve", lambda e: e.memset(Hs, 0.0), writes=["Hs"])
    P.op("dve", lambda e: e.memset(Hbf, 0.0), writes=["Hbf"])
    P.op("act", lambda e: e.activation(out=Aneg, in_=Aneg, func=AF.Exp), reads=["Aneg"], writes=["Aneg"])
    P.op("dve", lambda e: e.tensor_scalar(out=Aneg, in0=Aneg, scalar1=-1.0, scalar2=None, op0=ALU.mult),
         reads=["Aneg"], writes=["Aneg"])
    P.op("dve", lambda e: e.tensor_tensor(out=dtall, in0=dtall,
                                          in1=dtbt.unsqueeze(1).to_broadcast([64, NCH, 16]), op=ALU.add),
         reads=["dtall", "dtbt"], writes=["dtall"])
    P.op("act", lambda e: e.activation(out=dtall, in_=dtall, func=AF.Exp), reads=["dtall"], writes=["dtall"])
    P.op("act", lambda e: e.activation(out=dtall, in_=dtall, func=AF.Ln, bias=onec[0:64, :], scale=1.0),
         reads=["dtall", "onec"], writes=["dtall"])
    P.op("dve", lambda e: e.tensor_tensor(out=aall, in0=dtall,
                                          in1=Aneg.unsqueeze(1).to_broadcast([64, NCH, 16]), op=ALU.mult),
         reads=["dtall", "Aneg"], writes=["aall"])

    for blk in range(SA // 512):
        t0 = blk * 512
        xcb = xc[0]
        if t0 == 0:
            P.op("dve", lambda e: e.memset(xcb[:, :, 0:3], 0.0), writes=["xc"])
            P.dma("sp", xcb[:, :, 3:515], xbcs[:, 0:512].rearrange("(g p) t -> p g t", p=128),
                  reads=["xbcs"], writes=["xc"])
        else:
            P.dma("sp", xcb, xbcs[:, t0 - 3:t0 + 512].rearrange("(g p) t -> p g t", p=128),
                  reads=["xbcs"], writes=["xc"])
        for g in range(12):
            P.op("dve", lambda e, g=g: e.tensor_scalar(out=acc, in0=xcb[:, g, 3:515],
                                                      scalar1=cwt[:, g, 3:4], scalar2=None, op0=ALU.mult),
                 reads=["xc", "cwt"], writes=["acc"])
            for k in range(3):
                P.op("dve", lambda e, g=g, k=k: e.scalar_tensor_tensor(
                    out=acc, in0=xcb[:, g, k:k + 512], scalar=cwt[:, g, k:k + 1], in1=acc,
                    op0=ALU.mult, op1=ALU.add), reads=["xc", "cwt", "acc"], writes=["acc"])
            if g < 8:
                P.op("act", lambda e, g=g: e.activation(out=xcv[:, g, :], in_=acc, func=AF.Silu,
                                                       bias=cbt[:, g:g + 1], scale=1.0),
                     reads=["acc", "cbt"], writes=[("xcv", g)])
            else:
                P.op("act", lambda e, g=g: e.activation(out=bct[:, g - 8, :], in_=acc, func=AF.Silu,
                                                       bias=cbt[:, g:g + 1], scale=1.0),
                     reads=["acc", "cbt"], writes=[("bct", g - 8)])
        for cl in range(8):
            c = blk * 8 + cl
            own = c * 64 >= SC
            csl = slice(cl * 64, (cl + 1) * 64)
            a_c = aall[:, c, :]
            dt_c = dtall[:, c, :]
            for g in range(8):
                P.op("pe", lambda e, g=g, csl=csl: e.transpose(
                    bank(g // 4, 64, 128, (g % 4) * 128), xcv[:, g, csl], ident_f[:]),
                    reads=[("xcv", g), "ident_f"], writes=[("ps", g // 4)])
            for hb_ in range(2):
                P.op("act", lambda e, hb_=hb_: e.activation(
                    out=x32[:, hb_ * 512:(hb_ + 1) * 512], in_=bank(hb_, 64), func=AF.Copy),
                    reads=[("ps", hb_)], writes=[("x32", hb_)])
                P.op("act", lambda e, hb_=hb_: e.activation(
                    out=xbf[:, hb_ * 512:(hb_ + 1) * 512], in_=bank(hb_, 64), func=AF.Copy),
                    reads=[("ps", hb_)], writes=[("xbf", hb_)])
            for g2 in range(2):
                P.op("pe", lambda e, g2=g2, csl=csl: e.transpose(
                    bankbf(7, 64, 128, 512 + g2 * 128), bct[:, g2, csl], ident_b[:]),
                    reads=[("bct", g2), "ident_b"], writes=[("ps", 7, "b")])
            P.op("dve", lambda e: e.tensor_copy(out=Btok.rearrange("p a b -> p (a b)"),
                                                in_=bankbf(7, 64, 256, 512)),
                 reads=[("ps", 7, "b")], writes=["Btok"])
            P.op("dve", lambda e, a_c=a_c: e.tensor_tensor(
                out=Rt, in0=tri.unsqueeze(1).to_broadcast([64, 16, 64]),
                in1=a_c.unsqueeze(2).to_broadcast([64, 16, 64]), op=ALU.mult),
                reads=["tri", "aall"], writes=["Rt"])
            Rf = Rt.rearrange("p a b -> p (a b)")
            for hb_ in range(2):
                P.op("pe", lambda e, hb_=hb_: e.matmul(
                    bank(2 + hb_, 64), lhsT=umat, rhs=Rf[:, hb_ * 512:(hb_ + 1) * 512],
                    start=True, stop=True), reads=["umat", "Rt"], writes=[("ps", 2 + hb_)])
                P.op("pe", lambda e, hb_=hb_: e.matmul(
                    bank(4 + hb_), lhsT=ones64, rhs=Rf[:, hb_ * 512:(hb_ + 1) * 512],
                    start=True, stop=True), reads=["ones64", "Rt"], writes=[("ps", 4 + hb_)])
            Ef = Et.rearrange("p a b -> p (a b)")
            Ebf = Ebt.rearrange("p a b -> p (a b)")
            for hb_ in range(2):
                P.op("act", lambda e, hb_=hb_: e.activation(
                    out=Ef[:, hb_ * 512:(hb_ + 1) * 512], in_=bank(2 + hb_, 64), func=AF.Exp),
                    reads=[("ps", 2 + hb_)], writes=[("Et", hb_)])
                P.op("act", lambda e, hb_=hb_: e.activation(
                    out=Ebf[:, hb_ * 512:(hb_ + 1) * 512], in_=bank(4 + hb_), func=AF.Exp),
                    reads=[("ps", 4 + hb_)], writes=[("Ebt", hb_)])
            ekeys = [("Et", 0), ("Et", 1)]
            ebkeys = [("Ebt", 0), ("Ebt", 1)]
            P.op("dve", lambda e, dt_c=dt_c: e.tensor_tensor(out=wsc, in0=dt_c, in1=Et[:, :, 63],
                                                            op=ALU.mult),
                 reads=ekeys + ["dtall"], writes=["wsc"])
            P.op("dve", lambda e: e.tensor_tensor(
                out=xw.rearrange("p (h d) -> p h d", h=16),
                in0=x32.rearrange("p (h d) -> p h d", h=16),
                in1=wsc.unsqueeze(2).to_broadcast([64, 16, 64]), op=ALU.mult),
                reads=["wsc", ("x32", 0), ("x32", 1)], writes=["xw"])
            if own:
                if c * 64 == SC:
                    P.op("dve", lambda e: e.tensor_scalar(out=Hs, in0=Hs, scalar1=flagt[:, 0:1],
                                                          scalar2=None, op0=ALU.mult),
                         reads=["Hs", "flagt"], writes=["Hs"])
                    P.op("dve", lambda e: e.tensor_copy(out=Hbf, in_=Hs), reads=["Hs"], writes=["Hbf"])
                for g2 in range(2):
                    P.op("pe", lambda e, g2=g2, csl=csl: e.matmul(
                        bank(6, 64, 64, g2 * 64), lhsT=bct[:, g2, csl], rhs=bct[:, 2 + g2, csl],
                        start=True, stop=True), reads=[("bct", g2), ("bct", 2 + g2)],
                        writes=[("ps", 6, "cb")])
                P.op("dve", lambda e: e.tensor_tensor(
                    out=mcb, in0=bank(6, 64, 128).rearrange("p (g l) -> p g l", g=2),
                    in1=tri.unsqueeze(1).to_broadcast([64, 2, 64]), op=ALU.mult),
                    reads=[("ps", 6, "cb"), "tri"], writes=["mcb"])
                for g2 in range(2):
                    P.op("dve", lambda e, g2=g2: e.tensor_tensor(
                        out=Mtmp[:, g2 * 8:(g2 + 1) * 8, :], in0=Et[:, g2 * 8:(g2 + 1) * 8, :],
                        in1=mcb[:, g2:g2 + 1, :].to_broadcast([64, 8, 64]), op=ALU.mult),
                        reads=ekeys + ["mcb"], writes=[("Mtmp", g2)])
                P.op("dve", lambda e, dt_c=dt_c: e.tensor_tensor(
                    out=Mt, in0=Mtmp, in1=dt_c.unsqueeze(2).to_broadcast([64, 16, 64]), op=ALU.mult),
                    reads=[("Mtmp", 0), ("Mtmp", 1), "dtall"], writes=["Mt"])
                for g2 in range(2):
                    P.op("dve", lambda e, g2=g2, csl=csl: e.tensor_tensor(
                        out=Cs[:, g2 * 8:(g2 + 1) * 8, :], in0=Ebt[:, g2 * 8:(g2 + 1) * 8, :],
                        in1=bct[:, 2 + g2, csl].unsqueeze(1).to_broadcast([128, 8, 64]), op=ALU.mult),
                        reads=ebkeys + [("bct", 2 + g2)], writes=[("Cs", g2)])
                for hh in range(16):
                    yb = bank(hh // 8, 64, 64, (hh % 8) * 64)
                    P.op("pe", lambda e, hh=hh, yb=yb: e.matmul(
                        yb, lhsT=Mt[:, hh, :], rhs=xbf[:, hh * 64:(hh + 1) * 64], start=True, stop=False),
                        reads=["Mt", ("xbf", hh // 8), ("x32", hh // 8)], writes=[("ps", hh // 8)])
                    P.op("pe", lambda e, hh=hh, yb=yb: e.matmul(
                        yb, lhsT=Cs[:, hh, :], rhs=Hbf[:, hh * 64:(hh + 1) * 64], start=False, stop=True),
                        reads=[("Cs", hh // 8), "Hbf"], writes=[("ps", hh // 8)])
            for g2 in range(2):
                P.op("pe", lambda e, g2=g2: e.matmul(
                    bank(2 + g2), lhsT=Btok[:, g2, :], rhs=xw[:, g2 * 512:(g2 + 1) * 512],
                    start=True, stop=True), reads=["Btok", "xw"] + ekeys, writes=[("ps", 2 + g2)])
            P.op("dve", lambda e: e.tensor_tensor(
                out=Hs.rearrange("p (h d) -> p h d", h=16), in0=Hs.rearrange("p (h d) -> p h d", h=16),
                in1=Ebt[:, :, 63:64].to_broadcast([128, 16, 64]), op=ALU.mult),
                reads=["Hs", "Hbf"] + ebkeys, writes=["Hs"])
            for g2 in range(2):
                P.op("dve", lambda e, g2=g2: e.tensor_tensor(
                    out=Hs[:, g2 * 512:(g2 + 1) * 512], in0=Hs[:, g2 * 512:(g2 + 1) * 512],
                    in1=bank(2 + g2), op=ALU.add), reads=["Hs", ("ps", 2 + g2)], writes=["Hs"])
            if own:
                to = c * 64 - SC
                P.dma("sp", zt, zs[to:to + 64, :], reads=["zs"], writes=["zt"])
                P.op("dve", lambda e: e.tensor_tensor(
                    out=yt.rearrange("p (h d) -> p h d", h=16), in0=x32.rearrange("p (h d) -> p h d", h=16),
                    in1=Dt.unsqueeze(2).to_broadcast([64, 16, 64]), op=ALU.mult),
                    reads=[("x32", 0), ("x32", 1), "Dt"], writes=["yt"])
                for hb_ in range(2):
                    P.op("dve", lambda e, hb_=hb_: e.tensor_tensor(
                        out=yt[:, hb_ * 512:(hb_ + 1) * 512], in0=yt[:, hb_ * 512:(hb_ + 1) * 512],
                        in1=bank(hb_, 64), op=ALU.add), reads=["yt", ("ps", hb_)], writes=["yt"])
                P.op("act", lambda e: e.activation(out=zt, in_=zt, func=AF.Silu), reads=["zt"], writes=["zt"])
                P.op("dve", lambda e: e.tensor_tensor(out=yt, in0=yt, in1=zt, op=ALU.mult),
                     reads=["yt", "zt"], writes=["yt"])
                for g2 in range(2):
                    P.op("act", lambda e, g2=g2: e.activation(
                        out=ysq[:, g2 * 512:(g2 + 1) * 512], in_=yt[:, g2 * 512:(g2 + 1) * 512],
                        func=AF.Square, accum_out=ss[:, g2:g2 + 1]), reads=["yt"], writes=["ysq", ("ss", g2)])
                P.op("act", lambda e: e.activation(out=ss[:, 2:4], in_=ss[:, 0:2], func=AF.Sqrt,
                                                   bias=epsrms[0:64, :], scale=1.0 / 512),
                     reads=[("ss", 0), ("ss", 1), "eps2"], writes=["ss2"])
                P.op("dve", lambda e: e.reciprocal(out=ss[:, 4:6], in_=ss[:, 2:4]), reads=["ss2"],
                     writes=["ss3"])
                for g2 in range(2):
                    P.op("dve", lambda e, g2=g2: e.scalar_tensor_tensor(
                        out=ybf[:, g2 * 512:(g2 + 1) * 512], in0=yt[:, g2 * 512:(g2 + 1) * 512],
                        scalar=ss[:, 4 + g2:5 + g2], in1=snwt[:, g2 * 512:(g2 + 1) * 512],
                        op0=ALU.mult, op1=ALU.mult), reads=["yt", "ss3", "snwt"], writes=["ybf"])
                for g in range(8):
                    P.op("pe", lambda e, g=g: e.transpose(
                        bankbf(7, 128, 64, g * 64), ybf[:, g * 128:(g + 1) * 128], ident_b[0:64, 0:64]),
                        reads=["ybf", "ident_b"], writes=[("ps", 7)])
                P.op("act", lambda e: e.activation(out=ymo.rearrange("p a b -> p (a b)"),
                                                   in_=bankbf(7, 128, 512, 0), func=AF.Copy),
                     reads=[("ps", 7)], writes=["ymo"])
                P.dma("sp", mixT[1024:2048, to:to + 64].rearrange("(g p) t -> p g t", p=128), ymo,
                      reads=["ymo"], writes=["mixT"], accum=True)
            P.op("act", lambda e: e.activation(out=Hbf, in_=Hs, func=AF.Copy), reads=["Hs"], writes=["Hbf"])
    if stop == 3:
        P.emit(final_wait_keys=["mixT"])
        P.close()
        return nc
    P.barrier()
    A.reset()

    phase_begin(4)
    wo = A.alloc([16, D], BF16)
    mxt = [A.alloc([16, 512], BF16) for _ in range(2)]
    xot = [A.alloc([D], F32) for _ in range(2)]
    rt = [A.alloc([D], F32) for _ in range(2)]
    g1 = A.alloc([D], F32)
    b1 = A.alloc([D], F32)
    st6 = A.alloc([4, 6], F32)
    mv = A.alloc([8], F32)
    hT32 = A.alloc([16, 128], F32)
    hTb = A.alloc([16, 128], BF16)
    hbt = [A.alloc([D], BF16) for _ in range(2)]
    wr32 = A.alloc([16, 36], F32)
    brt = A.alloc([36], F32)
    lg = A.alloc([36], F32)
    rs = A.alloc([16], F32)
    Gt = A.alloc([4], F32)
    elm = A.alloc([4, 8], F32)
    top8 = A.alloc([8], F32)
    msk = A.alloc([32], F32)
    ext = A.alloc([32], F32)
    P.dma("sp", wo.rearrange("p a b -> p (a b)"), wout_b, reads=["wout_b"], writes=["wo"])
    P.dma("sp", g1, ln1g.partition_broadcast(128), writes=["g1"])
    P.dma("sp", b1, ln1b.partition_broadcast(128), writes=["b1"])
    P.dma("sp", wr32, wr.rearrange("(kc p) c -> p kc c", p=128), writes=["wr32"])
    P.dma("sp", brt, br.partition_broadcast(128), writes=["brt"])

    def layernorm(src, dst, gk, bk, gt, bt, rkey, okey, tag, st6, mv):
        for cc in range(4):
            P.op("dve", lambda e, cc=cc: e.bn_stats(out=st6[:, cc, :], in_=src[:, cc * 512:(cc + 1) * 512]),
                 reads=[rkey], writes=[("st6", tag)])
        P.op("dve", lambda e: e.bn_aggr(out=mv[:, 0:2], in_=st6.rearrange("p a b -> p (a b)")),
             reads=[("st6", tag)], writes=[("mv", tag)])
        P.op("act", lambda e: e.activation(out=mv[:, 2:3], in_=mv[:, 1:2], func=AF.Sqrt,
                                           bias=epsln[:], scale=1.0),
             reads=[("mv", tag), "eps"], writes=[("mv2", tag)])
        P.op("dve", lambda e: e.reciprocal(out=mv[:, 3:4], in_=mv[:, 2:3]), reads=[("mv2", tag)],
             writes=[("mv3", tag)])
        P.op("dve", lambda e: e.tensor_scalar(out=src, in0=src, scalar1=mv[:, 0:1], scalar2=mv[:, 3:4],
                                              op0=ALU.subtract, op1=ALU.mult),
             reads=[rkey, ("mv3", tag), ("mv", tag)], writes=[rkey])
        P.op("dve", lambda e: e.tensor_tensor(out=src, in0=src, in1=gt, op=ALU.mult),
             reads=[rkey, gk], writes=[rkey])
        P.op("dve", lambda e: e.tensor_tensor(out=dst, in0=src, in1=bt, op=ALU.add),
             reads=[rkey, bk], writes=[okey])

    for t4 in range(SO // 512):
        mb = t4 % 2
        P.dma("sp", mxt[mb], mixT[:, t4 * 512:(t4 + 1) * 512].rearrange("(kc p) t -> p kc t", p=128),
              reads=["mixT"], writes=[("mxt", mb)])
        for ts in range(4):
            ti = t4 * 4 + ts
            tb = ti % 2
            tok0 = ti * 128
            P.dma("sp", xot[tb], xo[tok0:tok0 + 128, :], writes=[("xot", tb)])
            for cbk in range(4):
                for kc in range(16):
                    P.op("pe", lambda e, cbk=cbk, kc=kc, ts=ts, mb=mb: e.matmul(
                        bank(cbk), lhsT=mxt[mb][:, kc, ts * 128:(ts + 1) * 128],
                        rhs=wo[:, kc, cbk * 512:(cbk + 1) * 512], start=(kc == 0), stop=(kc == 15)),
                        reads=[("mxt", mb), "wo"], writes=[("ps", cbk)])
                P.op("dve", lambda e, cbk=cbk, tb=tb: e.scalar_tensor_tensor(
                    out=rt[tb][:, cbk * 512:(cbk + 1) * 512], in0=xot[tb][:, cbk * 512:(cbk + 1) * 512],
                    scalar=DN_ALPHA, in1=bank(cbk), op0=ALU.mult, op1=ALU.add),
                    reads=[("xot", tb), ("ps", cbk)], writes=[("rt", tb)])
            layernorm(rt[tb], rt[tb], "g1", "b1", g1, b1, ("rt", tb), ("rt", tb), "a", st6, mv)
            P.dma("sp", h1s[tok0:tok0 + 128, :], rt[tb], reads=[("rt", tb)], writes=["h1s"], accum=True)
            P.op("act", lambda e, tb=tb: e.activation(out=hbt[tb], in_=rt[tb], func=AF.Copy),
                 reads=[("rt", tb)], writes=[("hbt", tb)])
            P.dma("sp", h1b[tok0:tok0 + 128, :], hbt[tb], reads=[("hbt", tb)], writes=["h1b"], accum=True)
            for half in range(2):
                for k8 in range(8):
                    kc = half * 8 + k8
                    P.op("pe", lambda e, kc=kc, k8=k8, half=half, tb=tb: e.transpose(
                        bank(4 + half * 2 + k8 // 4, 128, 128, (k8 % 4) * 128),
                        rt[tb][:, kc * 128:(kc + 1) * 128], ident_f[:]),
                        reads=[("rt", tb), "ident_f"], writes=[("ps", 4 + half * 2 + k8 // 4)])
                for q2 in range(2):
                    bq = 4 + half * 2 + q2
                    kc0 = half * 8 + q2 * 4
                    P.op("act", lambda e, bq=bq, kc0=kc0: e.activation(
                        out=hT32[:, kc0:kc0 + 4, :].rearrange("p a b -> p (a b)"), in_=bank(bq), func=AF.Copy),
                        reads=[("ps", bq)], writes=[("hT32", kc0)])
            for kc in range(16):
                P.op("pe", lambda e, kc=kc: e.matmul(
                    bank(0, 128, 36), lhsT=hT32[:, kc, :], rhs=wr32[:, kc, :], start=(kc == 0), stop=(kc == 15)),
                    reads=[("hT32", (kc // 4) * 4), "wr32"], writes=[("ps", 0)])
            P.op("dve", lambda e: e.tensor_tensor(out=lg, in0=bank(0, 128, 36), in1=brt, op=ALU.add),
                 reads=[("ps", 0), "brt"], writes=["lg"])
            gl = lg[:, 0:4]
            el = lg[:, 4:36].rearrange("p (g e) -> p g e", g=4)
            P.op("dve", lambda e: e.tensor_reduce(out=rs[:, 0:1], in_=gl, axis=AX.X, op=ALU.max),
                 reads=["lg"], writes=["rs0"])
            P.op("dve", lambda e: e.tensor_scalar(out=rs[:, 1:2], in0=rs[:, 0:1], scalar1=-1.0, scalar2=None,
                                                  op0=ALU.mult), reads=["rs0"], writes=["rs1"])
            P.op("act", lambda e: e.activation(out=Gt, in_=gl, func=AF.Exp, bias=rs[:, 1:2], scale=1.0,
                                               accum_out=rs[:, 2:3]),
                 reads=["lg", "rs1"], writes=["Gt", "rs2"])
            P.op("dve", lambda e: e.reciprocal(out=rs[:, 3:4], in_=rs[:, 2:3]), reads=["rs2"], writes=["rs3"])
            P.op("dve", lambda e: e.tensor_scalar(out=Gt, in0=gl, scalar1=rs[:, 0:1], scalar2=None,
                                                  op0=ALU.is_ge), reads=["lg", "rs0", "Gt"], writes=["Gt"])
            P.op("dve", lambda e: e.tensor_scalar(out=Gt, in0=Gt, scalar1=-NEG, scalar2=NEG,
                                                  op0=ALU.mult, op1=ALU.add), reads=["Gt"], writes=["Gt"])
            P.op("dve", lambda e: e.tensor_tensor(out=elm, in0=el, in1=Gt.unsqueeze(2).to_broadcast([128, 4, 8]),
                                                  op=ALU.add), reads=["lg", "Gt"], writes=["elm"])
            elf = elm.rearrange("p a b -> p (a b)")
            P.op("dve", lambda e: e.max(out=top8, in_=elf), reads=["elm"], writes=["top8"])
            P.op("dve", lambda e: e.tensor_scalar(out=msk, in0=elf, scalar1=top8[:, 1:2], scalar2=None,
                                                  op0=ALU.is_ge), reads=["elm", "top8"], writes=["msk"])
            P.op("dve", lambda e: e.tensor_scalar(out=rs[:, 4:5], in0=top8[:, 0:1], scalar1=-1.0, scalar2=None,
                                                  op0=ALU.mult), reads=["top8"], writes=["rs4"])
            P.op("act", lambda e: e.activation(out=ext, in_=elf, func=AF.Exp, bias=rs[:, 4:5], scale=1.0),
                 reads=["elm", "rs4"], writes=["ext"])
            P.op("act", lambda e: e.activation(out=rs[:, 5:6], in_=top8[:, 1:2], func=AF.Exp, bias=rs[:, 4:5],
                                               scale=1.0), reads=["top8", "rs4"], writes=["rs5"])
            P.op("dve", lambda e: e.tensor_scalar(out=rs[:, 6:7], in0=rs[:, 5:6], scalar1=1.0, scalar2=None,
                                                  op0=ALU.add), reads=["rs5"], writes=["rs6"])
            P.op("dve", lambda e: e.reciprocal(out=rs[:, 7:8], in_=rs[:, 6:7]), reads=["rs6"], writes=["rs7"])
            P.op("dve", lambda e: e.tensor_tensor(out=rs[:, 8:9], in0=rs[:, 7:8], in1=rs[:, 3:4], op=ALU.mult),
                 reads=["rs7", "rs3"], writes=["rs8"])
            P.op("dve", lambda e, ti=ti: e.scalar_tensor_tensor(
                out=Wall[:, ti * 32:(ti + 1) * 32], in0=ext, scalar=rs[:, 8:9], in1=msk,
                op0=ALU.mult, op1=ALU.mult), reads=["ext", "rs8", "msk"], writes=["Wall"])
    if stop == 4:
        wall_out = nc.dram_tensor("Wall_out", [128, (SO // 128) * 32], F32, kind="ExternalOutput").ap()
        P.dma("sp", wall_out, Wall[:], reads=["Wall"], writes=["wall_out"])
        P.emit(final_wait_keys=["h1s","h1b","wall_out"])
        P.close()
        return nc
    P.barrier()
    A.reset()

    phase_begin(5)
    if start == 5:
        wall_in = nc.dram_tensor("Wall_in", [128, (SO // 128) * 32], F32, kind="ExternalInput").ap()
        P.dma("sp", Wall[:], wall_in, writes=["Wall"])
    I32 = mybir.dt.int32
    U32 = mybir.dt.uint32
    didx = P.sb([128, NTT * 2], I32, "didx")
    wk = P.sb([128, NTT * 2], F32, "wk")
    gidx = P.sb([128, NBLK], I32, "gidx")
    lmat_b = A.alloc([128], BF16)
    ones_b = A.alloc([128], BF16)
    sel = A.alloc([NTT, 32], BF16)
    carry = A.alloc([32], F32)
    rank = A.alloc([NTT, 32], F32)
    dest = A.alloc([NTT, 32], F32)
    dsel = A.alloc([NTT, 32], F32)
    t8 = A.alloc([NTT, 8], F32)
    thr = A.alloc([32], F32)
    cmpt = A.alloc([32, 32], F32)
    nbe = A.alloc([32], F32)
    padded = A.alloc([32], F32)
    pend = A.alloc([32], F32)
    pstart = A.alloc([32], F32)
    onesf = A.alloc([32], F32)
    bthr = A.alloc([NBLK], F32)
    cmpb = A.alloc([NBLK, 32], F32)
    ebt = A.alloc([NBLK], F32)
    pidx = A.alloc([1], F32)
    didf = A.alloc([NTT, 2], F32)
    junk = A.alloc([32], F32)
    Wv = Wall[:].rearrange("p (t e) -> p t e", e=32)
    P.dma("pool", lmat_b, lmat_in, writes=["lmat_b"])
    P.dma("sp", thr, thr_in, writes=["thr"])
    P.dma("sp", bthr, bthr_in, writes=["bthr"])
    P.dma("sp", pidx, pidx_in, writes=["pidx"])
    P.op("dve", lambda e: e.memset(ones_b, 1.0), writes=["ones_b"])
    P.op("dve", lambda e: e.memset(onesf, 1.0), writes=["onesf"])
    P.op("dve", lambda e: e.memset(carry, 0.0), writes=["carry"])
    P.op("dve", lambda e: e.tensor_single_scalar(out=sel, in_=Wv, scalar=0.0, op=ALU.is_gt),
         reads=["Wall"], writes=["sel"])
    for ti in range(NTT):
        bk = ti % 2
        P.op("pe", lambda e, ti=ti, bk=bk: e.matmul(bank(bk, 128, 32, 0), lhsT=lmat_b, rhs=sel[:, ti, :],
                                                   start=True, stop=True),
             reads=["lmat_b", "sel"], writes=[("ps", bk)])
        P.op("pe", lambda e, ti=ti, bk=bk: e.matmul(bank(bk, 128, 32, 32), lhsT=ones_b, rhs=sel[:, ti, :],
                                                   start=True, stop=True),
             reads=["ones_b", "sel"], writes=[("ps", bk)])
        P.op("dve", lambda e, ti=ti, bk=bk: e.tensor_tensor(out=rank[:, ti, :], in0=bank(bk, 128, 32, 0),
                                                           in1=carry, op=ALU.add),
             reads=[("ps", bk), "carry"], writes=[("rank", ti)])
        P.op("dve", lambda e, bk=bk: e.tensor_tensor(out=carry, in0=bank(bk, 128, 32, 32), in1=carry, op=ALU.add),
             reads=[("ps", bk), "carry"], writes=["carry"])
    rkeys = [("rank", ti) for ti in range(NTT)]
    P.op("dve", lambda e: e.tensor_tensor(out=cmpt, in0=carry.unsqueeze(2).to_broadcast([128, 32, 32]),
                                          in1=thr.unsqueeze(1).to_broadcast([128, 32, 32]), op=ALU.is_gt),
         reads=["carry", "thr"], writes=["cmpt"])
    P.op("dve", lambda e: e.tensor_reduce(out=nbe, in_=cmpt, axis=AX.X, op=ALU.add), reads=["cmpt"], writes=["nbe"])
    P.op("dve", lambda e: e.tensor_scalar(out=padded, in0=nbe, scalar1=float(BLK), scalar2=None, op0=ALU.mult),
         reads=["nbe"], writes=["padded"])
    P.op("dve", lambda e: e.tensor_tensor_scan(out=pend, data0=onesf, data1=padded, initial=0.0,
                                               op0=ALU.mult, op1=ALU.add),
         reads=["onesf", "padded"], writes=["pend"])
    P.op("dve", lambda e: e.tensor_tensor(out=pstart, in0=pend, in1=padded, op=ALU.subtract),
         reads=["pend", "padded"], writes=["pstart"])
    P.op("dve", lambda e: e.tensor_tensor(out=dest, in0=rank, in1=pstart.unsqueeze(1).to_broadcast([128, NTT, 32]),
                                          op=ALU.add), reads=rkeys + ["pstart"], writes=["dest"])
    P.op("dve", lambda e: e.scalar_tensor_tensor(out=dsel.rearrange("p a b -> p (a b)"),
                                                 in0=dest.rearrange("p a b -> p (a b)"), scalar=1.0,
                                                 in1=sel.rearrange("p a b -> p (a b)"), op0=ALU.add, op1=ALU.mult),
         reads=["dest", "sel"], writes=["dsel"])
    for ti in range(NTT):
        P.op("dve", lambda e, ti=ti: e.max(out=t8[:, ti, :], in_=dsel[:, ti, :]), reads=["dsel"],
             writes=[("t8", ti)])
        for k in range(2):
            P.op("dve", lambda e, ti=ti, k=k: e.scalar_tensor_tensor(
                out=junk, in0=dsel[:, ti, :], scalar=t8[:, ti, k:k + 1], in1=Wv[:, ti, :],
                op0=ALU.is_equal, op1=ALU.mult, accum_out=wk[:, ti * 2 + k:ti * 2 + k + 1]),
                reads=["dsel", ("t8", ti), "Wall"], writes=["junk", ("wk", ti, k)])
    tkeys = [("t8", ti) for ti in range(NTT)]
    P.op("dve", lambda e: e.tensor_scalar(out=didf, in0=t8[:, :, 0:2], scalar1=-1.0, scalar2=None, op0=ALU.add),
         reads=tkeys, writes=["didf"])
    P.op("dve", lambda e: e.tensor_copy(out=didx[:], in_=didf.rearrange("p a b -> p (a b)")),
         reads=["didf"], writes=["didx"])
    P.op("dve", lambda e: e.tensor_tensor(out=cmpb, in0=pend.unsqueeze(1).to_broadcast([128, NBLK, 32]),
                                          in1=bthr.unsqueeze(2).to_broadcast([128, NBLK, 32]), op=ALU.is_le),
         reads=["pend", "bthr"], writes=["cmpb"])
    P.op("dve", lambda e: e.tensor_reduce(out=ebt, in_=cmpb, axis=AX.X, op=ALU.add), reads=["cmpb"], writes=["ebt"])
    P.op("dve", lambda e: e.tensor_scalar(out=ebt, in0=ebt, scalar1=float(NEXP - 1), scalar2=128.0,
                                          op0=ALU.min, op1=ALU.mult), reads=["ebt"], writes=["ebt"])
    P.op("dve", lambda e: e.tensor_scalar(out=ebt, in0=ebt, scalar1=pidx[:, 0:1], scalar2=None, op0=ALU.add),
         reads=["ebt", "pidx"], writes=["ebt"])
    P.op("dve", lambda e: e.tensor_copy(out=gidx[:], in_=ebt), reads=["ebt"], writes=["gidx"])
    if stop == 51:
        o1 = nc.dram_tensor("didx_o", [128, NTT * 2], I32, kind="ExternalOutput").ap()
        o2 = nc.dram_tensor("wk_o", [128, NTT * 2], F32, kind="ExternalOutput").ap()
        o3 = nc.dram_tensor("gidx_o", [128, NBLK], I32, kind="ExternalOutput").ap()
        o4 = nc.dram_tensor("pend_o", [128, 32], F32, kind="ExternalOutput").ap()
        P.dma("sp", o1, didx[:], reads=["didx"], writes=["o1"])
        P.dma("sp", o2, wk[:], reads=[("wk", ti, k) for ti in range(NTT) for k in range(2)], writes=["o2"])
        P.dma("sp", o3, gidx[:], reads=["gidx"], writes=["o3"])
        P.dma("sp", o4, pend, reads=["pend"], writes=["o4"])
        P.emit(final_wait_keys=["o1", "o2", "o3", "o4"])
        P.close()
        return nc
    P.barrier()
    A.reset()
    xrow = [A.alloc([D], BF16) for _ in range(2)]
    Gw = A.alloc([16, EH], BF16)
    Uw = A.alloc([16, EH], BF16)
    Dw = A.alloc([8, D], BF16)
    xblk = [A.alloc([D], BF16) for _ in range(2)]
    xTb5 = A.alloc([16, BLK], BF16)
    sg5 = [A.alloc([BLK], F32) for _ in range(2)]
    hid5 = A.alloc([8, BLK], BF16)
    yst = [A.alloc([D], F32) for _ in range(2)]
    for ti in range(NTT):
        xb_ = ti % 2
        P.dma("sp", xrow[xb_], h1b[ti * 128:(ti + 1) * 128, :], reads=["h1b"], writes=[("xrow", xb_), "spq"])
        for k in range(2):
            P.op("pool", lambda e, ti=ti, k=k, xb_=xb_: e.indirect_dma_start(
                out=xsort, out_offset=bass.IndirectOffsetOnAxis(
                    ap=didx[:, ti * 2 + k:ti * 2 + k + 1].bitcast(U32), axis=0),
                in_=xrow[xb_], in_offset=None),
                reads=[("xrow", xb_), "didx"], writes=["xsort", "poolq"], kind="d")
    erow = 0
    for b in range(NBLK):
        gio = bass.IndirectOffsetOnAxis
        P.op("pool", lambda e, b=b: e.indirect_dma_start(
            out=Gw.rearrange("p a b -> p (a b)"), out_offset=None, in_=wg_b,
            in_offset=bass.IndirectOffsetOnAxis(ap=gidx[:, b:b + 1].bitcast(U32), axis=0)),
            reads=["gidx", "wg_b"], writes=["Gw", "poolq"], kind="d")
        P.op("pool", lambda e, b=b: e.indirect_dma_start(
            out=Uw.rearrange("p a b -> p (a b)"), out_offset=None, in_=wu_b,
            in_offset=bass.IndirectOffsetOnAxis(ap=gidx[:, b:b + 1].bitcast(U32), axis=0)),
            reads=["gidx", "wu_b"], writes=["Uw", "poolq"], kind="d")
        P.op("pool", lambda e, b=b: e.indirect_dma_start(
            out=Dw.rearrange("p a b -> p (a b)"), out_offset=None, in_=wd_b,
            in_offset=bass.IndirectOffsetOnAxis(ap=gidx[:, b:b + 1].bitcast(U32), axis=0)),
            reads=["gidx", "wd_b"], writes=["Dw", "poolq"], kind="d")
        for st in range(2):
            r0 = b * BLK + st * 128
            P.dma("sp", xblk[st], xsort[r0:r0 + 128, :], reads=["xsort"], writes=[("xblk", st), "spq"])
            for k4 in range(4):
                bk = k4 % 2
                for kk in range(4):
                    kc = k4 * 4 + kk
                    P.op("pe", lambda e, st=st, kc=kc, kk=kk, bk=bk: e.transpose(
                        bankbf(bk, 128, 128, kk * 128), xblk[st][:, kc * 128:(kc + 1) * 128], ident_b[:]),
                        reads=[("xblk", st), "ident_b"], writes=[("ps", bk)])
                evac(xTb5[:, k4 * 4:(k4 + 1) * 4, st * 128:(st + 1) * 128],
                     bankbf(bk, 128, 512, 0).rearrange("p (a b) -> p a b", a=4),
                     [("ps", bk)], [("xT5", st, k4)])
        xkeys = [("xT5", st, k4) for st in range(2) for k4 in range(4)]
        for hc in range(8):
            bg = 2 + (hc % 2) * 2
            for kc in range(16):
                P.op("pe", lambda e, bg=bg, kc=kc, hc=hc: e.matmul(
                    bank(bg, 128, BLK), lhsT=Gw[:, kc, hc * 128:(hc + 1) * 128], rhs=xTb5[:, kc, :],
                    start=(kc == 0), stop=(kc == 15)), reads=["Gw"] + xkeys, writes=[("ps", bg)])
            for kc in range(16):
                P.op("pe", lambda e, bg=bg, kc=kc, hc=hc: e.matmul(
                    bank(bg + 1, 128, BLK), lhsT=Uw[:, kc, hc * 128:(hc + 1) * 128], rhs=xTb5[:, kc, :],
                    start=(kc == 0), stop=(kc == 15)), reads=["Uw"] + xkeys, writes=[("ps", bg + 1)])
            sgi = hc % 2
            P.op("act", lambda e, bg=bg, sgi=sgi: e.activation(out=sg5[sgi], in_=bank(bg, 128, BLK), func=AF.Silu),
                 reads=[("ps", bg)], writes=[("sg5", sgi)])
            P.op("dve", lambda e, bg=bg, sgi=sgi, hc=hc: e.tensor_tensor(
                out=hid5[:, hc, :], in0=sg5[sgi], in1=bank(bg + 1, 128, BLK), op=ALU.mult),
                reads=[("sg5", sgi), ("ps", bg + 1)], writes=[("hid5", hc)])
        hkeys = [("hid5", hc) for hc in range(8)]
        for st in range(2):
            r0 = b * BLK + st * 128
            for cbk in range(4):
                bd = 6 + cbk % 2
                for fc in range(8):
                    P.op("pe", lambda e, bd=bd, fc=fc, st=st, cbk=cbk: e.matmul(
                        bank(bd), lhsT=hid5[:, fc, st * 128:(st + 1) * 128],
                        rhs=Dw[:, fc, cbk * 512:(cbk + 1) * 512], start=(fc == 0), stop=(fc == 7)),
                        reads=hkeys + ["Dw"], writes=[("ps", bd)])
                evac(yst[st][:, cbk * 512:(cbk + 1) * 512], bank(bd), [("ps", bd)], [("yst", st, cbk)])
            P.dma("sp", ysort[r0:r0 + 128, :], yst[st], reads=[("yst", st, c_) for c_ in range(4)],
                  writes=["ysort", "spq"])
    P.barrier()
    A.reset()
    g2t = A.alloc([D], F32)
    b2t = A.alloc([D], F32)
    y0t = [A.alloc([D], F32) for _ in range(2)]
    y1t = [A.alloc([D], F32) for _ in range(2)]
    h1t = [A.alloc([D], F32) for _ in range(2)]
    st6b = A.alloc([4, 6], F32)
    mvb = A.alloc([8], F32)
    P.dma("sp", g2t, ln2g.partition_broadcast(128), writes=["g2"])
    P.dma("sp", b2t, ln2b.partition_broadcast(128), writes=["b2"])
    for ti in range(NTT):
        hb_ = ti % 2
        tok0 = ti * 128
        P.dma("sp", h1t[hb_], h1s[tok0:tok0 + 128, :], reads=["h1s"], writes=[("h1t", hb_), "spq"])
        for k, yt_ in ((0, y0t), (1, y1t)):
            P.op("pool", lambda e, ti=ti, k=k, yt_=yt_, hb_=hb_: e.indirect_dma_start(
                out=yt_[hb_], out_offset=None, in_=ysort,
                in_offset=bass.IndirectOffsetOnAxis(ap=didx[:, ti * 2 + k:ti * 2 + k + 1].bitcast(U32), axis=0)),
                reads=["didx", "ysort"], writes=[("yk", k, hb_), "poolq"], kind="d")
        P.op("dve", lambda e, ti=ti, hb_=hb_: e.tensor_scalar(
            out=y0t[hb_], in0=y0t[hb_], scalar1=wk[:, ti * 2:ti * 2 + 1], scalar2=None, op0=ALU.mult),
            reads=[("yk", 0, hb_)] + [("wk", ti, 0)], writes=[("yk", 0, hb_)])
        P.op("dve", lambda e, ti=ti, hb_=hb_: e.scalar_tensor_tensor(
            out=y0t[hb_], in0=y1t[hb_], scalar=wk[:, ti * 2 + 1:ti * 2 + 2], in1=y0t[hb_],
            op0=ALU.mult, op1=ALU.add),
            reads=[("yk", 0, hb_), ("yk", 1, hb_), ("wk", ti, 1)], writes=[("yk", 0, hb_)])
        P.op("dve", lambda e, hb_=hb_: e.scalar_tensor_tensor(
            out=h1t[hb_], in0=h1t[hb_], scalar=DN_ALPHA, in1=y0t[hb_], op0=ALU.mult, op1=ALU.add),
            reads=[("h1t", hb_), ("yk", 0, hb_)], writes=[("h1t", hb_)])
        layernorm(h1t[hb_], h1t[hb_], "g2", "b2", g2t, b2t, ("h1t", hb_), ("h1t", hb_), "b", st6b, mvb)
        P.dma("sp", out[tok0:tok0 + 128, :], h1t[hb_], reads=[("h1t", hb_)], writes=["out", "spq"])
    P.emit(final_wait_keys=["out"])
    P.close()
    return nc


def _consts(S, hf):
    SO = S // 2
    SC = S - SO
    slopes = np.exp2(-8.0 * np.arange(1, 9, dtype=np.float64) / 8)
    tok = np.arange(S)
    blk = tok // 128
    rel = tok % 128
    ktab = np.zeros((8, 4, S), np.float32)
    qtab = np.zeros((8, 4, SO), np.float32)
    for h in range(8):
        sl = slopes[h]
        ktab[h, 0] = 1.0
        ktab[h, 1] = 1.0
        ktab[h, 2] = sl * 128 * blk
        ktab[h, 3] = sl * rel
        if hf == 0:
            ktab[h, 2, :SC] += NEG
        qtab[h, 0] = -sl * 128 * blk[SC:]
        qtab[h, 1] = -sl * rel[SC:]
        qtab[h, 2] = 1.0
        qtab[h, 3] = 1.0
    ttab = np.zeros((128, 8, 896), np.float32)
    s = np.arange(128)[:, None]
    t = np.arange(128)[None, :]
    for h in range(8):
        d = np.zeros((128, 128), np.float64)
        fut = (s > t) & ((s // 64) == (t // 64))
        d[fut] = (-2.0 * slopes[h] * (s - t))[fut]
        d[(s // 64) > (t // 64)] = NEG
        ttab[:, h, 0:384] = NEG
        ttab[:, h, 384:512] = d
    j = np.arange(64)[:, None]
    l = np.arange(64)[None, :]
    tri = (j <= l).astype(np.float32)
    umat = (j > l).astype(np.float32)
    flag = np.full((128, 1), 1.0 if hf == 1 else 0.0, np.float32)
    nblk = (2 * SO) // 256 + NEXP
    a = np.arange(128)
    lmat = (a[:, None] < a[None, :]).astype(np.float32)
    thr256 = np.tile((256.0 * np.arange(32, dtype=np.float32))[None], (128, 1))
    bthr = np.tile((256.0 * np.arange(nblk, dtype=np.float32))[None], (128, 1))
    pidx = a.astype(np.float32)[:, None]
    return dict(ktab=ktab, qtab=qtab, ttab=ttab.reshape(128, 8 * 896), tri=tri, umat=umat, flag=flag,
                ident=np.eye(128, dtype=np.float32), lmat=lmat, thr256=thr256, bthr=bthr, pidx=pidx)


_CACHE = {}


def kernel(x, w_in, lambda_q1, lambda_k1, lambda_q2, lambda_k2, attn_norm_w, conv_w, conv_b,
           dt_bias, a_log, d_skip, ssm_norm_w, w_out, ln1_g, ln1_b, w_router_group,
           b_router_group, w_router_expert, b_router_expert, w_gate, w_up, w_down,
           ln2_g, ln2_b):
    x = np.asarray(x, np.float32)
    B, S, _ = x.shape
    SO = S // 2
    f = lambda a: np.ascontiguousarray(np.asarray(a, np.float32))
    if S not in _CACHE:
        _CACHE[S] = build_program(S)
    nc = _CACHE[S]
    wr = np.concatenate([f(w_router_group)[0], np.transpose(f(w_router_expert)[0], (1, 0, 2)).reshape(D, 32)], 1)
    br = np.concatenate([f(b_router_group)[0], f(b_router_expert)[0].reshape(32)])[None]
    lamv = np.concatenate([f(lambda_q1)[0], f(lambda_k1)[0], f(lambda_q2)[0], f(lambda_k2)[0]])[None]
    cw = np.ascontiguousarray(f(conv_w)[0].T.reshape(12, 128, 4).transpose(1, 0, 2).reshape(128, 48))
    cb = np.ascontiguousarray(f(conv_b)[0].reshape(12, 128).T)
    shared = dict(
        w_in=f(w_in)[0], w_out=f(w_out)[0], w_gate=f(w_gate)[0], w_up=f(w_up)[0], w_down=f(w_down)[0],
        wr=f(wr), br=f(br), lamv=f(lamv), anw=f(attn_norm_w), cw=cw, cb=cb, dtb=f(dt_bias), alog=f(a_log),
        dsk=f(d_skip), snw=f(ssm_norm_w), ln1g=f(ln1_g), ln1b=f(ln1_b), ln2g=f(ln2_g), ln2b=f(ln2_b))
    in_maps = []
    for b in range(B):
        for hf in range(2):
            m = dict(shared)
            m.update(_consts(S, hf))
            own = x[b, hf * SO:(hf + 1) * SO]
            ctx = x[b, 0:SO] if hf == 1 else np.zeros_like(own)
            m["xT"] = np.ascontiguousarray(np.concatenate([ctx, own], 0).T)
            m["xo"] = np.ascontiguousarray(own)
            in_maps.append(m)
    res = run_bass_kernel_spmd(nc, in_maps, core_ids=list(range(B * 2)))
    outp = np.zeros((B, S, D), np.float32)
    for b in range(B):
        for hf in range(2):
            outp[b, hf * SO:(hf + 1) * SO] = res.results[b * 2 + hf]["out"]
    return outp
```

```python
from contextlib import ExitStack
import math
import numpy as np
import concourse.bass as bass
import concourse.mybir as mybir
from concourse.bass_utils import run_bass_kernel_spmd

F32 = mybir.dt.float32
BF16 = mybir.dt.bfloat16
AF = mybir.ActivationFunctionType
ALU = mybir.AluOpType
AX = mybir.AxisListType

D = 2048
IN_COLS = 5648
NEXP = 32
EH = 1024
DN_ALPHA = 2.0 ** 0.25
LN_EPS = 1e-5
RMS_EPS = 1e-6
LAMBDA_INIT = 0.8 - 0.6 * math.exp(0.0)
NEG = -30000.0


class Prog:
    ENGS = ("pe", "act", "dve", "pool", "sp")

    def __init__(self, nc):
        self.nc = nc
        self.ops = []
        self.stack = ExitStack()
        self._n = 0
        self.barrier_at = []
        self.maxops = None

    def sb(self, shape, dtype, name=None):
        self._n += 1
        return self.stack.enter_context(
            self.nc.sbuf_tensor(name or f"sb{self._n}", list(shape), dtype))

    def op(self, eng, fn, reads=(), writes=(), kind="c", accum=False):
        if self.maxops is not None and len(self.ops) >= self.maxops:
            return
        import sys as _sys
        fr = _sys._getframe(1)
        if fr.f_code.co_name in ("dma", "evac", "convert", "layernorm"):
            fr = fr.f_back
        self.ops.append(dict(eng=eng, kind=kind, fn=fn, reads=tuple(reads),
                             writes=tuple(writes), accum=accum, line=fr.f_lineno))

    def dma(self, eng, out, in_, reads=(), writes=(), accum=False, **kw):
        self.op(eng, lambda e: e.dma_start(out=out, in_=in_, **kw), reads, writes,
                kind="d", accum=accum)

    def barrier(self):
        self.barrier_at.append(len(self.ops))

    def emit(self, final_wait_keys=()):
        nc = self.nc
        ops = self.ops
        n = len(ops)
        writers = {}
        gen_first = {}
        readers = {}
        deps = [None] * n
        needed = [False] * n
        bset = set()
        last_of = {}
        barriers = set(self.barrier_at)
        KSLOT = 16
        dcount = {}
        for o in ops:
            if o["kind"] == "d":
                c_ = dcount.get(o["eng"], 0)
                o["slot"] = c_ % KSLOT
                dcount[o["eng"]] = c_ + 1
            else:
                o["slot"] = 0

        def skof(o):
            return (o["eng"], o["kind"], o["slot"])

        def bank_of(k):
            if isinstance(k, tuple) and k[0] == "ps":
                return ("bankx", k[1])
            if isinstance(k, tuple) and k[0] == "O":
                return ("bankx", 4 + k[1] * 2 + k[2] // 2)
            return None

        for o in ops:
            if o["kind"] != "c":
                continue
            bx = {bank_of(k) for k in o["reads"] + o["writes"]} - {None}
            if bx and not o.get("bx_done"):
                o["writes"] = o["writes"] + tuple(bx)
                o["bx_done"] = True
        for i, o in enumerate(ops):
            if i in barriers:
                bset = set(last_of.values())
            d = set(bset)
            sk = skof(o)
            for k in o["reads"]:
                d.update(writers.get(k, {}).values())
            if not o["accum"]:
                for k in o["writes"]:
                    d.update(writers.get(k, {}).values())
                    d.update(readers.get(k, ()))
            else:
                for k in o["writes"]:
                    if k in gen_first:
                        d.add(gen_first[k])
                    if isinstance(k, tuple) and k[0] == "bankx":
                        d.update(writers.get(k, {}).values())
                        d.update(readers.get(k, ()))
            d.discard(i)
            if o["eng"] == "pe" and o["kind"] == "c":
                d = {j for j in d if not (ops[j]["eng"] == "pe" and ops[j]["kind"] == "c")}
            deps[i] = d
            for j in d:
                needed[j] = True
            for k in o["writes"]:
                if o["accum"]:
                    writers.setdefault(k, {})[sk] = i
                else:
                    writers[k] = {sk: i}
                    readers[k] = []
                    gen_first[k] = i
            for k in o["reads"]:
                readers.setdefault(k, []).append(i)
            last_of[sk] = i
            if o["kind"] == "d":
                needed[i] = True
        last_writer = {k: max(v.values()) for k, v in writers.items()}
        final = set()
        for k in final_wait_keys:
            final.update(writers.get(k, {}).values())
        for j in final:
            needed[j] = True
        semkeys = sorted({skof(o) for o in ops})
        sems = {sk: self.stack.enter_context(nc.semaphore(f"s_{sk[0]}_{sk[1]}_{sk[2]}"))
                for sk in semkeys}
        cnt = {sk: 0 for sk in semkeys}
        val = [0] * n
        for i, o in enumerate(ops):
            if needed[i]:
                sk = skof(o)
                cnt[sk] += 16 if o["kind"] == "d" else 1
                val[i] = cnt[sk]
        self.sem_counts = cnt
        engmap = {"pe": "tensor", "act": "scalar", "dve": "vector", "pool": "gpsimd",
                  "sp": "sync"}
        block = self.stack.enter_context(nc.Block())
        for eng in self.ENGS:
            idxs = [i for i, o in enumerate(ops) if o["eng"] == eng]
            if not idxs and eng != "sp":
                continue

            def body(e, idxs=idxs, eng=eng):
                waited = {}
                for i in idxs:
                    o = ops[i]
                    need = {}
                    for j in deps[i]:
                        sk = skof(ops[j])
                        if val[j] > need.get(sk, 0):
                            need[sk] = val[j]
                    for sk, v in need.items():
                        if waited.get(sk, 0) < v:
                            e.wait_ge(sems[sk], v)
                            waited[sk] = v
                    ins = o["fn"](e)
                    if needed[i]:
                        ins.then_inc(sems[skof(o)],
                                     16 if o["kind"] == "d" else 1)
                if eng == "sp":
                    need = {}
                    for j in final:
                        sk = skof(ops[j])
                        need[sk] = max(need.get(sk, 0), val[j])
                    for sk, v in need.items():
                        if waited.get(sk, 0) < v:
                            e.wait_ge(sems[sk], v)

            getattr(block, engmap[eng])(body)

    def close(self):
        self.stack.close()


def _dsz(dt):
    return 4 if dt == F32 else 2


class Arena:
    def __init__(self, P, nbytes):
        self.t = P.sb([128, nbytes // 4], F32, name="arena")
        self.off = 0
        self.cap = nbytes

    def alloc(self, free, dtype, parts=128):
        free = tuple(free)
        nel = int(np.prod(free))
        sz = (nel * _dsz(dtype) + 63) // 64 * 64
        assert self.off + sz <= self.cap, (self.off, sz, self.cap)
        ap = self.t[0:parts, self.off // 4:(self.off + sz) // 4]
        self.off += sz
        if dtype != F32:
            ap = ap.bitcast(dtype)
        ap = ap[:, 0:nel]
        if len(free) == 2:
            ap = ap.rearrange("p (a b) -> p a b", a=free[0])
        elif len(free) == 3:
            ap = ap.rearrange("p (a b c) -> p a b c", a=free[0], b=free[1])
        return ap

    def reset(self):
        self.off = 0


def win_groups():
    g = []
    for h in range(8):
        g.append(("q", h * 128, 128))
    for h in range(8):
        g.append(("k", 1024 + h * 128, 128))
    for i in range(2):
        g.append(("v", 2048 + i * 512, 512))
    for i in range(2):
        g.append(("z", 3072 + i * 512, 512))
    for i in range(12):
        g.append(("x", 4096 + i * 128, 128))
    g.append(("d", 5632, 16))
    return g


def build_program(S, stop=None, start=0):
    SO = S // 2
    SA = S
    SC = S - SO
    NB = SA // 128
    nc = bass.Bass("TRN2", target_bir_lowering=False)

    def din(name, shape, dt=F32):
        kind = "ExternalInput"
        if start > 0 and name in ("w_in", "w_out", "w_gate", "w_up", "w_down", "xT"):
            kind = "Internal"
        return nc.dram_tensor(name, list(shape), dt, kind=kind).ap()

    PROD = dict(win_b=0, wout_b=0, wg_b=0, wu_b=0, wd_b=0, qs=1, ks=1, vs=1, zs=1, xbcs=1, dts=1,
                mixT=3, h1s=4, h1T=4, xsort=5, ysort=5, h1b=4)

    def dscr(name, shape, dt):
        dbg = stop is not None and (stop == 0 or not name.startswith("w"))
        kind = "ExternalOutput" if dbg else "Internal"
        if start > 0 and PROD[name] < start and not name.startswith("w"):
            kind = "ExternalInput"
        if start > 0 and name.startswith("w"):
            kind = "ExternalInput" if (start == 4 and name == "wout_b") else "Internal"
        return nc.dram_tensor(name, list(shape), dt, kind=kind).ap()

    xT = din("xT", [D, SA])
    xo = din("xo", [SO, D])
    w_in = din("w_in", [D, IN_COLS])
    w_out = din("w_out", [D, D])
    w_gate = din("w_gate", [NEXP, D, EH])
    w_up = din("w_up", [NEXP, D, EH])
    w_down = din("w_down", [NEXP, EH, D])
    wr = din("wr", [D, 36])
    br = din("br", [1, 36])
    lamv = din("lamv", [1, 256])
    anw = din("anw", [1, 128])
    cw = din("cw", [128, 48])
    cb = din("cb", [128, 12])
    dtb = din("dtb", [1, 16])
    alog = din("alog", [1, 16])
    dsk = din("dsk", [1, 16])
    snw = din("snw", [1, 1024])
    ln1g = din("ln1g", [1, D])
    ln1b = din("ln1b", [1, D])
    ln2g = din("ln2g", [1, D])
    ln2b = din("ln2b", [1, D])
    ident_in = din("ident", [128, 128])
    qtab = din("qtab", [8, 4, SO])
    ktab = din("ktab", [8, 4, SA])
    ttab = din("ttab", [128, 8 * 896])
    tri_in = din("tri", [64, 64])
    umat_in = din("umat", [64, 64])
    flag_in = din("flag", [128, 1])
    out = nc.dram_tensor("out", [SO, D], F32, kind="ExternalOutput").ap()

    groups = win_groups()
    goff = []
    o = 0
    for (_, _, gc) in groups:
        goff.append(o)
        o += 128 * 16 * gc
    win_b = dscr("win_b", [o], BF16)
    wout_b = dscr("wout_b", [128, 16 * D], BF16)
    wg_b = dscr("wg_b", [NEXP * 128, 16 * EH], BF16)
    wu_b = dscr("wu_b", [NEXP * 128, 16 * EH], BF16)
    wd_b = dscr("wd_b", [NEXP * 128, 8 * D], BF16)
    BLK = 256
    NTT = SO // 128
    NBLK = (2 * SO) // BLK + NEXP
    xsort = dscr("xsort", [NBLK * BLK, D], BF16)
    ysort = dscr("ysort", [NBLK * BLK, D], F32)
    h1b = dscr("h1b", [SO, D], BF16)
    lmat_in = din("lmat", [128, 128])
    thr_in = din("thr256", [128, 32])
    bthr_in = din("bthr", [128, NBLK])
    pidx_in = din("pidx", [128, 1])
    qs = dscr("qs", [16, 64, SO], BF16)
    ks = dscr("ks", [16, 64, SA], BF16)
    vs = dscr("vs", [SA, 1024], BF16)
    zs = dscr("zs", [SO, 1024], F32)
    xbcs = dscr("xbcs", [1536, SA], F32)
    dts = dscr("dts", [SA, 16], F32)
    mixT = dscr("mixT", [D, SO], BF16)
    h1s = dscr("h1s", [SO, D], F32)
    h1T = dscr("h1T", [D, SO], BF16)

    P = Prog(nc)
    A = Arena(P, 176 * 1024)
    pp = P.stack.enter_context(nc.psum_tensor("pp", [128, 4096], F32))

    def bank(b, parts=128, n=512, off=0):
        return pp[0:parts, b * 512 + off:b * 512 + off + n]

    def bankbf(b, parts=128, n=1024, off=0):
        return pp[0:parts, b * 512:(b + 1) * 512].bitcast(BF16)[:, off:off + n]

    ident_f = P.sb([128, 128], F32, "ident_f")
    ident_b = P.sb([128, 128], BF16, "ident_b")
    Wall = P.sb([128, (SO // 128) * 32], F32, "Wall")
    epsln = P.sb([128, 1], F32, "epsln")
    epsrms = P.sb([128, 1], F32, "epsrms")
    P.dma("sp", ident_f[:], ident_in, writes=["ident_f"])
    P.dma("pool", ident_b[:], ident_in, writes=["ident_b"])
    P.op("dve", lambda e: e.memset(epsln[:], LN_EPS), writes=["eps"])
    P.op("dve", lambda e: e.memset(epsrms[:], RMS_EPS), writes=["eps2"])
    onec = P.sb([128, 1], F32, "onec")
    P.op("dve", lambda e: e.memset(onec[:], 1.0), writes=["onec"])

    n_setup = len(P.ops)

    def phase_begin(k):
        if start == k and k > 0:
            del P.ops[n_setup:]
            P.barrier_at.clear()
            import os
            if os.environ.get("KMAXOPS"):
                P.maxops = n_setup + int(os.environ["KMAXOPS"])

    evac_rr = [0]

    def evac(out_ap, in_ap, reads, writes, scale=None, accum=False):
        evac_rr[0] ^= 1
        if evac_rr[0]:
            if scale is None:
                P.op("act", lambda e: e.activation(out=out_ap, in_=in_ap, func=AF.Copy),
                     reads, writes, accum=accum)
            else:
                P.op("act", lambda e: e.activation(out=out_ap, in_=in_ap, func=AF.Copy,
                                                   scale=scale), reads, writes, accum=accum)
        else:
            if scale is None:
                P.op("dve", lambda e: e.tensor_copy(out=out_ap, in_=in_ap), reads, writes, accum=accum)
            else:
                P.op("dve", lambda e: e.tensor_scalar(out=out_ap, in0=in_ap, scalar1=scale,
                                                      scalar2=None, op0=ALU.mult),
                     reads, writes, accum=accum)

    cast_rr = [0]
    pend_st = []

    def convert(src_ap, dst_ap, free_shape, tag, dst3d=False):
        b = cast_rr[0] % 3
        cast_rr[0] += 1
        st = stg[b]
        cv = cvt[b]
        nel = int(np.prod(free_shape))
        sv = st[:, 0:nel]
        if len(free_shape) == 2:
            sv = sv.rearrange("p (a b) -> p a b", a=free_shape[0])
        P.dma("sp", sv, src_ap, reads=[], writes=[("stg", b)])
        eng = ("dve", "act")[(cast_rr[0] - 1) % 2]
        if eng == "act":
            P.op("act", lambda e: e.activation(out=cv[:, 0:nel], in_=st[:, 0:nel], func=AF.Copy),
                 reads=[("stg", b)], writes=[("cvt", b)])
        else:
            P.op(eng, lambda e: e.tensor_copy(out=cv[:, 0:nel], in_=st[:, 0:nel]),
                 reads=[("stg", b)], writes=[("cvt", b)])
        cvv = cv[:, 0:nel]
        if dst3d:
            cvv = cvv.rearrange("p (a b) -> p a b", a=free_shape[0])
        pend_st.append(lambda: P.dma("sp", dst_ap, cvv, reads=[("cvt", b)], writes=[tag], accum=True))
        if len(pend_st) > 2:
            pend_st.pop(0)()

    stg = [A.alloc([4096], F32) for _ in range(3)]
    cvt = [A.alloc([4096], BF16) for _ in range(3)]
    for gi, (kind, c0, gc) in enumerate(groups):
        nel = 16 * gc
        dst = win_b[goff[gi]:goff[gi] + 128 * nel].rearrange("(p f) -> p f", p=128)
        if nel <= 4096:
            convert(w_in[:, c0:c0 + gc].rearrange("(kc p) c -> p kc c", p=128), dst,
                    [16, gc], "win_b")
        else:
            for half in range(2):
                convert(w_in[half * 1024:(half + 1) * 1024, c0:c0 + gc]
                        .rearrange("(kc p) c -> p kc c", p=128),
                        dst[:, half * 8 * gc:(half + 1) * 8 * gc], [8, gc], "win_b")
    for kc2 in range(8):
        convert(w_out[kc2 * 256:(kc2 + 1) * 256, :].rearrange("(kc p) c -> p kc c", p=128),
                wout_b[:, kc2 * 2 * D:(kc2 + 1) * 2 * D], [2, D], "wout_b")
    for e_ in range(NEXP):
        for q in range(4):
            convert(w_gate[e_, :, q * 256:(q + 1) * 256].rearrange("(kc p) c -> p kc c", p=128),
                    wg_b[e_ * 128:(e_ + 1) * 128, :].rearrange("p (kc c) -> p kc c", kc=16)[:, :, q * 256:(q + 1) * 256],
                    [16, 256], "wg_b", dst3d=True)
            convert(w_up[e_, :, q * 256:(q + 1) * 256].rearrange("(kc p) c -> p kc c", p=128),
                    wu_b[e_ * 128:(e_ + 1) * 128, :].rearrange("p (kc c) -> p kc c", kc=16)[:, :, q * 256:(q + 1) * 256],
                    [16, 256], "wu_b", dst3d=True)
            convert(w_down[e_, q * 256:(q + 1) * 256, :].rearrange("(kc p) c -> p kc c", p=128),
                    wd_b[e_ * 128:(e_ + 1) * 128, q * 2 * D:(q + 1) * 2 * D], [2, D], "wd_b")
    while pend_st:
        pend_st.pop(0)()
    if stop == 0:
        P.emit(final_wait_keys=["win_b","wout_b","wg_b","wu_b","wd_b"])
        P.close()
        return nc
    P.barrier()
    A.reset()

    phase_begin(1)
    xTb = [A.alloc([16, 512], BF16) for _ in range(2)]
    wbuf = [A.alloc([16 * 512], BF16) for _ in range(3)]
    ebuf = [A.alloc([512], F32) for _ in range(4)]
    NT = SA // 512
    pend_c = []
    wrr = 0
    err = 0
    prr = 0
    for tt in range(NT):
        own = tt * 512 >= SC
        t0 = tt * 512
        to = t0 - SC
        xb = xTb[tt % 2]
        xk = ("xTb", tt % 2)
        P.dma("pool", xb, xT[:, t0:t0 + 512].rearrange("(kc p) t -> p kc t", p=128),
              writes=[xk])
        for gi, (kind, c0, gc) in enumerate(groups):
            if kind in ("q", "z") and not own:
                continue
            wb = wbuf[wrr % 3]
            wk = ("wbuf", wrr % 3)
            wrr += 1
            wv = wb[:, 0:16 * gc].rearrange("p (kc c) -> p kc c", kc=16)
            P.dma("sp", wb[:, 0:16 * gc],
                  win_b[goff[gi]:goff[gi] + 128 * 16 * gc].rearrange("(p f) -> p f", p=128),
                  reads=["win_b"], writes=[wk])
            if len(pend_c) >= 2:
                P.ops.extend(pend_c.pop(0))
            mark_c = len(P.ops)
            if kind in ("q", "k"):
                h = (c0 % 1024) // 128
                for m in range(2):
                    b = prr % 8
                    prr += 1
                    for kc in range(16):
                        P.op("pe", lambda e, b=b, kc=kc, m=m, wv=wv, xb=xb: e.matmul(
                            bank(b, 64), lhsT=wv[:, kc, m * 64:(m + 1) * 64], rhs=xb[:, kc, :],
                            start=(kc == 0), stop=(kc == 15)),
                            reads=[wk, xk], writes=[("ps", b)])
                    eb = ebuf[err % 4]
                    ek = ("ebuf", err % 4)
                    err += 1
                    ebv = eb.bitcast(BF16)[0:64, 0:512]
                    evac(ebv, bank(b, 64), [("ps", b)], [ek],
                         scale=(0.125 if kind == "q" else None))
                    if kind == "q":
                        P.dma("sp", qs[h * 2 + m, :, to:to + 512], ebv, reads=[ek], writes=["qs"], accum=True)
                    else:
                        P.dma("sp", ks[h * 2 + m, :, t0:t0 + 512], ebv, reads=[ek], writes=["ks"], accum=True)
            elif kind == "x":
                g = (c0 - 4096) // 128
                b = prr % 8
                prr += 1
                for kc in range(16):
                    P.op("pe", lambda e, b=b, kc=kc, wv=wv, xb=xb: e.matmul(
                        bank(b), lhsT=wv[:, kc, :], rhs=xb[:, kc, :],
                        start=(kc == 0), stop=(kc == 15)),
                        reads=[wk, xk], writes=[("ps", b)])
                eb = ebuf[err % 4]
                ek = ("ebuf", err % 4)
                err += 1
                evac(eb, bank(b), [("ps", b)], [ek])
                P.dma("sp", xbcs[g * 128:(g + 1) * 128, t0:t0 + 512], eb, reads=[ek],
                      writes=["xbcs"], accum=True)
            else:
                for ts in range(4):
                    b = prr % 8
                    prr += 1
                    for kc in range(16):
                        P.op("pe", lambda e, b=b, kc=kc, wv=wv, xb=xb, ts=ts, gc=gc: e.matmul(
                            bank(b, 128, gc), lhsT=xb[:, kc, ts * 128:(ts + 1) * 128],
                            rhs=wv[:, kc, :], start=(kc == 0), stop=(kc == 15)),
                            reads=[wk, xk], writes=[("ps", b)])
                    eb = ebuf[err % 4]
                    ek = ("ebuf", err % 4)
                    err += 1
                    if kind == "v":
                        ebv = eb.bitcast(BF16)[:, 0:512]
                        evac(ebv, bank(b), [("ps", b)], [ek])
                        P.dma("sp", vs[t0 + ts * 128:t0 + (ts + 1) * 128, c0 - 2048:c0 - 2048 + 512],
                              ebv, reads=[ek], writes=["vs"], accum=True)
                    elif kind == "z":
                        evac(eb, bank(b), [("ps", b)], [ek])
                        P.dma("sp", zs[to + ts * 128:to + (ts + 1) * 128, c0 - 3072:c0 - 3072 + 512],
                              eb, reads=[ek], writes=["zs"], accum=True)
                    else:
                        evac(eb[:, 0:16], bank(b, 128, 16), [("ps", b)], [ek])
                        P.dma("sp", dts[t0 + ts * 128:t0 + (ts + 1) * 128, :], eb[:, 0:16],
                              reads=[ek], writes=["dts"], accum=True)
            pend_c.append(P.ops[mark_c:])
            del P.ops[mark_c:]
    while pend_c:
        P.ops.extend(pend_c.pop(0))
    if stop == 1:
        P.emit(final_wait_keys=["qs","ks","vs","zs","xbcs","dts"])
        P.close()
        return nc
    P.barrier()
    A.reset()

    phase_begin(2)
    KA = [[A.alloc([SA], BF16, parts=68) for m in range(2)] for _ in range(2)]
    QA = [[A.alloc([SO], BF16, parts=68) for m in range(2)] for _ in range(2)]
    VA = [A.alloc([NB, 130], BF16) for _ in range(2)]
    PT = [A.alloc([512], BF16) for _ in range(3)]
    TT = A.alloc([8, 896], BF16)
    lamt = A.alloc([256], F32)
    lsc = A.alloc([64], F32)
    lam4 = A.alloc([8], F32)
    nwb = A.alloc([128], F32)
    ep_a = [A.alloc([128], F32) for _ in range(2)]
    ep_o = [A.alloc([128], F32) for _ in range(2)]
    ep_j = A.alloc([128], F32)
    ep_s = [A.alloc([8], F32) for _ in range(2)]
    ob = [A.alloc([128], BF16) for _ in range(2)]
    mo = [A.alloc([512], BF16) for _ in range(2)]

    P.dma("pool", TT, ttab.rearrange("p (h c) -> p h c", h=8), writes=["TT"])
    P.dma("sp", lamt, lamv.partition_broadcast(128), writes=["lamt"])
    P.dma("sp", nwb, anw.partition_broadcast(128), writes=["nwb"])
    P.op("dve", lambda e: e.tensor_scalar(out=nwb, in0=nwb, scalar1=(1.0 - LAMBDA_INIT),
                                          scalar2=None, op0=ALU.mult), reads=["nwb"], writes=["nwb"])
    for i in range(2):
        P.op("dve", lambda e, i=i: e.tensor_tensor(out=lsc, in0=lamt[:, i * 128:i * 128 + 64],
                                                  in1=lamt[:, i * 128 + 64:i * 128 + 128],
                                                  op=ALU.mult), reads=["lamt"], writes=["lsc"])
        P.op("dve", lambda e, i=i: e.tensor_reduce(out=lam4[:, i:i + 1], in_=lsc, axis=AX.X,
                                                  op=ALU.add), reads=["lsc"], writes=["lam4"])
    P.op("act", lambda e: e.activation(out=lam4[:, 4:6], in_=lam4[:, 0:2], func=AF.Exp),
         reads=["lam4"], writes=["lam4"])
    P.op("dve", lambda e: e.scalar_tensor_tensor(out=lam4[:, 2:3], in0=lam4[:, 5:6],
                                                 scalar=-LAMBDA_INIT, in1=lam4[:, 4:5],
                                                 op0=ALU.add, op1=ALU.subtract),
         reads=["lam4"], writes=["neglam"])
    neglam = lam4[:, 2:3]
    for vb in range(2):
        P.op("dve", lambda e, vb=vb: e.memset(VA[vb][:, :, 128:130], 1.0), writes=[("VA", vb)])

    NQT = SO // 512
    strr = 0
    ptrr = 0
    eprr = 0
    for h in range(8):
        hb = h % 2
        kkey = ("KA", hb)
        qkey = ("QA", hb)
        vkey = ("VA", hb)
        for m in range(2):
            P.dma("sp", KA[hb][m][0:64, :], ks[h * 2 + m], reads=["ks"], writes=[kkey], accum=(m > 0))
            for c0_ in range(0, SA, 2048):
                c1_ = min(SA, c0_ + 2048)
                P.dma("pool", KA[hb][m][64:68, c0_:c1_], ktab[h, :, c0_:c1_], writes=[kkey], accum=True)
            P.dma("sp", QA[hb][m][0:64, :], qs[h * 2 + m], reads=["qs"], writes=[qkey], accum=(m > 0))
            for c0_ in range(0, SO, 2048):
                c1_ = min(SO, c0_ + 2048)
                P.dma("pool", QA[hb][m][64:68, c0_:c1_], qtab[h, :, c0_:c1_], writes=[qkey], accum=True)
        for j0 in range(0, NB, 16):
            j1 = min(NB, j0 + 16)
            P.dma("sp", VA[hb][:, j0:j1, 0:128],
                  vs[j0 * 128:j1 * 128, h * 128:(h + 1) * 128].rearrange("(j p) d -> p j d", p=128),
                  reads=["vs"], writes=[vkey], accum=(j0 > 0))
        for qt in range(NQT):
            gb = (SC + qt * 512) // 128
            okeys = [[("O", m, i) for i in range(4)] for m in range(2)]
            its = [(j, m) for j in range(gb + 4) for m in range(2)]
            slots = []
            for (j, m) in its:
                slots.append((strr % 3, ptrr % 3))
                strr += 1
                ptrr += 1

            def emit_qk(idx, its=its, slots=slots, gb=gb, qt=qt, hb=hb, h=h, kkey=kkey, qkey=qkey):
                j, m = its[idx]
                sb_, pb = slots[idx]
                r = j - gb
                P.op("pe", lambda e, sb_=sb_, m=m, j=j, qt=qt, hb=hb, r=r: e.matmul(
                    bank(sb_), lhsT=KA[hb][m][:, j * 128:(j + 1) * 128],
                    rhs=QA[hb][m][:, qt * 512:(qt + 1) * 512], start=True, stop=(r < 0)),
                    reads=[kkey, qkey], writes=[("ps", sb_)])
                if r >= 0:
                    P.op("pe", lambda e, sb_=sb_, h=h, r=r: e.matmul(
                        bank(sb_), lhsT=ident_b[:], rhs=TT[:, h, (3 - r) * 128:(3 - r) * 128 + 512],
                        start=False, stop=True),
                        reads=["ident_b", "TT"], writes=[("ps", sb_)])

            def emit_rest(idx, its=its, slots=slots, gb=gb, hb=hb, vkey=vkey, okeys=okeys):
                j, m = its[idx]
                sb_, pb = slots[idx]
                r = j - gb
                P.op("act", lambda e, sb_=sb_, pb=pb: e.activation(
                    out=PT[pb], in_=bank(sb_), func=AF.Exp),
                    reads=[("ps", sb_)], writes=[("PT", pb)])
                for i in range(4):
                    if r >= 0 and i < r:
                        continue
                    ob_ = 4 + m * 2 + i // 2
                    P.op("pe", lambda e, pb=pb, i=i, j=j, hb=hb, ob_=ob_, gb=gb: e.matmul(
                        bank(ob_, 128, 129, (i % 2) * 256), lhsT=PT[pb][:, i * 128:(i + 1) * 128],
                        rhs=VA[hb][:, j, 0:129], start=(j == 0 and i % 2 == 0), stop=(j == gb + i)),
                        reads=[("PT", pb), vkey], writes=[okeys[m][i]])

            LA = 2
            for idx in range(len(its) + LA):
                if idx < len(its):
                    emit_qk(idx)
                if idx - LA >= 0:
                    emit_rest(idx - LA)
            mb = (h * NQT + qt) % 2
            for i in range(4):
                eb_ = eprr % 2
                O0 = bank(4 + i // 2, 128, 129, (i % 2) * 256)
                O1 = bank(6 + i // 2, 128, 129, (i % 2) * 256)
                es = ep_s[eb_]
                ek = ("ep", eb_)
                P.op("dve", lambda e, es=es, O0=O0: e.reciprocal(out=es[:, 0:1], in_=O0[:, 128:129]),
                     reads=[okeys[0][i]], writes=[ek])
                P.op("dve", lambda e, es=es, O1=O1: e.reciprocal(out=es[:, 1:2], in_=O1[:, 128:129]),
                     reads=[okeys[1][i]], writes=[ek])
                P.op("dve", lambda e, es=es: e.tensor_tensor(out=es[:, 2:3], in0=es[:, 1:2], in1=neglam,
                                                             op=ALU.mult),
                     reads=[ek, "neglam"], writes=[ek])
                P.op("dve", lambda e, es=es, O0=O0, eb_=eb_: e.tensor_scalar(
                    out=ep_a[eb_], in0=O0[:, 0:128], scalar1=es[:, 0:1], scalar2=None, op0=ALU.mult),
                    reads=[ek, okeys[0][i]], writes=[("epa", eb_)])
                P.op("dve", lambda e, es=es, O1=O1, eb_=eb_: e.scalar_tensor_tensor(
                    out=ep_o[eb_], in0=O1[:, 0:128], scalar=es[:, 2:3], in1=ep_a[eb_],
                    op0=ALU.mult, op1=ALU.add),
                    reads=[ek, okeys[1][i], ("epa", eb_)], writes=[("epo", eb_)])
                P.op("act", lambda e, es=es, eb_=eb_: e.activation(
                    out=ep_j, in_=ep_o[eb_], func=AF.Square, accum_out=es[:, 3:4]),
                    reads=[("epo", eb_)], writes=[("ep2", eb_), "epj"])
                P.op("act", lambda e, es=es: e.activation(
                    out=es[:, 4:5], in_=es[:, 3:4], func=AF.Sqrt, bias=epsrms[:], scale=1.0 / 128),
                    reads=[("ep2", eb_), "eps2"], writes=[("ep3", eb_)])
                P.op("dve", lambda e, es=es: e.reciprocal(out=es[:, 5:6], in_=es[:, 4:5]),
                     reads=[("ep3", eb_)], writes=[("ep4", eb_)])
                P.op("dve", lambda e, es=es, eb_=eb_: e.scalar_tensor_tensor(
                    out=ob[eb_], in0=ep_o[eb_], scalar=es[:, 5:6], in1=nwb, op0=ALU.mult, op1=ALU.mult),
                    reads=[("ep4", eb_), ("epo", eb_), "nwb"], writes=[("ob", eb_)])
                P.op("pe", lambda e, eb_=eb_, i=i: e.transpose(
                    bankbf(3, 128, 128, i * 128), ob[eb_], ident_b[:]),
                    reads=[("ob", eb_), "ident_b"], writes=[("ps", 3, i)])
                evac(mo[mb][:, i * 128:(i + 1) * 128], bankbf(3, 128, 128, i * 128),
                     [("ps", 3, i)], [("mo", mb)], accum=(i > 0))
                eprr += 1
            P.dma("sp", mixT[h * 128:(h + 1) * 128, qt * 512:(qt + 1) * 512], mo[mb],
                  reads=[("mo", mb)], writes=["mixT"], accum=True)
    if stop == 2:
        P.emit(final_wait_keys=["mixT"])
        P.close()
        return nc
    P.barrier()
    A.reset()

    phase_begin(3)
    NCH = SA // 64
    cwt = A.alloc([12, 4], F32)
    cbt = A.alloc([12], F32)
    tri = A.alloc([64], F32, parts=64)
    umat = A.alloc([64], F32, parts=64)
    ones64 = A.alloc([128], F32, parts=64)
    flagt = A.alloc([1], F32)
    dtbt = A.alloc([16], F32, parts=64)
    Aneg = A.alloc([16], F32, parts=64)
    Dt = A.alloc([16], F32, parts=64)
    snwt = A.alloc([1024], F32, parts=64)
    dtall = A.alloc([NCH, 16], F32, parts=64)
    aall = A.alloc([NCH, 16], F32, parts=64)
    xc = [A.alloc([12, 515], F32) for _ in range(1)]
    acc = A.alloc([512], F32)
    xcv = A.alloc([8, 512], F32)
    bct = A.alloc([4, 512], BF16)
    Rt = A.alloc([16, 64], F32, parts=64)
    Et = A.alloc([16, 64], F32, parts=64)
    Ebt = A.alloc([16, 64], F32)
    mcb = A.alloc([2, 64], F32, parts=64)
    Mt = A.alloc([16, 64], BF16, parts=64)
    Mtmp = A.alloc([16, 64], F32, parts=64)
    x32 = A.alloc([1024], F32, parts=64)
    xbf = A.alloc([1024], BF16, parts=64)
    wsc = A.alloc([16], F32, parts=64)
    xw = A.alloc([1024], BF16, parts=64)
    Btok = A.alloc([2, 128], BF16, parts=64)
    Cs = A.alloc([16, 64], BF16)
    Hs = A.alloc([1024], F32)
    Hbf = A.alloc([1024], BF16)
    yt = A.alloc([1024], F32, parts=64)
    zt = A.alloc([1024], F32, parts=64)
    ysq = A.alloc([1024], F32, parts=64)
    ss = A.alloc([8], F32, parts=64)
    ybf = A.alloc([1024], BF16, parts=64)
    ymo = A.alloc([8, 64], BF16)

    P.dma("sp", cwt, cw.rearrange("p (g k) -> p g k", g=12), writes=["cwt"])
    P.dma("sp", cbt, cb, writes=["cbt"])
    P.dma("sp", tri, tri_in, writes=["tri"])
    P.dma("sp", umat, umat_in, writes=["umat"])
    P.dma("sp", flagt, flag_in, writes=["flagt"])
    P.dma("sp", dtbt, dtb.partition_broadcast(64), writes=["dtbt"])
    P.dma("sp", Aneg, alog.partition_broadcast(64), writes=["Aneg"])
    P.dma("sp", Dt, dsk.partition_broadcast(64), writes=["Dt"])
    P.dma("sp", snwt, snw.partition_broadcast(64), writes=["snwt"])
    for c0_ in range(0, NCH, 32):
        c1_ = min(NCH, c0_ + 32)
        P.dma("sp", dtall[:, c0_:c1_, :], dts[c0_ * 64:c1_ * 64, :].rearrange("(c l) h -> l c h", l=64),
              reads=["dts"], writes=["dtall"], accum=(c0_ > 0))
    P.op("dve", lambda e: e.memset(ones64, 1.0), writes=["ones64"])
    P.op("dve", lambda e: e.memset(Hs, 0.0), writes=["Hs"])
    P.op("dve", lambda e: e.memset(Hbf, 0.0), writes=["Hbf"])
    P.op("act", lambda e: e.activation(out=Aneg, in_=Aneg, func=AF.Exp), reads=["Aneg"], writes=["Aneg"])
    P.op("dve", lambda e: e.tensor_scalar(out=Aneg, in0=Aneg, scalar1=-1.0, scalar2=None, op0=ALU.mult),
         reads=["Aneg"], writes=["Aneg"])
    P.op("dve", lambda e: e.tensor_tensor(out=dtall, in0=dtall,
                                          in1=dtbt.unsqueeze(1).to_broadcast([64, NCH, 16]), op=ALU.add),
         reads=["dtall", "dtbt"], writes=["dtall"])
    P.op("act", lambda e: e.activation(out=dtall, in_=dtall, func=AF.Exp), reads=["dtall"], writes=["dtall"])
    P.op("act", lambda e: e.activation(out=dtall, in_=dtall, func=AF.Ln, bias=onec[0:64, :], scale=1.0),
         reads=["dtall", "onec"], writes=["dtall"])
    P.op("dve", lambda e: e.tensor_tensor(out=aall, in0=dtall,
                                          in1=Aneg.unsqueeze(1).to_broadcast([64, NCH, 16]), op=ALU.mult),
         reads=["dtall", "Aneg"], writes=["aall"])

    for blk in range(SA // 512):
        t0 = blk * 512
        xcb = xc[0]
        if t0 == 0:
            P.op("dve", lambda e: e.memset(xcb[:, :, 0:3], 0.0), writes=["xc"])
            P.dma("sp", xcb[:, :, 3:515], xbcs[:, 0:512].rearrange("(g p) t -> p g t", p=128),
                  reads=["xbcs"], writes=["xc"])
        else:
            P.dma("sp", xcb, xbcs[:, t0 - 3:t0 + 512].rearrange("(g p) t -> p g t", p=128),
                  reads=["xbcs"], writes=["xc"])
        for g in range(12):
            P.op("dve", lambda e, g=g: e.tensor_scalar(out=acc, in0=xcb[:, g, 3:515],
                                                      scalar1=cwt[:, g, 3:4], scalar2=None, op0=ALU.mult),
                 reads=["xc", "cwt"], writes=["acc"])
            for k in range(3):
                P.op("dve", lambda e, g=g, k=k: e.scalar_tensor_tensor(
                    out=acc, in0=xcb[:, g, k:k + 512], scalar=cwt[:, g, k:k + 1], in1=acc,
                    op0=ALU.mult, op1=ALU.add), reads=["xc", "cwt", "acc"], writes=["acc"])
            if g < 8:
                P.op("act", lambda e, g=g: e.activation(out=xcv[:, g, :], in_=acc, func=AF.Silu,
                                                       bias=cbt[:, g:g + 1], scale=1.0),
                     reads=["acc", "cbt"], writes=[("xcv", g)])
            else:
                P.op("act", lambda e, g=g: e.activation(out=bct[:, g - 8, :], in_=acc, func=AF.Silu,
                                                       bias=cbt[:, g:g + 1], scale=1.0),
                     reads=["acc", "cbt"], writes=[("bct", g - 8)])
        for cl in range(8):
            c = blk * 8 + cl
            own = c * 64 >= SC
            csl = slice(cl * 64, (cl + 1) * 64)
            a_c = aall[:, c, :]
            dt_c = dtall[:, c, :]
            for g in range(8):
                P.op("pe", lambda e, g=g, csl=csl: e.transpose(
                    bank(g // 4, 64, 128, (g % 4) * 128), xcv[:, g, csl], ident_f[:]),
                    reads=[("xcv", g), "ident_f"], writes=[("ps", g // 4)])
            for hb_ in range(2):
                P.op("act", lambda e, hb_=hb_: e.activation(
                    out=x32[:, hb_ * 512:(hb_ + 1) * 512], in_=bank(hb_, 64), func=AF.Copy),
                    reads=[("ps", hb_)], writes=[("x32", hb_)])
                P.op("act", lambda e, hb_=hb_: e.activation(
                    out=xbf[:, hb_ * 512:(hb_ + 1) * 512], in_=bank(hb_, 64), func=AF.Copy),
                    reads=[("ps", hb_)], writes=[("xbf", hb_)])
            for g2 in range(2):
                P.op("pe", lambda e, g2=g2, csl=csl: e.transpose(
                    bankbf(7, 64, 128, 512 + g2 * 128), bct[:, g2, csl], ident_b[:]),
                    reads=[("bct", g2), "ident_b"], writes=[("ps", 7, "b")])
            P.op("dve", lambda e: e.tensor_copy(out=Btok.rearrange("p a b -> p (a b)"),
                                                in_=bankbf(7, 64, 256, 512)),
                 reads=[("ps", 7, "b")], writes=["Btok"])
            P.op("dve", lambda e, a_c=a_c: e.tensor_tensor(
                out=Rt, in0=tri.unsqueeze(1).to_broadcast([64, 16, 64]),
                in1=a_c.unsqueeze(2).to_broadcast([64, 16, 64]), op=ALU.mult),
                reads=["tri", "aall"], writes=["Rt"])
            Rf = Rt.rearrange("p a b -> p (a b)")
            for hb_ in range(2):
                P.op("pe", lambda e, hb_=hb_: e.matmul(
                    bank(2 + hb_, 64), lhsT=umat, rhs=Rf[:, hb_ * 512:(hb_ + 1) * 512],
                    start=True, stop=True), reads=["umat", "Rt"], writes=[("ps", 2 + hb_)])
                P.op("pe", lambda e, hb_=hb_: e.matmul(
                    bank(4 + hb_), lhsT=ones64, rhs=Rf[:, hb_ * 512:(hb_ + 1) * 512],
                    start=True, stop=True), reads=["ones64", "Rt"], writes=[("ps", 4 + hb_)])
            Ef = Et.rearrange("p a b -> p (a b)")
            Ebf = Ebt.rearrange("p a b -> p (a b)")
            for hb_ in range(2):
                P.op("act", lambda e, hb_=hb_: e.activation(
                    out=Ef[:, hb_ * 512:(hb_ + 1) * 512], in_=bank(2 + hb_, 64), func=AF.Exp),
                    reads=[("ps", 2 + hb_)], writes=[("Et", hb_)])
                P.op("act", lambda e, hb_=hb_: e.activation(
                    out=Ebf[:, hb_ * 512:(hb_ + 1) * 512], in_=bank(4 + hb_), func=AF.Exp),
                    reads=[("ps", 4 + hb_)], writes=[("Ebt", hb_)])
            ekeys = [("Et", 0), ("Et", 1)]
            ebkeys = [("Ebt", 0), ("Ebt", 1)]
            P.op("dve", lambda e, dt_c=dt_c: e.tensor_tensor(out=wsc, in0=dt_c, in1=Et[:, :, 63],
                                                            op=ALU.mult),
                 reads=ekeys + ["dtall"], writes=["wsc"])
            P.op("dve", lambda e: e.tensor_tensor(
                out=xw.rearrange("p (h d) -> p h d", h=16),
                in0=x32.rearrange("p (h d) -> p h d", h=16),
                in1=wsc.unsqueeze(2).to_broadcast([64, 16, 64]), op=ALU.mult),
                reads=["wsc", ("x32", 0), ("x32", 1)], writes=["xw"])
            if own:
                if c * 64 == SC:
                    P.op("dve", lambda e: e.tensor_scalar(out=Hs, in0=Hs, scalar1=flagt[:, 0:1],
                                                          scalar2=None, op0=ALU.mult),
                         reads=["Hs", "flagt"], writes=["Hs"])
                    P.op("dve", lambda e: e.tensor_copy(out=Hbf, in_=Hs), reads=["Hs"], writes=["Hbf"])
                for g2 in range(2):
                    P.op("pe", lambda e, g2=g2, csl=csl: e.matmul(
                        bank(6, 64, 64, g2 * 64), lhsT=bct[:, g2, csl], rhs=bct[:, 2 + g2, csl],
                        start=True, stop=True), reads=[("bct", g2), ("bct", 2 + g2)],
                        writes=[("ps", 6, "cb")])
                P.op("dve", lambda e: e.tensor_tensor(
                    out=mcb, in0=bank(6, 64, 128).rearrange("p (g l) -> p g l", g=2),
                    in1=tri.unsqueeze(1).to_broadcast([64, 2, 64]), op=ALU.mult),
                    reads=[("ps", 6, "cb"), "tri"], writes=["mcb"])
                for g2 in range(2):
                    P.op("dve", lambda e, g2=g2: e.tensor_tensor(
                        out=Mtmp[:, g2 * 8:(g2 + 1) * 8, :], in0=Et[:, g2 * 8:(g2 + 1) * 8, :],
                        in1=mcb[:, g2:g2 + 1, :].to_broadcast([64, 8, 64]), op=ALU.mult),
                        reads=ekeys + ["mcb"], writes=[("Mtmp", g2)])
                P.op("dve", lambda e, dt_c=dt_c: e.tensor_tensor(
                    out=Mt, in0=Mtmp, in1=dt_c.unsqueeze(2).to_broadcast([64, 16, 64]), op=ALU.mult),
                    reads=[("Mtmp", 0), ("Mtmp", 1), "dtall"], writes=["Mt"])
                for g2 in range(2):
                    P.op("dve", lambda e, g2=g2, csl=csl: e.tensor_tensor(
                        out=Cs[:, g2 * 8:(g2 + 1) * 8, :], in0=Ebt[:, g2 * 8:(g2 + 1) * 8, :],
                        in1=bct[:, 2 + g2, csl].unsqueeze(1).to_broadcast([128, 8, 64]), op=ALU.mult),
                        reads=ebkeys + [("bct", 2 + g2)], writes=[("Cs", g2)])
                for hh in range(16):
                    yb = bank(hh // 8, 64, 64, (hh % 8) * 64)
                    P.op("pe", lambda e, hh=hh, yb=yb: e.matmul(
                        yb, lhsT=Mt[:, hh, :], rhs=xbf[:, hh * 64:(hh + 1) * 64], start=True, stop=False),
                        reads=["Mt", ("xbf", hh // 8), ("x32", hh // 8)], writes=[("ps", hh // 8)])
                    P.op("pe", lambda e, hh=hh, yb=yb: e.matmul(
                        yb, lhsT=Cs[:, hh, :], rhs=Hbf[:, hh * 64:(hh + 1) * 64], start=False, stop=True),
                        reads=[("Cs", hh // 8), "Hbf"], writes=[("ps", hh // 8)])
            for g2 in range(2):
                P.op("pe", lambda e, g2=g2: e.matmul(
                    bank(2 + g2), lhsT=Btok[:, g2, :], rhs=xw[:, g2 * 512:(g2 + 1) * 512],
                    start=True, stop=True), reads=["Btok", "xw"] + ekeys, writes=[("ps", 2 + g2)])
            P.op("dve", lambda e: e.tensor_tensor(
                out=Hs.rearrange("p (h d) -> p h d", h=16), in0=Hs.rearrange("p (h d) -> p h d", h=16),
                in1=Ebt[:, :, 63:64].to_broadcast([128, 16, 64]), op=ALU.mult),
                reads=["Hs", "Hbf"] + ebkeys, writes=["Hs"])
            for g2 in range(2):
                P.op("dve", lambda e, g2=g2: e.tensor_tensor(
                    out=Hs[:, g2 * 512:(g2 + 1) * 512], in0=Hs[:, g2 * 512:(g2 + 1) * 512],
                    in1=bank(2 + g2), op=ALU.add), reads=["Hs", ("ps", 2 + g2)], writes=["Hs"])
            if own:
                to = c * 64 - SC
                P.dma("sp", zt, zs[to:to + 64, :], reads=["zs"], writes=["zt"])
                P.op("dve", lambda e: e.tensor_tensor(
                    out=yt.rearrange("p (h d) -> p h d", h=16), in0=x32.rearrange("p (h d) -> p h d", h=16),
                    in1=Dt.unsqueeze(2).to_broadcast([64, 16, 64]), op=ALU.mult),
                    reads=[("x32", 0), ("x32", 1), "Dt"], writes=["yt"])
                for hb_ in range(2):
                    P.op("dve", lambda e, hb_=hb_: e.tensor_tensor(
                        out=yt[:, hb_ * 512:(hb_ + 1) * 512], in0=yt[:, hb_ * 512:(hb_ + 1) * 512],
                        in1=bank(hb_, 64), op=ALU.add), reads=["yt", ("ps", hb_)], writes=["yt"])
                P.op("act", lambda e: e.activation(out=zt, in_=zt, func=AF.Silu), reads=["zt"], writes=["zt"])
                P.op("dve", lambda e: e.tensor_tensor(out=yt, in0=yt, in1=zt, op=ALU.mult),
                     reads=["yt", "zt"], writes=["yt"])
                for g2 in range(2):
                    P.op("act", lambda e, g2=g2: e.activation(
                        out=ysq[:, g2 * 512:(g2 + 1) * 512], in_=yt[:, g2 * 512:(g2 + 1) * 512],
                        func=AF.Square, accum_out=ss[:, g2:g2 + 1]), reads=["yt"], writes=["ysq", ("ss", g2)])
                P.op("act", lambda e: e.activation(out=ss[:, 2:4], in_=ss[:, 0:2], func=AF.Sqrt,
                                                   bias=epsrms[0:64, :], scale=1.0 / 512),
                     reads=[("ss", 0), ("ss", 1), "eps2"], writes=["ss2"])
                P.op("dve", lambda e: e.reciprocal(out=ss[:, 4:6], in_=ss[:, 2:4]), reads=["ss2"],
                     writes=["ss3"])
                for g2 in range(2):
                    P.op("dve", lambda e, g2=g2: e.scalar_tensor_tensor(
                        out=ybf[:, g2 * 512:(g2 + 1) * 512], in0=yt[:, g2 * 512:(g2 + 1) * 512],
                        scalar=ss[:, 4 + g2:5 + g2], in1=snwt[:, g2 * 512:(g2 + 1) * 512],
                        op0=ALU.mult, op1=ALU.mult), reads=["yt", "ss3", "snwt"], writes=["ybf"])
                for g in range(8):
                    P.op("pe", lambda e, g=g: e.transpose(
                        bankbf(7, 128, 64, g * 64), ybf[:, g * 128:(g + 1) * 128], ident_b[0:64, 0:64]),
                        reads=["ybf", "ident_b"], writes=[("ps", 7)])
                P.op("act", lambda e: e.activation(out=ymo.rearrange("p a b -> p (a b)"),
                                                   in_=bankbf(7, 128, 512, 0), func=AF.Copy),
                     reads=[("ps", 7)], writes=["ymo"])
                P.dma("sp", mixT[1024:2048, to:to + 64].rearrange("(g p) t -> p g t", p=128), ymo,
                      reads=["ymo"], writes=["mixT"], accum=True)
            P.op("act", lambda e: e.activation(out=Hbf, in_=Hs, func=AF.Copy), reads=["Hs"], writes=["Hbf"])
    if stop == 3:
        P.emit(final_wait_keys=["mixT"])
        P.close()
        return nc
    P.barrier()
    A.reset()

    phase_begin(4)
    wo = A.alloc([16, D], BF16)
    mxt = [A.alloc([16, 512], BF16) for _ in range(2)]
    xot = [A.alloc([D], F32) for _ in range(2)]
    rt = [A.alloc([D], F32) for _ in range(2)]
    g1 = A.alloc([D], F32)
    b1 = A.alloc([D], F32)
    st6 = A.alloc([4, 6], F32)
    mv = A.alloc([8], F32)
    hT32 = A.alloc([16, 128], F32)
    hTb = A.alloc([16, 128], BF16)
    hbt = [A.alloc([D], BF16) for _ in range(2)]
    wr32 = A.alloc([16, 36], F32)
    brt = A.alloc([36], F32)
    lg = A.alloc([36], F32)
    rs = A.alloc([16], F32)
    Gt = A.alloc([4], F32)
    elm = A.alloc([4, 8], F32)
    top8 = A.alloc([8], F32)
    msk = A.alloc([32], F32)
    ext = A.alloc([32], F32)
    P.dma("sp", wo.rearrange("p a b -> p (a b)"), wout_b, reads=["wout_b"], writes=["wo"])
    P.dma("sp", g1, ln1g.partition_broadcast(128), writes=["g1"])
    P.dma("sp", b1, ln1b.partition_broadcast(128), writes=["b1"])
    P.dma("sp", wr32, wr.rearrange("(kc p) c -> p kc c", p=128), writes=["wr32"])
    P.dma("sp", brt, br.partition_broadcast(128), writes=["brt"])

    def layernorm(src, dst, gk, bk, gt, bt, rkey, okey, tag, st6, mv):
        for cc in range(4):
            P.op("dve", lambda e, cc=cc: e.bn_stats(out=st6[:, cc, :], in_=src[:, cc * 512:(cc + 1) * 512]),
                 reads=[rkey], writes=[("st6", tag)])
        P.op("dve", lambda e: e.bn_aggr(out=mv[:, 0:2], in_=st6.rearrange("p a b -> p (a b)")),
             reads=[("st6", tag)], writes=[("mv", tag)])
        P.op("act", lambda e: e.activation(out=mv[:, 2:3], in_=mv[:, 1:2], func=AF.Sqrt,
                                           bias=epsln[:], scale=1.0),
             reads=[("mv", tag), "eps"], writes=[("mv2", tag)])
        P.op("dve", lambda e: e.reciprocal(out=mv[:, 3:4], in_=mv[:, 2:3]), reads=[("mv2", tag)],
             writes=[("mv3", tag)])
        P.op("dve", lambda e: e.tensor_scalar(out=src, in0=src, scalar1=mv[:, 0:1], scalar2=mv[:, 3:4],
                                              op0=ALU.subtract, op1=ALU.mult),
             reads=[rkey, ("mv3", tag), ("mv", tag)], writes=[rkey])
        P.op("dve", lambda e: e.tensor_tensor(out=src, in0=src, in1=gt, op=ALU.mult),
             reads=[rkey, gk], writes=[rkey])
        P.op("dve", lambda e: e.tensor_tensor(out=dst, in0=src, in1=bt, op=ALU.add),
             reads=[rkey, bk], writes=[okey])

    for t4 in range(SO // 512):
        mb = t4 % 2
        P.dma("sp", mxt[mb], mixT[:, t4 * 512:(t4 + 1) * 512].rearrange("(kc p) t -> p kc t", p=128),
              reads=["mixT"], writes=[("mxt", mb)])
        for ts in range(4):
            ti = t4 * 4 + ts
            tb = ti % 2
            tok0 = ti * 128
            P.dma("sp", xot[tb], xo[tok0:tok0 + 128, :], writes=[("xot", tb)])
            for cbk in range(4):
                for kc in range(16):
                    P.op("pe", lambda e, cbk=cbk, kc=kc, ts=ts, mb=mb: e.matmul(
                        bank(cbk), lhsT=mxt[mb][:, kc, ts * 128:(ts + 1) * 128],
                        rhs=wo[:, kc, cbk * 512:(cbk + 1) * 512], start=(kc == 0), stop=(kc == 15)),
                        reads=[("mxt", mb), "wo"], writes=[("ps", cbk)])
                P.op("dve", lambda e, cbk=cbk, tb=tb: e.scalar_tensor_tensor(
                    out=rt[tb][:, cbk * 512:(cbk + 1) * 512], in0=xot[tb][:, cbk * 512:(cbk + 1) * 512],
                    scalar=DN_ALPHA, in1=bank(cbk), op0=ALU.mult, op1=ALU.add),
                    reads=[("xot", tb), ("ps", cbk)], writes=[("rt", tb)])
            layernorm(rt[tb], rt[tb], "g1", "b1", g1, b1, ("rt", tb), ("rt", tb), "a", st6, mv)
            P.dma("sp", h1s[tok0:tok0 + 128, :], rt[tb], reads=[("rt", tb)], writes=["h1s"], accum=True)
            P.op("act", lambda e, tb=tb: e.activation(out=hbt[tb], in_=rt[tb], func=AF.Copy),
                 reads=[("rt", tb)], writes=[("hbt", tb)])
            P.dma("sp", h1b[tok0:tok0 + 128, :], hbt[tb], reads=[("hbt", tb)], writes=["h1b"], accum=True)
            for half in range(2):
                for k8 in range(8):
                    kc = half * 8 + k8
                    P.op("pe", lambda e, kc=kc, k8=k8, half=half, tb=tb: e.transpose(
                        bank(4 + half * 2 + k8 // 4, 128, 128, (k8 % 4) * 128),
                        rt[tb][:, kc * 128:(kc + 1) * 128], ident_f[:]),
                        reads=[("rt", tb), "ident_f"], writes=[("ps", 4 + half * 2 + k8 // 4)])
                for q2 in range(2):
                    bq = 4 + half * 2 + q2
                    kc0 = half * 8 + q2 * 4
                    P.op("act", lambda e, bq=bq, kc0=kc0: e.activation(
                        out=hT32[:, kc0:kc0 + 4, :].rearrange("p a b -> p (a b)"), in_=bank(bq), func=AF.Copy),
                        reads=[("ps", bq)], writes=[("hT32", kc0)])
            for kc in range(16):
                P.op("pe", lambda e, kc=kc: e.matmul(
                    bank(0, 128, 36), lhsT=hT32[:, kc, :], rhs=wr32[:, kc, :], start=(kc == 0), stop=(kc == 15)),
                    reads=[("hT32", (kc // 4) * 4), "wr32"], writes=[("ps", 0)])
            P.op("dve", lambda e: e.tensor_tensor(out=lg, in0=bank(0, 128, 36), in1=brt, op=ALU.add),
                 reads=[("ps", 0), "brt"], writes=["lg"])
            gl = lg[:, 0:4]
            el = lg[:, 4:36].rearrange("p (g e) -> p g e", g=4)
            P.op("dve", lambda e: e.tensor_reduce(out=rs[:, 0:1], in_=gl, axis=AX.X, op=ALU.max),
                 reads=["lg"], writes=["rs0"])
            P.op("dve", lambda e: e.tensor_scalar(out=rs[:, 1:2], in0=rs[:, 0:1], scalar1=-1.0, scalar2=None,
                                                  op0=ALU.mult), reads=["rs0"], writes=["rs1"])
            P.op("act", lambda e: e.activation(out=Gt, in_=gl, func=AF.Exp, bias=rs[:, 1:2], scale=1.0,
                                               accum_out=rs[:, 2:3]),
                 reads=["lg", "rs1"], writes=["Gt", "rs2"])
            P.op("dve", lambda e: e.reciprocal(out=rs[:, 3:4], in_=rs[:, 2:3]), reads=["rs2"], writes=["rs3"])
            P.op("dve", lambda e: e.tensor_scalar(out=Gt, in0=gl, scalar1=rs[:, 0:1], scalar2=None,
                                                  op0=ALU.is_ge), reads=["lg", "rs0", "Gt"], writes=["Gt"])
            P.op("dve", lambda e: e.tensor_scalar(out=Gt, in0=Gt, scalar1=-NEG, scalar2=NEG,
                                                  op0=ALU.mult, op1=ALU.add), reads=["Gt"], writes=["Gt"])
            P.op("dve", lambda e: e.tensor_tensor(out=elm, in0=el, in1=Gt.unsqueeze(2).to_broadcast([128, 4, 8]),
                                                  op=ALU.add), reads=["lg", "Gt"], writes=["elm"])
            elf = elm.rearrange("p a b -> p (a b)")
            P.op("dve", lambda e: e.max(out=top8, in_=elf), reads=["elm"], writes=["top8"])
            P.op("dve", lambda e: e.tensor_scalar(out=msk, in0=elf, scalar1=top8[:, 1:2], scalar2=None,
                                                  op0=ALU.is_ge), reads=["elm", "top8"], writes=["msk"])
            P.op("dve", lambda e: e.tensor_scalar(out=rs[:, 4:5], in0=top8[:, 0:1], scalar1=-1.0, scalar2=None,
                                                  op0=ALU.mult), reads=["top8"], writes=["rs4"])
            P.op("act", lambda e: e.activation(out=ext, in_=elf, func=AF.Exp, bias=rs[:, 4:5], scale=1.0),
                 reads=["elm", "rs4"], writes=["ext"])
            P.op("act", lambda e: e.activation(out=rs[:, 5:6], in_=top8[:, 1:2], func=AF.Exp, bias=rs[:, 4:5],
                                               scale=1.0), reads=["top8", "rs4"], writes=["rs5"])
            P.op("dve", lambda e: e.tensor_scalar(out=rs[:, 6:7], in0=rs[:, 5:6], scalar1=1.0, scalar2=None,
                                                  op0=ALU.add), reads=["rs5"], writes=["rs6"])
            P.op("dve", lambda e: e.reciprocal(out=rs[:, 7:8], in_=rs[:, 6:7]), reads=["rs6"], writes=["rs7"])
            P.op("dve", lambda e: e.tensor_tensor(out=rs[:, 8:9], in0=rs[:, 7:8], in1=rs[:, 3:4], op=ALU.mult),
                 reads=["rs7", "rs3"], writes=["rs8"])
            P.op("dve", lambda e, ti=ti: e.scalar_tensor_tensor(
                out=Wall[:, ti * 32:(ti + 1) * 32], in0=ext, scalar=rs[:, 8:9], in1=msk,
                op0=ALU.mult, op1=ALU.mult), reads=["ext", "rs8", "msk"], writes=["Wall"])
    if stop == 4:
        wall_out = nc.dram_tensor("Wall_out", [128, (SO // 128) * 32], F32, kind="ExternalOutput").ap()
        P.dma("sp", wall_out, Wall[:], reads=["Wall"], writes=["wall_out"])
        P.emit(final_wait_keys=["h1s","h1b","wall_out"])
        P.close()
        return nc
    P.barrier()
    A.reset()

    phase_begin(5)
    if start == 5:
        wall_in = nc.dram_tensor("Wall_in", [128, (SO // 128) * 32], F32, kind="ExternalInput").ap()
        P.dma("sp", Wall[:], wall_in, writes=["Wall"])
    I32 = mybir.dt.int32
    U32 = mybir.dt.uint32
    didx = P.sb([128, NTT * 2], I32, "didx")
    wk = P.sb([128, NTT * 2], F32, "wk")
    gidx = P.sb([128, NBLK], I32, "gidx")
    lmat_b = A.alloc([128], BF16)
    ones_b = A.alloc([128], BF16)
    sel = A.alloc([NTT, 32], BF16)
    carry = A.alloc([32], F32)
    rank = A.alloc([NTT, 32], F32)
    dest = A.alloc([NTT, 32], F32)
    dsel = A.alloc([NTT, 32], F32)
    t8 = A.alloc([NTT, 8], F32)
    thr = A.alloc([32], F32)
    cmpt = A.alloc([32, 32], F32)
    nbe = A.alloc([32], F32)
    padded = A.alloc([32], F32)
    pend = A.alloc([32], F32)
    pstart = A.alloc([32], F32)
    onesf = A.alloc([32], F32)
    bthr = A.alloc([NBLK], F32)
    cmpb = A.alloc([NBLK, 32], F32)
    ebt = A.alloc([NBLK], F32)
    pidx = A.alloc([1], F32)
    didf = A.alloc([NTT, 2], F32)
    junk = A.alloc([32], F32)
    Wv = Wall[:].rearrange("p (t e) -> p t e", e=32)
    P.dma("pool", lmat_b, lmat_in, writes=["lmat_b"])
    P.dma("sp", thr, thr_in, writes=["thr"])
    P.dma("sp", bthr, bthr_in, writes=["bthr"])
    P.dma("sp", pidx, pidx_in, writes=["pidx"])
    P.op("dve", lambda e: e.memset(ones_b, 1.0), writes=["ones_b"])
    P.op("dve", lambda e: e.memset(onesf, 1.0), writes=["onesf"])
    P.op("dve", lambda e: e.memset(carry, 0.0), writes=["carry"])
    P.op("dve", lambda e: e.tensor_single_scalar(out=sel, in_=Wv, scalar=0.0, op=ALU.is_gt),
         reads=["Wall"], writes=["sel"])
    for ti in range(NTT):
        bk = ti % 2
        P.op("pe", lambda e, ti=ti, bk=bk: e.matmul(bank(bk, 128, 32, 0), lhsT=lmat_b, rhs=sel[:, ti, :],
                                                   start=True, stop=True),
             reads=["lmat_b", "sel"], writes=[("ps", bk)])
        P.op("pe", lambda e, ti=ti, bk=bk: e.matmul(bank(bk, 128, 32, 32), lhsT=ones_b, rhs=sel[:, ti, :],
                                                   start=True, stop=True),
             reads=["ones_b", "sel"], writes=[("ps", bk)])
        P.op("dve", lambda e, ti=ti, bk=bk: e.tensor_tensor(out=rank[:, ti, :], in0=bank(bk, 128, 32, 0),
                                                           in1=carry, op=ALU.add),
             reads=[("ps", bk), "carry"], writes=[("rank", ti)])
        P.op("dve", lambda e, bk=bk: e.tensor_tensor(out=carry, in0=bank(bk, 128, 32, 32), in1=carry, op=ALU.add),
             reads=[("ps", bk), "carry"], writes=["carry"])
    rkeys = [("rank", ti) for ti in range(NTT)]
    P.op("dve", lambda e: e.tensor_tensor(out=cmpt, in0=carry.unsqueeze(2).to_broadcast([128, 32, 32]),
                                          in1=thr.unsqueeze(1).to_broadcast([128, 32, 32]), op=ALU.is_gt),
         reads=["carry", "thr"], writes=["cmpt"])
    P.op("dve", lambda e: e.tensor_reduce(out=nbe, in_=cmpt, axis=AX.X, op=ALU.add), reads=["cmpt"], writes=["nbe"])
    P.op("dve", lambda e: e.tensor_scalar(out=padded, in0=nbe, scalar1=float(BLK), scalar2=None, op0=ALU.mult),
         reads=["nbe"], writes=["padded"])
    P.op("dve", lambda e: e.tensor_tensor_scan(out=pend, data0=onesf, data1=padded, initial=0.0,
                                               op0=ALU.mult, op1=ALU.add),
         reads=["onesf", "padded"], writes=["pend"])
    P.op("dve", lambda e: e.tensor_tensor(out=pstart, in0=pend, in1=padded, op=ALU.subtract),
         reads=["pend", "padded"], writes=["pstart"])
    P.op("dve", lambda e: e.tensor_tensor(out=dest, in0=rank, in1=pstart.unsqueeze(1).to_broadcast([128, NTT, 32]),
                                          op=ALU.add), reads=rkeys + ["pstart"], writes=["dest"])
    P.op("dve", lambda e: e.scalar_tensor_tensor(out=dsel.rearrange("p a b -> p (a b)"),
                                                 in0=dest.rearrange("p a b -> p (a b)"), scalar=1.0,
                                                 in1=sel.rearrange("p a b -> p (a b)"), op0=ALU.add, op1=ALU.mult),
         reads=["dest", "sel"], writes=["dsel"])
    for ti in range(NTT):
        P.op("dve", lambda e, ti=ti: e.max(out=t8[:, ti, :], in_=dsel[:, ti, :]), reads=["dsel"],
             writes=[("t8", ti)])
        for k in range(2):
            P.op("dve", lambda e, ti=ti, k=k: e.scalar_tensor_tensor(
                out=junk, in0=dsel[:, ti, :], scalar=t8[:, ti, k:k + 1], in1=Wv[:, ti, :],
                op0=ALU.is_equal, op1=ALU.mult, accum_out=wk[:, ti * 2 + k:ti * 2 + k + 1]),
                reads=["dsel", ("t8", ti), "Wall"], writes=["junk", ("wk", ti, k)])
    tkeys = [("t8", ti) for ti in range(NTT)]
    P.op("dve", lambda e: e.tensor_scalar(out=didf, in0=t8[:, :, 0:2], scalar1=-1.0, scalar2=None, op0=ALU.add),
         reads=tkeys, writes=["didf"])
    P.op("dve", lambda e: e.tensor_copy(out=didx[:], in_=didf.rearrange("p a b -> p (a b)")),
         reads=["didf"], writes=["didx"])
    P.op("dve", lambda e: e.tensor_tensor(out=cmpb, in0=pend.unsqueeze(1).to_broadcast([128, NBLK, 32]),
                                          in1=bthr.unsqueeze(2).to_broadcast([128, NBLK, 32]), op=ALU.is_le),
         reads=["pend", "bthr"], writes=["cmpb"])
    P.op("dve", lambda e: e.tensor_reduce(out=ebt, in_=cmpb, axis=AX.X, op=ALU.add), reads=["cmpb"], writes=["ebt"])
    P.op("dve", lambda e: e.tensor_scalar(out=ebt, in0=ebt, scalar1=float(NEXP - 1), scalar2=128.0,
                                          op0=ALU.min, op1=ALU.mult), reads=["ebt"], writes=["ebt"])
    P.op("dve", lambda e: e.tensor_scalar(out=ebt, in0=ebt, scalar1=pidx[:, 0:1], scalar2=None, op0=ALU.add),
         reads=["ebt", "pidx"], writes=["ebt"])
    P.op("dve", lambda e: e.tensor_copy(out=gidx[:], in_=ebt), reads=["ebt"], writes=["gidx"])
    if stop == 51:
        o1 = nc.dram_tensor("didx_o", [128, NTT * 2], I32, kind="ExternalOutput").ap()
        o2 = nc.dram_tensor("wk_o", [128, NTT * 2], F32, kind="ExternalOutput").ap()
        o3 = nc.dram_tensor("gidx_o", [128, NBLK], I32, kind="ExternalOutput").ap()
        o4 = nc.dram_tensor("pend_o", [128, 32], F32, kind="ExternalOutput").ap()
        P.dma("sp", o1, didx[:], reads=["didx"], writes=["o1"])
        P.dma("sp", o2, wk[:], reads=[("wk", ti, k) for ti in range(NTT) for k in range(2)], writes=["o2"])
        P.dma("sp", o3, gidx[:], reads=["gidx"], writes=["o3"])
        P.dma("sp", o4, pend, reads=["pend"], writes=["o4"])
        P.emit(final_wait_keys=["o1", "o2", "o3", "o4"])
        P.close()
        return nc
    P.barrier()
    A.reset()
    xrow = [A.alloc([D], BF16) for _ in range(2)]
    Gw = A.alloc([16, EH], BF16)
    Uw = A.alloc([16, EH], BF16)
    Dw = A.alloc([8, D], BF16)
    xblk = [A.alloc([D], BF16) for _ in range(2)]
    xTb5 = A.alloc([16, BLK], BF16)
    sg5 = [A.alloc([BLK], F32) for _ in range(2)]
    hid5 = A.alloc([8, BLK], BF16)
    yst = [A.alloc([D], F32) for _ in range(2)]
    for ti in range(NTT):
        xb_ = ti % 2
        P.dma("sp", xrow[xb_], h1b[ti * 128:(ti + 1) * 128, :], reads=["h1b"], writes=[("xrow", xb_), "spq"])
        for k in range(2):
            P.op("pool", lambda e, ti=ti, k=k, xb_=xb_: e.indirect_dma_start(
                out=xsort, out_offset=bass.IndirectOffsetOnAxis(
                    ap=didx[:, ti * 2 + k:ti * 2 + k + 1].bitcast(U32), axis=0),
                in_=xrow[xb_], in_offset=None),
                reads=[("xrow", xb_), "didx"], writes=["xsort", "poolq"], kind="d")
    erow = 0
    for b in range(NBLK):
        gio = bass.IndirectOffsetOnAxis
        P.op("pool", lambda e, b=b: e.indirect_dma_start(
            out=Gw.rearrange("p a b -> p (a b)"), out_offset=None, in_=wg_b,
            in_offset=bass.IndirectOffsetOnAxis(ap=gidx[:, b:b + 1].bitcast(U32), axis=0)),
            reads=["gidx", "wg_b"], writes=["Gw", "poolq"], kind="d")
        P.op("pool", lambda e, b=b: e.indirect_dma_start(
            out=Uw.rearrange("p a b -> p (a b)"), out_offset=None, in_=wu_b,
            in_offset=bass.IndirectOffsetOnAxis(ap=gidx[:, b:b + 1].bitcast(U32), axis=0)),
            reads=["gidx", "wu_b"], writes=["Uw", "poolq"], kind="d")
        P.op("pool", lambda e, b=b: e.indirect_dma_start(
            out=Dw.rearrange("p a b -> p (a b)"), out_offset=None, in_=wd_b,
            in_offset=bass.IndirectOffsetOnAxis(ap=gidx[:, b:b + 1].bitcast(U32), axis=0)),
            reads=["gidx", "wd_b"], writes=["Dw", "poolq"], kind="d")
        for st in range(2):
            r0 = b * BLK + st * 128
            P.dma("sp", xblk[st], xsort[r0:r0 + 128, :], reads=["xsort"], writes=[("xblk", st), "spq"])
            for k4 in range(4):
                bk = k4 % 2
                for kk in range(4):
                    kc = k4 * 4 + kk
                    P.op("pe", lambda e, st=st, kc=kc, kk=kk, bk=bk: e.transpose(
                        bankbf(bk, 128, 128, kk * 128), xblk[st][:, kc * 128:(kc + 1) * 128], ident_b[:]),
                        reads=[("xblk", st), "ident_b"], writes=[("ps", bk)])
                evac(xTb5[:, k4 * 4:(k4 + 1) * 4, st * 128:(st + 1) * 128],
                     bankbf(bk, 128, 512, 0).rearrange("p (a b) -> p a b", a=4),
                     [("ps", bk)], [("xT5", st, k4)])
        xkeys = [("xT5", st, k4) for st in range(2) for k4 in range(4)]
        for hc in range(8):
            bg = 2 + (hc % 2) * 2
            for kc in range(16):
                P.op("pe", lambda e, bg=bg, kc=kc, hc=hc: e.matmul(
                    bank(bg, 128, BLK), lhsT=Gw[:, kc, hc * 128:(hc + 1) * 128], rhs=xTb5[:, kc, :],
                    start=(kc == 0), stop=(kc == 15)), reads=["Gw"] + xkeys, writes=[("ps", bg)])
            for kc in range(16):
                P.op("pe", lambda e, bg=bg, kc=kc, hc=hc: e.matmul(
                    bank(bg + 1, 128, BLK), lhsT=Uw[:, kc, hc * 128:(hc + 1) * 128], rhs=xTb5[:, kc, :],
                    start=(kc == 0), stop=(kc == 15)), reads=["Uw"] + xkeys, writes=[("ps", bg + 1)])
            sgi = hc % 2
            P.op("act", lambda e, bg=bg, sgi=sgi: e.activation(out=sg5[sgi], in_=bank(bg, 128, BLK), func=AF.Silu),
                 reads=[("ps", bg)], writes=[("sg5", sgi)])
            P.op("dve", lambda e, bg=bg, sgi=sgi, hc=hc: e.tensor_tensor(
                out=hid5[:, hc, :], in0=sg5[sgi], in1=bank(bg + 1, 128, BLK), op=ALU.mult),
                reads=[("sg5", sgi), ("ps", bg + 1)], writes=[("hid5", hc)])
        hkeys = [("hid5", hc) for hc in range(8)]
        for st in range(2):
            r0 = b * BLK + st * 128
            for cbk in range(4):
                bd = 6 + cbk % 2
                for fc in range(8):
                    P.op("pe", lambda e, bd=bd, fc=fc, st=st, cbk=cbk: e.matmul(
                        bank(bd), lhsT=hid5[:, fc, st * 128:(st + 1) * 128],
                        rhs=Dw[:, fc, cbk * 512:(cbk + 1) * 512], start=(fc == 0), stop=(fc == 7)),
                        reads=hkeys + ["Dw"], writes=[("ps", bd)])
                evac(yst[st][:, cbk * 512:(cbk + 1) * 512], bank(bd), [("ps", bd)], [("yst", st, cbk)])
            P.dma("sp", ysort[r0:r0 + 128, :], yst[st], reads=[("yst", st, c_) for c_ in range(4)],
                  writes=["ysort", "spq"])
    P.barrier()
    A.reset()
    g2t = A.alloc([D], F32)
    b2t = A.alloc([D], F32)
    y0t = [A.alloc([D], F32) for _ in range(2)]
    y1t = [A.alloc([D], F32) for _ in range(2)]
    h1t = [A.alloc([D], F32) for _ in range(2)]
    st6b = A.alloc([4, 6], F32)
    mvb = A.alloc([8], F32)
    P.dma("sp", g2t, ln2g.partition_broadcast(128), writes=["g2"])
    P.dma("sp", b2t, ln2b.partition_broadcast(128), writes=["b2"])
    for ti in range(NTT):
        hb_ = ti % 2
        tok0 = ti * 128
        P.dma("sp", h1t[hb_], h1s[tok0:tok0 + 128, :], reads=["h1s"], writes=[("h1t", hb_), "spq"])
        for k, yt_ in ((0, y0t), (1, y1t)):
            P.op("pool", lambda e, ti=ti, k=k, yt_=yt_, hb_=hb_: e.indirect_dma_start(
                out=yt_[hb_], out_offset=None, in_=ysort,
                in_offset=bass.IndirectOffsetOnAxis(ap=didx[:, ti * 2 + k:ti * 2 + k + 1].bitcast(U32), axis=0)),
                reads=["didx", "ysort"], writes=[("yk", k, hb_), "poolq"], kind="d")
        P.op("dve", lambda e, ti=ti, hb_=hb_: e.tensor_scalar(
            out=y0t[hb_], in0=y0t[hb_], scalar1=wk[:, ti * 2:ti * 2 + 1], scalar2=None, op0=ALU.mult),
            reads=[("yk", 0, hb_)] + [("wk", ti, 0)], writes=[("yk", 0, hb_)])
        P.op("dve", lambda e, ti=ti, hb_=hb_: e.scalar_tensor_tensor(
            out=y0t[hb_], in0=y1t[hb_], scalar=wk[:, ti * 2 + 1:ti * 2 + 2], in1=y0t[hb_],
            op0=ALU.mult, op1=ALU.add),
            reads=[("yk", 0, hb_), ("yk", 1, hb_), ("wk", ti, 1)], writes=[("yk", 0, hb_)])
        P.op("dve", lambda e, hb_=hb_: e.scalar_tensor_tensor(
            out=h1t[hb_], in0=h1t[hb_], scalar=DN_ALPHA, in1=y0t[hb_], op0=ALU.mult, op1=ALU.add),
            reads=[("h1t", hb_), ("yk", 0, hb_)], writes=[("h1t", hb_)])
        layernorm(h1t[hb_], h1t[hb_], "g2", "b2", g2t, b2t, ("h1t", hb_), ("h1t", hb_), "b", st6b, mvb)
        P.dma("sp", out[tok0:tok0 + 128, :], h1t[hb_], reads=[("h1t", hb_)], writes=["out", "spq"])
    P.emit(final_wait_keys=["out"])
    P.close()
    return nc


def _consts(S, hf):
    SO = S // 2
    SC = S - SO
    slopes = np.exp2(-8.0 * np.arange(1, 9, dtype=np.float64) / 8)
    tok = np.arange(S)
    blk = tok // 128
    rel = tok % 128
    ktab = np.zeros((8, 4, S), np.float32)
    qtab = np.zeros((8, 4, SO), np.float32)
    for h in range(8):
        sl = slopes[h]
        ktab[h, 0] = 1.0
        ktab[h, 1] = 1.0
        ktab[h, 2] = sl * 128 * blk
        ktab[h, 3] = sl * rel
        if hf == 0:
            ktab[h, 2, :SC] += NEG
        qtab[h, 0] = -sl * 128 * blk[SC:]
        qtab[h, 1] = -sl * rel[SC:]
        qtab[h, 2] = 1.0
        qtab[h, 3] = 1.0
    ttab = np.zeros((128, 8, 896), np.float32)
    s = np.arange(128)[:, None]
    t = np.arange(128)[None, :]
    for h in range(8):
        d = np.zeros((128, 128), np.float64)
        fut = (s > t) & ((s // 64) == (t // 64))
        d[fut] = (-2.0 * slopes[h] * (s - t))[fut]
        d[(s // 64) > (t // 64)] = NEG
        ttab[:, h, 0:384] = NEG
        ttab[:, h, 384:512] = d
    j = np.arange(64)[:, None]
    l = np.arange(64)[None, :]
    tri = (j <= l).astype(np.float32)
    umat = (j > l).astype(np.float32)
    flag = np.full((128, 1), 1.0 if hf == 1 else 0.0, np.float32)
    nblk = (2 * SO) // 256 + NEXP
    a = np.arange(128)
    lmat = (a[:, None] < a[None, :]).astype(np.float32)
    thr256 = np.tile((256.0 * np.arange(32, dtype=np.float32))[None], (128, 1))
    bthr = np.tile((256.0 * np.arange(nblk, dtype=np.float32))[None], (128, 1))
    pidx = a.astype(np.float32)[:, None]
    return dict(ktab=ktab, qtab=qtab, ttab=ttab.reshape(128, 8 * 896), tri=tri, umat=umat, flag=flag,
                ident=np.eye(128, dtype=np.float32), lmat=lmat, thr256=thr256, bthr=bthr, pidx=pidx)


_CACHE = {}


def kernel(x, w_in, lambda_q1, lambda_k1, lambda_q2, lambda_k2, attn_norm_w, conv_w, conv_b,
           dt_bias, a_log, d_skip, ssm_norm_w, w_out, ln1_g, ln1_b, w_router_group,
           b_router_group, w_router_expert, b_router_expert, w_gate, w_up, w_down,
           ln2_g, ln2_b):
    x = np.asarray(x, np.float32)
    B, S, _ = x.shape
    SO = S // 2
    f = lambda a: np.ascontiguousarray(np.asarray(a, np.float32))
    if S not in _CACHE:
        _CACHE[S] = build_program(S)
    nc = _CACHE[S]
    wr = np.concatenate([f(w_router_group)[0], np.transpose(f(w_router_expert)[0], (1, 0, 2)).reshape(D, 32)], 1)
    br = np.concatenate([f(b_router_group)[0], f(b_router_expert)[0].reshape(32)])[None]
    lamv = np.concatenate([f(lambda_q1)[0], f(lambda_k1)[0], f(lambda_q2)[0], f(lambda_k2)[0]])[None]
    cw = np.ascontiguousarray(f(conv_w)[0].T.reshape(12, 128, 4).transpose(1, 0, 2).reshape(128, 48))
    cb = np.ascontiguousarray(f(conv_b)[0].reshape(12, 128).T)
    shared = dict(
        w_in=f(w_in)[0], w_out=f(w_out)[0], w_gate=f(w_gate)[0], w_up=f(w_up)[0], w_down=f(w_down)[0],
        wr=f(wr), br=f(br), lamv=f(lamv), anw=f(attn_norm_w), cw=cw, cb=cb, dtb=f(dt_bias), alog=f(a_log),
        dsk=f(d_skip), snw=f(ssm_norm_w), ln1g=f(ln1_g), ln1b=f(ln1_b), ln2g=f(ln2_g), ln2b=f(ln2_b))
    in_maps = []
    for b in range(B):
        for hf in range(2):
            m = dict(shared)
            m.update(_consts(S, hf))
            own = x[b, hf * SO:(hf + 1) * SO]
            ctx = x[b, 0:SO] if hf == 1 else np.zeros_like(own)
            m["xT"] = np.ascontiguousarray(np.concatenate([ctx, own], 0).T)
            m["xo"] = np.ascontiguousarray(own)
            in_maps.append(m)
    res = run_bass_kernel_spmd(nc, in_maps, core_ids=list(range(B * 2)))
    outp = np.zeros((B, S, D), np.float32)
    for b in range(B):
        for hf in range(2):
            outp[b, hf * SO:(hf + 1) * SO] = res.results[b * 2 + hf]["out"]
    return outp
```

```python
from contextlib import ExitStack
import math
import numpy as np
import concourse.bass as bass
import concourse.mybir as mybir
from concourse.bass_utils import run_bass_kernel_spmd

F32 = mybir.dt.float32
BF16 = mybir.dt.bfloat16
AF = mybir.ActivationFunctionType
ALU = mybir.AluOpType
AX = mybir.AxisListType

D = 2048
IN_COLS = 5648
NEXP = 32
EH = 1024
DN_ALPHA = 2.0 ** 0.25
LN_EPS = 1e-5
RMS_EPS = 1e-6
LAMBDA_INIT = 0.8 - 0.6 * math.exp(0.0)
NEG = -30000.0


class Prog:
    ENGS = ("pe", "act", "dve", "pool", "sp")

    def __init__(self, nc):
        self.nc = nc
        self.ops = []
        self.stack = ExitStack()
        self._n = 0
        self.barrier_at = []
        self.maxops = None

    def sb(self, shape, dtype, name=None):
        self._n += 1
        return self.stack.enter_context(
            self.nc.sbuf_tensor(name or f"sb{self._n}", list(shape), dtype))

    def op(self, eng, fn, reads=(), writes=(), kind="c", accum=False):
        if self.maxops is not None and len(self.ops) >= self.maxops:
            return
        import sys as _sys
        fr = _sys._getframe(1)
        if fr.f_code.co_name in ("dma", "evac", "convert", "layernorm"):
            fr = fr.f_back
        self.ops.append(dict(eng=eng, kind=kind, fn=fn, reads=tuple(reads),
                             writes=tuple(writes), accum=accum, line=fr.f_lineno))

    def dma(self, eng, out, in_, reads=(), writes=(), accum=False, **kw):
        self.op(eng, lambda e: e.dma_start(out=out, in_=in_, **kw), reads, writes,
                kind="d", accum=accum)

    def barrier(self):
        self.barrier_at.append(len(self.ops))

    def emit(self, final_wait_keys=()):
        nc = self.nc
        ops = self.ops
        n = len(ops)
        writers = {}
        gen_first = {}
        readers = {}
        deps = [None] * n
        needed = [False] * n
        bset = set()
        last_of = {}
        barriers = set(self.barrier_at)
        KSLOT = 16
        dcount = {}
        for o in ops:
            if o["kind"] == "d":
                c_ = dcount.get(o["eng"], 0)
                o["slot"] = c_ % KSLOT
                dcount[o["eng"]] = c_ + 1
            else:
                o["slot"] = 0

        def skof(o):
            return (o["eng"], o["kind"], o["slot"])

        def bank_of(k):
            if isinstance(k, tuple) and k[0] == "ps":
                return ("bankx", k[1])
            if isinstance(k, tuple) and k[0] == "O":
                return ("bankx", 4 + k[1] * 2 + k[2] // 2)
            return None

        for o in ops:
            if o["kind"] != "c":
                continue
            bx = {bank_of(k) for k in o["reads"] + o["writes"]} - {None}
            if bx and not o.get("bx_done"):
                o["writes"] = o["writes"] + tuple(bx)
                o["bx_done"] = True
        for i, o in enumerate(ops):
            if i in barriers:
                bset = set(last_of.values())
            d = set(bset)
            sk = skof(o)
            for k in o["reads"]:
                d.update(writers.get(k, {}).values())
            if not o["accum"]:
                for k in o["writes"]:
                    d.update(writers.get(k, {}).values())
                    d.update(readers.get(k, ()))
            else:
                for k in o["writes"]:
                    if k in gen_first:
                        d.add(gen_first[k])
                    if isinstance(k, tuple) and k[0] == "bankx":
                        d.update(writers.get(k, {}).values())
                        d.update(readers.get(k, ()))
            d.discard(i)
            if o["eng"] == "pe" and o["kind"] == "c":
                d = {j for j in d if not (ops[j]["eng"] == "pe" and ops[j]["kind"] == "c")}
            deps[i] = d
            for j in d:
                needed[j] = True
            for k in o["writes"]:
                if o["accum"]:
                    writers.setdefault(k, {})[sk] = i
                else:
                    writers[k] = {sk: i}
                    readers[k] = []
                    gen_first[k] = i
            for k in o["reads"]:
                readers.setdefault(k, []).append(i)
            last_of[sk] = i
            if o["kind"] == "d":
                needed[i] = True
        last_writer = {k: max(v.values()) for k, v in writers.items()}
        final = set()
        for k in final_wait_keys:
            final.update(writers.get(k, {}).values())
        for j in final:
            needed[j] = True
        semkeys = sorted({skof(o) for o in ops})
        sems = {sk: self.stack.enter_context(nc.semaphore(f"s_{sk[0]}_{sk[1]}_{sk[2]}"))
                for sk in semkeys}
        cnt = {sk: 0 for sk in semkeys}
        val = [0] * n
        for i, o in enumerate(ops):
            if needed[i]:
                sk = skof(o)
                cnt[sk] += 16 if o["kind"] == "d" else 1
                val[i] = cnt[sk]
        self.sem_counts = cnt
        engmap = {"pe": "tensor", "act": "scalar", "dve": "vector", "pool": "gpsimd",
                  "sp": "sync"}
        block = self.stack.enter_context(nc.Block())
        for eng in self.ENGS:
            idxs = [i for i, o in enumerate(ops) if o["eng"] == eng]
            if not idxs and eng != "sp":
                continue

            def body(e, idxs=idxs, eng=eng):
                waited = {}
                for i in idxs:
                    o = ops[i]
                    need = {}
                    for j in deps[i]:
                        sk = skof(ops[j])
                        if val[j] > need.get(sk, 0):
                            need[sk] = val[j]
                    for sk, v in need.items():
                        if waited.get(sk, 0) < v:
                            e.wait_ge(sems[sk], v)
                            waited[sk] = v
                    ins = o["fn"](e)
                    if needed[i]:
                        ins.then_inc(sems[skof(o)],
                                     16 if o["kind"] == "d" else 1)
                if eng == "sp":
                    need = {}
                    for j in final:
                        sk = skof(ops[j])
                        need[sk] = max(need.get(sk, 0), val[j])
                    for sk, v in need.items():
                        if waited.get(sk, 0) < v:
                            e.wait_ge(sems[sk], v)

            getattr(block, engmap[eng])(body)

    def close(self):
        self.stack.close()


def _dsz(dt):
    return 4 if dt == F32 else 2


class Arena:
    def __init__(self, P, nbytes):
        self.t = P.sb([128, nbytes // 4], F32, name="arena")
        self.off = 0
        self.cap = nbytes

    def alloc(self, free, dtype, parts=128):
        free = tuple(free)
        nel = int(np.prod(free))
        sz = (nel * _dsz(dtype) + 63) // 64 * 64
        assert self.off + sz <= self.cap, (self.off, sz, self.cap)
        ap = self.t[0:parts, self.off // 4:(self.off + sz) // 4]
        self.off += sz
        if dtype != F32:
            ap = ap.bitcast(dtype)
        ap = ap[:, 0:nel]
        if len(free) == 2:
            ap = ap.rearrange("p (a b) -> p a b", a=free[0])
        elif len(free) == 3:
            ap = ap.rearrange("p (a b c) -> p a b c", a=free[0], b=free[1])
        return ap

    def reset(self):
        self.off = 0


def win_groups():
    g = []
    for h in range(8):
        g.append(("q", h * 128, 128))
    for h in range(8):
        g.append(("k", 1024 + h * 128, 128))
    for i in range(2):
        g.append(("v", 2048 + i * 512, 512))
    for i in range(2):
        g.append(("z", 3072 + i * 512, 512))
    for i in range(12):
        g.append(("x", 4096 + i * 128, 128))
    g.append(("d", 5632, 16))
    return g


def build_program(S, stop=None, start=0):
    SO = S // 2
    SA = S
    SC = S - SO
    NB = SA // 128
    nc = bass.Bass("TRN2", target_bir_lowering=False)

    def din(name, shape, dt=F32):
        kind = "ExternalInput"
        if start > 0 and name in ("w_in", "w_out", "w_gate", "w_up", "w_down", "xT"):
            kind = "Internal"
        return nc.dram_tensor(name, list(shape), dt, kind=kind).ap()

    PROD = dict(win_b=0, wout_b=0, wg_b=0, wu_b=0, wd_b=0, qs=1, ks=1, vs=1, zs=1, xbcs=1, dts=1,
                mixT=3, h1s=4, h1T=4, xsort=5, ysort=5, h1b=4)

    def dscr(name, shape, dt):
        dbg = stop is not None and (stop == 0 or not name.startswith("w"))
        kind = "ExternalOutput" if dbg else "Internal"
        if start > 0 and PROD[name] < start and not name.startswith("w"):
            kind = "ExternalInput"
        if start > 0 and name.startswith("w"):
            kind = "ExternalInput" if (start == 4 and name == "wout_b") else "Internal"
        return nc.dram_tensor(name, list(shape), dt, kind=kind).ap()

    xT = din("xT", [D, SA])
    xo = din("xo", [SO, D])
    w_in = din("w_in", [D, IN_COLS])
    w_out = din("w_out", [D, D])
    w_gate = din("w_gate", [NEXP, D, EH])
    w_up = din("w_up", [NEXP, D, EH])
    w_down = din("w_down", [NEXP, EH, D])
    wr = din("wr", [D, 36])
    br = din("br", [1, 36])
    lamv = din("lamv", [1, 256])
    anw = din("anw", [1, 128])
    cw = din("cw", [128, 48])
    cb = din("cb", [128, 12])
    dtb = din("dtb", [1, 16])
    alog = din("alog", [1, 16])
    dsk = din("dsk", [1, 16])
    snw = din("snw", [1, 1024])
    ln1g = din("ln1g", [1, D])
    ln1b = din("ln1b", [1, D])
    ln2g = din("ln2g", [1, D])
    ln2b = din("ln2b", [1, D])
    ident_in = din("ident", [128, 128])
    qtab = din("qtab", [8, 4, SO])
    ktab = din("ktab", [8, 4, SA])
    ttab = din("ttab", [128, 8 * 896])
    tri_in = din("tri", [64, 64])
    umat_in = din("umat", [64, 64])
    flag_in = din("flag", [128, 1])
    out = nc.dram_tensor("out", [SO, D], F32, kind="ExternalOutput").ap()

    groups = win_groups()
    goff = []
    o = 0
    for (_, _, gc) in groups:
        goff.append(o)
        o += 128 * 16 * gc
    win_b = dscr("win_b", [o], BF16)
    wout_b = dscr("wout_b", [128, 16 * D], BF16)
    wg_b = dscr("wg_b", [NEXP * 128, 16 * EH], BF16)
    wu_b = dscr("wu_b", [NEXP * 128, 16 * EH], BF16)
    wd_b = dscr("wd_b", [NEXP * 128, 8 * D], BF16)
    BLK = 256
    NTT = SO // 128
    NBLK = (2 * SO) // BLK + NEXP
    xsort = dscr("xsort", [NBLK * BLK, D], BF16)
    ysort = dscr("ysort", [NBLK * BLK, D], F32)
    h1b = dscr("h1b", [SO, D], BF16)
    lmat_in = din("lmat", [128, 128])
    thr_in = din("thr256", [128, 32])
    bthr_in = din("bthr", [128, NBLK])
    pidx_in = din("pidx", [128, 1])
    qs = dscr("qs", [16, 64, SO], BF16)
    ks = dscr("ks", [16, 64, SA], BF16)
    vs = dscr("vs", [SA, 1024], BF16)
    zs = dscr("zs", [SO, 1024], F32)
    xbcs = dscr("xbcs", [1536, SA], F32)
    dts = dscr("dts", [SA, 16], F32)
    mixT = dscr("mixT", [D, SO], BF16)
    h1s = dscr("h1s", [SO, D], F32)
    h1T = dscr("h1T", [D, SO], BF16)

    P = Prog(nc)
    A = Arena(P, 176 * 1024)
    pp = P.stack.enter_context(nc.psum_tensor("pp", [128, 4096], F32))

    def bank(b, parts=128, n=512, off=0):
        return pp[0:parts, b * 512 + off:b * 512 + off + n]

    def bankbf(b, parts=128, n=1024, off=0):
        return pp[0:parts, b * 512:(b + 1) * 512].bitcast(BF16)[:, off:off + n]

    ident_f = P.sb([128, 128], F32, "ident_f")
    ident_b = P.sb([128, 128], BF16, "ident_b")
    Wall = P.sb([128, (SO // 128) * 32], F32, "Wall")
    epsln = P.sb([128, 1], F32, "epsln")
    epsrms = P.sb([128, 1], F32, "epsrms")
    P.dma("sp", ident_f[:], ident_in, writes=["ident_f"])
    P.dma("pool", ident_b[:], ident_in, writes=["ident_b"])
    P.op("dve", lambda e: e.memset(epsln[:], LN_EPS), writes=["eps"])
    P.op("dve", lambda e: e.memset(epsrms[:], RMS_EPS), writes=["eps2"])
    onec = P.sb([128, 1], F32, "onec")
    P.op("dve", lambda e: e.memset(onec[:], 1.0), writes=["onec"])

    n_setup = len(P.ops)

    def phase_begin(k):
        if start == k and k > 0:
            del P.ops[n_setup:]
            P.barrier_at.clear()
            import os
            if os.environ.get("KMAXOPS"):
                P.maxops = n_setup + int(os.environ["KMAXOPS"])

    evac_rr = [0]

    def evac(out_ap, in_ap, reads, writes, scale=None, accum=False):
        evac_rr[0] ^= 1
        if evac_rr[0]:
            if scale is None:
                P.op("act", lambda e: e.activation(out=out_ap, in_=in_ap, func=AF.Copy),
                     reads, writes, accum=accum)
            else:
                P.op("act", lambda e: e.activation(out=out_ap, in_=in_ap, func=AF.Copy,
                                                   scale=scale), reads, writes, accum=accum)
        else:
            if scale is None:
                P.op("dve", lambda e: e.tensor_copy(out=out_ap, in_=in_ap), reads, writes, accum=accum)
            else:
                P.op("dve", lambda e: e.tensor_scalar(out=out_ap, in0=in_ap, scalar1=scale,
                                                      scalar2=None, op0=ALU.mult),
                     reads, writes, accum=accum)

    cast_rr = [0]
    pend_st = []

    def convert(src_ap, dst_ap, free_shape, tag, dst3d=False):
        b = cast_rr[0] % 3
        cast_rr[0] += 1
        st = stg[b]
        cv = cvt[b]
        nel = int(np.prod(free_shape))
        sv = st[:, 0:nel]
        if len(free_shape) == 2:
            sv = sv.rearrange("p (a b) -> p a b", a=free_shape[0])
        P.dma("sp", sv, src_ap, reads=[], writes=[("stg", b)])
        eng = ("dve", "act")[(cast_rr[0] - 1) % 2]
        if eng == "act":
            P.op("act", lambda e: e.activation(out=cv[:, 0:nel], in_=st[:, 0:nel], func=AF.Copy),
                 reads=[("stg", b)], writes=[("cvt", b)])
        else:
            P.op(eng, lambda e: e.tensor_copy(out=cv[:, 0:nel], in_=st[:, 0:nel]),
                 reads=[("stg", b)], writes=[("cvt", b)])
        cvv = cv[:, 0:nel]
        if dst3d:
            cvv = cvv.rearrange("p (a b) -> p a b", a=free_shape[0])
        pend_st.append(lambda: P.dma("sp", dst_ap, cvv, reads=[("cvt", b)], writes=[tag], accum=True))
        if len(pend_st) > 2:
            pend_st.pop(0)()

    stg = [A.alloc([4096], F32) for _ in range(3)]
    cvt = [A.alloc([4096], BF16) for _ in range(3)]
    for gi, (kind, c0, gc) in enumerate(groups):
        nel = 16 * gc
        dst = win_b[goff[gi]:goff[gi] + 128 * nel].rearrange("(p f) -> p f", p=128)
        if nel <= 4096:
            convert(w_in[:, c0:c0 + gc].rearrange("(kc p) c -> p kc c", p=128), dst,
                    [16, gc], "win_b")
        else:
            for half in range(2):
                convert(w_in[half * 1024:(half + 1) * 1024, c0:c0 + gc]
                        .rearrange("(kc p) c -> p kc c", p=128),
                        dst[:, half * 8 * gc:(half + 1) * 8 * gc], [8, gc], "win_b")
    for kc2 in range(8):
        convert(w_out[kc2 * 256:(kc2 + 1) * 256, :].rearrange("(kc p) c -> p kc c", p=128),
                wout_b[:, kc2 * 2 * D:(kc2 + 1) * 2 * D], [2, D], "wout_b")
    for e_ in range(NEXP):
        for q in range(4):
            convert(w_gate[e_, :, q * 256:(q + 1) * 256].rearrange("(kc p) c -> p kc c", p=128),
                    wg_b[e_ * 128:(e_ + 1) * 128, :].rearrange("p (kc c) -> p kc c", kc=16)[:, :, q * 256:(q + 1) * 256],
                    [16, 256], "wg_b", dst3d=True)
            convert(w_up[e_, :, q * 256:(q + 1) * 256].rearrange("(kc p) c -> p kc c", p=128),
                    wu_b[e_ * 128:(e_ + 1) * 128, :].rearrange("p (kc c) -> p kc c", kc=16)[:, :, q * 256:(q + 1) * 256],
                    [16, 256], "wu_b", dst3d=True)
            convert(w_down[e_, q * 256:(q + 1) * 256, :].rearrange("(kc p) c -> p kc c", p=128),
                    wd_b[e_ * 128:(e_ + 1) * 128, q * 2 * D:(q + 1) * 2 * D], [2, D], "wd_b")
    while pend_st:
        pend_st.pop(0)()
    if stop == 0:
        P.emit(final_wait_keys=["win_b","wout_b","wg_b","wu_b","wd_b"])
        P.close()
        return nc
    P.barrier()
    A.reset()

    phase_begin(1)
    xTb = [A.alloc([16, 512], BF16) for _ in range(2)]
    wbuf = [A.alloc([16 * 512], BF16) for _ in range(3)]
    ebuf = [A.alloc([512], F32) for _ in range(4)]
    NT = SA // 512
    pend_c = []
    wrr = 0
    err = 0
    prr = 0
    for tt in range(NT):
        own = tt * 512 >= SC
        t0 = tt * 512
        to = t0 - SC
        xb = xTb[tt % 2]
        xk = ("xTb", tt % 2)
        P.dma("pool", xb, xT[:, t0:t0 + 512].rearrange("(kc p) t -> p kc t", p=128),
              writes=[xk])
        for gi, (kind, c0, gc) in enumerate(groups):
            if kind in ("q", "z") and not own:
                continue
            wb = wbuf[wrr % 3]
            wk = ("wbuf", wrr % 3)
            wrr += 1
            wv = wb[:, 0:16 * gc].rearrange("p (kc c) -> p kc c", kc=16)
            P.dma("sp", wb[:, 0:16 * gc],
                  win_b[goff[gi]:goff[gi] + 128 * 16 * gc].rearrange("(p f) -> p f", p=128),
                  reads=["win_b"], writes=[wk])
            if len(pend_c) >= 2:
                P.ops.extend(pend_c.pop(0))
            mark_c = len(P.ops)
            if kind in ("q", "k"):
                h = (c0 % 1024) // 128
                b = prr % 8
                prr += 1
                for kc in range(16):
                    P.op("pe", lambda e, b=b, kc=kc, wv=wv, xb=xb: e.matmul(
                        bank(b), lhsT=wv[:, kc, :], rhs=xb[:, kc, :],
                        start=(kc == 0), stop=(kc == 15)),
                        reads=[wk, xk], writes=[("ps", b)])
                eb = ebuf[err % 4]
                ek = ("ebuf", err % 4)
                err += 1
                ebv = eb.bitcast(BF16)[:, 0:512]
                evac(ebv, bank(b), [("ps", b)], [ek], scale=(0.125 if kind == "q" else None))
                if kind == "q":
                    P.dma("sp", qs[h * 2:h * 2 + 2, :, to:to + 512].rearrange("m d t -> (m d) t"), ebv,
                          reads=[ek], writes=["qs"], accum=True)
                else:
                    P.dma("sp", ks[h * 2:h * 2 + 2, :, t0:t0 + 512].rearrange("m d t -> (m d) t"), ebv,
                          reads=[ek], writes=["ks"], accum=True)
            elif kind == "x":
                g = (c0 - 4096) // 128
                b = prr % 8
                prr += 1
                for kc in range(16):
                    P.op("pe", lambda e, b=b, kc=kc, wv=wv, xb=xb: e.matmul(
                        bank(b), lhsT=wv[:, kc, :], rhs=xb[:, kc, :],
                        start=(kc == 0), stop=(kc == 15)),
                        reads=[wk, xk], writes=[("ps", b)])
                eb = ebuf[err % 4]
                ek = ("ebuf", err % 4)
                err += 1
                evac(eb, bank(b), [("ps", b)], [ek])
                P.dma("sp", xbcs[g * 128:(g + 1) * 128, t0:t0 + 512], eb, reads=[ek],
                      writes=["xbcs"], accum=True)
            else:
                for ts in range(4):
                    b = prr % 8
                    prr += 1
                    for kc in range(16):
                        P.op("pe", lambda e, b=b, kc=kc, wv=wv, xb=xb, ts=ts, gc=gc: e.matmul(
                            bank(b, 128, gc), lhsT=xb[:, kc, ts * 128:(ts + 1) * 128],
                            rhs=wv[:, kc, :], start=(kc == 0), stop=(kc == 15)),
                            reads=[wk, xk], writes=[("ps", b)])
                    eb = ebuf[err % 4]
                    ek = ("ebuf", err % 4)
                    err += 1
                    if kind == "v":
                        ebv = eb.bitcast(BF16)[:, 0:512]
                        evac(ebv, bank(b), [("ps", b)], [ek])
                        P.dma("sp", vs[t0 + ts * 128:t0 + (ts + 1) * 128, c0 - 2048:c0 - 2048 + 512],
                              ebv, reads=[ek], writes=["vs"], accum=True)
                    elif kind == "z":
                        evac(eb, bank(b), [("ps", b)], [ek])
                        P.dma("sp", zs[to + ts * 128:to + (ts + 1) * 128, c0 - 3072:c0 - 3072 + 512],
                              eb, reads=[ek], writes=["zs"], accum=True)
                    else:
                        evac(eb[:, 0:16], bank(b, 128, 16), [("ps", b)], [ek])
                        P.dma("sp", dts[t0 + ts * 128:t0 + (ts + 1) * 128, :], eb[:, 0:16],
                              reads=[ek], writes=["dts"], accum=True)
            pend_c.append(P.ops[mark_c:])
            del P.ops[mark_c:]
    while pend_c:
        P.ops.extend(pend_c.pop(0))
    if stop == 1:
        P.emit(final_wait_keys=["qs","ks","vs","zs","xbcs","dts"])
        P.close()
        return nc
    P.barrier()
    A.reset()

    phase_begin(2)
    KA = [[A.alloc([SA], BF16, parts=68) for m in range(2)] for _ in range(2)]
    QA = [[A.alloc([SO], BF16, parts=68) for m in range(2)] for _ in range(2)]
    VA = [A.alloc([NB, 130], BF16) for _ in range(2)]
    PT = [A.alloc([512], BF16) for _ in range(3)]
    TT = A.alloc([8, 896], BF16)
    lamt = A.alloc([256], F32)
    lsc = A.alloc([64], F32)
    lam4 = A.alloc([8], F32)
    nwb = A.alloc([128], F32)
    ep_a = [A.alloc([128], F32) for _ in range(2)]
    ep_o = [A.alloc([128], F32) for _ in range(2)]
    ep_j = A.alloc([128], F32)
    ep_s = [A.alloc([8], F32) for _ in range(2)]
    ob = [A.alloc([128], BF16) for _ in range(2)]
    mo = [A.alloc([512], BF16) for _ in range(2)]

    P.dma("pool", TT, ttab.rearrange("p (h c) -> p h c", h=8), writes=["TT"])
    P.dma("sp", lamt, lamv.partition_broadcast(128), writes=["lamt"])
    P.dma("sp", nwb, anw.partition_broadcast(128), writes=["nwb"])
    P.op("dve", lambda e: e.tensor_scalar(out=nwb, in0=nwb, scalar1=(1.0 - LAMBDA_INIT),
                                          scalar2=None, op0=ALU.mult), reads=["nwb"], writes=["nwb"])
    for i in range(2):
        P.op("dve", lambda e, i=i: e.tensor_tensor(out=lsc, in0=lamt[:, i * 128:i * 128 + 64],
                                                  in1=lamt[:, i * 128 + 64:i * 128 + 128],
                                                  op=ALU.mult), reads=["lamt"], writes=["lsc"])
        P.op("dve", lambda e, i=i: e.tensor_reduce(out=lam4[:, i:i + 1], in_=lsc, axis=AX.X,
                                                  op=ALU.add), reads=["lsc"], writes=["lam4"])
    P.op("act", lambda e: e.activation(out=lam4[:, 4:6], in_=lam4[:, 0:2], func=AF.Exp),
         reads=["lam4"], writes=["lam4"])
    P.op("dve", lambda e: e.scalar_tensor_tensor(out=lam4[:, 2:3], in0=lam4[:, 5:6],
                                                 scalar=-LAMBDA_INIT, in1=lam4[:, 4:5],
                                                 op0=ALU.add, op1=ALU.subtract),
         reads=["lam4"], writes=["neglam"])
    neglam = lam4[:, 2:3]
    for vb in range(2):
        P.op("dve", lambda e, vb=vb: e.memset(VA[vb][:, :, 128:130], 1.0), writes=[("VA", vb)])

    NQT = SO // 512
    strr = 0
    ptrr = 0
    eprr = 0
    for h in range(8):
        hb = h % 2
        kkey = ("KA", hb)
        qkey = ("QA", hb)
        vkey = ("VA", hb)
        for m in range(2):
            P.dma("sp", KA[hb][m][0:64, :], ks[h * 2 + m], reads=["ks"], writes=[kkey], accum=(m > 0))
            for c0_ in range(0, SA, 2048):
                c1_ = min(SA, c0_ + 2048)
                P.dma("pool", KA[hb][m][64:68, c0_:c1_], ktab[h, :, c0_:c1_], writes=[kkey], accum=True)
            P.dma("sp", QA[hb][m][0:64, :], qs[h * 2 + m], reads=["qs"], writes=[qkey], accum=(m > 0))
            for c0_ in range(0, SO, 2048):
                c1_ = min(SO, c0_ + 2048)
                P.dma("pool", QA[hb][m][64:68, c0_:c1_], qtab[h, :, c0_:c1_], writes=[qkey], accum=True)
        for j0 in range(0, NB, 16):
            j1 = min(NB, j0 + 16)
            P.dma("sp", VA[hb][:, j0:j1, 0:128],
                  vs[j0 * 128:j1 * 128, h * 128:(h + 1) * 128].rearrange("(j p) d -> p j d", p=128),
                  reads=["vs"], writes=[vkey], accum=(j0 > 0))
        for qt in range(NQT):
            gb = (SC + qt * 512) // 128
            okeys = [[("O", m, i) for i in range(4)] for m in range(2)]
            its = [(j, m) for j in range(gb + 4) for m in range(2)]
            slots = []
            for (j, m) in its:
                slots.append((strr % 3, ptrr % 3))
                strr += 1
                ptrr += 1

            def emit_qk(idx, its=its, slots=slots, gb=gb, qt=qt, hb=hb, h=h, kkey=kkey, qkey=qkey):
                j, m = its[idx]
                sb_, pb = slots[idx]
                r = j - gb
                P.op("pe", lambda e, sb_=sb_, m=m, j=j, qt=qt, hb=hb, r=r: e.matmul(
                    bank(sb_), lhsT=KA[hb][m][:, j * 128:(j + 1) * 128],
                    rhs=QA[hb][m][:, qt * 512:(qt + 1) * 512], start=True, stop=(r < 0)),
                    reads=[kkey, qkey], writes=[("ps", sb_)])
                if r >= 0:
                    P.op("pe", lambda e, sb_=sb_, h=h, r=r: e.matmul(
                        bank(sb_), lhsT=ident_b[:], rhs=TT[:, h, (3 - r) * 128:(3 - r) * 128 + 512],
                        start=False, stop=True),
                        reads=["ident_b", "TT"], writes=[("ps", sb_)])

            def emit_rest(idx, its=its, slots=slots, gb=gb, hb=hb, vkey=vkey, okeys=okeys):
                j, m = its[idx]
                sb_, pb = slots[idx]
                r = j - gb
                P.op("act", lambda e, sb_=sb_, pb=pb: e.activation(
                    out=PT[pb], in_=bank(sb_), func=AF.Exp),
                    reads=[("ps", sb_)], writes=[("PT", pb)])
                for i in range(4):
                    if r >= 0 and i < r:
                        continue
                    ob_ = 4 + m * 2 + i // 2
                    P.op("pe", lambda e, pb=pb, i=i, j=j, hb=hb, ob_=ob_, gb=gb: e.matmul(
                        bank(ob_, 128, 129, (i % 2) * 256), lhsT=PT[pb][:, i * 128:(i + 1) * 128],
                        rhs=VA[hb][:, j, 0:129], start=(j == 0 and i % 2 == 0), stop=(j == gb + i)),
                        reads=[("PT", pb), vkey], writes=[okeys[m][i]])

            LA = 2
            for idx in range(len(its) + LA):
                if idx < len(its):
                    emit_qk(idx)
                if idx - LA >= 0:
                    emit_rest(idx - LA)
            mb = (h * NQT + qt) % 2
            for i in range(4):
                eb_ = eprr % 2
                O0 = bank(4 + i // 2, 128, 129, (i % 2) * 256)
                O1 = bank(6 + i // 2, 128, 129, (i % 2) * 256)
                es = ep_s[eb_]
                ek = ("ep", eb_)
                P.op("dve", lambda e, es=es, O0=O0: e.reciprocal(out=es[:, 0:1], in_=O0[:, 128:129]),
                     reads=[okeys[0][i]], writes=[ek])
                P.op("dve", lambda e, es=es, O1=O1: e.reciprocal(out=es[:, 1:2], in_=O1[:, 128:129]),
                     reads=[okeys[1][i]], writes=[ek])
                P.op("dve", lambda e, es=es: e.tensor_tensor(out=es[:, 2:3], in0=es[:, 1:2], in1=neglam,
                                                             op=ALU.mult),
                     reads=[ek, "neglam"], writes=[ek])
                P.op("dve", lambda e, es=es, O0=O0, eb_=eb_: e.tensor_scalar(
                    out=ep_a[eb_], in0=O0[:, 0:128], scalar1=es[:, 0:1], scalar2=None, op0=ALU.mult),
                    reads=[ek, okeys[0][i]], writes=[("epa", eb_)])
                P.op("dve", lambda e, es=es, O1=O1, eb_=eb_: e.scalar_tensor_tensor(
                    out=ep_o[eb_], in0=O1[:, 0:128], scalar=es[:, 2:3], in1=ep_a[eb_],
                    op0=ALU.mult, op1=ALU.add),
                    reads=[ek, okeys[1][i], ("epa", eb_)], writes=[("epo", eb_)])
                P.op("act", lambda e, es=es, eb_=eb_: e.activation(
                    out=ep_j, in_=ep_o[eb_], func=AF.Square, accum_out=es[:, 3:4]),
                    reads=[("epo", eb_)], writes=[("ep2", eb_), "epj"])
                P.op("act", lambda e, es=es: e.activation(
                    out=es[:, 4:5], in_=es[:, 3:4], func=AF.Sqrt, bias=epsrms[:], scale=1.0 / 128),
                    reads=[("ep2", eb_), "eps2"], writes=[("ep3", eb_)])
                P.op("dve", lambda e, es=es: e.reciprocal(out=es[:, 5:6], in_=es[:, 4:5]),
                     reads=[("ep3", eb_)], writes=[("ep4", eb_)])
                P.op("dve", lambda e, es=es, eb_=eb_: e.scalar_tensor_tensor(
                    out=ob[eb_], in0=ep_o[eb_], scalar=es[:, 5:6], in1=nwb, op0=ALU.mult, op1=ALU.mult),
                    reads=[("ep4", eb_), ("epo", eb_), "nwb"], writes=[("ob", eb_)])
                P.op("pe", lambda e, eb_=eb_, i=i: e.transpose(
                    bankbf(3, 128, 128, i * 128), ob[eb_], ident_b[:]),
                    reads=[("ob", eb_), "ident_b"], writes=[("ps", 3, i)])
                evac(mo[mb][:, i * 128:(i + 1) * 128], bankbf(3, 128, 128, i * 128),
                     [("ps", 3, i)], [("mo", mb)], accum=(i > 0))
                eprr += 1
            P.dma("sp", mixT[h * 128:(h + 1) * 128, qt * 512:(qt + 1) * 512], mo[mb],
                  reads=[("mo", mb)], writes=["mixT"], accum=True)
    if stop == 2:
        P.emit(final_wait_keys=["mixT"])
        P.close()
        return nc
    P.barrier()
    A.reset()

    phase_begin(3)
    NCH = SA // 64
    cwt = A.alloc([12, 4], F32)
    cbt = A.alloc([12], F32)
    tri = A.alloc([64], F32, parts=64)
    umat = A.alloc([64], F32, parts=64)
    ones64 = A.alloc([128], F32, parts=64)
    flagt = A.alloc([1], F32)
    dtbt = A.alloc([16], F32, parts=64)
    Aneg = A.alloc([16], F32, parts=64)
    Dt = A.alloc([16], F32, parts=64)
    snwt = A.alloc([1024], F32, parts=64)
    dtall = A.alloc([NCH, 16], F32, parts=64)
    aall = A.alloc([NCH, 16], F32, parts=64)
    xc = [A.alloc([12, 515], F32) for _ in range(1)]
    acc = A.alloc([512], F32)
    xcv = A.alloc([8, 512], F32)
    bct = A.alloc([4, 512], BF16)
    Rt = A.alloc([16, 64], F32, parts=64)
    Et = A.alloc([16, 64], F32, parts=64)
    Ebt = A.alloc([16, 64], F32)
    mcb = A.alloc([2, 64], F32, parts=64)
    Mt = A.alloc([16, 64], BF16, parts=64)
    Mtmp = A.alloc([16, 64], F32, parts=64)
    x32 = A.alloc([1024], F32, parts=64)
    xbf = A.alloc([1024], BF16, parts=64)
    wsc = A.alloc([16], F32, parts=64)
    xw = A.alloc([1024], BF16, parts=64)
    Btok = A.alloc([2, 128], BF16, parts=64)
    Cs = A.alloc([16, 64], BF16)
    Hs = A.alloc([1024], F32)
    Hbf = A.alloc([1024], BF16)
    yt = A.alloc([1024], F32, parts=64)
    zt = A.alloc([1024], F32, parts=64)
    ysq = A.alloc([1024], F32, parts=64)
    ss = A.alloc([8], F32, parts=64)
    ybf = A.alloc([1024], BF16, parts=64)
    ymo = A.alloc([8, 64], BF16)

    P.dma("sp", cwt, cw.rearrange("p (g k) -> p g k", g=12), writes=["cwt"])
    P.dma("sp", cbt, cb, writes=["cbt"])
    P.dma("sp", tri, tri_in, writes=["tri"])
    P.dma("sp", umat, umat_in, writes=["umat"])
    P.dma("sp", flagt, flag_in, writes=["flagt"])
    P.dma("sp", dtbt, dtb.partition_broadcast(64), writes=["dtbt"])
    P.dma("sp", Aneg, alog.partition_broadcast(64), writes=["Aneg"])
    P.dma("sp", Dt, dsk.partition_broadcast(64), writes=["Dt"])
    P.dma("sp", snwt, snw.partition_broadcast(64), writes=["snwt"])
    for c0_ in range(0, NCH, 32):
        c1_ = min(NCH, c0_ + 32)
        P.dma("sp", dtall[:, c0_:c1_, :], dts[c0_ * 64:c1_ * 64, :].rearrange("(c l) h -> l c h", l=64),
              reads=["dts"], writes=["dtall"], accum=(c0_ > 0))
    P.op("dve", lambda e: e.memset(ones64, 1.0), writes=["ones64"])
    P.op("dve", lambda e: e.memset(Hs, 0.0), writes=["Hs"])
    P.op("dve", lambda e: e.memset(Hbf, 0.0), writes=["Hbf"])
    P.op("act", lambda e: e.activation(out=Aneg, in_=Aneg, func=AF.Exp), reads=["Aneg"], writes=["Aneg"])
    P.op("dve", lambda e: e.tensor_scalar(out=Aneg, in0=Aneg, scalar1=-1.0, scalar2=None, op0=ALU.mult),
         reads=["Aneg"], writes=["Aneg"])
    P.op("dve", lambda e: e.tensor_tensor(out=dtall, in0=dtall,
                                          in1=dtbt.unsqueeze(1).to_broadcast([64, NCH, 16]), op=ALU.add),
         reads=["dtall", "dtbt"], writes=["dtall"])
    P.op("act", lambda e: e.activation(out=dtall, in_=dtall, func=AF.Exp), reads=["dtall"], writes=["dtall"])
    P.op("act", lambda e: e.activation(out=dtall, in_=dtall, func=AF.Ln, bias=onec[0:64, :], scale=1.0),
         reads=["dtall", "onec"], writes=["dtall"])
    P.op("dve", lambda e: e.tensor_tensor(out=aall, in0=dtall,
                                          in1=Aneg.unsqueeze(1).to_broadcast([64, NCH, 16]), op=ALU.mult),
         reads=["dtall", "Aneg"], writes=["aall"])

    for blk in range(SA // 512):
        t0 = blk * 512
        xcb = xc[0]
        if t0 == 0:
            P.op("dve", lambda e: e.memset(xcb[:, :, 0:3], 0.0), writes=["xc"])
            P.dma("sp", xcb[:, :, 3:515], xbcs[:, 0:512].rearrange("(g p) t -> p g t", p=128),
                  reads=["xbcs"], writes=["xc"])
        else:
            P.dma("sp", xcb, xbcs[:, t0 - 3:t0 + 512].rearrange("(g p) t -> p g t", p=128),
                  reads=["xbcs"], writes=["xc"])
        for g in range(12):
            P.op("dve", lambda e, g=g: e.tensor_scalar(out=acc, in0=xcb[:, g, 3:515],
                                                      scalar1=cwt[:, g, 3:4], scalar2=None, op0=ALU.mult),
                 reads=["xc", "cwt"], writes=["acc"])
            for k in range(3):
                P.op("dve", lambda e, g=g, k=k: e.scalar_tensor_tensor(
                    out=acc, in0=xcb[:, g, k:k + 512], scalar=cwt[:, g, k:k + 1], in1=acc,
                    op0=ALU.mult, op1=ALU.add), reads=["xc", "cwt", "acc"], writes=["acc"])
            if g < 8:
                P.op("act", lambda e, g=g: e.activation(out=xcv[:, g, :], in_=acc, func=AF.Silu,
                                                       bias=cbt[:, g:g + 1], scale=1.0),
                     reads=["acc", "cbt"], writes=[("xcv", g)])
            else:
                P.op("act", lambda e, g=g: e.activation(out=bct[:, g - 8, :], in_=acc, func=AF.Silu,
                                                       bias=cbt[:, g:g + 1], scale=1.0),
                     reads=["acc", "cbt"], writes=[("bct", g - 8)])
        for cl in range(8):
            c = blk * 8 + cl
            own = c * 64 >= SC
            csl = slice(cl * 64, (cl + 1) * 64)
            a_c = aall[:, c, :]
            dt_c = dtall[:, c, :]
            for g in range(8):
                P.op("pe", lambda e, g=g, csl=csl: e.transpose(
                    bank(g // 4, 64, 128, (g % 4) * 128), xcv[:, g, csl], ident_f[:]),
                    reads=[("xcv", g), "ident_f"], writes=[("ps", g // 4)])
            for hb_ in range(2):
                P.op("act", lambda e, hb_=hb_: e.activation(
                    out=x32[:, hb_ * 512:(hb_ + 1) * 512], in_=bank(hb_, 64), func=AF.Copy),
                    reads=[("ps", hb_)], writes=[("x32", hb_)])
                P.op("act", lambda e, hb_=hb_: e.activation(
                    out=xbf[:, hb_ * 512:(hb_ + 1) * 512], in_=bank(hb_, 64), func=AF.Copy),
                    reads=[("ps", hb_)], writes=[("xbf", hb_)])
            for g2 in range(2):
                P.op("pe", lambda e, g2=g2, csl=csl: e.transpose(
                    bankbf(7, 64, 128, 512 + g2 * 128), bct[:, g2, csl], ident_b[:]),
                    reads=[("bct", g2), "ident_b"], writes=[("ps", 7, "b")])
            P.op("dve", lambda e: e.tensor_copy(out=Btok.rearrange("p a b -> p (a b)"),
                                                in_=bankbf(7, 64, 256, 512)),
                 reads=[("ps", 7, "b")], writes=["Btok"])
            P.op("dve", lambda e, a_c=a_c: e.tensor_tensor(
                out=Rt, in0=tri.unsqueeze(1).to_broadcast([64, 16, 64]),
                in1=a_c.unsqueeze(2).to_broadcast([64, 16, 64]), op=ALU.mult),
                reads=["tri", "aall"], writes=["Rt"])
            Rf = Rt.rearrange("p a b -> p (a b)")
            for hb_ in range(2):
                P.op("pe", lambda e, hb_=hb_: e.matmul(
                    bank(2 + hb_, 64), lhsT=umat, rhs=Rf[:, hb_ * 512:(hb_ + 1) * 512],
                    start=True, stop=True), reads=["umat", "Rt"], writes=[("ps", 2 + hb_)])
                P.op("pe", lambda e, hb_=hb_: e.matmul(
                    bank(4 + hb_), lhsT=ones64, rhs=Rf[:, hb_ * 512:(hb_ + 1) * 512],
                    start=True, stop=True), reads=["ones64", "Rt"], writes=[("ps", 4 + hb_)])
            Ef = Et.rearrange("p a b -> p (a b)")
            Ebf = Ebt.rearrange("p a b -> p (a b)")
            for hb_ in range(2):
                P.op("act", lambda e, hb_=hb_: e.activation(
                    out=Ef[:, hb_ * 512:(hb_ + 1) * 512], in_=bank(2 + hb_, 64), func=AF.Exp),
                    reads=[("ps", 2 + hb_)], writes=[("Et", hb_)])
                P.op("act", lambda e, hb_=hb_: e.activation(
                    out=Ebf[:, hb_ * 512:(hb_ + 1) * 512], in_=bank(4 + hb_), func=AF.Exp),
                    reads=[("ps", 4 + hb_)], writes=[("Ebt", hb_)])
            ekeys = [("Et", 0), ("Et", 1)]
            ebkeys = [("Ebt", 0), ("Ebt", 1)]
            P.op("dve", lambda e, dt_c=dt_c: e.tensor_tensor(out=wsc, in0=dt_c, in1=Et[:, :, 63],
                                                            op=ALU.mult),
                 reads=ekeys + ["dtall"], writes=["wsc"])
            P.op("dve", lambda e: e.tensor_tensor(
                out=xw.rearrange("p (h d) -> p h d", h=16),
                in0=x32.rearrange("p (h d) -> p h d", h=16),
                in1=wsc.unsqueeze(2).to_broadcast([64, 16, 64]), op=ALU.mult),
                reads=["wsc", ("x32", 0), ("x32", 1)], writes=["xw"])
            if own:
                if c * 64 == SC:
                    P.op("dve", lambda e: e.tensor_scalar(out=Hs, in0=Hs, scalar1=flagt[:, 0:1],
                                                          scalar2=None, op0=ALU.mult),
                         reads=["Hs", "flagt"], writes=["Hs"])
                    P.op("dve", lambda e: e.tensor_copy(out=Hbf, in_=Hs), reads=["Hs"], writes=["Hbf"])
                for g2 in range(2):
                    P.op("pe", lambda e, g2=g2, csl=csl: e.matmul(
                        bank(6, 64, 64, g2 * 64), lhsT=bct[:, g2, csl], rhs=bct[:, 2 + g2, csl],
                        start=True, stop=True), reads=[("bct", g2), ("bct", 2 + g2)],
                        writes=[("ps", 6, "cb")])
                P.op("dve", lambda e: e.tensor_tensor(
                    out=mcb, in0=bank(6, 64, 128).rearrange("p (g l) -> p g l", g=2),
                    in1=tri.unsqueeze(1).to_broadcast([64, 2, 64]), op=ALU.mult),
                    reads=[("ps", 6, "cb"), "tri"], writes=["mcb"])
                for g2 in range(2):
                    P.op("dve", lambda e, g2=g2: e.tensor_tensor(
                        out=Mtmp[:, g2 * 8:(g2 + 1) * 8, :], in0=Et[:, g2 * 8:(g2 + 1) * 8, :],
                        in1=mcb[:, g2:g2 + 1, :].to_broadcast([64, 8, 64]), op=ALU.mult),
                        reads=ekeys + ["mcb"], writes=[("Mtmp", g2)])
                P.op("dve", lambda e, dt_c=dt_c: e.tensor_tensor(
                    out=Mt, in0=Mtmp, in1=dt_c.unsqueeze(2).to_broadcast([64, 16, 64]), op=ALU.mult),
                    reads=[("Mtmp", 0), ("Mtmp", 1), "dtall"], writes=["Mt"])
                for g2 in range(2):
                    P.op("dve", lambda e, g2=g2, csl=csl: e.tensor_tensor(
                        out=Cs[:, g2 * 8:(g2 + 1) * 8, :], in0=Ebt[:, g2 * 8:(g2 + 1) * 8, :],
                        in1=bct[:, 2 + g2, csl].unsqueeze(1).to_broadcast([128, 8, 64]), op=ALU.mult),
                        reads=ebkeys + [("bct", 2 + g2)], writes=[("Cs", g2)])
                for hh in range(16):
                    yb = bank(hh // 8, 64, 64, (hh % 8) * 64)
                    P.op("pe", lambda e, hh=hh, yb=yb: e.matmul(
                        yb, lhsT=Mt[:, hh, :], rhs=xbf[:, hh * 64:(hh + 1) * 64], start=True, stop=False),
                        reads=["Mt", ("xbf", hh // 8), ("x32", hh // 8)], writes=[("ps", hh // 8)])
                    P.op("pe", lambda e, hh=hh, yb=yb: e.matmul(
                        yb, lhsT=Cs[:, hh, :], rhs=Hbf[:, hh * 64:(hh + 1) * 64], start=False, stop=True),
                        reads=[("Cs", hh // 8), "Hbf"], writes=[("ps", hh // 8)])
            for g2 in range(2):
                P.op("pe", lambda e, g2=g2: e.matmul(
                    bank(2 + g2), lhsT=Btok[:, g2, :], rhs=xw[:, g2 * 512:(g2 + 1) * 512],
                    start=True, stop=True), reads=["Btok", "xw"] + ekeys, writes=[("ps", 2 + g2)])
            P.op("dve", lambda e: e.tensor_tensor(
                out=Hs.rearrange("p (h d) -> p h d", h=16), in0=Hs.rearrange("p (h d) -> p h d", h=16),
                in1=Ebt[:, :, 63:64].to_broadcast([128, 16, 64]), op=ALU.mult),
                reads=["Hs", "Hbf"] + ebkeys, writes=["Hs"])
            for g2 in range(2):
                P.op("dve", lambda e, g2=g2: e.tensor_tensor(
                    out=Hs[:, g2 * 512:(g2 + 1) * 512], in0=Hs[:, g2 * 512:(g2 + 1) * 512],
                    in1=bank(2 + g2), op=ALU.add), reads=["Hs", ("ps", 2 + g2)], writes=["Hs"])
            if own:
                to = c * 64 - SC
                P.dma("sp", zt, zs[to:to + 64, :], reads=["zs"], writes=["zt"])
                P.op("dve", lambda e: e.tensor_tensor(
                    out=yt.rearrange("p (h d) -> p h d", h=16), in0=x32.rearrange("p (h d) -> p h d", h=16),
                    in1=Dt.unsqueeze(2).to_broadcast([64, 16, 64]), op=ALU.mult),
                    reads=[("x32", 0), ("x32", 1), "Dt"], writes=["yt"])
                for hb_ in range(2):
                    P.op("dve", lambda e, hb_=hb_: e.tensor_tensor(
                        out=yt[:, hb_ * 512:(hb_ + 1) * 512], in0=yt[:, hb_ * 512:(hb_ + 1) * 512],
                        in1=bank(hb_, 64), op=ALU.add), reads=["yt", ("ps", hb_)], writes=["yt"])
                P.op("act", lambda e: e.activation(out=zt, in_=zt, func=AF.Silu), reads=["zt"], writes=["zt"])
                P.op("dve", lambda e: e.tensor_tensor(out=yt, in0=yt, in1=zt, op=ALU.mult),
                     reads=["yt", "zt"], writes=["yt"])
                for g2 in range(2):
                    P.op("act", lambda e, g2=g2: e.activation(
                        out=ysq[:, g2 * 512:(g2 + 1) * 512], in_=yt[:, g2 * 512:(g2 + 1) * 512],
                        func=AF.Square, accum_out=ss[:, g2:g2 + 1]), reads=["yt"], writes=["ysq", ("ss", g2)])
                P.op("act", lambda e: e.activation(out=ss[:, 2:4], in_=ss[:, 0:2], func=AF.Sqrt,
                                                   bias=epsrms[0:64, :], scale=1.0 / 512),
                     reads=[("ss", 0), ("ss", 1), "eps2"], writes=["ss2"])
                P.op("dve", lambda e: e.reciprocal(out=ss[:, 4:6], in_=ss[:, 2:4]), reads=["ss2"],
                     writes=["ss3"])
                for g2 in range(2):
                    P.op("dve", lambda e, g2=g2: e.scalar_tensor_tensor(
                        out=ybf[:, g2 * 512:(g2 + 1) * 512], in0=yt[:, g2 * 512:(g2 + 1) * 512],
                        scalar=ss[:, 4 + g2:5 + g2], in1=snwt[:, g2 * 512:(g2 + 1) * 512],
                        op0=ALU.mult, op1=ALU.mult), reads=["yt", "ss3", "snwt"], writes=["ybf"])
                for g in range(8):
                    P.op("pe", lambda e, g=g: e.transpose(
                        bankbf(7, 128, 64, g * 64), ybf[:, g * 128:(g + 1) * 128], ident_b[0:64, 0:64]),
                        reads=["ybf", "ident_b"], writes=[("ps", 7)])
                P.op("act", lambda e: e.activation(out=ymo.rearrange("p a b -> p (a b)"),
                                                   in_=bankbf(7, 128, 512, 0), func=AF.Copy),
                     reads=[("ps", 7)], writes=["ymo"])
                P.dma("sp", mixT[1024:2048, to:to + 64].rearrange("(g p) t -> p g t", p=128), ymo,
                      reads=["ymo"], writes=["mixT"], accum=True)
            P.op("act", lambda e: e.activation(out=Hbf, in_=Hs, func=AF.Copy), reads=["Hs"], writes=["Hbf"])
    if stop == 3:
        P.emit(final_wait_keys=["mixT"])
        P.close()
        return nc
    P.barrier()
    A.reset()

    phase_begin(4)
    wo = A.alloc([16, D], BF16)
    mxt = [A.alloc([16, 512], BF16) for _ in range(2)]
    xot = [A.alloc([D], F32) for _ in range(2)]
    rt = [A.alloc([D], F32) for _ in range(2)]
    g1 = A.alloc([D], F32)
    b1 = A.alloc([D], F32)
    st6 = A.alloc([4, 6], F32)
    mv = A.alloc([8], F32)
    hT32 = A.alloc([16, 128], F32)
    hTb = A.alloc([16, 128], BF16)
    hbt = [A.alloc([D], BF16) for _ in range(2)]
    wr32 = A.alloc([16, 36], F32)
    brt = A.alloc([36], F32)
    lg = A.alloc([36], F32)
    rs = A.alloc([16], F32)
    Gt = A.alloc([4], F32)
    elm = A.alloc([4, 8], F32)
    top8 = A.alloc([8], F32)
    msk = A.alloc([32], F32)
    ext = A.alloc([32], F32)
    P.dma("sp", wo.rearrange("p a b -> p (a b)"), wout_b, reads=["wout_b"], writes=["wo"])
    P.dma("sp", g1, ln1g.partition_broadcast(128), writes=["g1"])
    P.dma("sp", b1, ln1b.partition_broadcast(128), writes=["b1"])
    P.dma("sp", wr32, wr.rearrange("(kc p) c -> p kc c", p=128), writes=["wr32"])
    P.dma("sp", brt, br.partition_broadcast(128), writes=["brt"])

    def layernorm(src, dst, gk, bk, gt, bt, rkey, okey, tag, st6, mv):
        for cc in range(4):
            P.op("dve", lambda e, cc=cc: e.bn_stats(out=st6[:, cc, :], in_=src[:, cc * 512:(cc + 1) * 512]),
                 reads=[rkey], writes=[("st6", tag)])
        P.op("dve", lambda e: e.bn_aggr(out=mv[:, 0:2], in_=st6.rearrange("p a b -> p (a b)")),
             reads=[("st6", tag)], writes=[("mv", tag)])
        P.op("act", lambda e: e.activation(out=mv[:, 2:3], in_=mv[:, 1:2], func=AF.Sqrt,
                                           bias=epsln[:], scale=1.0),
             reads=[("mv", tag), "eps"], writes=[("mv2", tag)])
        P.op("dve", lambda e: e.reciprocal(out=mv[:, 3:4], in_=mv[:, 2:3]), reads=[("mv2", tag)],
             writes=[("mv3", tag)])
        P.op("dve", lambda e: e.tensor_scalar(out=src, in0=src, scalar1=mv[:, 0:1], scalar2=mv[:, 3:4],
                                              op0=ALU.subtract, op1=ALU.mult),
             reads=[rkey, ("mv3", tag), ("mv", tag)], writes=[rkey])
        P.op("dve", lambda e: e.tensor_tensor(out=src, in0=src, in1=gt, op=ALU.mult),
             reads=[rkey, gk], writes=[rkey])
        P.op("dve", lambda e: e.tensor_tensor(out=dst, in0=src, in1=bt, op=ALU.add),
             reads=[rkey, bk], writes=[okey])

    for t4 in range(SO // 512):
        mb = t4 % 2
        P.dma("sp", mxt[mb], mixT[:, t4 * 512:(t4 + 1) * 512].rearrange("(kc p) t -> p kc t", p=128),
              reads=["mixT"], writes=[("mxt", mb)])
        for ts in range(4):
            ti = t4 * 4 + ts
            tb = ti % 2
            tok0 = ti * 128
            P.dma("sp", xot[tb], xo[tok0:tok0 + 128, :], writes=[("xot", tb)])
            for cbk in range(4):
                for kc in range(16):
                    P.op("pe", lambda e, cbk=cbk, kc=kc, ts=ts, mb=mb: e.matmul(
                        bank(cbk), lhsT=mxt[mb][:, kc, ts * 128:(ts + 1) * 128],
                        rhs=wo[:, kc, cbk * 512:(cbk + 1) * 512], start=(kc == 0), stop=(kc == 15)),
                        reads=[("mxt", mb), "wo"], writes=[("ps", cbk)])
                P.op("dve", lambda e, cbk=cbk, tb=tb: e.scalar_tensor_tensor(
                    out=rt[tb][:, cbk * 512:(cbk + 1) * 512], in0=xot[tb][:, cbk * 512:(cbk + 1) * 512],
                    scalar=DN_ALPHA, in1=bank(cbk), op0=ALU.mult, op1=ALU.add),
                    reads=[("xot", tb), ("ps", cbk)], writes=[("rt", tb)])
            layernorm(rt[tb], rt[tb], "g1", "b1", g1, b1, ("rt", tb), ("rt", tb), "a", st6, mv)
            P.dma("sp", h1s[tok0:tok0 + 128, :], rt[tb], reads=[("rt", tb)], writes=["h1s"], accum=True)
            P.op("act", lambda e, tb=tb: e.activation(out=hbt[tb], in_=rt[tb], func=AF.Copy),
                 reads=[("rt", tb)], writes=[("hbt", tb)])
            P.dma("sp", h1b[tok0:tok0 + 128, :], hbt[tb], reads=[("hbt", tb)], writes=["h1b"], accum=True)
            for half in range(2):
                for k8 in range(8):
                    kc = half * 8 + k8
                    P.op("pe", lambda e, kc=kc, k8=k8, half=half, tb=tb: e.transpose(
                        bank(4 + half * 2 + k8 // 4, 128, 128, (k8 % 4) * 128),
                        rt[tb][:, kc * 128:(kc + 1) * 128], ident_f[:]),
                        reads=[("rt", tb), "ident_f"], writes=[("ps", 4 + half * 2 + k8 // 4)])
                for q2 in range(2):
                    bq = 4 + half * 2 + q2
                    kc0 = half * 8 + q2 * 4
                    P.op("act", lambda e, bq=bq, kc0=kc0: e.activation(
                        out=hT32[:, kc0:kc0 + 4, :].rearrange("p a b -> p (a b)"), in_=bank(bq), func=AF.Copy),
                        reads=[("ps", bq)], writes=[("hT32", kc0)])
            for kc in range(16):
                P.op("pe", lambda e, kc=kc: e.matmul(
                    bank(0, 128, 36), lhsT=hT32[:, kc, :], rhs=wr32[:, kc, :], start=(kc == 0), stop=(kc == 15)),
                    reads=[("hT32", (kc // 4) * 4), "wr32"], writes=[("ps", 0)])
            P.op("dve", lambda e: e.tensor_tensor(out=lg, in0=bank(0, 128, 36), in1=brt, op=ALU.add),
                 reads=[("ps", 0), "brt"], writes=["lg"])
            gl = lg[:, 0:4]
            el = lg[:, 4:36].rearrange("p (g e) -> p g e", g=4)
            P.op("dve", lambda e: e.tensor_reduce(out=rs[:, 0:1], in_=gl, axis=AX.X, op=ALU.max),
                 reads=["lg"], writes=["rs0"])
            P.op("dve", lambda e: e.tensor_scalar(out=rs[:, 1:2], in0=rs[:, 0:1], scalar1=-1.0, scalar2=None,
                                                  op0=ALU.mult), reads=["rs0"], writes=["rs1"])
            P.op("act", lambda e: e.activation(out=Gt, in_=gl, func=AF.Exp, bias=rs[:, 1:2], scale=1.0,
                                               accum_out=rs[:, 2:3]),
                 reads=["lg", "rs1"], writes=["Gt", "rs2"])
            P.op("dve", lambda e: e.reciprocal(out=rs[:, 3:4], in_=rs[:, 2:3]), reads=["rs2"], writes=["rs3"])
            P.op("dve", lambda e: e.tensor_scalar(out=Gt, in0=gl, scalar1=rs[:, 0:1], scalar2=None,
                                                  op0=ALU.is_ge), reads=["lg", "rs0", "Gt"], writes=["Gt"])
            P.op("dve", lambda e: e.tensor_scalar(out=Gt, in0=Gt, scalar1=-NEG, scalar2=NEG,
                                                  op0=ALU.mult, op1=ALU.add), reads=["Gt"], writes=["Gt"])
            P.op("dve", lambda e: e.tensor_tensor(out=elm, in0=el, in1=Gt.unsqueeze(2).to_broadcast([128, 4, 8]),
                                                  op=ALU.add), reads=["lg", "Gt"], writes=["elm"])
            elf = elm.rearrange("p a b -> p (a b)")
            P.op("dve", lambda e: e.max(out=top8, in_=elf), reads=["elm"], writes=["top8"])
            P.op("dve", lambda e: e.tensor_scalar(out=msk, in0=elf, scalar1=top8[:, 1:2], scalar2=None,
                                                  op0=ALU.is_ge), reads=["elm", "top8"], writes=["msk"])
            P.op("dve", lambda e: e.tensor_scalar(out=rs[:, 4:5], in0=top8[:, 0:1], scalar1=-1.0, scalar2=None,
                                                  op0=ALU.mult), reads=["top8"], writes=["rs4"])
            P.op("act", lambda e: e.activation(out=ext, in_=elf, func=AF.Exp, bias=rs[:, 4:5], scale=1.0),
                 reads=["elm", "rs4"], writes=["ext"])
            P.op("act", lambda e: e.activation(out=rs[:, 5:6], in_=top8[:, 1:2], func=AF.Exp, bias=rs[:, 4:5],
                                               scale=1.0), reads=["top8", "rs4"], writes=["rs5"])
            P.op("dve", lambda e: e.tensor_scalar(out=rs[:, 6:7], in0=rs[:, 5:6], scalar1=1.0, scalar2=None,
                                                  op0=ALU.add), reads=["rs5"], writes=["rs6"])
            P.op("dve", lambda e: e.reciprocal(out=rs[:, 7:8], in_=rs[:, 6:7]), reads=["rs6"], writes=["rs7"])
            P.op("dve", lambda e: e.tensor_tensor(out=rs[:, 8:9], in0=rs[:, 7:8], in1=rs[:, 3:4], op=ALU.mult),
                 reads=["rs7", "rs3"], writes=["rs8"])
            P.op("dve", lambda e, ti=ti: e.scalar_tensor_tensor(
                out=Wall[:, ti * 32:(ti + 1) * 32], in0=ext, scalar=rs[:, 8:9], in1=msk,
                op0=ALU.mult, op1=ALU.mult), reads=["ext", "rs8", "msk"], writes=["Wall"])
    if stop == 4:
        wall_out = nc.dram_tensor("Wall_out", [128, (SO // 128) * 32], F32, kind="ExternalOutput").ap()
        P.dma("sp", wall_out, Wall[:], reads=["Wall"], writes=["wall_out"])
        P.emit(final_wait_keys=["h1s","h1b","wall_out"])
        P.close()
        return nc
    P.barrier()
    A.reset()

    phase_begin(5)
    if start == 5:
        wall_in = nc.dram_tensor("Wall_in", [128, (SO // 128) * 32], F32, kind="ExternalInput").ap()
        P.dma("sp", Wall[:], wall_in, writes=["Wall"])
    I32 = mybir.dt.int32
    U32 = mybir.dt.uint32
    didx = P.sb([128, NTT * 2], I32, "didx")
    wk = P.sb([128, NTT * 2], F32, "wk")
    gidx = P.sb([128, NBLK], I32, "gidx")
    lmat_b = A.alloc([128], BF16)
    ones_b = A.alloc([128], BF16)
    sel = A.alloc([NTT, 32], BF16)
    carry = A.alloc([32], F32)
    rank = A.alloc([NTT, 32], F32)
    dest = A.alloc([NTT, 32], F32)
    dsel = A.alloc([NTT, 32], F32)
    t8 = A.alloc([NTT, 8], F32)
    thr = A.alloc([32], F32)
    cmpt = A.alloc([32, 32], F32)
    nbe = A.alloc([32], F32)
    padded = A.alloc([32], F32)
    pend = A.alloc([32], F32)
    pstart = A.alloc([32], F32)
    onesf = A.alloc([32], F32)
    bthr = A.alloc([NBLK], F32)
    cmpb = A.alloc([NBLK, 32], F32)
    ebt = A.alloc([NBLK], F32)
    pidx = A.alloc([1], F32)
    didf = A.alloc([NTT, 2], F32)
    junk = A.alloc([32], F32)
    Wv = Wall[:].rearrange("p (t e) -> p t e", e=32)
    P.dma("pool", lmat_b, lmat_in, writes=["lmat_b"])
    P.dma("sp", thr, thr_in, writes=["thr"])
    P.dma("sp", bthr, bthr_in, writes=["bthr"])
    P.dma("sp", pidx, pidx_in, writes=["pidx"])
    P.op("dve", lambda e: e.memset(ones_b, 1.0), writes=["ones_b"])
    P.op("dve", lambda e: e.memset(onesf, 1.0), writes=["onesf"])
    P.op("dve", lambda e: e.memset(carry, 0.0), writes=["carry"])
    P.op("dve", lambda e: e.tensor_single_scalar(out=sel, in_=Wv, scalar=0.0, op=ALU.is_gt),
         reads=["Wall"], writes=["sel"])
    for ti in range(NTT):
        bk = ti % 2
        P.op("pe", lambda e, ti=ti, bk=bk: e.matmul(bank(bk, 128, 32, 0), lhsT=lmat_b, rhs=sel[:, ti, :],
                                                   start=True, stop=True),
             reads=["lmat_b", "sel"], writes=[("ps", bk)])
        P.op("pe", lambda e, ti=ti, bk=bk: e.matmul(bank(bk, 128, 32, 32), lhsT=ones_b, rhs=sel[:, ti, :],
                                                   start=True, stop=True),
             reads=["ones_b", "sel"], writes=[("ps", bk)])
        P.op("dve", lambda e, ti=ti, bk=bk: e.tensor_tensor(out=rank[:, ti, :], in0=bank(bk, 128, 32, 0),
                                                           in1=carry, op=ALU.add),
             reads=[("ps", bk), "carry"], writes=[("rank", ti)])
        P.op("dve", lambda e, bk=bk: e.tensor_tensor(out=carry, in0=bank(bk, 128, 32, 32), in1=carry, op=ALU.add),
             reads=[("ps", bk), "carry"], writes=["carry"])
    rkeys = [("rank", ti) for ti in range(NTT)]
    P.op("dve", lambda e: e.tensor_tensor(out=cmpt, in0=carry.unsqueeze(2).to_broadcast([128, 32, 32]),
                                          in1=thr.unsqueeze(1).to_broadcast([128, 32, 32]), op=ALU.is_gt),
         reads=["carry", "thr"], writes=["cmpt"])
    P.op("dve", lambda e: e.tensor_reduce(out=nbe, in_=cmpt, axis=AX.X, op=ALU.add), reads=["cmpt"], writes=["nbe"])
    P.op("dve", lambda e: e.tensor_scalar(out=padded, in0=nbe, scalar1=float(BLK), scalar2=None, op0=ALU.mult),
         reads=["nbe"], writes=["padded"])
    P.op("dve", lambda e: e.tensor_tensor_scan(out=pend, data0=onesf, data1=padded, initial=0.0,
                                               op0=ALU.mult, op1=ALU.add),
         reads=["onesf", "padded"], writes=["pend"])
    P.op("dve", lambda e: e.tensor_tensor(out=pstart, in0=pend, in1=padded, op=ALU.subtract),
         reads=["pend", "padded"], writes=["pstart"])
    P.op("dve", lambda e: e.tensor_tensor(out=dest, in0=rank, in1=pstart.unsqueeze(1).to_broadcast([128, NTT, 32]),
                                          op=ALU.add), reads=rkeys + ["pstart"], writes=["dest"])
    P.op("dve", lambda e: e.scalar_tensor_tensor(out=dsel.rearrange("p a b -> p (a b)"),
                                                 in0=dest.rearrange("p a b -> p (a b)"), scalar=1.0,
                                                 in1=sel.rearrange("p a b -> p (a b)"), op0=ALU.add, op1=ALU.mult),
         reads=["dest", "sel"], writes=["dsel"])
    for ti in range(NTT):
        P.op("dve", lambda e, ti=ti: e.max(out=t8[:, ti, :], in_=dsel[:, ti, :]), reads=["dsel"],
             writes=[("t8", ti)])
        for k in range(2):
            P.op("dve", lambda e, ti=ti, k=k: e.scalar_tensor_tensor(
                out=junk, in0=dsel[:, ti, :], scalar=t8[:, ti, k:k + 1], in1=Wv[:, ti, :],
                op0=ALU.is_equal, op1=ALU.mult, accum_out=wk[:, ti * 2 + k:ti * 2 + k + 1]),
                reads=["dsel", ("t8", ti), "Wall"], writes=["junk", ("wk", ti, k)])
    tkeys = [("t8", ti) for ti in range(NTT)]
    P.op("dve", lambda e: e.tensor_scalar(out=didf, in0=t8[:, :, 0:2], scalar1=-1.0, scalar2=None, op0=ALU.add),
         reads=tkeys, writes=["didf"])
    P.op("dve", lambda e: e.tensor_copy(out=didx[:], in_=didf.rearrange("p a b -> p (a b)")),
         reads=["didf"], writes=["didx"])
    P.op("dve", lambda e: e.tensor_tensor(out=cmpb, in0=pend.unsqueeze(1).to_broadcast([128, NBLK, 32]),
                                          in1=bthr.unsqueeze(2).to_broadcast([128, NBLK, 32]), op=ALU.is_le),
         reads=["pend", "bthr"], writes=["cmpb"])
    P.op("dve", lambda e: e.tensor_reduce(out=ebt, in_=cmpb, axis=AX.X, op=ALU.add), reads=["cmpb"], writes=["ebt"])
    P.op("dve", lambda e: e.tensor_scalar(out=ebt, in0=ebt, scalar1=float(NEXP - 1), scalar2=128.0,
                                          op0=ALU.min, op1=ALU.mult), reads=["ebt"], writes=["ebt"])
    P.op("dve", lambda e: e.tensor_scalar(out=ebt, in0=ebt, scalar1=pidx[:, 0:1], scalar2=None, op0=ALU.add),
         reads=["ebt", "pidx"], writes=["ebt"])
    P.op("dve", lambda e: e.tensor_copy(out=gidx[:], in_=ebt), reads=["ebt"], writes=["gidx"])
    if stop == 51:
        o1 = nc.dram_tensor("didx_o", [128, NTT * 2], I32, kind="ExternalOutput").ap()
        o2 = nc.dram_tensor("wk_o", [128, NTT * 2], F32, kind="ExternalOutput").ap()
        o3 = nc.dram_tensor("gidx_o", [128, NBLK], I32, kind="ExternalOutput").ap()
        o4 = nc.dram_tensor("pend_o", [128, 32], F32, kind="ExternalOutput").ap()
        P.dma("sp", o1, didx[:], reads=["didx"], writes=["o1"])
        P.dma("sp", o2, wk[:], reads=[("wk", ti, k) for ti in range(NTT) for k in range(2)], writes=["o2"])
        P.dma("sp", o3, gidx[:], reads=["gidx"], writes=["o3"])
        P.dma("sp", o4, pend, reads=["pend"], writes=["o4"])
        P.emit(final_wait_keys=["o1", "o2", "o3", "o4"])
        P.close()
        return nc
    P.barrier()
    A.reset()
    xrow = [A.alloc([D], BF16) for _ in range(2)]
    Gw = A.alloc([16, EH], BF16)
    Uw = A.alloc([16, EH], BF16)
    Dw = A.alloc([8, D], BF16)
    xblk = [A.alloc([D], BF16) for _ in range(2)]
    xTb5 = A.alloc([16, BLK], BF16)
    sg5 = [A.alloc([BLK], F32) for _ in range(2)]
    hid5 = A.alloc([8, BLK], BF16)
    yst = [A.alloc([D], F32) for _ in range(2)]
    for ti in range(NTT):
        xb_ = ti % 2
        P.dma("sp", xrow[xb_], h1b[ti * 128:(ti + 1) * 128, :], reads=["h1b"], writes=[("xrow", xb_)])
        for k in range(2):
            P.op("pool", lambda e, ti=ti, k=k, xb_=xb_: e.indirect_dma_start(
                out=xsort, out_offset=bass.IndirectOffsetOnAxis(
                    ap=didx[:, ti * 2 + k:ti * 2 + k + 1].bitcast(U32), axis=0),
                in_=xrow[xb_], in_offset=None),
                reads=[("xrow", xb_), "didx"], writes=["xsort"], kind="d")
    erow = 0
    for b in range(NBLK):
        gio = bass.IndirectOffsetOnAxis
        P.op("pool", lambda e, b=b: e.indirect_dma_start(
            out=Gw.rearrange("p a b -> p (a b)"), out_offset=None, in_=wg_b,
            in_offset=bass.IndirectOffsetOnAxis(ap=gidx[:, b:b + 1].bitcast(U32), axis=0)),
            reads=["gidx", "wg_b"], writes=["Gw"], kind="d")
        P.op("pool", lambda e, b=b: e.indirect_dma_start(
            out=Uw.rearrange("p a b -> p (a b)"), out_offset=None, in_=wu_b,
            in_offset=bass.IndirectOffsetOnAxis(ap=gidx[:, b:b + 1].bitcast(U32), axis=0)),
            reads=["gidx", "wu_b"], writes=["Uw"], kind="d")
        P.op("pool", lambda e, b=b: e.indirect_dma_start(
            out=Dw.rearrange("p a b -> p (a b)"), out_offset=None, in_=wd_b,
            in_offset=bass.IndirectOffsetOnAxis(ap=gidx[:, b:b + 1].bitcast(U32), axis=0)),
            reads=["gidx", "wd_b"], writes=["Dw"], kind="d")
        for st in range(2):
            r0 = b * BLK + st * 128
            P.dma("sp", xblk[st], xsort[r0:r0 + 128, :], reads=["xsort"], writes=[("xblk", st)])
            for k4 in range(4):
                bk = k4 % 2
                for kk in range(4):
                    kc = k4 * 4 + kk
                    P.op("pe", lambda e, st=st, kc=kc, kk=kk, bk=bk: e.transpose(
                        bankbf(bk, 128, 128, kk * 128), xblk[st][:, kc * 128:(kc + 1) * 128], ident_b[:]),
                        reads=[("xblk", st), "ident_b"], writes=[("ps", bk)])
                evac(xTb5[:, k4 * 4:(k4 + 1) * 4, st * 128:(st + 1) * 128],
                     bankbf(bk, 128, 512, 0).rearrange("p (a b) -> p a b", a=4),
                     [("ps", bk)], [("xT5", st, k4)])
        xkeys = [("xT5", st, k4) for st in range(2) for k4 in range(4)]
        for hc in range(8):
            bg = 2 + (hc % 2) * 2
            for kc in range(16):
                P.op("pe", lambda e, bg=bg, kc=kc, hc=hc: e.matmul(
                    bank(bg, 128, BLK), lhsT=Gw[:, kc, hc * 128:(hc + 1) * 128], rhs=xTb5[:, kc, :],
                    start=(kc == 0), stop=(kc == 15)), reads=["Gw"] + xkeys, writes=[("ps", bg)])
            for kc in range(16):
                P.op("pe", lambda e, bg=bg, kc=kc, hc=hc: e.matmul(
                    bank(bg + 1, 128, BLK), lhsT=Uw[:, kc, hc * 128:(hc + 1) * 128], rhs=xTb5[:, kc, :],
                    start=(kc == 0), stop=(kc == 15)), reads=["Uw"] + xkeys, writes=[("ps", bg + 1)])
            sgi = hc % 2
            P.op("act", lambda e, bg=bg, sgi=sgi: e.activation(out=sg5[sgi], in_=bank(bg, 128, BLK), func=AF.Silu),
                 reads=[("ps", bg)], writes=[("sg5", sgi)])
            P.op("dve", lambda e, bg=bg, sgi=sgi, hc=hc: e.tensor_tensor(
                out=hid5[:, hc, :], in0=sg5[sgi], in1=bank(bg + 1, 128, BLK), op=ALU.mult),
                reads=[("sg5", sgi), ("ps", bg + 1)], writes=[("hid5", hc)])
        hkeys = [("hid5", hc) for hc in range(8)]
        for st in range(2):
            r0 = b * BLK + st * 128
            for cbk in range(4):
                bd = 6 + cbk % 2
                for fc in range(8):
                    P.op("pe", lambda e, bd=bd, fc=fc, st=st, cbk=cbk: e.matmul(
                        bank(bd), lhsT=hid5[:, fc, st * 128:(st + 1) * 128],
                        rhs=Dw[:, fc, cbk * 512:(cbk + 1) * 512], start=(fc == 0), stop=(fc == 7)),
                        reads=hkeys + ["Dw"], writes=[("ps", bd)])
                evac(yst[st][:, cbk * 512:(cbk + 1) * 512], bank(bd), [("ps", bd)], [("yst", st, cbk)])
            P.dma("sp", ysort[r0:r0 + 128, :], yst[st], reads=[("yst", st, c_) for c_ in range(4)],
                  writes=["ysort"], accum=True)
    P.barrier()
    A.reset()
    g2t = A.alloc([D], F32)
    b2t = A.alloc([D], F32)
    y0t = [A.alloc([D], F32) for _ in range(2)]
    y1t = [A.alloc([D], F32) for _ in range(2)]
    h1t = [A.alloc([D], F32) for _ in range(2)]
    st6b = A.alloc([4, 6], F32)
    mvb = A.alloc([8], F32)
    P.dma("sp", g2t, ln2g.partition_broadcast(128), writes=["g2"])
    P.dma("sp", b2t, ln2b.partition_broadcast(128), writes=["b2"])
    for ti in range(NTT):
        hb_ = ti % 2
        tok0 = ti * 128
        P.dma("sp", h1t[hb_], h1s[tok0:tok0 + 128, :], reads=["h1s"], writes=[("h1t", hb_)])
        for k, yt_ in ((0, y0t), (1, y1t)):
            P.op("pool", lambda e, ti=ti, k=k, yt_=yt_, hb_=hb_: e.indirect_dma_start(
                out=yt_[hb_], out_offset=None, in_=ysort,
                in_offset=bass.IndirectOffsetOnAxis(ap=didx[:, ti * 2 + k:ti * 2 + k + 1].bitcast(U32), axis=0)),
                reads=["didx", "ysort"], writes=[("yk", k, hb_)], kind="d")
        P.op("dve", lambda e, ti=ti, hb_=hb_: e.tensor_scalar(
            out=y0t[hb_], in0=y0t[hb_], scalar1=wk[:, ti * 2:ti * 2 + 1], scalar2=None, op0=ALU.mult),
            reads=[("yk", 0, hb_)] + [("wk", ti, 0)], writes=[("yk", 0, hb_)])
        P.op("dve", lambda e, ti=ti, hb_=hb_: e.scalar_tensor_tensor(
            out=y0t[hb_], in0=y1t[hb_], scalar=wk[:, ti * 2 + 1:ti * 2 + 2], in1=y0t[hb_],
            op0=ALU.mult, op1=ALU.add),
            reads=[("yk", 0, hb_), ("yk", 1, hb_), ("wk", ti, 1)], writes=[("yk", 0, hb_)])
        P.op("dve", lambda e, hb_=hb_: e.scalar_tensor_tensor(
            out=h1t[hb_], in0=h1t[hb_], scalar=DN_ALPHA, in1=y0t[hb_], op0=ALU.mult, op1=ALU.add),
            reads=[("h1t", hb_), ("yk", 0, hb_)], writes=[("h1t", hb_)])
        layernorm(h1t[hb_], h1t[hb_], "g2", "b2", g2t, b2t, ("h1t", hb_), ("h1t", hb_), "b", st6b, mvb)
        P.dma("sp", out[tok0:tok0 + 128, :], h1t[hb_], reads=[("h1t", hb_)], writes=["out"], accum=True)
    P.emit(final_wait_keys=["out"])
    P.close()
    return nc


def _consts(S, hf):
    SO = S // 2
    SC = S - SO
    slopes = np.exp2(-8.0 * np.arange(1, 9, dtype=np.float64) / 8)
    tok = np.arange(S)
    blk = tok // 128
    rel = tok % 128
    ktab = np.zeros((8, 4, S), np.float32)
    qtab = np.zeros((8, 4, SO), np.float32)
    for h in range(8):
        sl = slopes[h]
        ktab[h, 0] = 1.0
        ktab[h, 1] = 1.0
        ktab[h, 2] = sl * 128 * blk
        ktab[h, 3] = sl * rel
        if hf == 0:
            ktab[h, 2, :SC] += NEG
        qtab[h, 0] = -sl * 128 * blk[SC:]
        qtab[h, 1] = -sl * rel[SC:]
        qtab[h, 2] = 1.0
        qtab[h, 3] = 1.0
    ttab = np.zeros((128, 8, 896), np.float32)
    s = np.arange(128)[:, None]
    t = np.arange(128)[None, :]
    for h in range(8):
        d = np.zeros((128, 128), np.float64)
        fut = (s > t) & ((s // 64) == (t // 64))
        d[fut] = (-2.0 * slopes[h] * (s - t))[fut]
        d[(s // 64) > (t // 64)] = NEG
        ttab[:, h, 0:384] = NEG
        ttab[:, h, 384:512] = d
    j = np.arange(64)[:, None]
    l = np.arange(64)[None, :]
    tri = (j <= l).astype(np.float32)
    umat = (j > l).astype(np.float32)
    flag = np.full((128, 1), 1.0 if hf == 1 else 0.0, np.float32)
    nblk = (2 * SO) // 256 + NEXP
    a = np.arange(128)
    lmat = (a[:, None] < a[None, :]).astype(np.float32)
    thr256 = np.tile((256.0 * np.arange(32, dtype=np.float32))[None], (128, 1))
    bthr = np.tile((256.0 * np.arange(nblk, dtype=np.float32))[None], (128, 1))
    pidx = a.astype(np.float32)[:, None]
    return dict(ktab=ktab, qtab=qtab, ttab=ttab.reshape(128, 8 * 896), tri=tri, umat=umat, flag=flag,
                ident=np.eye(128, dtype=np.float32), lmat=lmat, thr256=thr256, bthr=bthr, pidx=pidx)


_CACHE = {}


def kernel(x, w_in, lambda_q1, lambda_k1, lambda_q2, lambda_k2, attn_norm_w, conv_w, conv_b,
           dt_bias, a_log, d_skip, ssm_norm_w, w_out, ln1_g, ln1_b, w_router_group,
           b_router_group, w_router_expert, b_router_expert, w_gate, w_up, w_down,
           ln2_g, ln2_b):
    x = np.asarray(x, np.float32)
    B, S, _ = x.shape
    SO = S // 2
    f = lambda a: np.ascontiguousarray(np.asarray(a, np.float32))
    if S not in _CACHE:
        _CACHE[S] = build_program(S)
    nc = _CACHE[S]
    wr = np.concatenate([f(w_router_group)[0], np.transpose(f(w_router_expert)[0], (1, 0, 2)).reshape(D, 32)], 1)
    br = np.concatenate([f(b_router_group)[0], f(b_router_expert)[0].reshape(32)])[None]
    lamv = np.concatenate([f(lambda_q1)[0], f(lambda_k1)[0], f(lambda_q2)[0], f(lambda_k2)[0]])[None]
    cw = np.ascontiguousarray(f(conv_w)[0].T.reshape(12, 128, 4).transpose(1, 0, 2).reshape(128, 48))
    cb = np.ascontiguousarray(f(conv_b)[0].reshape(12, 128).T)
    shared = dict(
        w_in=f(w_in)[0], w_out=f(w_out)[0], w_gate=f(w_gate)[0], w_up=f(w_up)[0], w_down=f(w_down)[0],
        wr=f(wr), br=f(br), lamv=f(lamv), anw=f(attn_norm_w), cw=cw, cb=cb, dtb=f(dt_bias), alog=f(a_log),
        dsk=f(d_skip), snw=f(ssm_norm_w), ln1g=f(ln1_g), ln1b=f(ln1_b), ln2g=f(ln2_g), ln2b=f(ln2_b))
    in_maps = []
    for b in range(B):
        for hf in range(2):
            m = dict(shared)
            m.update(_consts(S, hf))
            own = x[b, hf * SO:(hf + 1) * SO]
            ctx = x[b, 0:SO] if hf == 1 else np.zeros_like(own)
            m["xT"] = np.ascontiguousarray(np.concatenate([ctx, own], 0).T)
            m["xo"] = np.ascontiguousarray(own)
            in_maps.append(m)
    res = run_bass_kernel_spmd(nc, in_maps, core_ids=list(range(B * 2)))
    outp = np.zeros((B, S, D), np.float32)
    for b in range(B):
        for hf in range(2):
            outp[b, hf * SO:(hf + 1) * SO] = res.results[b * 2 + hf]["out"]
    return outp
```
